# Optimizing a Trainium2 kernel written in Bass

```python
import jax, jax.numpy as jnp
from jax import lax
import numpy as np

D_MODEL = 1024
BATCH = 4
SEQ = 4096
DEPTH = 2

HEAD_DIM = 64
A_HEADS = D_MODEL // 256
B_HEADS = D_MODEL // 256
C_HEADS = D_MODEL // 128
A_WIDTH = A_HEADS * HEAD_DIM
B_WIDTH = B_HEADS * HEAD_DIM
C_WIDTH = C_HEADS * HEAD_DIM
D_MIX = A_WIDTH + B_WIDTH + C_WIDTH
CHUNK = 128
LN_EPS = 1e-5
RW_DECAY_LORA = 64
RW_A_LORA = 64
RW_GATE_LORA = 128
GN_EPS = 64e-5
C_KV_GROUPS = 2
C_Q_PER_GROUP = C_HEADS // C_KV_GROUPS
C_KV_WIDTH = C_KV_GROUPS * HEAD_DIM
CMP_BLOCK = 32
CMP_STRIDE = 16
CMP_HIDDEN = 2 * HEAD_DIM
SLC_BLOCK = 64
SLC_TOPK = 16
WINDOW = 512
Q_BLOCK = 128
NEG_INF = -1e30
FORCE = 1e4
ROPE_THETA = 500000.0
ROPE_DIM = HEAD_DIM // 4
D_FF = 2816
N_EXPERTS = 8
TOP_K = 2
PLE_DIM = 256
RMS_EPS = 1e-6
N_DENSE = (DEPTH + 1) // 2
N_MOE = DEPTH // 2
A_SIZES = (A_WIDTH, A_WIDTH)
B_SIZES = (B_WIDTH, B_WIDTH, B_WIDTH, RW_DECAY_LORA, RW_A_LORA, RW_GATE_LORA)
C_SIZES = (C_WIDTH,) + (C_KV_WIDTH,) * 6 + (3 * C_HEADS,)
A_COLS = 2 * A_WIDTH
B_COLS = 3 * B_WIDTH + RW_DECAY_LORA + RW_A_LORA + RW_GATE_LORA
C_COLS = C_WIDTH + 6 * C_KV_WIDTH + 3 * C_HEADS
D_IN = A_COLS + B_COLS + C_COLS

kernel_name = "hybrid_gmlp_rwkv7_nsa_moe_block"


def split_cols(z, sizes):
    out, off = [], 0
    for s in sizes:
        out.append(z[..., off:off + s])
        off += s
    return out


def rms_norm(x, g):
    xf = x.astype(jnp.float32)
    y = xf * lax.rsqrt(jnp.mean(xf * xf, axis=-1, keepdims=True) + RMS_EPS)
    return (y * g.astype(jnp.float32)).astype(x.dtype)


def layer_norm(x, g, b, eps):
    xf = x.astype(jnp.float32)
    mu = jnp.mean(xf, axis=-1, keepdims=True)
    var = jnp.mean(jnp.square(xf - mu), axis=-1, keepdims=True)
    return ((xf - mu) * lax.rsqrt(var + eps) * g.astype(jnp.float32) + b.astype(jnp.float32)).astype(x.dtype)


def rope_tables(positions):
    inv = 1.0 / (ROPE_THETA ** (jnp.arange(0, ROPE_DIM, 2, dtype=jnp.float32) / ROPE_DIM))
    ang = positions.astype(jnp.float32)[..., None] * inv
    return jnp.cos(ang), jnp.sin(ang)


def apply_partial_rope(x, cos, sin):
    bshape = cos.shape[:2] + (1,) * (x.ndim - 3) + cos.shape[-1:]
    c = cos.reshape(bshape).astype(x.dtype)
    s = sin.reshape(bshape).astype(x.dtype)
    half = ROPE_DIM // 2
    x1, x2, xp = x[..., :half], x[..., half:ROPE_DIM], x[..., ROPE_DIM:]
    return jnp.concatenate([x1 * c - x2 * s, x1 * s + x2 * c, xp], axis=-1)


def masked_softmax(s, mask):
    s32 = jnp.where(mask, s.astype(jnp.float32), NEG_INF)
    pr = jax.nn.softmax(s32, axis=-1)
    return jnp.where(mask, pr, 0.0).astype(s.dtype)


def swiglu(x, w1, w3, w2):
    return (jax.nn.silu(x @ w1) * (x @ w3)) @ w2


def chunked_spatial_gating(z_a, ln_g, ln_b, w_s, b_s):
    Bn, S, _ = z_a.shape
    nc = S // CHUNK
    u, v = split_cols(jax.nn.gelu(z_a), A_SIZES)
    u = u.reshape(Bn, nc, CHUNK, A_HEADS, HEAD_DIM)
    v = layer_norm(v.reshape(Bn, nc, CHUNK, A_HEADS, HEAD_DIM), ln_g, ln_b, LN_EPS)
    causal = jnp.tril(jnp.ones((CHUNK, CHUNK), dtype=bool))
    w = jnp.where(causal[None], w_s, 0.0).astype(v.dtype)
    mixed = jnp.einsum('hts,bnshd->bnthd', w, v) + b_s.T[None, None, :, :, None]
    return (u * mixed).reshape(Bn, S, A_WIDTH)


def rwkv7_time_mix(z_b, mu, w0, w_up, a0, a_up, g_up, k_k, k_a, r_k, gn_g, gn_b):
    Bn, S, _ = z_b.shape
    f32 = jnp.float32
    z_prev = jnp.pad(z_b[:, :-1], ((0, 0), (1, 0), (0, 0)))
    z = z_b + (z_prev - z_b) * mu
    r, k, v, wd, ad, gd = split_cols(z, B_SIZES)
    w_logit = -jax.nn.softplus(-(w0 + jnp.tanh(wd) @ w_up).astype(f32)) - 0.5
    decay = jnp.exp(-jnp.exp(w_logit))
    a = jax.nn.sigmoid((a0 + ad @ a_up).astype(f32))
    g = jax.nn.sigmoid(gd) @ g_up
    hs = lambda t: t.astype(f32).reshape(Bn, S, B_HEADS, HEAD_DIM)
    kk = hs(k * k_k)
    kk = kk * lax.rsqrt(jnp.maximum(jnp.sum(kk * kk, axis=-1, keepdims=True), 1e-24))
    k_a_h = k_a.astype(f32).reshape(B_HEADS, HEAD_DIM)
    k = hs(k) * (1.0 + (hs(a) - 1.0) * k_a_h)
    r_h, v_h, a_h, w_h = hs(r), hs(v), hs(a), hs(decay)

    def step(state, inp):
        r_t, w_t, k_t, v_t, kk_t, a_t = inp
        sa = jnp.einsum('bhvk,bhk->bhv', state, -kk_t)
        state = (state * w_t[:, :, None, :] + sa[..., None] * (kk_t * a_t)[:, :, None, :]
                 + v_t[..., None] * k_t[:, :, None, :])
        return state, jnp.einsum('bhvk,bhk->bhv', state, r_t)

    xs = tuple(jnp.moveaxis(t, 1, 0) for t in (r_h, w_h, k, v_h, kk, a_h))
    s0 = jnp.zeros((Bn, B_HEADS, HEAD_DIM, HEAD_DIM), f32)
    _, ys = lax.scan(step, s0, xs)
    y = jnp.moveaxis(ys, 0, 1)
    mu_y = jnp.mean(y, axis=-1, keepdims=True)
    var_y = jnp.mean(jnp.square(y - mu_y), axis=-1, keepdims=True)
    y = ((y - mu_y) * lax.rsqrt(var_y + GN_EPS)).reshape(Bn, S, B_WIDTH)
    y = y * gn_g.astype(f32) + gn_b.astype(f32)
    bonus = jnp.sum(r_h * k * r_k.astype(f32), axis=-1, keepdims=True) * v_h
    y = y + bonus.reshape(Bn, S, B_WIDTH)
    return (y * g.astype(f32)).astype(z_b.dtype)


def nsa_attention(z_c, cos, sin, cmp_pos, kc_w1, kc_w2, vc_w1, vc_w2):
    Bn, S, _ = z_c.shape
    G, J, Dh = C_KV_GROUPS, C_Q_PER_GROUP, HEAD_DIM
    scale = HEAD_DIM ** -0.5
    q, kc, vc, ks, vs, kw, vw, gates = split_cols(z_c, C_SIZES)
    q = q.reshape(Bn, S, G, J, Dh)
    kv = lambda t: t.reshape(Bn, S, G, Dh)
    kc, vc, ks, vs, kw, vw = map(kv, (kc, vc, ks, vs, kw, vw))
    gates = jax.nn.sigmoid(gates.astype(jnp.float32)).astype(z_c.dtype).reshape(Bn, S, G, J, 3)
    q_rot = apply_partial_rope(q, cos, sin)
    ks = apply_partial_rope(ks, cos, sin)
    kw = apply_partial_rope(kw, cos, sin)

    n_cmp = (S - CMP_BLOCK) // CMP_STRIDE + 1
    cmp_idx = np.arange(n_cmp)[:, None] * CMP_STRIDE + np.arange(CMP_BLOCK)[None, :]
    cmp_end = jnp.asarray(cmp_idx[:, -1])

    def compress(t, w1, w2):
        blk = t[:, cmp_idx] + cmp_pos[None, None, :, None, :]
        blk = blk.transpose(0, 1, 3, 2, 4).reshape(Bn, n_cmp, G, CMP_BLOCK * Dh)
        return jax.nn.gelu(blk @ w1) @ w2

    k_cmp = compress(kc, kc_w1, kc_w2)
    v_cmp = compress(vc, vc_w1, vc_w2)

    n_slc = S // SLC_BLOCK
    n_sel = min(SLC_TOPK, n_slc)
    slc_start = np.arange(n_slc) * SLC_BLOCK
    overlap = jnp.asarray(((cmp_idx[:, :1] < slc_start[None, :] + SLC_BLOCK)
                           & (cmp_idx[:, -1:] >= slc_start[None, :])).astype(np.float32))
    ks_blk = ks.reshape(Bn, n_slc, SLC_BLOCK, G, Dh).transpose(0, 3, 1, 2, 4)
    vs_blk = vs.reshape(Bn, n_slc, SLC_BLOCK, G, Dh).transpose(0, 3, 1, 2, 4)
    kw_pad = jnp.pad(kw, ((0, 0), (WINDOW, 0), (0, 0), (0, 0)))
    vw_pad = jnp.pad(vw, ((0, 0), (WINDOW, 0), (0, 0), (0, 0)))
    bi = jnp.arange(Bn)[:, None, None, None]
    gi = jnp.arange(G)[None, :, None, None]
    m_ids = jnp.arange(n_slc)

    def query_block(qb):
        start = qb * Q_BLOCK
        t = start + jnp.arange(Q_BLOCK)
        q_raw = lax.dynamic_slice_in_dim(q, start, Q_BLOCK, axis=1)
        q_r = lax.dynamic_slice_in_dim(q_rot, start, Q_BLOCK, axis=1)
        g_t = lax.dynamic_slice_in_dim(gates, start, Q_BLOCK, axis=1)
        s_c = jnp.einsum('btgjd,bngd->bgjtn', q_raw, k_cmp) * scale
        p_c = masked_softmax(s_c, cmp_end[None, :] <= t[:, None])
        o_c = jnp.einsum('bgjtn,bngd->btgjd', p_c, v_cmp)
        imp = jnp.einsum('bgjtn,nm->bgtm', p_c.astype(jnp.float32), overlap)
        blk_t = (t // SLC_BLOCK)[:, None]
        valid = m_ids[None, :] <= blk_t
        forced = (m_ids[None, :] == 0) | (m_ids[None, :] == blk_t) | (m_ids[None, :] == blk_t - 1)
        imp = jnp.where(valid, imp + FORCE * forced.astype(jnp.float32), -FORCE)
        top_v, top_i = lax.top_k(imp, n_sel)
        sel_ok = top_v > -0.5 * FORCE
        k_sel = ks_blk[bi, gi, top_i]
        v_sel = vs_blk[bi, gi, top_i]
        kpos = top_i[..., None] * SLC_BLOCK + jnp.arange(SLC_BLOCK)
        m_s = (sel_ok[..., None] & (kpos <= t[None, None, :, None, None]))
        m_s = m_s.reshape(Bn, G, 1, Q_BLOCK, n_sel * SLC_BLOCK)
        s_s = jnp.einsum('btgjd,bgtksd->bgjtks', q_r, k_sel) * scale
        p_s = masked_softmax(s_s.reshape(Bn, G, J, Q_BLOCK, n_sel * SLC_BLOCK), m_s)
        p_s = p_s.reshape(Bn, G, J, Q_BLOCK, n_sel, SLC_BLOCK)
        o_s = jnp.einsum('bgjtks,bgtksd->btgjd', p_s, v_sel)
        k_win = lax.dynamic_slice_in_dim(kw_pad, start, WINDOW + Q_BLOCK, axis=1)
        v_win = lax.dynamic_slice_in_dim(vw_pad, start, WINDOW + Q_BLOCK, axis=1)
        kpos_w = start - WINDOW + jnp.arange(WINDOW + Q_BLOCK)
        m_w = ((kpos_w[None, :] <= t[:, None]) & (kpos_w[None, :] > t[:, None] - WINDOW)
               & (kpos_w[None, :] >= 0))
        s_w = jnp.einsum('btgjd,bkgd->bgjtk', q_r, k_win) * scale
        p_w = masked_softmax(s_w, m_w)
        o_w = jnp.einsum('bgjtk,bkgd->btgjd', p_w, v_win)
        o = g_t[..., 0:1] * o_c + g_t[..., 1:2] * o_s + g_t[..., 2:3] * o_w
        return o.reshape(Bn, Q_BLOCK, C_WIDTH)

    out = lax.map(query_block, jnp.arange(S // Q_BLOCK))
    return out.transpose(1, 0, 2, 3).reshape(Bn, S, C_WIDTH)


def moe_swiglu(x, router_w, router_b, w1, w3, w2):
    logits = (x @ router_w + router_b).astype(jnp.float32)
    top_v, top_i = lax.top_k(logits, TOP_K)
    top_p = jax.nn.softmax(top_v, axis=-1)
    gate = jnp.sum(jax.nn.one_hot(top_i, N_EXPERTS, dtype=jnp.float32) * top_p[..., None], axis=-2)
    gate = gate.astype(x.dtype)
    out = jnp.zeros_like(x)
    for e in range(N_EXPERTS):
        out = out + gate[..., e:e + 1] * swiglu(x, w1[e], w3[e], w2[e])
    return out


def setup_inputs(seed: int = 0) -> dict:
    key = jax.random.key(seed)
    ks = iter(jax.random.split(key, 64))
    f32 = jnp.float32

    def nrm(shape, scale):
        return scale * jax.random.normal(next(ks), shape, f32)

    def gain(shape):
        return 1.0 + 0.1 * jax.random.normal(next(ks), shape, f32)

    L = DEPTH
    x = nrm((BATCH, SEQ, D_MODEL), 1.0)
    p = nrm((DEPTH, BATCH, SEQ, PLE_DIM), 1.0)
    offset = jax.random.randint(next(ks), (BATCH, 1), 0, 1024, dtype=jnp.int32)
    positions = offset + jnp.arange(SEQ, dtype=jnp.int32)[None, :]
    return {
        "x": x, "p": p, "positions": positions,
        "g_mix": gain((L, D_MODEL)),
        "w_in": nrm((L, D_MODEL, D_IN), D_MODEL ** -0.5),
        "w_out": nrm((L, D_MIX, D_MODEL), D_MIX ** -0.5),
        "gm_ln_g": gain((L, A_HEADS, HEAD_DIM)),
        "gm_ln_b": nrm((L, A_HEADS, HEAD_DIM), 0.1),
        "gm_ws": nrm((L, A_HEADS, CHUNK, CHUNK), CHUNK ** -0.5),
        "gm_bs": gain((L, A_HEADS, CHUNK)),
        "rw_mu": jax.random.uniform(next(ks), (L, B_COLS), f32),
        "rw_w0": nrm((L, B_WIDTH), 0.5),
        "rw_w_up": nrm((L, RW_DECAY_LORA, B_WIDTH), 0.1),
        "rw_a0": nrm((L, B_WIDTH), 0.5),
        "rw_a_up": nrm((L, RW_A_LORA, B_WIDTH), RW_A_LORA ** -0.5),
        "rw_g_up": nrm((L, RW_GATE_LORA, B_WIDTH), RW_GATE_LORA ** -0.5),
        "rw_k_k": 0.85 + nrm((L, B_WIDTH), 0.1),
        "rw_k_a": gain((L, B_WIDTH)),
        "rw_r_k": nrm((L, B_HEADS, HEAD_DIM), 0.1),
        "rw_gn_g": gain((L, B_WIDTH)),
        "rw_gn_b": nrm((L, B_WIDTH), 0.1),
        "nsa_cmp_pos": nrm((L, CMP_BLOCK, HEAD_DIM), 0.1),
        "nsa_kc_w1": nrm((L, CMP_BLOCK * HEAD_DIM, CMP_HIDDEN), (CMP_BLOCK * HEAD_DIM) ** -0.5),
        "nsa_kc_w2": nrm((L, CMP_HIDDEN, HEAD_DIM), CMP_HIDDEN ** -0.5),
        "nsa_vc_w1": nrm((L, CMP_BLOCK * HEAD_DIM, CMP_HIDDEN), (CMP_BLOCK * HEAD_DIM) ** -0.5),
        "nsa_vc_w2": nrm((L, CMP_HIDDEN, HEAD_DIM), CMP_HIDDEN ** -0.5),
        "g_ffn": gain((L, D_MODEL)),
        "ffn_w1": nrm((N_DENSE, D_MODEL, D_FF), D_MODEL ** -0.5),
        "ffn_w3": nrm((N_DENSE, D_MODEL, D_FF), D_MODEL ** -0.5),
        "ffn_w2": nrm((N_DENSE, D_FF, D_MODEL), D_FF ** -0.5),
        "router_w": nrm((N_MOE, D_MODEL, N_EXPERTS), D_MODEL ** -0.5),
        "router_b": nrm((N_MOE, N_EXPERTS), 0.01),
        "moe_w1": nrm((N_MOE, N_EXPERTS, D_MODEL, D_FF), D_MODEL ** -0.5),
        "moe_w3": nrm((N_MOE, N_EXPERTS, D_MODEL, D_FF), D_MODEL ** -0.5),
        "moe_w2": nrm((N_MOE, N_EXPERTS, D_FF, D_MODEL), D_FF ** -0.5),
        "g_ple": gain((L, D_MODEL)),
        "ple_gate_w": nrm((L, D_MODEL, D_MODEL), D_MODEL ** -0.5),
        "ple_proj_w": nrm((L, PLE_DIM, D_MODEL), PLE_DIM ** -0.5),
        "g_final": gain((D_MODEL,)),
    }


def reference(x, p, positions, g_mix, w_in, w_out, gm_ln_g, gm_ln_b, gm_ws, gm_bs,
              rw_mu, rw_w0, rw_w_up, rw_a0, rw_a_up, rw_g_up, rw_k_k, rw_k_a, rw_r_k, rw_gn_g, rw_gn_b,
              nsa_cmp_pos, nsa_kc_w1, nsa_kc_w2, nsa_vc_w1, nsa_vc_w2,
              g_ffn, ffn_w1, ffn_w3, ffn_w2, router_w, router_b, moe_w1, moe_w3, moe_w2,
              g_ple, ple_gate_w, ple_proj_w, g_final):
    cos, sin = rope_tables(positions)
    h = x
    for i in range(DEPTH):
        hn = rms_norm(h, g_mix[i])
        z = hn @ w_in[i]
        z_a, z_b, z_c = split_cols(z, (A_COLS, B_COLS, C_COLS))
        y_a = chunked_spatial_gating(z_a, gm_ln_g[i], gm_ln_b[i], gm_ws[i], gm_bs[i])
        y_b = rwkv7_time_mix(z_b, rw_mu[i], rw_w0[i], rw_w_up[i], rw_a0[i], rw_a_up[i], rw_g_up[i],
                             rw_k_k[i], rw_k_a[i], rw_r_k[i], rw_gn_g[i], rw_gn_b[i])
        y_c = nsa_attention(z_c, cos, sin, nsa_cmp_pos[i], nsa_kc_w1[i], nsa_kc_w2[i],
                            nsa_vc_w1[i], nsa_vc_w2[i])
        h = h + jnp.concatenate([y_a, y_b, y_c], axis=-1) @ w_out[i]
        hn = rms_norm(h, g_ffn[i])
        if i % 2 == 0:
            j = i // 2
            f = swiglu(hn, ffn_w1[j], ffn_w3[j], ffn_w2[j])
        else:
            j = i // 2
            f = moe_swiglu(hn, router_w[j], router_b[j], moe_w1[j], moe_w3[j], moe_w2[j])
        h = h + f
        gate = jax.nn.sigmoid(rms_norm(h, g_ple[i]) @ ple_gate_w[i])
        h = h + (p[i] @ ple_proj_w[i]) * gate
    return rms_norm(h, g_final)
```

```python
import numpy as np
from contextlib import ExitStack
import concourse.bass as bass
import concourse.mybir as mybir
from concourse.bass_utils import run_bass_kernel_spmd

F32 = mybir.dt.float32
BF16 = mybir.dt.bfloat16
I32 = mybir.dt.int32
AF = mybir.ActivationFunctionType
ALU = mybir.AluOpType
AX = mybir.AxisListType

EPOCH = 20000
NDMA = 24


class Prog:
    ENGS = ("pe", "act", "dve", "pool", "sp")

    def __init__(self, nc, stack):
        self.nc = nc
        self.stack = stack
        self.ops = {e: [] for e in self.ENGS}
        self.count = {e: 0 for e in self.ENGS}
        self.sems = {}
        self.seen = {e: {} for e in self.ENGS}
        self.lastw = {}
        self.readers = {}
        self.ndma = 0
        self.dma_last = {}
        self.final_tokens = []
        self.barrier_toks = []

    def sem(self, key):
        if key not in self.sems:
            self.sems[key] = self.stack.enter_context(self.nc.semaphore("s_" + "_".join(map(str, key))))
        return self.sems[key]

    def barrier(self):
        toks = []
        for e in self.ENGS:
            n = self.count[e]
            if n > 0:
                ep, v = divmod(n - 1, EPOCH)
                toks.append((("c", e, ep), v + 1))
        for si, val in self.dma_last.items():
            toks.append((("d", si), val))
        toks.extend(getattr(self, "cc_toks", []))
        self.barrier_toks = toks

    def _deps(self, eng, reads, writes):
        toks = list(self.barrier_toks)
        for k in reads:
            if k in self.lastw:
                toks.append(self.lastw[k])
        for k in writes:
            if k in self.lastw:
                toks.append(self.lastw[k])
            toks.extend(self.readers.get(k, ()))
        need = {}
        for (sk, val) in toks:
            if self.seen[eng].get(sk, 0) >= val:
                continue
            if need.get(sk, 0) < val:
                need[sk] = val
        for sk, val in need.items():
            self.seen[eng][sk] = val
        return list(need.items())

    def _record(self, tok, reads, writes):
        for k in writes:
            self.lastw[k] = tok
            self.readers[k] = []
        for k in reads:
            if k in writes:
                continue
            self.readers.setdefault(k, []).append(tok)

    def op(self, eng, fn, reads=(), writes=(), same_ok=False):
        waits = self._deps(eng, reads, writes)
        if same_ok:
            waits = [(wk, v) for (wk, v) in waits if not (wk[0] == "c" and wk[1] == eng)]
        n = self.count[eng]
        ep, v = divmod(n, EPOCH)
        sk = ("c", eng, ep)
        self.sem(sk)
        tok = (sk, v + 1)
        self.count[eng] = n + 1
        self.ops[eng].append((fn, waits, sk, 1))
        self._record(tok, reads, writes)
        return tok

    def dma(self, eng, out, in_, reads=(), writes=(), **kw):
        i = self.ndma
        self.ndma += 1
        si = i % NDMA
        sk = ("d", si)
        self.sem(sk)
        prev = self.dma_last.get(si, 0)
        waits = self._deps(eng, reads, writes)
        if prev > 0 and self.seen[eng].get(sk, 0) < prev:
            waits.append((sk, prev))
            self.seen[eng][sk] = prev
        val = prev + 16
        self.dma_last[si] = val
        tok = (sk, val)

        def fn(e, out=out, in_=in_, kw=kw):
            return e.dma_start(out=out, in_=in_, **kw)
        self.ops[eng].append((fn, waits, sk, 16))
        self._record(tok, reads, writes)
        return tok

    def collective(self, fn, reads=(), writes=()):
        k = getattr(self, "ncc", 0)
        self.ncc = k + 1
        sk = ("cc", k)
        self.sem(sk)
        waits = self._deps("pool", reads, writes)
        self.ops["pool"].append((fn, waits, sk, None))
        tok = (sk, 1)
        self._record(tok, reads, writes)
        self.cc_toks = getattr(self, "cc_toks", []) + [tok]
        return tok

    def emit(self, last=False, final_waits_eng="sp"):
        nc = self.nc
        fin = []
        if last:
            for tok in self.final_tokens:
                fin.append(tok)
        with nc.Block() as block:
            def mk(engname):
                def body(e):
                    for (fn, waits, sk, inc) in self.ops[engname]:
                        for (wk, val) in waits:
                            e.wait_ge(self.sems[wk], val)
                        inst = fn(e)
                        if inc is None:
                            inst.then_inc(self.sems[sk])
                        else:
                            inst.then_inc(self.sems[sk], inc)
                    if engname == final_waits_eng:
                        for (wk, val) in fin:
                            e.wait_ge(self.sems[wk], val)
                return body
            block.tensor(mk("pe"))
            block.scalar(mk("act"))
            block.vector(mk("dve"))
            block.gpsimd(mk("pool"))
            block.sync(mk("sp"))
        self.ops = {e: [] for e in self.ENGS}


D = 1024
DFF = 2816
NFC = 22
TF = 2048
HALF = 1024
NT = 8


def phase_F(nc, P, ps, psb, pre, E, moe, final, h_in, ydst, out_ap, is_last):
    di = lambda n, s, dt=F32: nc.dram_tensor(pre + n, s, dt, kind="ExternalInput").ap()
    p_in = di("p", [TF, 256])
    w_out = di("w_out", [D, D])
    g_ffn = di("g_ffn", [128, 8])
    w1 = di("w1", [E, NFC, 128, 8, 128])
    w3 = di("w3", [E, NFC, 128, 8, 128])
    w2 = di("w2", [E, DFF, D])
    g_ple = di("g_ple", [128, 8])
    ple_gate = di("ple_gate", [D, D])
    ple_proj = di("ple_proj", [256, D])
    identf = di("identf", [128, 128])
    selv_in = di("selv", [128, 2])
    if moe:
        rw = di("rw", [128, 8, 8])
        rb = di("rb", [128, 8])
    if final:
        g_fin = di("g_fin", [128, D])

    with ExitStack() as st:
        sb = lambda name, shape, dt: st.enter_context(nc.sbuf_tensor(pre + "s_" + name, shape, dt))
        hacc = sb("hacc", [128, NT, D], F32)
        hnT = sb("hnT", [128, 8, HALF], BF16)
        actT = sb("actT", [128, NFC * HALF], BF16)
        w2b = sb("w2b", [128, NFC * D], BF16)
        stg = sb("stg", [128, 6, 8, 128], F32)
        w13b = sb("w13b", [128, 6, 8, 128], BF16)
        stg2 = sb("stg2", [128, 3, D], F32)
        idf = sb("idf", [128, 128], F32)
        idb = sb("idb", [128, 128], BF16)
        gf = sb("gf", [128, 8], F32)
        gp = sb("gp", [128, 8], F32)
        ss = sb("ss", [128, 4], F32)
        gates = sb("gates", [128, NT, 8], F32)
        sm = sb("sm", [128, 64], F32)
        silu = sb("silu", [128, 2, 512], BF16)
        if moe:
            rws = sb("rws", [128, 8, 8], F32)
            rbs = sb("rbs", [128, 8], F32)
        if final:
            gfin = sb("gfin", [128, D], F32)

        woutb = actT[:, 0:8 * D].rearrange("p (c n) -> p c n", c=8)
        pgb = actT[:, 8 * D:16 * D].rearrange("p (c n) -> p c n", c=8)
        ppb = actT[:, 16 * D:18 * D].rearrange("p (c n) -> p c n", c=2)
        o = 0
        def carve(nbytes_bf16, dt, pat=None, **kw):
            nonlocal o
            v = w2b[:, o:o + nbytes_bf16]
            o += nbytes_bf16
            if dt == F32:
                v = v.bitcast(F32)
            if pat:
                v = v.rearrange(pat, **kw)
            return v
        xs = carve(2 * D, F32)
        ys = carve(2 * D, F32)
        ys2 = carve(2 * D, F32)
        yb = carve(D, BF16)
        yT = carve(D, BF16, "p (c t) -> p c t", c=8)
        hnb = carve(D, BF16)
        hn32 = carve(2 * D, F32)
        hnT32 = carve(2 * D, F32, "p (c t) -> p c t", c=8)
        pst = carve(2 * 256, F32)
        pbf = carve(256, BF16)
        pT = carve(256, BF16, "p (c t) -> p c t", c=2)
        gsb = carve(2 * D, F32)
        junk = carve(2 * D, F32)
        osb = carve(2 * D, F32)

        P.dma("sp", idf[:], identf, writes=["idf"])
        P.dma("sp", gf[:], g_ffn, writes=["gf"])
        P.dma("sp", gp[:], g_ple, writes=["gp"])
        selv = sb("selv", [128, 2], F32)
        P.dma("sp", selv[:], selv_in, writes=["selv"])
        P.op("dve", lambda e: e.tensor_copy(out=idb[:], in_=idf[:]), reads=["idf"], writes=["idb"])
        if moe:
            P.dma("sp", rws[:], rw, writes=["rws"])
            P.dma("sp", rbs[:], rb, writes=["rbs"])
            P.op("dve", lambda e: e.tensor_tensor(out=rws[:], in0=rws[:], in1=gf[:].unsqueeze(2).to_broadcast([128, 8, 8]), op=ALU.mult),
                 reads=["rws", "gf"], writes=["rws"])
        if final:
            P.dma("sp", gfin[:], g_fin, writes=["gfin"])

        def load_w_bf16(dst, src, nchunks, gscale, tag):
            for c in range(nchunks):
                s = c % 2
                P.dma("sp", stg2[:, s, :], src[c * 128:(c + 1) * 128, :], writes=[("stg2", s)])
                if gscale is not None:
                    P.op("pool", lambda e, c=c, s=s: e.tensor_scalar(out=dst[:, c, :], in0=stg2[:, s, :], scalar1=gscale[:, c:c + 1], scalar2=None, op0=ALU.mult),
                         reads=[("stg2", s), "gp"], writes=[tag])
                else:
                    P.op("pool", lambda e, c=c, s=s: e.tensor_copy(out=dst[:, c, :], in_=stg2[:, s, :]), reads=[("stg2", s)], writes=[tag])

        def rms(src_ap, key, col):
            P.op("act", lambda e: e.activation(out=junk, in_=src_ap, func=AF.Square, accum_out=ss[:, col:col + 1]), reads=[key], writes=["junk", ("ss", col)])
            P.op("dve", lambda e: e.tensor_scalar(out=ss[:, col:col + 1], in0=ss[:, col:col + 1], scalar1=1.0 / D, scalar2=1e-6, op0=ALU.mult, op1=ALU.add),
                 reads=[("ss", col)], writes=[("ss", col)])
            P.op("act", lambda e: e.sqrt(out=ss[:, col:col + 1], in_=ss[:, col:col + 1]), reads=[("ss", col)], writes=[("ss", col)])
            P.op("dve", lambda e: e.reciprocal(out=ss[:, col:col + 1], in_=ss[:, col:col + 1]), reads=[("ss", col)], writes=[("ss", col)])

        def transposes(psbank, src_bf, n, ident, srckey, pskey):
            def f(e):
                for c in range(n):
                    i = e.transpose(out=psbank[:, c * 128:(c + 1) * 128], in_=src_bf[:, c * 128:(c + 1) * 128], identity=ident)
                return i
            P.op("pe", f, reads=[srckey, "idb", "idf"], writes=[pskey])

        for half in range(2):
            tb = half * HALF
            P.barrier()
            load_w_bf16(woutb, w_out, 8, None, "woutb")
            for i in range(NT):
                t0 = tb + i * 128
                P.dma("sp", xs, h_in[t0:t0 + 128, :], writes=["xs"])
                tl = i * 128 + half * HALF
                for k_, yk in ((0, ys), (1, ys2)):
                    for r_ in range(2):
                        tt = k_ * TF + tl
                        row = (tt // 1024) * 2048 + r_ * 1024 + (tt % 1024)
                        P.dma("sp", yk[:, r_ * 512:(r_ + 1) * 512], ydst[row:row + 128, :], writes=["ys" if k_ == 0 else "ys2"])
                P.op("dve", lambda e: e.tensor_scalar(out=ys, in0=ys, scalar1=selv[:, 0:1], scalar2=None, op0=ALU.mult), reads=["ys", "selv"], writes=["ys"])
                P.op("dve", lambda e: e.scalar_tensor_tensor(out=ys, in0=ys2, scalar=selv[:, 1:2], in1=ys, op0=ALU.mult, op1=ALU.add), reads=["ys", "ys2", "selv"], writes=["ys"])
                P.op("pool", lambda e: e.tensor_copy(out=yb, in_=ys), reads=["ys"], writes=["yb"])
                transposes(psb[0], yb, 8, idb[:], "yb", "ps0")
                P.op("act", lambda e: e.activation(out=yT.rearrange("p c t -> p (c t)"), in_=psb[0], func=AF.Copy), reads=["ps0"], writes=["yT"])
                for hf in range(2):
                    def mm(e, hf=hf):
                        for c in range(8):
                            ins = e.matmul(ps[1 + hf][:], lhsT=yT[:, c, :], rhs=woutb[:, c, hf * 512:(hf + 1) * 512], start=(c == 0), stop=(c == 7))
                        return ins
                    P.op("pe", mm, reads=["yT", "woutb"], writes=[f"ps{1 + hf}"])
                    P.op("dve", lambda e, hf=hf, i=i: e.tensor_tensor(out=hacc[:, i, hf * 512:(hf + 1) * 512], in0=ps[1 + hf][:], in1=xs[:, hf * 512:(hf + 1) * 512], op=ALU.add),
                         reads=[f"ps{1 + hf}", "xs"], writes=[("hacc", i)])
                rms(hacc[:, i, :], ("hacc", i), 0)
                P.op("dve", lambda e, i=i: e.tensor_scalar(out=hnb, in0=hacc[:, i, :], scalar1=ss[:, 0:1], scalar2=None, op0=ALU.mult),
                     reads=[("hacc", i), ("ss", 0)], writes=["hnb"])
                transposes(psb[3], hnb, 8, idb[:], "hnb", "ps3")
                P.op("act", lambda e, i=i: e.activation(out=hnT[:, :, i * 128:(i + 1) * 128], in_=psb[3].rearrange("p (c t) -> p c t", c=8), func=AF.Copy),
                     reads=["ps3"], writes=[("hnT", i)])
                if moe:
                    P.op("pool", lambda e, i=i: e.tensor_scalar(out=hn32, in0=hacc[:, i, :], scalar1=ss[:, 0:1], scalar2=None, op0=ALU.mult),
                         reads=[("hacc", i), ("ss", 0)], writes=["hn32"])
                    for q in range(2):
                        def trf(e, q=q):
                            for c in range(4):
                                cc = q * 4 + c
                                ins = e.transpose(out=ps[4 + q][:, c * 128:(c + 1) * 128], in_=hn32[:, cc * 128:(cc + 1) * 128], identity=idf[:])
                            return ins
                        P.op("pe", trf, reads=["hn32", "idf"], writes=[f"ps{4 + q}"])
                        P.op("act", lambda e, q=q: e.activation(out=hnT32[:, q * 4:(q + 1) * 4, :], in_=ps[4 + q][:].rearrange("p (c t) -> p c t", c=4), func=AF.Copy),
                             reads=[f"ps{4 + q}"], writes=[("hnT32", q)])
                    def mml(e):
                        for c in range(8):
                            ins = e.matmul(ps[6][:, 0:8], lhsT=hnT32[:, c, :], rhs=rws[:, c, :], start=(c == 0), stop=(c == 7))
                        return ins
                    P.op("pe", mml, reads=[("hnT32", 0), ("hnT32", 1), "rws"], writes=["ps6"])
                    lg = sm[:, 0:8]
                    mx = sm[:, 8:16]
                    dd = sm[:, 16:17]
                    p1 = sm[:, 17:18]
                    p2 = sm[:, 18:19]
                    t1 = sm[:, 24:32]
                    P.op("dve", lambda e: e.tensor_tensor(out=lg, in0=ps[6][:, 0:8], in1=rbs[:], op=ALU.add), reads=["ps6", "rbs"], writes=["lg"])
                    P.op("dve", lambda e: e.max(out=mx, in_=lg), reads=["lg"], writes=["mx"])
                    P.op("dve", lambda e: e.tensor_tensor(out=dd, in0=mx[:, 1:2], in1=mx[:, 0:1], op=ALU.subtract), reads=["mx"], writes=["dd"])
                    P.op("act", lambda e: e.activation(out=dd, in_=dd, func=AF.Exp), reads=["dd"], writes=["dd"])
                    P.op("dve", lambda e: e.tensor_scalar(out=p1, in0=dd, scalar1=1.0, scalar2=None, op0=ALU.add), reads=["dd"], writes=["p1"])
                    P.op("dve", lambda e: e.reciprocal(out=p1, in_=p1), reads=["p1"], writes=["p1"])
                    P.op("dve", lambda e: e.tensor_tensor(out=p2, in0=dd, in1=p1, op=ALU.mult), reads=["dd", "p1"], writes=["p2"])
                    P.op("dve", lambda e: e.tensor_scalar(out=t1, in0=lg, scalar1=mx[:, 0:1], scalar2=p1, op0=ALU.is_equal, op1=ALU.mult),
                         reads=["lg", "mx", "p1"], writes=["t1"])
                    P.op("dve", lambda e, i=i: e.tensor_scalar(out=gates[:, i, :], in0=lg, scalar1=mx[:, 1:2], scalar2=p2, op0=ALU.is_equal, op1=ALU.mult),
                         reads=["lg", "mx", "p2"], writes=[("gates", i)])
                    P.op("dve", lambda e, i=i: e.tensor_tensor(out=gates[:, i, :], in0=gates[:, i, :], in1=t1, op=ALU.add),
                         reads=[("gates", i), "t1"], writes=[("gates", i)])

            P.barrier()
            for ex in range(E):
                nslot = 0
                for fc in range(NFC):
                    s = fc % 3
                    P.dma("sp", stg[:, s, :, :], w1[ex, fc], writes=[("stg", s)])
                    P.dma("sp", stg[:, 3 + s, :, :], w3[ex, fc], writes=[("stg", 3 + s)])
                    gb = gf[:].unsqueeze(2).to_broadcast([128, 8, 128])
                    P.op("pool", lambda e, s=s: e.tensor_tensor(out=w13b[:, s, :, :], in0=stg[:, s, :, :], in1=gb, op=ALU.mult),
                         reads=[("stg", s), "gf"], writes=[("w13b", s)])
                    P.op("dve", lambda e, s=s: e.tensor_tensor(out=w13b[:, 3 + s, :, :], in0=stg[:, 3 + s, :, :], in1=gb, op=ALU.mult),
                         reads=[("stg", 3 + s), "gf"], writes=[("w13b", 3 + s)])
                    P.dma("act", stg2[:, s, :], w2[ex, fc * 128:(fc + 1) * 128, :], writes=[("stg2", s)])
                    P.op("act", lambda e, s=s, fc=fc: e.activation(out=w2b[:, fc * D:(fc + 1) * D], in_=stg2[:, s, :], func=AF.Copy),
                         reads=[("stg2", s)], writes=[("w2b", fc)])
                    for g in range(2):
                        def mm13(e, g=g, s=s):
                            for wi in range(2):
                                for c in range(8):
                                    ins = e.matmul(ps[2 * g + wi][:], lhsT=w13b[:, 3 * wi + s, c, :], rhs=hnT[:, c, g * 512:(g + 1) * 512], start=(c == 0), stop=(c == 7))
                            return ins
                        P.op("pe", mm13, reads=[("w13b", s), ("w13b", 3 + s)] + [("hnT", i) for i in range(NT)], writes=[f"ps{2 * g}", f"ps{2 * g + 1}"])
                        P.op("act", lambda e, g=g: e.activation(out=silu[:, g, :], in_=ps[2 * g][:], func=AF.Silu), reads=[f"ps{2 * g}"], writes=[("silu", g)])
                        P.op("dve", lambda e, g=g, fc=fc: e.tensor_tensor(out=actT[:, fc * HALF + g * 512: fc * HALF + (g + 1) * 512], in0=silu[:, g, :], in1=ps[2 * g + 1][:], op=ALU.mult),
                             reads=[("silu", g), f"ps{2 * g + 1}"], writes=[("actT", fc)])
                for i in range(NT):
                    for hf in range(2):
                        b = 4 + (nslot % 4)
                        nslot += 1
                        def mm2(e, i=i, hf=hf, b=b):
                            for fc in range(NFC):
                                ins = e.matmul(ps[b][:], lhsT=actT[:, fc * HALF + i * 128: fc * HALF + (i + 1) * 128], rhs=w2b[:, fc * D + hf * 512: fc * D + (hf + 1) * 512],
                                               start=(fc == 0), stop=(fc == NFC - 1))
                            return ins
                        P.op("pe", mm2, reads=[("actT", fc) for fc in range(NFC)] + [("w2b", fc) for fc in range(NFC)], writes=[f"ps{b}"])
                        if moe:
                            P.op("dve", lambda e, i=i, hf=hf, b=b, ex=ex: e.scalar_tensor_tensor(out=hacc[:, i, hf * 512:(hf + 1) * 512], in0=ps[b][:], scalar=gates[:, i, ex:ex + 1],
                                                                                               in1=hacc[:, i, hf * 512:(hf + 1) * 512], op0=ALU.mult, op1=ALU.add),
                                 reads=[f"ps{b}", ("gates", i), ("hacc", i)], writes=[("hacc", i)])
                        else:
                            P.op("dve", lambda e, i=i, hf=hf, b=b: e.tensor_tensor(out=hacc[:, i, hf * 512:(hf + 1) * 512], in0=ps[b][:], in1=hacc[:, i, hf * 512:(hf + 1) * 512], op=ALU.add),
                                 reads=[f"ps{b}", ("hacc", i)], writes=[("hacc", i)])

            P.barrier()
            load_w_bf16(pgb, ple_gate, 8, gp, "pgb")
            load_w_bf16(ppb, ple_proj, 2, None, "ppb")
            for i in range(NT):
                t0 = tb + i * 128
                P.dma("sp", pst, p_in[t0:t0 + 128, :], writes=["pst"])
                P.op("pool", lambda e: e.tensor_copy(out=pbf, in_=pst), reads=["pst"], writes=["pbf"])
                transposes(psb[0], pbf, 2, idb[:], "pbf", "ps0")
                P.op("act", lambda e: e.activation(out=pT.rearrange("p c t -> p (c t)"), in_=psb[0][:, 0:256], func=AF.Copy), reads=["ps0"], writes=["pT"])
                rms(hacc[:, i, :], ("hacc", i), 1)
                P.op("dve", lambda e, i=i: e.tensor_scalar(out=hnb, in0=hacc[:, i, :], scalar1=ss[:, 1:2], scalar2=None, op0=ALU.mult),
                     reads=[("hacc", i), ("ss", 1)], writes=["hnb"])
                transposes(psb[3], hnb, 8, idb[:], "hnb", "ps3")
                P.op("act", lambda e: e.activation(out=yT.rearrange("p c t -> p (c t)"), in_=psb[3], func=AF.Copy), reads=["ps3"], writes=["yT"])
                for hf in range(2):
                    def mmg(e, hf=hf):
                        for c in range(8):
                            ins = e.matmul(ps[1 + hf][:], lhsT=yT[:, c, :], rhs=pgb[:, c, hf * 512:(hf + 1) * 512], start=(c == 0), stop=(c == 7))
                        return ins
                    P.op("pe", mmg, reads=["yT", "pgb"], writes=[f"ps{1 + hf}"])
                    P.op("act", lambda e, hf=hf: e.activation(out=gsb[:, hf * 512:(hf + 1) * 512], in_=ps[1 + hf][:], func=AF.Sigmoid), reads=[f"ps{1 + hf}"], writes=[("gsb", hf)])
                    def mmp(e, hf=hf):
                        for c in range(2):
                            ins = e.matmul(ps[4 + hf][:], lhsT=pT[:, c, :], rhs=ppb[:, c, hf * 512:(hf + 1) * 512], start=(c == 0), stop=(c == 1))
                        return ins
                    P.op("pe", mmp, reads=["pT", "ppb"], writes=[f"ps{4 + hf}"])
                    P.op("dve", lambda e, hf=hf: e.tensor_tensor(out=gsb[:, hf * 512:(hf + 1) * 512], in0=gsb[:, hf * 512:(hf + 1) * 512], in1=ps[4 + hf][:], op=ALU.mult),
                         reads=[("gsb", hf), f"ps{4 + hf}"], writes=[("gsb", hf)])
                P.op("dve", lambda e, i=i: e.tensor_tensor(out=osb, in0=gsb, in1=hacc[:, i, :], op=ALU.add), reads=[("gsb", 0), ("gsb", 1), ("hacc", i)], writes=["osb"])
                if final:
                    rms(osb, "osb", 2)
                    P.op("dve", lambda e: e.scalar_tensor_tensor(out=osb, in0=osb, scalar=ss[:, 2:3], in1=gfin[:], op0=ALU.mult, op1=ALU.mult),
                         reads=["osb", ("ss", 2), "gfin"], writes=["osb"])
                tk = P.dma("sp", out_ap[t0:t0 + 128, :], osb, reads=["osb"], writes=["f_out"])
                if is_last:
                    P.final_tokens.append(tk)
        P.emit(last=is_last)


def prep_F_inputs(layer, hs, ys, inputs, moe, final):
    i = layer
    j = i // 2
    def tochunks(w):
        E = w.shape[0]
        return np.ascontiguousarray(w.reshape(E, 8, 128, NFC, 128).transpose(0, 3, 2, 1, 4))
    if moe:
        w1, w3, w2 = inputs["moe_w1"][j], inputs["moe_w3"][j], inputs["moe_w2"][j]
    else:
        w1, w3, w2 = inputs["ffn_w1"][j][None], inputs["ffn_w3"][j][None], inputs["ffn_w2"][j][None]
    pc = lambda g: np.ascontiguousarray(g.reshape(8, 128).T)
    common = {
        "w_out": inputs["w_out"][i], "g_ffn": pc(inputs["g_ffn"][i]), "w1": tochunks(w1), "w3": tochunks(w3), "w2": np.ascontiguousarray(w2),
        "g_ple": pc(inputs["g_ple"][i]), "ple_gate": inputs["ple_gate_w"][i], "ple_proj": inputs["ple_proj_w"][i],
        "identf": np.eye(128, dtype=np.float32),
    }
    if moe:
        common["rw"] = np.ascontiguousarray(inputs["router_w"][j].reshape(8, 128, 8).transpose(1, 0, 2))
        common["rb"] = np.ascontiguousarray(np.broadcast_to(inputs["router_b"][j][None, :], (128, 8)))
    if final:
        common["g_fin"] = np.ascontiguousarray(np.broadcast_to(inputs["g_final"][None, :], (128, D)))
    pl = inputs["p"][i].reshape(-1, 256)
    maps = []
    for c in range(8):
        m = dict(common)
        m["h"] = np.ascontiguousarray(hs[c * TF:(c + 1) * TF])
        m["y"] = np.ascontiguousarray(ys[c * TF:(c + 1) * TF])
        m["p"] = np.ascontiguousarray(pl[c * TF:(c + 1) * TF])
        maps.append(m)
    return maps


S = 4096
NTILE = 32


class Proj:
    def __init__(self, nc, P, st, h_in, identf, ncol, w_dram, g_dram, shift_cols=None, mu_dram=None, pre="", hmap=None):
        self.nc, self.P = nc, P
        self.hmap = hmap if hmap is not None else (lambda i: i * 128)
        sb = lambda name, shape, dt: st.enter_context(nc.sbuf_tensor(pre + "s_" + name, shape, dt))
        self.h_in = h_in
        self.xs = sb("pj_xs", [128, 2, D], F32)
        self.junk = sb("pj_junk", [128, D], F32)
        self.ss = sb("pj_ss", [128, 2], F32)
        self.hnb = sb("pj_hnb", [128, D], BF16)
        self.hnT = sb("pj_hnT", [128, 3, 8, 129], BF16)
        self.idf = sb("pj_idf", [128, 128], F32)
        self.idb = sb("pj_idb", [128, 128], BF16)
        self.g = sb("pj_g", [128, 8], F32)
        self.wb = sb("pj_wb", [128, 8, ncol], BF16)
        self.ncol = ncol
        stg = sb("pj_stg", [128, 2, ncol], F32)
        P.dma("sp", self.idf[:], identf, writes=["idf"])
        P.dma("sp", self.g[:], g_dram, writes=["pj_g"])
        P.op("dve", lambda e: e.tensor_copy(out=self.idb[:], in_=self.idf[:]), reads=["idf"], writes=["idb"])
        P.op("pool", lambda e: e.memset(self.hnT[:], 0.0), writes=[("hnT", 0), ("hnT", 1), ("hnT", 2)])
        for c in range(8):
            s = c % 2
            P.dma("sp", stg[:, s, :], w_dram[c * 128:(c + 1) * 128, :], writes=[("pj_stg", s)])
            P.op("pool", lambda e, c=c, s=s: e.tensor_scalar(out=self.wb[:, c, :], in0=stg[:, s, :], scalar1=self.g[:, c:c + 1], scalar2=None, op0=ALU.mult),
                 reads=[("pj_stg", s), "pj_g"], writes=["pj_wb"])
        if shift_cols is not None:
            a, b = shift_cols
            n = b - a
            self.wprev = sb("pj_wprev", [128, 8, n], BF16)
            mu = sb("pj_mu", [128, n], F32)
            P.dma("sp", mu[:], mu_dram, writes=["pj_mu"])
            mub = mu[:].unsqueeze(1).to_broadcast([128, 8, n])
            P.op("dve", lambda e: e.tensor_tensor(out=self.wprev[:], in0=self.wb[:, :, a:b], in1=mub, op=ALU.mult), reads=["pj_wb", "pj_mu"], writes=["pj_wprev"])
            P.op("dve", lambda e: e.tensor_tensor(out=self.wb[:, :, a:b], in0=self.wb[:, :, a:b], in1=self.wprev[:], op=ALU.subtract), reads=["pj_wb", "pj_wprev"], writes=["pj_wb"])
        self.shift_cols = shift_cols

    def tile(self, i, psbank_bf, pskey):
        P = self.P
        s = i % 2
        xs = self.xs[:, s, :]
        r0 = self.hmap(i)
        P.dma("sp", xs, self.h_in[r0:r0 + 128, :], writes=[("pj_xs", s)])
        ssc = self.ss[:, s:s + 1]
        k = ("pj_ss", s)
        P.op("act", lambda e: e.activation(out=self.junk[:], in_=xs, func=AF.Square, accum_out=ssc), reads=[("pj_xs", s)], writes=["pj_junk", k])
        P.op("dve", lambda e: e.tensor_scalar(out=ssc, in0=ssc, scalar1=1.0 / D, scalar2=1e-6, op0=ALU.mult, op1=ALU.add), reads=[k], writes=[k])
        P.op("act", lambda e: e.sqrt(out=ssc, in_=ssc), reads=[k], writes=[k])
        P.op("dve", lambda e: e.reciprocal(out=ssc, in_=ssc), reads=[k], writes=[k])
        P.op("dve", lambda e: e.tensor_scalar(out=self.hnb[:], in0=xs, scalar1=ssc, scalar2=None, op0=ALU.mult), reads=[("pj_xs", s), k], writes=["pj_hnb"])

        def tr(e):
            for c in range(8):
                ins = e.transpose(out=psbank_bf[:, c * 128:(c + 1) * 128], in_=self.hnb[:, c * 128:(c + 1) * 128], identity=self.idb[:])
            return ins
        P.op("pe", tr, reads=["pj_hnb", "idb"], writes=[pskey])
        sh, sn = i % 3, (i + 1) % 3
        P.op("act", lambda e: e.activation(out=self.hnT[:, sh, :, 1:129], in_=psbank_bf.rearrange("p (c t) -> p c t", c=8), func=AF.Copy), reads=[pskey], writes=[("hnT", sh)])
        P.op("pool", lambda e: e.tensor_copy(out=self.hnT[:, sn, :, 0:1], in_=self.hnT[:, sh, :, 128:129]), reads=[("hnT", sh)], writes=[("hnT", sn)])

    def mm_tok(self, e, i, ps_ap, c0, c1, start=True, stop=True):
        s = i % 3
        sh = self.shift_cols is not None and c0 >= self.shift_cols[0] and c1 <= self.shift_cols[1]
        n = 16 if sh else 8
        k = 0
        for c in range(8):
            ins = e.matmul(ps_ap, lhsT=self.hnT[:, s, c, 1:129], rhs=self.wb[:, c, c0:c1], start=(start and k == 0), stop=(stop and k == n - 1))
            k += 1
        if sh:
            a = self.shift_cols[0]
            for c in range(8):
                ins = e.matmul(ps_ap, lhsT=self.hnT[:, s, c, 0:128], rhs=self.wprev[:, c, c0 - a:c1 - a], start=False, stop=(stop and k == n - 1))
                k += 1
        return ins

    def mm_feat(self, e, i, ps_ap, c0, c1):
        s = i % 3
        sh = self.shift_cols is not None and c0 >= self.shift_cols[0] and c1 <= self.shift_cols[1]
        n = 16 if sh else 8
        k = 0
        for c in range(8):
            ins = e.matmul(ps_ap, lhsT=self.wb[:, c, c0:c1], rhs=self.hnT[:, s, c, 1:129], start=(k == 0), stop=(k == n - 1))
            k += 1
        if sh:
            a = self.shift_cols[0]
            for c in range(8):
                ins = e.matmul(ps_ap, lhsT=self.wprev[:, c, c0 - a:c1 - a], rhs=self.hnT[:, s, c, 0:128], start=False, stop=(k == n - 1))
                k += 1
        return ins

    def keys(self, i):
        return [("hnT", i % 3), "pj_wb", "pj_wprev"]


def pc8(g):
    return np.ascontiguousarray(np.asarray(g).reshape(8, 128).T)


def bc128(v):
    v = np.asarray(v, dtype=np.float32).reshape(1, -1)
    return np.ascontiguousarray(np.broadcast_to(v, (128, v.shape[1])))


class MAops:
    def __init__(self, nc, P, st, pre, pj, c0, psA, keyA, psB, keyB, ysrc):
        self.P, self.pj, self.c0, self.psA, self.keyA, self.psB, self.keyB, self.ysrc = P, pj, c0, psA, keyA, psB, keyB, ysrc
        di = lambda n, s, dt=F32: nc.dram_tensor(pre + n, s, dt, kind="ExternalInput").ap()
        sb = lambda name, shape, dt: st.enter_context(nc.sbuf_tensor(pre + "s_" + name, shape, dt))
        lng = di("lng", [128, 128]); lnb = di("lnb", [128, 128]); ws = di("ws", [2, 128, 128]); tril = di("tril", [128, 128]); bs = di("bs", [128, 2])
        self.lngs = sb("lngs", [128, 128], F32); self.lnbs = sb("lnbs", [128, 128], F32)
        wss = sb("wss", [128, 2, 128], F32); trl = sb("trl", [128, 128], F32)
        self.wT = sb("wT", [128, 2, 128], BF16); self.bss = sb("bss", [128, 2], F32)
        self.uv = sb("uv", [128, 2, 256], F32); self.st1 = sb("st1", [128, 2, 8], F32)
        self.vc = sb("vc", [128, 2, 2, 64], F32); self.sq = sb("sq", [128, 2, 64], F32)
        self.vn = sb("vn", [128, 2, 2, 64], BF16); self.yo = sb("yo", [128, 2, 128], F32)
        P.dma("sp", self.lngs[:], lng, writes=["a_lngs"]); P.dma("sp", self.lnbs[:], lnb, writes=["a_lnbs"])
        P.dma("sp", trl[:], tril, writes=["a_trl"]); P.dma("sp", self.bss[:], bs, writes=["a_bss"])
        for hh in range(2):
            P.dma("sp", wss[:, hh, :], ws[hh], writes=[("a_wss", hh)])
            P.op("dve", lambda e, hh=hh: e.tensor_tensor(out=wss[:, hh, :], in0=wss[:, hh, :], in1=trl[:], op=ALU.mult), reads=[("a_wss", hh), "a_trl"], writes=[("a_wss", hh)])
            P.op("pe", lambda e, hh=hh: e.transpose(out=psB[:, hh * 128:(hh + 1) * 128], in_=wss[:, hh, :], identity=pj.idf[:]), reads=[("a_wss", hh), "idf"], writes=[keyB])
            P.op("act", lambda e, hh=hh: e.activation(out=self.wT[:, hh, :], in_=psB[:, hh * 128:(hh + 1) * 128], func=AF.Copy), reads=[keyB], writes=["a_wT"])

    def tile(self, i):
        P, pj, c0, psA, keyA, psB, keyB = self.P, self.pj, self.c0, self.psA, self.keyA, self.psB, self.keyB
        so = i % 2
        uv = self.uv[:, so, :]; st1 = self.st1[:, so, :]; vc = self.vc[:, so]; sq = self.sq; vn = self.vn[:, so]; yo = self.yo
        K = lambda n: ("a_" + n, so)
        P.op("pe", lambda e: pj.mm_tok(e, i, psA[:, 0:256], c0, c0 + 256), reads=pj.keys(i), writes=[keyA])
        P.op("act", lambda e: e.activation(out=uv, in_=psA[:, 0:256], func=AF.Gelu_apprx_tanh), reads=[keyA], writes=[K("uv")])
        v3 = uv[:, 128:256].rearrange("p (h d) -> p h d", h=2)
        g3 = lambda ap: ap.rearrange("p (h d) -> p h d", h=2)
        P.op("dve", lambda e: e.tensor_reduce(out=st1[:, 0:2], in_=v3, axis=AX.X, op=ALU.add), reads=[K("uv")], writes=[K("st_m")])
        P.op("dve", lambda e: e.tensor_scalar(out=st1[:, 0:2], in0=st1[:, 0:2], scalar1=1.0 / 64, scalar2=None, op0=ALU.mult), reads=[K("st_m")], writes=[K("st_m")])
        P.op("pool", lambda e: e.tensor_tensor(out=vc, in0=v3, in1=st1[:, 0:2].unsqueeze(2).to_broadcast([128, 2, 64]), op=ALU.subtract), reads=[K("uv"), K("st_m")], writes=[K("vc")])
        P.op("pool", lambda e: e.tensor_tensor(out=sq[:], in0=vc, in1=vc, op=ALU.mult), reads=[K("vc")], writes=["a_sq"])
        P.op("dve", lambda e: e.tensor_reduce(out=st1[:, 2:4], in_=sq[:], axis=AX.X, op=ALU.add), reads=["a_sq"], writes=[K("st_v")])
        P.op("dve", lambda e: e.tensor_scalar(out=st1[:, 2:4], in0=st1[:, 2:4], scalar1=1.0 / 64, scalar2=1e-5, op0=ALU.mult, op1=ALU.add), reads=[K("st_v")], writes=[K("st_v")])
        P.op("act", lambda e: e.sqrt(out=st1[:, 2:4], in_=st1[:, 2:4]), reads=[K("st_v")], writes=[K("st_v")])
        P.op("dve", lambda e: e.reciprocal(out=st1[:, 2:4], in_=st1[:, 2:4]), reads=[K("st_v")], writes=[K("st_v")])
        P.op("pool", lambda e: e.tensor_tensor(out=vc, in0=vc, in1=st1[:, 2:4].unsqueeze(2).to_broadcast([128, 2, 64]), op=ALU.mult), reads=[K("vc"), K("st_v")], writes=[K("vc")])
        P.op("pool", lambda e: e.tensor_tensor(out=vc, in0=vc, in1=g3(self.lngs[:]), op=ALU.mult), reads=[K("vc"), "a_lngs"], writes=[K("vc")])
        P.op("pool", lambda e: e.tensor_tensor(out=vn, in0=vc, in1=g3(self.lnbs[:]), op=ALU.add), reads=[K("vc"), "a_lnbs"], writes=[K("vn")])

        def mix(e):
            for hh in range(2):
                ins = e.matmul(psB[:, hh * 64:(hh + 1) * 64], lhsT=self.wT[:, hh, :], rhs=vn[:, hh, :], start=True, stop=True)
            return ins
        P.op("pe", mix, reads=["a_wT", K("vn")], writes=[keyB])
        for hh in range(2):
            P.op("dve", lambda e, hh=hh: e.scalar_tensor_tensor(out=yo[:, so, hh * 64:(hh + 1) * 64], in0=psB[:, hh * 64:(hh + 1) * 64], scalar=self.bss[:, hh:hh + 1],
                                                            in1=uv[:, hh * 64:(hh + 1) * 64], op0=ALU.add, op1=ALU.mult),
                 reads=[keyB, "a_bss", K("uv")], writes=[K("yo")])
        P.dma("sp", self.ysrc[i * 128:(i + 1) * 128, 0:128], yo[:, so, :], reads=[K("yo")], writes=["ysrc"])


def phase_MA(nc, P, ps, psb, pre, h_in, ysrc, hmap=None, is_last=False):
    di = lambda n, s, dt=F32: nc.dram_tensor(pre + n, s, dt, kind="ExternalInput").ap()
    identf = di("identf", [128, 128])
    wc = di("wc", [D, 256])
    g_mix = di("g_mix", [128, 8])
    lng = di("lng", [128, 128])
    lnb = di("lnb", [128, 128])
    ws = di("ws", [2, 128, 128])
    tril = di("tril", [128, 128])
    bs = di("bs", [128, 2])
    with ExitStack() as st:
        sb = lambda name, shape, dt: st.enter_context(nc.sbuf_tensor(pre + "s_" + name, shape, dt))
        pj = Proj(nc, P, st, h_in, identf, 256, wc, g_mix, pre=pre, hmap=hmap)
        lngs = sb("lngs", [128, 128], F32)
        lnbs = sb("lnbs", [128, 128], F32)
        wss = sb("wss", [128, 2, 128], F32)
        trl = sb("trl", [128, 128], F32)
        wT = sb("wT", [128, 2, 128], BF16)
        bss = sb("bss", [128, 2], F32)
        uv = sb("uv", [128, 256], F32)
        st1 = sb("st1", [128, 8], F32)
        vc = sb("vc", [128, 2, 64], F32)
        sq = sb("sq", [128, 2, 64], F32)
        vn = sb("vn", [128, 2, 64], BF16)
        yo = sb("yo", [128, 2, 128], F32)
        P.dma("sp", lngs[:], lng, writes=["lngs"])
        P.dma("sp", lnbs[:], lnb, writes=["lnbs"])
        P.dma("sp", trl[:], tril, writes=["trl"])
        P.dma("sp", bss[:], bs, writes=["bss"])
        for hh in range(2):
            P.dma("sp", wss[:, hh, :], ws[hh], writes=[("wss", hh)])
            P.op("dve", lambda e, hh=hh: e.tensor_tensor(out=wss[:, hh, :], in0=wss[:, hh, :], in1=trl[:], op=ALU.mult), reads=[("wss", hh), "trl"], writes=[("wss", hh)])
            P.op("pe", lambda e, hh=hh: e.transpose(out=ps[7][:, hh * 128:(hh + 1) * 128], in_=wss[:, hh, :], identity=pj.idf[:]), reads=[("wss", hh), "idf"], writes=["ps7"])
            P.op("act", lambda e, hh=hh: e.activation(out=wT[:, hh, :], in_=ps[7][:, hh * 128:(hh + 1) * 128], func=AF.Copy), reads=["ps7"], writes=["wT"])
        for i in range(NTILE):
            pj.tile(i, psb[0], "ps0")
            P.op("pe", lambda e, i=i: pj.mm_tok(e, i, ps[1][:, 0:256], 0, 256), reads=pj.keys(i), writes=["ps1"])
            P.op("act", lambda e: e.activation(out=uv[:], in_=ps[1][:, 0:256], func=AF.Gelu_apprx_tanh), reads=["ps1"], writes=["uv"])
            v3 = uv[:, 128:256].rearrange("p (h d) -> p h d", h=2)
            P.op("dve", lambda e: e.tensor_reduce(out=st1[:, 0:2], in_=v3, axis=AX.X, op=ALU.add), reads=["uv"], writes=["st_m"])
            P.op("dve", lambda e: e.tensor_scalar(out=st1[:, 0:2], in0=st1[:, 0:2], scalar1=1.0 / 64, scalar2=None, op0=ALU.mult), reads=["st_m"], writes=["st_m"])
            P.op("dve", lambda e: e.tensor_tensor(out=vc[:], in0=v3, in1=st1[:, 0:2].unsqueeze(2).to_broadcast([128, 2, 64]), op=ALU.subtract), reads=["uv", "st_m"], writes=["vc"])
            P.op("dve", lambda e: e.tensor_tensor(out=sq[:], in0=vc[:], in1=vc[:], op=ALU.mult), reads=["vc"], writes=["sq"])
            P.op("dve", lambda e: e.tensor_reduce(out=st1[:, 2:4], in_=sq[:], axis=AX.X, op=ALU.add), reads=["sq"], writes=["st_v"])
            P.op("dve", lambda e: e.tensor_scalar(out=st1[:, 2:4], in0=st1[:, 2:4], scalar1=1.0 / 64, scalar2=1e-5, op0=ALU.mult, op1=ALU.add), reads=["st_v"], writes=["st_v"])
            P.op("act", lambda e: e.sqrt(out=st1[:, 2:4], in_=st1[:, 2:4]), reads=["st_v"], writes=["st_v"])
            P.op("dve", lambda e: e.reciprocal(out=st1[:, 2:4], in_=st1[:, 2:4]), reads=["st_v"], writes=["st_v"])
            P.op("dve", lambda e: e.tensor_tensor(out=vc[:], in0=vc[:], in1=st1[:, 2:4].unsqueeze(2).to_broadcast([128, 2, 64]), op=ALU.mult), reads=["vc", "st_v"], writes=["vc"])
            P.op("dve", lambda e: e.tensor_tensor(out=vc[:], in0=vc[:], in1=lngs[:].rearrange("p (h d) -> p h d", h=2), op=ALU.mult), reads=["vc", "lngs"], writes=["vc"])
            P.op("dve", lambda e: e.tensor_tensor(out=vn[:], in0=vc[:], in1=lnbs[:].rearrange("p (h d) -> p h d", h=2), op=ALU.add), reads=["vc", "lnbs"], writes=["vn"])
            def mix(e):
                for hh in range(2):
                    ins = e.matmul(ps[2][:, hh * 64:(hh + 1) * 64], lhsT=wT[:, hh, :], rhs=vn[:, hh, :], start=True, stop=True)
                return ins
            P.op("pe", mix, reads=["wT", "vn"], writes=["ps2"])
            so = i % 2
            for hh in range(2):
                P.op("dve", lambda e, hh=hh, so=so: e.scalar_tensor_tensor(out=yo[:, so, hh * 64:(hh + 1) * 64], in0=ps[2][:, hh * 64:(hh + 1) * 64], scalar=bss[:, hh:hh + 1],
                                                                       in1=uv[:, hh * 64:(hh + 1) * 64], op0=ALU.add, op1=ALU.mult),
                     reads=["ps2", "bss", "uv"], writes=[("yo", so)])
            P.dma("sp", ysrc[i * 128:(i + 1) * 128, 0:128], yo[:, so, :], reads=[("yo", so)], writes=["ysrc"])
        P.emit(last=is_last)


def prep_MA(layer, h, inputs):
    i = layer
    maps = []
    for c in range(8):
        b, gi = c // 2, c % 2
        w = inputs["w_in"][i]
        wc = np.concatenate([w[:, gi * 128:(gi + 1) * 128], w[:, 256 + gi * 128:256 + (gi + 1) * 128]], axis=1)
        maps.append({
            "h": np.ascontiguousarray(h[b]), "identf": np.eye(128, dtype=np.float32), "wc": np.ascontiguousarray(wc),
            "g_mix": pc8(inputs["g_mix"][i]),
            "lng": bc128(inputs["gm_ln_g"][i][2 * gi:2 * gi + 2].reshape(-1)), "lnb": bc128(inputs["gm_ln_b"][i][2 * gi:2 * gi + 2].reshape(-1)),
            "ws": np.ascontiguousarray(inputs["gm_ws"][i][2 * gi:2 * gi + 2]), "tril": np.tril(np.ones((128, 128), np.float32)),
            "bs": np.ascontiguousarray(inputs["gm_bs"][i][2 * gi:2 * gi + 2].T),
        })
    return maps


def phase_MB(nc, P, ps, psb, pre, h_in, ysrc, scr, hmap=None, ntile=NTILE, upto=9, is_last=False):
    di = lambda n, s, dt=F32: nc.dram_tensor(pre + n, s, dt, kind="ExternalInput").ap()
    identf = di("identf", [128, 128])
    wc = di("wc", [D, 896])
    g_mix = di("g_mix", [128, 8])
    mu = di("mu", [128, 640])
    wa_up = di("wa_up", [128, 128])
    g_up = di("g_up", [128, 128])
    w0a0 = di("w0a0", [1, 256])
    cvec = di("cvec", [128, 5, 128])
    sel_in = di("sel", [20, 5, 128])
    mcum_in = di("mcum", [128, 128])
    NST = 32
    with ExitStack() as st:
        sb = lambda name, shape, dt: st.enter_context(nc.sbuf_tensor(pre + "s_" + name, shape, dt))
        pj = Proj(nc, P, st, h_in, identf, 896, wc, g_mix, shift_cols=(0, 640), mu_dram=mu, pre=pre, hmap=hmap)
        ma = MAops(nc, P, st, pre + "a_", pj, 640, ps[6], "ps6", ps[7], "ps7", ysrc)
        waf = sb("waf", [128, 128], F32); wab = sb("wab", [128, 128], BF16)
        guf = sb("guf", [128, 128], F32); gub = sb("gub", [128, 128], BF16)
        w0f = sb("w0f", [1, 256], F32); w0b = sb("w0b", [1, 256], BF16)
        ones = sb("ones", [1, 128], BF16)
        cv = sb("cv", [128, 5, 128], F32)
        self_ = sb("self", [20, 5, 128], F32)
        sel = sb("selt", [20, 5, 128], BF16)
        ldT = sb("ldT", [128, 128], BF16)
        gdT = sb("gdT", [128, 128], BF16)
        sg = sb("sg", [128, 128], F32)
        aa = sb("aa", [128, 128], F32)
        rkv = sb("rkv", [128, 384], F32)
        kk0 = sb("kk0", [128, 2, 64], F32)
        sq = sb("sq", [128, 2, 64], F32)
        st1 = sb("st1", [128, 8], F32)
        tmp = sb("tmp", [128, 128], F32)
        strm = sb("strm", [128, 2, 5, 128], F32)
        g_all = sb("g_all", [128, ntile, 128], F32)
        bon_all = sb("bon_all", [128, ntile, 128], F32)
        vT_all = sb("vT_all", [128, ntile * 128], F32)
        yT_all = sb("yT_all", [128, ntile * 128], F32)
        rows = sb("rows", [20, 2, NST * 64], BF16)
        shl = sb("shl", [128, 2, 2, 5, 128], BF16)
        sdf = sb("sdf", [128, 5, 128], F32)
        Sst = sb("Sst", [128, 64], F32)
        T1 = sb("T1", [128, 64], F32)
        junk = sb("junk", [128, 64], F32)
        sa = sb("sa", [128, 1], F32)
        fill = sb("fill", [128, 2], F32)
        junk2 = sb("junk2", [128, 64], F32)
        prev_step = None
        mcum = sb("mcum", [128, 128], F32)
        pinc = sb("pinc", [128, 128], F32); pinv = sb("pinv", [128, 128], F32); pexc = sb("pexc", [128, 128], F32); csx = sb("csx", [128, 128], F32)
        Sb2 = sb("Sb2", [128, 2, 64], F32); Ubuf = sb("Ubuf", [128, 64], F32)
        P.dma("sp", mcum[:], mcum_in, writes=["mcum"])
        T1p = sb("T1p", [128, 64], F32)
        wr_sb = sb("wr_sb", [128, 2, 512], F32)
        yo = sb("yo", [128, 2, 128], F32)
        yc = sb("yc", [128, 2, 64], F32)

        P.dma("sp", waf[:], wa_up, writes=["waf"]); P.op("dve", lambda e: e.tensor_copy(out=wab[:], in_=waf[:]), reads=["waf"], writes=["wab"])
        P.dma("sp", guf[:], g_up, writes=["guf"]); P.op("dve", lambda e: e.tensor_copy(out=gub[:], in_=guf[:]), reads=["guf"], writes=["gub"])
        P.dma("sp", w0f[:], w0a0, writes=["w0f"]); P.op("dve", lambda e: e.tensor_copy(out=w0b[:], in_=w0f[:]), reads=["w0f"], writes=["w0b"])
        P.op("dve", lambda e: e.memset(ones[:], 1.0), writes=["ones"])
        P.dma("sp", cv[:], cvec, writes=["cv"])
        P.dma("sp", self_[:], sel_in, writes=["self"])
        P.op("dve", lambda e: e.tensor_copy(out=sel[:], in_=self_[:]), reads=["self"], writes=["sel"])
        KK, KA, RK, GNG, GNB = range(5)
        h3 = lambda ap: ap.rearrange("p (h d) -> p h d", h=2)

        pj.tile(0, psb[0], "ps0")
        for i in range(ntile):
            so = i % 2
            if i + 1 < ntile:
                pj.tile(i + 1, psb[0], "ps0")
            P.op("pe", lambda e, i=i: pj.mm_tok(e, i, ps[1][:, 0:384], 0, 384), reads=pj.keys(i), writes=["ps1"])
            P.op("pe", lambda e, i=i: pj.mm_feat(e, i, ps[2][:, 0:128], 384, 512), reads=pj.keys(i), writes=["ps2"])
            P.op("pe", lambda e, i=i: pj.mm_feat(e, i, ps[3][:, 0:128], 512, 640), reads=pj.keys(i), writes=["ps3"])
            P.op("act", lambda e: e.activation(out=ldT[0:64, :], in_=ps[2][0:64, 0:128], func=AF.Tanh), reads=["ps2"], writes=["ldT0"])
            P.op("act", lambda e: e.activation(out=ldT[64:128, :], in_=ps[2][64:128, 0:128], func=AF.Copy), reads=["ps2"], writes=["ldT1"])
            P.op("act", lambda e: e.activation(out=gdT[:], in_=ps[3][:, 0:128], func=AF.Sigmoid), reads=["ps3"], writes=["gdT"])
            P.op("act", lambda e: e.activation(out=rkv[:], in_=ps[1][:, 0:384], func=AF.Copy), reads=["ps1"], writes=["rkv"])
            def ups(e):
                e.matmul(ps[4][:, 0:128], lhsT=ldT[0:64, :], rhs=wab[0:64, :], start=True, stop=False)
                e.matmul(ps[4][:, 0:128], lhsT=ones[:], rhs=w0b[:, 0:128], start=False, stop=True)
                e.matmul(ps[4][:, 128:256], lhsT=ldT[64:128, :], rhs=wab[64:128, :], start=True, stop=False)
                e.matmul(ps[4][:, 128:256], lhsT=ones[:], rhs=w0b[:, 128:256], start=False, stop=True)
                return e.matmul(ps[4][:, 256:384], lhsT=gdT[:], rhs=gub[:], start=True, stop=True)
            P.op("pe", ups, reads=["ldT0", "ldT1", "gdT", "wab", "gub", "w0b", "ones"], writes=["ps4"])
            P.op("act", lambda e: e.activation(out=sg[:], in_=ps[4][:, 0:128], func=AF.Sigmoid), reads=["ps4"], writes=["sg"])
            P.op("pe", lambda e: e.matmul(ps[6][:, 128:256], lhsT=mcum[:], rhs=sg[:], start=True, stop=True), reads=["mcum", "sg"], writes=["ps6"])
            P.op("act", lambda e, so=so: e.activation(out=strm[:, so, 0, :], in_=ps[6][:, 128:256], func=AF.Exp, scale=-0.6065306597126334), reads=["ps6"], writes=[("strm", so, 0)])
            P.op("act", lambda e: e.activation(out=pinv[:], in_=ps[6][:, 128:256], func=AF.Exp, scale=0.6065306597126334), reads=["ps6"], writes=["pinv"])
            P.op("act", lambda e: e.activation(out=csx[:], in_=ps[6][:, 128:256], func=AF.Copy), reads=["ps6"], writes=["csx"])
            P.op("pool", lambda e: e.tensor_tensor(out=csx[:], in0=csx[:], in1=sg[:], op=ALU.subtract), reads=["csx", "sg"], writes=["csx"])
            P.op("act", lambda e: e.activation(out=pexc[:], in_=csx[:], func=AF.Exp, scale=-0.6065306597126334), reads=["csx"], writes=["pexc"])
            P.op("act", lambda e: e.activation(out=aa[:], in_=ps[4][:, 128:256], func=AF.Sigmoid), reads=["ps4"], writes=["aa"])
            P.op("act", lambda e, i=i: e.activation(out=g_all[:, i, :], in_=ps[4][:, 256:384], func=AF.Copy), reads=["ps4"], writes=[("g_all", i)])
            r_ = rkv[:, 0:128]; k_ = rkv[:, 128:256]; v_ = rkv[:, 256:384]
            P.op("pe", lambda e: e.transpose(out=ps[5][:, 0:128], in_=v_, identity=pj.idf[:]), reads=["rkv", "idf"], writes=["ps5"])
            P.op("act", lambda e, i=i: e.activation(out=vT_all[:, i * 128:(i + 1) * 128], in_=ps[5][:, 0:128], func=AF.Copy), reads=["ps5"], writes=[("vT", i)])
            P.op("dve", lambda e: e.tensor_tensor(out=kk0[:], in0=h3(k_), in1=h3(cv[:, KK, :]), op=ALU.mult), reads=["rkv", "cv"], writes=["kk0"])
            P.op("dve", lambda e: e.tensor_tensor(out=sq[:], in0=kk0[:], in1=kk0[:], op=ALU.mult), reads=["kk0"], writes=["sq"])
            P.op("dve", lambda e: e.tensor_reduce(out=st1[:, 0:2], in_=sq[:], axis=AX.X, op=ALU.add), reads=["sq"], writes=["st_k"])
            P.op("dve", lambda e: e.tensor_scalar(out=st1[:, 0:2], in0=st1[:, 0:2], scalar1=1e-24, scalar2=None, op0=ALU.max), reads=["st_k"], writes=["st_k"])
            P.op("act", lambda e: e.sqrt(out=st1[:, 0:2], in_=st1[:, 0:2]), reads=["st_k"], writes=["st_k"])
            P.op("dve", lambda e: e.reciprocal(out=st1[:, 0:2], in_=st1[:, 0:2]), reads=["st_k"], writes=["st_k"])
            P.op("dve", lambda e, so=so: e.tensor_tensor(out=h3(strm[:, so, 1, :]), in0=kk0[:], in1=st1[:, 0:2].unsqueeze(2).to_broadcast([128, 2, 64]), op=ALU.mult),
                 reads=["kk0", "st_k"], writes=[("strm", so, 1)])
            P.op("dve", lambda e, so=so: e.scalar_tensor_tensor(out=strm[:, so, 2, :], in0=strm[:, so, 1, :], scalar=-1.0, in1=aa[:], op0=ALU.mult, op1=ALU.mult),
                 reads=[("strm", so, 1), "aa"], writes=[("strm", so, 2)])
            P.op("dve", lambda e: e.scalar_tensor_tensor(out=tmp[:], in0=aa[:], scalar=-1.0, in1=cv[:, KA, :], op0=ALU.add, op1=ALU.mult), reads=["aa", "cv"], writes=["tmp"])
            P.op("dve", lambda e, so=so: e.scalar_tensor_tensor(out=strm[:, so, 3, :], in0=tmp[:], scalar=1.0, in1=k_, op0=ALU.add, op1=ALU.mult),
                 reads=["tmp", "rkv"], writes=[("strm", so, 3)])
            P.op("pool", lambda e, so=so: e.tensor_tensor(out=strm[:, so, 4, :], in0=r_, in1=strm[:, so, 0, :], op=ALU.mult), reads=["rkv", ("strm", so, 0)], writes=[("strm", so, 4)])
            P.op("dve", lambda e, so=so: e.tensor_tensor(out=tmp[:], in0=r_, in1=strm[:, so, 3, :], op=ALU.mult), reads=["rkv", ("strm", so, 3), "tmp"], writes=["tmp"])
            P.op("dve", lambda e: e.tensor_tensor(out=tmp[:], in0=tmp[:], in1=cv[:, RK, :], op=ALU.mult), reads=["tmp", "cv"], writes=["tmp"])
            P.op("dve", lambda e: e.tensor_reduce(out=st1[:, 2:4], in_=h3(tmp[:]), axis=AX.X, op=ALU.add), reads=["tmp"], writes=["st_b"])
            P.op("dve", lambda e, i=i: e.tensor_tensor(out=h3(bon_all[:, i, :]), in0=h3(v_), in1=st1[:, 2:4].unsqueeze(2).to_broadcast([128, 2, 64]), op=ALU.mult),
                 reads=["rkv", "st_b"], writes=[("bon", i)])
            P.op("pool", lambda e, so=so: e.tensor_tensor(out=strm[:, so, 1, :], in0=strm[:, so, 1, :], in1=pexc[:], op=ALU.mult), reads=[("strm", so, 1), ("strm", so, 2), "pexc"], writes=[("strm", so, 1)])
            P.op("pool", lambda e, so=so: e.tensor_tensor(out=strm[:, so, 2, :], in0=strm[:, so, 2, :], in1=pinv[:], op=ALU.mult), reads=[("strm", so, 2), "pinv"], writes=[("strm", so, 2)])
            P.op("pool", lambda e, so=so: e.tensor_tensor(out=strm[:, so, 3, :], in0=strm[:, so, 3, :], in1=pinv[:], op=ALU.mult), reads=[("strm", so, 3), "pinv", "tmp"], writes=[("strm", so, 3)])
            ma.tile(i)
            skeys = [("strm", so, s_) for s_ in range(5)]
            P.op("pool", lambda e, so=so: e.tensor_copy(out=shl[:, so, 0], in_=strm[:, so]), reads=skeys, writes=[("shl", so, 0)])
            P.op("pool", lambda e, so=so: e.tensor_tensor(out=sdf[:], in0=strm[:, so], in1=shl[:, so, 0], op=ALU.subtract), reads=skeys + [("shl", so, 0)], writes=["sdf"])
            P.op("pool", lambda e, so=so: e.tensor_copy(out=shl[:, so, 1], in_=sdf[:]), reads=["sdf"], writes=[("shl", so, 1)])
            for hl in range(2):
                for s_ in range(5):
                    r0 = hl * 10 + 2 * s_
                    P.dma("sp" if hl == 0 else "pool", scr[r0:r0 + 2, i * 128:(i + 1) * 128, :].rearrange("h t k -> t h k"), h3(shl[:, so, hl, s_, :]),
                          reads=[("shl", so, hl)], writes=[("scr", i)])

        P.barrier()
        P.op("dve", lambda e: e.memset(Sb2[:], 0.0), writes=[("S", 0), ("S", 1)])
        nsteps = ntile * 128
        SW, SKK, SNB, SK, SR = range(5)
        SLOT = {0: 0, 4: 1, 1: 2, 2: 3, 3: 4}
        def bcap(par, s_, j):
            f = SLOT[s_] * 256
            return ps[par * 3 + f // 512][:, (f % 512) + j * 64:(f % 512) + (j + 1) * 64]
        def bcblk(par, s_):
            f = SLOT[s_] * 256
            return ps[par * 3 + f // 512][:, (f % 512):(f % 512) + 256]
        if upto < 3:
            P.op('dve', lambda e: e.memset(yT_all[:], 0.0), writes=[('yT', i) for i in range(ntile)])
        for ch in range(nsteps // NST if upto >= 2 else 0):
            slot = ch % 2
            P.dma("sp", rows[:, slot, :].rearrange("p (t k) -> p t k", k=64), scr[:, ch * NST:(ch + 1) * NST, :],
                  reads=[("scr", (ch * NST) // 128)], writes=[("rows", slot)])
            for gg in range(NST // 4):
                g = ch * (NST // 4) + gg
                par = g % 2
                def bc(e, par=par, slot=slot, gg=gg):
                    for s_ in range(5):
                        ins = e.matmul(bcblk(par, s_), lhsT=sel[:, s_, :], rhs=rows[:, slot, gg * 256:(gg + 1) * 256], start=True, stop=True)
                    return ins
                P.op("pe", bc, reads=[("rows", slot), "sel"], writes=[("bc", par)])
                if upto >= 3:
                    pass
                for j in range(4 if upto >= 3 else 0):
                    t = g * 4 + j
                    ti = t // 128
                    cb = ch % 2
                    Sx = Sb2[:, cb, :]
                    kS = ("S", cb)
                    last_in_chunk = (t % NST == NST - 1)

                    def yop(pt, ppar, pj_, pcb, same=True):
                        P.op("dve", lambda e, ppar=ppar, pj_=pj_, pt=pt, pcb=pcb: e.scalar_tensor_tensor(out=junk2[:], in0=Sb2[:, pcb, :], scalar=1.0, in1=bcap(ppar, SR, pj_), op0=ALU.mult, op1=ALU.mult,
                                                                                                   accum_out=yT_all[:, pt:pt + 1]),
                             reads=[("S", pcb), ("bc", ppar)], writes=["junk2", ("yT", pt // 128)], same_ok=same)
                    P.op("dve", lambda e, par=par, j=j, Sx=Sx: e.scalar_tensor_tensor(out=junk[:], in0=Sx, scalar=1.0, in1=bcap(par, SKK, j), op0=ALU.mult, op1=ALU.mult, accum_out=sa[:]),
                         reads=[kS, ("bc", par)], writes=["junk", "sa"], same_ok=(t > 0))
                    P.op("dve", lambda e, par=par, j=j, t=t, Sx=Sx: e.scalar_tensor_tensor(out=Ubuf[:], in0=bcap(par, SK, j), scalar=vT_all[:, t:t + 1], in1=Sx, op0=ALU.mult, op1=ALU.add),
                         reads=[kS, ("bc", par), ("vT", ti)], writes=["Ubuf"], same_ok=(t > 0))
                    if prev_step is not None:
                        yop(*prev_step)
                    P.op("dve", lambda e, par=par, j=j, Sx=Sx: e.scalar_tensor_tensor(out=Sx, in0=bcap(par, SNB, j), scalar=sa[:], in1=Ubuf[:], op0=ALU.mult, op1=ALU.add),
                         reads=["sa", "Ubuf", ("bc", par)], writes=[kS], same_ok=True)
                    prev_step = (t, par, j, cb)
                    if last_in_chunk:
                        yop(*prev_step)
                        prev_step = None
                        P.op("dve", lambda e, par=par, j=j, cb=cb: e.tensor_tensor(out=Sb2[:, 1 - cb, :], in0=Sb2[:, cb, :], in1=bcap(par, SW, j), op=ALU.mult),
                             reads=[("S", cb), ("bc", par)], writes=[("S", 1 - cb)], same_ok=True)
                        P.op("dve", lambda e: e.memset(fill[:, 0:1], 0.0), writes=["fill"], same_ok=True)

        P.barrier()
        for i in range(ntile):
            so = i % 2
            P.op("pe", lambda e, i=i: e.transpose(out=ps[5][:, 0:128], in_=yT_all[:, i * 128:(i + 1) * 128], identity=pj.idf[:]), reads=[("yT", i), "idf"], writes=["ps5"])
            y3 = ps[5][:, 0:128].rearrange("p (h d) -> p h d", h=2)
            P.op("dve", lambda e: e.tensor_reduce(out=st1[:, 0:2], in_=y3, axis=AX.X, op=ALU.add), reads=["ps5"], writes=["st_k"])
            P.op("dve", lambda e: e.tensor_scalar(out=st1[:, 0:2], in0=st1[:, 0:2], scalar1=1.0 / 64, scalar2=None, op0=ALU.mult), reads=["st_k"], writes=["st_k"])
            P.op("dve", lambda e: e.tensor_tensor(out=yc[:], in0=y3, in1=st1[:, 0:2].unsqueeze(2).to_broadcast([128, 2, 64]), op=ALU.subtract), reads=["ps5", "st_k"], writes=["yc"])
            P.op("dve", lambda e: e.tensor_tensor(out=sq[:], in0=yc[:], in1=yc[:], op=ALU.mult), reads=["yc"], writes=["sq"])
            P.op("dve", lambda e: e.tensor_reduce(out=st1[:, 2:4], in_=sq[:], axis=AX.X, op=ALU.add), reads=["sq"], writes=["st_b"])
            P.op("dve", lambda e: e.tensor_scalar(out=st1[:, 2:4], in0=st1[:, 2:4], scalar1=1.0 / 64, scalar2=64e-5, op0=ALU.mult, op1=ALU.add), reads=["st_b"], writes=["st_b"])
            P.op("act", lambda e: e.sqrt(out=st1[:, 2:4], in_=st1[:, 2:4]), reads=["st_b"], writes=["st_b"])
            P.op("dve", lambda e: e.reciprocal(out=st1[:, 2:4], in_=st1[:, 2:4]), reads=["st_b"], writes=["st_b"])
            P.op("dve", lambda e: e.tensor_tensor(out=yc[:], in0=yc[:], in1=st1[:, 2:4].unsqueeze(2).to_broadcast([128, 2, 64]), op=ALU.mult), reads=["yc", "st_b"], writes=["yc"])
            ycf = yc[:].rearrange("p h d -> p (h d)")
            P.op("dve", lambda e: e.tensor_tensor(out=ycf, in0=ycf, in1=cv[:, GNG, :], op=ALU.mult), reads=["yc", "cv"], writes=["yc"])
            P.op("dve", lambda e: e.tensor_tensor(out=ycf, in0=ycf, in1=cv[:, GNB, :], op=ALU.add), reads=["yc", "cv"], writes=["yc"])
            P.op("dve", lambda e, i=i: e.tensor_tensor(out=ycf, in0=ycf, in1=bon_all[:, i, :], op=ALU.add), reads=["yc", ("bon", i)], writes=["yc"])
            P.op("dve", lambda e, i=i, so=so: e.tensor_tensor(out=yo[:, so, :], in0=ycf, in1=g_all[:, i, :], op=ALU.mult), reads=["yc", ("g_all", i)], writes=[("yo", so)])
            P.dma("sp", ysrc[i * 128:(i + 1) * 128, 128:256], yo[:, so, :], reads=[("yo", so)], writes=["ysrc"])
        P.emit(last=is_last)


def prep_MB(layer, h, inputs):
    i = layer
    maps = []
    w = inputs["w_in"][i]
    sel = np.zeros((20, 5, 128), np.float32)
    for hl in range(2):
        for s_ in range(5):
            for hh in range(2):
                sel[hl * 10 + s_ * 2 + hh, s_, hh * 64:(hh + 1) * 64] = 1.0
    for c in range(8):
        b, gi = c // 2, c % 2
        hc = slice(gi * 128, (gi + 1) * 128)
        cols = np.concatenate([512 + np.arange(gi * 128, (gi + 1) * 128), 768 + np.arange(gi * 128, (gi + 1) * 128), 1024 + np.arange(gi * 128, (gi + 1) * 128),
                               np.arange(1280, 1536)])
        maps.append({
            "h": np.ascontiguousarray(h[b]), "identf": np.eye(128, dtype=np.float32),
            "wc": np.ascontiguousarray(np.concatenate([w[:, cols], w[:, gi * 128:(gi + 1) * 128], w[:, 256 + gi * 128:256 + (gi + 1) * 128]], axis=1)),
            "a_lng": bc128(inputs["gm_ln_g"][i][2 * gi:2 * gi + 2].reshape(-1)), "a_lnb": bc128(inputs["gm_ln_b"][i][2 * gi:2 * gi + 2].reshape(-1)),
            "a_ws": np.ascontiguousarray(inputs["gm_ws"][i][2 * gi:2 * gi + 2]), "a_tril": np.tril(np.ones((128, 128), np.float32)),
            "a_bs": np.ascontiguousarray(inputs["gm_bs"][i][2 * gi:2 * gi + 2].T),
            "g_mix": pc8(inputs["g_mix"][i]), "mu": bc128(inputs["rw_mu"][i][cols - 512]),
            "wa_up": np.ascontiguousarray(np.concatenate([inputs["rw_w_up"][i][:, hc], inputs["rw_a_up"][i][:, hc]], axis=0)),
            "g_up": np.ascontiguousarray(inputs["rw_g_up"][i][:, hc]),
            "w0a0": np.concatenate([inputs["rw_w0"][i][hc], inputs["rw_a0"][i][hc]])[None, :].astype(np.float32),
            "cvec": np.ascontiguousarray(np.stack([bc128(inputs["rw_k_k"][i][hc]), bc128(inputs["rw_k_a"][i][hc]), bc128(inputs["rw_r_k"][i].reshape(-1)[hc]),
                                                   bc128(inputs["rw_gn_g"][i][hc]), bc128(inputs["rw_gn_b"][i][hc])], axis=1)),
            "sel": sel,
            "mcum": ((np.arange(128)[:, None] // 32 == np.arange(128)[None, :] // 32) & (np.arange(128)[:, None] <= np.arange(128)[None, :])).astype(np.float32),
        })
    return maps


NEGB = -30000.0
MC_BR = 'csw'


def phase_MC(nc, P, ps, psb, pre, h_in, ysrc, hmap=None, ntile=NTILE, upto=9, is_last=False):
    import ml_dtypes
    di = lambda n, s, dt=F32: nc.dram_tensor(pre + n, s, dt, kind="ExternalInput").ap()
    identf = di("identf", [128, 128])
    wc = di("wc", [D, 652])
    g_mix = di("g_mix", [128, 8])
    pos_in = di("pos", [128, NTILE], I32)
    invf = di("invf", [128, 8])
    w1kc = di("w1kc", [2048, 128]); w1vc = di("w1vc", [2048, 128])
    w2kc = di("w2kc", [128, 64]); w2vc = di("w2vc", [128, 64])
    posT = di("posT", [64, 32])
    rconst = di("rconst", [128, 2, 65])
    cmask = di("cmask", [NTILE, 128, 2, 128], BF16)
    selc = di("selc", [NTILE, 128, 2, 64])
    Ef_in = di("Ef", [64, S], BF16)
    tri_in = di("tri", [128, 2, 128], BF16)
    with ExitStack() as st:
        sb = lambda name, shape, dt: st.enter_context(nc.sbuf_tensor(pre + "s_" + name, shape, dt))
        pj = Proj(nc, P, st, h_in, identf, 652, wc, g_mix, pre=pre, hmap=hmap)
        qrT = sb("qrT", [64, 4, S], BF16)
        qwT = sb("qwT", [64, 4, S], BF16)
        kT4 = sb("kT4", [64, 4, S], BF16)
        vaug = sb("vaug", [128, 2, NTILE, 65], BF16)
        ksE = sb("ksE", [128, S], BF16)
        qs = sb("qs", [128, 2, 4, 128], BF16)
        tri = sb("tris", [128, 2, 128], BF16)
        w1s = sb("w1s", [64, 32, 128], F32)
        w1b = sb("w1b", [64, 2, 32, 128], BF16)
        w2f = sb("w2f", [128, 2, 64], F32); w2b = sb("w2b", [128, 2, 64], BF16)
        posf = sb("posf", [64, 32], F32); posb = sb("posb", [64, 32], BF16)
        rcf = sb("rcf", [128, 2, 65], F32)
        R_ = sb("R_", [128, 2, 129], BF16)
        posi = sb("posi", [128, NTILE], I32); posfl = sb("posfl", [128, NTILE], F32)
        inv = sb("inv", [128, 8], F32)
        ang = sb("ang", [128, NTILE, 8], F32)
        cs = sb("cs", [128, NTILE, 8], F32); sn = sb("sn", [128, NTILE, 8], F32)
        gsig = sb("gsig", [128, NTILE, 12], F32)
        xq = sb("xq", [128, 512], F32)
        xqb = sb("xqb", [128, 512], BF16)
        qkr = sb("qkr", [128, 6, 64], BF16)
        ra = sb("ra", [128, 6, 8], F32); rb_ = sb("rb_", [128, 6, 8], F32)
        bias_sb = sb("bias_sb", [128, 2], F32)
        gelT = sb("gelT", [128, 2, 256], BF16)
        kcmpT = sb("kcmpT", [64, 256], BF16)
        cm = sb("cm", [128, 2, 2, 128], BF16)
        scs = sb("scs", [128, 2, 2, 64], F32)
        eT = sb("eT", [128, 2, 4, 128], BF16)
        imp = sb("imp", [128, 64], F32)
        imp2 = sb("imp2", [128, 64], F32)
        rep = sb("rep", [128, 64], F32)
        mx = sb("mx", [128, 8], F32)
        selbb = sb("selbb", [128, 128], BF16)
        selbT = sb("selbT", [64, 128], BF16)
        sm = sb("sm", [128, 16], F32)
        oacc = sb("oacc", [128, 2, 4, 64], F32)

        ld = lambda dst, src, key: P.dma("sp", dst, src, writes=[key])
        ld(ksE[64:128, :], Ef_in, "ksE_E"); ld(tri[:], tri_in, "tri")
        P.op("pool", lambda e: e.memset(selbb[:], 0.0), writes=["selbb"])
        ld(posi[:], pos_in, "posi"); ld(inv[:], invf, "inv")
        ld(rcf[:], rconst, "rcf"); ld(posf[:], posT, "posf")
        ld(w2f[:, 0, :], w2kc, "w2f0"); ld(w2f[:, 1, :], w2vc, "w2f1")
        P.op("dve", lambda e: e.tensor_copy(out=w2b[:], in_=w2f[:]), reads=["w2f0", "w2f1"], writes=["w2b"])
        P.op("dve", lambda e: e.tensor_copy(out=posb[:], in_=posf[:]), reads=["posf"], writes=["posb"])
        P.op("dve", lambda e: e.memset(R_[:], 0.0), writes=["R"])
        P.op("dve", lambda e: e.tensor_copy(out=R_[:, :, 0:65], in_=rcf[:]), reads=["rcf", "R"], writes=["R"])
        P.op("pool", lambda e: e.memset(vaug[:], 1.0), writes=["vaug_init"])
        P.op("pool", lambda e: e.memset(gelT[:], 0.0), writes=["gelT0", "gelT1"])
        for w in range(2):
            P.dma("sp", w1s[:], (w1kc if w == 0 else w1vc).rearrange("(l d) h -> d l h", d=64), writes=["w1s"])
            P.op("pool", lambda e, w=w: e.tensor_copy(out=w1b[:, w, :, :], in_=w1s[:]), reads=["w1s"], writes=[("w1b", w)])
        P.op("dve", lambda e: e.tensor_copy(out=posfl[:], in_=posi[:]), reads=["posi"], writes=["posfl"])
        P.op("dve", lambda e: e.tensor_tensor(out=ang[:], in0=posfl[:].unsqueeze(2).to_broadcast([128, NTILE, 8]), in1=inv[:].unsqueeze(1).to_broadcast([128, NTILE, 8]), op=ALU.mult),
             reads=["posfl", "inv"], writes=["ang"])
        PI = float(np.pi)
        angi = sb("angi", [128, NTILE, 8], I32)
        angf = sb("angf", [128, NTILE, 8], F32)
        for (dst, off, key) in ((sn, 0.5, "sn"), (cs, 0.75, "cs")):
            P.op("dve", lambda e, dst=dst, off=off: e.tensor_scalar(out=dst[:], in0=ang[:], scalar1=1.0 / (2 * PI), scalar2=off, op0=ALU.mult, op1=ALU.add), reads=["ang"], writes=[key])
            P.op("dve", lambda e, dst=dst: e.tensor_copy(out=angi[:], in_=dst[:]), reads=[key], writes=["angi"])
            P.op("dve", lambda e: e.tensor_copy(out=angf[:], in_=angi[:]), reads=["angi"], writes=["angf"])
            P.op("dve", lambda e, dst=dst: e.tensor_tensor(out=dst[:], in0=dst[:], in1=angf[:], op=ALU.subtract), reads=[key, "angf"], writes=[key])
            P.op("dve", lambda e, dst=dst: e.tensor_scalar(out=angf[:], in0=dst[:], scalar1=0.0, scalar2=None, op0=ALU.is_lt), reads=[key], writes=["angf"])
            P.op("dve", lambda e, dst=dst: e.tensor_tensor(out=dst[:], in0=dst[:], in1=angf[:], op=ALU.add), reads=[key, "angf"], writes=[key])
            P.op("dve", lambda e, dst=dst: e.tensor_scalar(out=dst[:], in0=dst[:], scalar1=2 * PI, scalar2=-PI, op0=ALU.mult, op1=ALU.add), reads=[key], writes=[key])
            P.op("dve", lambda e, dst=dst: e.tensor_scalar(out=dst[:], in0=dst[:], scalar1=PI, scalar2=-PI, op0=ALU.min, op1=ALU.max), reads=[key], writes=[key])
            P.op("act", lambda e, dst=dst: e.activation(out=dst[:], in_=dst[:], func=AF.Sin), reads=[key], writes=[key])

        pj.tile(0, psb[0], "ps0")
        for i in range(ntile):
            if i + 1 < ntile:
                pj.tile(i + 1, psb[0], "ps0")
            P.op("pe", lambda e, i=i: pj.mm_tok(e, i, ps[1][:], 0, 512), reads=pj.keys(i), writes=["ps1"])
            P.op("pe", lambda e, i=i: pj.mm_tok(e, i, ps[2][:, 0:140], 512, 652), reads=pj.keys(i), writes=["ps2"])
            P.op("act", lambda e: e.activation(out=xq[:], in_=ps[1][:], func=AF.Copy), reads=["ps1"], writes=["xq"])
            P.op("act", lambda e, i=i: e.activation(out=vaug[:, :, i, 0:64], in_=ps[2][:, 0:128].rearrange("p (a d) -> p a d", a=2), func=AF.Copy), reads=["ps2", "vaug_init"], writes=[("vaug", i)])
            P.op("act", lambda e, i=i: e.activation(out=gsig[:, i, :], in_=ps[2][:, 128:140], func=AF.Sigmoid), reads=["ps2"], writes=[("gsig", i)])
            P.op("pool", lambda e: e.tensor_copy(out=xqb[:], in_=xq[:]), reads=["xq"], writes=["xqb"])
            X = xq[:, 0:384].rearrange("p (h d) -> p h d", h=6)
            cb = cs[:, i, :].unsqueeze(1).to_broadcast([128, 6, 8])
            sbb = sn[:, i, :].unsqueeze(1).to_broadcast([128, 6, 8])
            P.op("pool", lambda e, X=X: e.tensor_copy(out=qkr[:, :, 16:64], in_=X[:, :, 16:64]), reads=["xq"], writes=["qkr_c"])
            P.op("dve", lambda e, X=X, cb=cb: e.tensor_tensor(out=ra[:], in0=X[:, :, 0:8], in1=cb, op=ALU.mult), reads=["xq", "cs"], writes=["ra"])
            P.op("dve", lambda e, X=X, sbb=sbb: e.tensor_tensor(out=rb_[:], in0=X[:, :, 8:16], in1=sbb, op=ALU.mult), reads=["xq", "sn"], writes=["rb"])
            P.op("dve", lambda e: e.tensor_tensor(out=qkr[:, :, 0:8], in0=ra[:], in1=rb_[:], op=ALU.subtract), reads=["ra", "rb"], writes=["qkr_a"])
            P.op("dve", lambda e, X=X, sbb=sbb: e.tensor_tensor(out=ra[:], in0=X[:, :, 0:8], in1=sbb, op=ALU.mult), reads=["xq", "sn", "ra"], writes=["ra"])
            P.op("dve", lambda e, X=X, cb=cb: e.tensor_tensor(out=rb_[:], in0=X[:, :, 8:16], in1=cb, op=ALU.mult), reads=["xq", "cs", "rb"], writes=["rb"])
            P.op("dve", lambda e: e.tensor_tensor(out=qkr[:, :, 8:16], in0=ra[:], in1=rb_[:], op=ALU.add), reads=["ra", "rb"], writes=["qkr_b"])
            def trs(e):
                for j in range(4):
                    e.transpose(out=psb[3][0:64, j * 128:(j + 1) * 128], in_=qkr[:, j, :], identity=pj.idb[:])
                for j in range(4):
                    e.transpose(out=psb[5][0:64, j * 128:(j + 1) * 128], in_=xqb[:, j * 64:(j + 1) * 64], identity=pj.idb[:])
                e.transpose(out=psb[4][0:64, 0:128], in_=qkr[:, 4, :], identity=pj.idb[:])
                e.transpose(out=psb[4][0:64, 128:256], in_=qkr[:, 5, :], identity=pj.idb[:])
                e.transpose(out=psb[4][0:64, 256:384], in_=xqb[:, 384:448], identity=pj.idb[:])
                return e.transpose(out=psb[4][0:64, 384:512], in_=xqb[:, 448:512], identity=pj.idb[:])
            P.op("pe", trs, reads=["qkr_a", "qkr_b", "qkr_c", "xqb", "idb"], writes=["ps3", "ps4", "ps5"])
            tsl = slice(i * 128, (i + 1) * 128)
            P.op("act", lambda e, tsl=tsl: e.activation(out=qrT[:, :, tsl], in_=psb[3][0:64, 0:512].rearrange("p (j t) -> p j t", j=4), func=AF.Copy), reads=["ps3"], writes=[("qrT", i)])
            P.op("dve", lambda e, tsl=tsl: e.tensor_copy(out=qwT[:, :, tsl], in_=psb[5][0:64, 0:512].rearrange("p (j t) -> p j t", j=4)), reads=["ps5"], writes=[("qwT", i)])
            P.op("act", lambda e, tsl=tsl: e.activation(out=kT4[:, :, tsl], in_=psb[4][0:64, 0:512].rearrange("p (j t) -> p j t", j=4), func=AF.Copy), reads=["ps4"], writes=[("kT4", i)])
            P.op("act", lambda e, tsl=tsl: e.activation(out=ksE[0:64, tsl], in_=psb[4][0:64, 0:128], func=AF.Copy), reads=["ps4"], writes=[("ksE", i)])

        P.barrier()
        ncmp = (ntile * 128 - 32) // 16 + 1
        for w in range(2 if upto >= 2 else 0):
            src = 2 + w
            def hid(e, w=w, src=src):
                for l in range(32):
                    ins = e.matmul(ps[0][:, 0:ncmp], lhsT=w1b[:, w, l, :], rhs=kT4[:, src, l:l + 16 * (ncmp - 1) + 1:16], start=(l == 0), stop=(l == 31))
                return ins
            P.op("pe", hid, reads=[("w1b", w)], writes=["ps0"])
            def pbias(e, w=w):
                for l in range(32):
                    ins = e.matmul(ps[1][:, 0:1], lhsT=w1b[:, w, l, :], rhs=posb[:, l:l + 1], start=(l == 0), stop=(l == 31))
                return ins
            P.op("pe", pbias, reads=[("w1b", w), "posb"], writes=["ps1"])
            P.op("dve", lambda e, w=w: e.tensor_copy(out=bias_sb[:, w:w + 1], in_=ps[1][:, 0:1]), reads=["ps1"], writes=[("bias", w)])
            P.op("act", lambda e, w=w: e.activation(out=gelT[:, w, 0:ncmp], in_=ps[0][:, 0:ncmp], func=AF.Gelu_apprx_tanh, bias=bias_sb[:, w:w + 1]), reads=["ps0", ("bias", w), f"gelT{w}"], writes=[f"gelT{w}"])
        if upto >= 2:
            P.op("pe", lambda e: e.matmul(ps[2][0:64, 0:256], lhsT=w2b[:, 0, :], rhs=gelT[:, 0, :], start=True, stop=True), reads=["w2b", "gelT0"], writes=["ps2"])
            P.op("act", lambda e: e.activation(out=kcmpT[:], in_=ps[2][0:64, 0:256], func=AF.Copy), reads=["ps2"], writes=["kcmpT"])
        for cn in range(2 if upto >= 2 else 0):
            P.op("pe", lambda e, cn=cn: e.matmul(ps[3][:, cn * 64:(cn + 1) * 64], lhsT=gelT[:, 1, cn * 128:(cn + 1) * 128], rhs=w2b[:, 1, :], start=True, stop=True), reads=["w2b", "gelT1"], writes=["ps3"])
            P.op("dve", lambda e, cn=cn: e.tensor_copy(out=R_[:, cn, 65:129], in_=ps[3][:, cn * 64:(cn + 1) * 64]), reads=["ps3", "R"], writes=["R"])

        P.barrier()
        nsc = 0
        if upto < 3:
            P.op('dve', lambda e: e.memset(oacc[:], 0.0), writes=[('oacc', 0), ('oacc', 1)])
            P.dma('sp', ysrc[0:128, 256:512], oacc[:, 0].rearrange('p j d -> p (j d)'), reads=[('oacc', 0)], writes=['ysrc'])
        for qb in range(ntile if upto >= 3 else 0):
            tsl = slice(qb * 128, (qb + 1) * 128)
            so = qb % 2
            P.dma("sp", cm[:, so], cmask[qb], writes=[("cm", so)])
            P.dma("sp", scs[:, so], selc[qb], writes=[("scs", so)])
            ncn = 2 if qb >= 16 else 1
            for cn in range(ncn):
                sbk = nsc % 2; nsc += 1
                def sc(e, cn=cn, sbk=sbk, tsl=tsl, so=so):
                    e.matmul(ps[sbk][:], lhsT=kcmpT[:, cn * 128:(cn + 1) * 128], rhs=qwT[:, :, tsl], start=True, stop=False)
                    return e.matmul(ps[sbk][:], lhsT=pj.idb[:], rhs=cm[:, so, cn, :].unsqueeze(1).to_broadcast([128, 4, 128]), start=False, stop=True)
                P.op("pe", sc, reads=["kcmpT", ("qwT", qb), ("cm", so), "idb"], writes=[f"ps{sbk}"])
                P.op("act", lambda e, sbk=sbk: e.activation(out=eT[:, sbk].rearrange("p j t -> p (j t)"), in_=ps[sbk][:], func=AF.Exp, scale=0.125), reads=[f"ps{sbk}"], writes=[("eT", sbk)])
                def pv(e, cn=cn, sbk=sbk, ncn=ncn):
                    for j in range(4):
                        ins = e.matmul(ps[2 + j // 2][:, (j % 2) * 129:(j % 2) * 129 + 129], lhsT=eT[:, sbk, j, :], rhs=R_[:, cn, :], start=(cn == 0 and j % 2 == 0), stop=(cn == ncn - 1), skip_group_check=True)
                    return ins
                P.op("pe", pv, reads=[("eT", sbk), "R"], writes=["ps2", "ps3"])
            P.op("dve", lambda e: e.memset(imp[:], 0.0), writes=["imp"])
            for j in range(4):
                pso = ps[2 + j // 2][:, (j % 2) * 129:(j % 2) * 129 + 129]
                ri = sm[:, j:j + 1]; rg = sm[:, 4 + j:5 + j]
                P.op("dve", lambda e, pso=pso, ri=ri: e.tensor_scalar(out=ri, in0=pso[:, 64:65], scalar1=1e-30, scalar2=None, op0=ALU.add), reads=["ps2", "ps3"], writes=[("ri", j)])
                P.op("dve", lambda e, ri=ri: e.reciprocal(out=ri, in_=ri), reads=[("ri", j)], writes=[("ri", j)])
                P.op("dve", lambda e, pso=pso, ri=ri: e.scalar_tensor_tensor(out=imp[:], in0=pso[:, 0:64], scalar=ri, in1=imp[:], op0=ALU.mult, op1=ALU.add), reads=["ps2", "ps3", ("ri", j), "imp"], writes=["imp"])
                P.op("dve", lambda e, ri=ri, rg=rg, j=j, qb=qb: e.tensor_tensor(out=rg, in0=ri, in1=gsig[:, qb, 3 * j:3 * j + 1], op=ALU.mult), reads=[("ri", j), ("gsig", qb)], writes=[("rg", j)])
                P.op("dve", lambda e, pso=pso, rg=rg, j=j, so=so: e.tensor_scalar(out=oacc[:, so, j, :], in0=pso[:, 65:129], scalar1=rg, scalar2=None, op0=ALU.mult), reads=["ps2", "ps3", ("rg", j)], writes=[("oacc", so)])
            if upto < 4:
                P.dma('sp', ysrc[tsl, 256:512], oacc[:, so].rearrange('p j d -> p (j d)'), reads=[('oacc', so)], writes=['ysrc'])
                continue
            P.op("dve", lambda e, so=so: e.tensor_tensor(out=imp2[:], in0=imp[:], in1=scs[:, so, 0, :], op=ALU.mult), reads=["imp", ("scs", so)], writes=["imp2"])
            P.op("dve", lambda e, so=so: e.tensor_tensor(out=imp2[:], in0=imp2[:], in1=scs[:, so, 1, :], op=ALU.add), reads=["imp2", ("scs", so)], writes=["imp2"])
            P.op("dve", lambda e: e.max(out=mx[:], in_=imp2[:]), reads=["imp2"], writes=["mx"])
            P.op("dve", lambda e: e.match_replace(out=rep[:], in_to_replace=mx[:], in_values=imp2[:], imm_value=-1e30), reads=["imp2", "mx"], writes=["rep"])
            P.op("dve", lambda e: e.max(out=mx[:], in_=rep[:]), reads=["rep"], writes=["mx"])
            P.op("dve", lambda e: e.tensor_scalar(out=mx[:, 7:8], in0=mx[:, 7:8], scalar1=-5000.0, scalar2=None, op0=ALU.max), reads=["mx"], writes=["mx"])
            P.op("dve", lambda e: e.tensor_scalar(out=rep[:], in0=imp2[:], scalar1=mx[:, 7:8], scalar2=None, op0=ALU.is_ge), reads=["imp2", "mx", "rep"], writes=["rep"])
            P.op("dve", lambda e: e.tensor_scalar(out=selbb[:, 64:128], in0=rep[:], scalar1=-NEGB, scalar2=NEGB, op0=ALU.mult, op1=ALU.add), reads=["rep", "selbb"], writes=["selbb"])
            P.op("pe", lambda e: e.transpose(out=psb[6][:, 0:128], in_=selbb[:], identity=pj.idb[:]), reads=["selbb", "idb"], writes=["ps6"])
            P.op("act", lambda e, so=so: e.activation(out=qs[64:128, so], in_=psb[6][64:128, 0:128].unsqueeze(1).to_broadcast([64, 4, 128]), func=AF.Copy), reads=["ps6"], writes=[("qs_b", so)])
            P.op("pool", lambda e, so=so, tsl=tsl: e.tensor_copy(out=qs[0:64, so], in_=qrT[:, :, tsl]), reads=[("qrT", qb)], writes=[("qs_a", so)])
            jobs = [("s", c) for c in range(qb + 1)] + [("w", c) for c in range(max(0, qb - 4), qb + 1)]
            pend = None
            first = {"s": True, "w": True}
            last_c = {"s": qb, "w": qb}
            for job in jobs + [(None, None)]:
                kind, c = job
                cur = None
                if kind is not None:
                    sbk = nsc % 2; nsc += 1
                    ksrc = 0 if kind == "s" else 1
                    def sc2(e, kind=kind, c=c, sbk=sbk, ksrc=ksrc, tsl=tsl, qb=qb):
                        extra = []
                        if c == qb:
                            extra.append((pj.idb[:], tri[:, 0, :].unsqueeze(1).to_broadcast([128, 4, 128])))
                        if kind == "w" and c == qb - 4:
                            extra.append((pj.idb[:], tri[:, 1, :].unsqueeze(1).to_broadcast([128, 4, 128])))
                        if kind == "s":
                            ins = e.matmul(ps[sbk][:], lhsT=ksE[:, c * 128:(c + 1) * 128], rhs=qs[:, qb % 2], start=True, stop=(len(extra) == 0))
                        else:
                            ins = e.matmul(ps[sbk][:], lhsT=kT4[:, ksrc, c * 128:(c + 1) * 128], rhs=qrT[:, :, tsl], start=True, stop=(len(extra) == 0))
                        for n_, (l_, r_) in enumerate(extra):
                            ins = e.matmul(ps[sbk][:], lhsT=l_, rhs=r_, start=False, stop=(n_ == len(extra) - 1))
                        return ins
                    P.op("pe", sc2, reads=[("kT4", c), ("ksE", c), "ksE_E", ("qrT", qb), ("qs_a", qb % 2), ("qs_b", qb % 2), "tri", "idb"], writes=[f"ps{sbk}"])
                    P.op("act", lambda e, sbk=sbk: e.activation(out=eT[:, sbk].rearrange("p j t -> p (j t)"), in_=ps[sbk][:], func=AF.Exp, scale=0.125), reads=[f"ps{sbk}"], writes=[("eT", sbk)])
                    cur = (kind, c, sbk)
                if pend is not None:
                    pk, pc, pb = pend
                    bank = 4 if pk == "s" else 5
                    vi = 0 if pk == "s" else 1
                    c0 = 0 if pk == "s" else max(0, qb - 4)
                    def pv2(e, pk=pk, pc=pc, pb=pb, bank=bank, vi=vi, c0=c0, qb=qb):
                        for j in range(4):
                            ins = e.matmul(ps[bank][:, j * 65:(j + 1) * 65], lhsT=eT[:, pb, j, :], rhs=vaug[:, vi, pc, :], start=(pc == c0 and j == 0), stop=(pc == qb), skip_group_check=True)
                        return ins
                    P.op("pe", pv2, reads=[("eT", pb), ("vaug", pc)], writes=[f"ps{bank}"])
                pend = cur
            for j in range(4):
                for (bank, gcol, tag) in ((4, 1, "s"), (5, 2, "w")):
                    if tag not in MC_BR:
                        continue
                    pso = ps[bank][:, j * 65:(j + 1) * 65]
                    ri = sm[:, 8:9]
                    P.op("dve", lambda e, pso=pso, ri=ri: e.reciprocal(out=ri, in_=pso[:, 64:65]), reads=[f"ps{bank}"], writes=["ri2"])
                    P.op("dve", lambda e, ri=ri, j=j, gcol=gcol, qb=qb: e.tensor_tensor(out=ri, in0=ri, in1=gsig[:, qb, 3 * j + gcol:3 * j + gcol + 1], op=ALU.mult), reads=["ri2", ("gsig", qb)], writes=["ri2"])
                    P.op("dve", lambda e, pso=pso, ri=ri, j=j, so=so: e.scalar_tensor_tensor(out=oacc[:, so, j, :], in0=pso[:, 0:64], scalar=ri, in1=oacc[:, so, j, :], op0=ALU.mult, op1=ALU.add),
                         reads=[f"ps{bank}", "ri2", ("oacc", so)], writes=[("oacc", so)])
            P.dma("sp", ysrc[tsl, 256:512], oacc[:, so].rearrange("p j d -> p (j d)"), reads=[("oacc", so)], writes=["ysrc"])
        P.emit(last=is_last)


def nsa_consts():
    import ml_dtypes
    bf = ml_dtypes.bfloat16
    n = np.arange(256)[:, None]
    cmask = np.zeros((NTILE, 128, 2, 128), np.float32)
    for qb in range(NTILE):
        t = qb * 128 + np.arange(128)[None, :]
        ok = (16 * n + 31 <= t) & (n < 255)
        m = np.where(ok, 0.0, NEGB).astype(np.float32)
        cmask[qb] = m.reshape(2, 128, 128).transpose(1, 0, 2)
    selc = np.zeros((NTILE, 128, 2, 64), np.float32)
    mids = np.arange(64)[None, :]
    for qb in range(NTILE):
        t = qb * 128 + np.arange(128)[:, None]
        blk = t // 64
        valid = mids <= blk
        forced = (mids == 0) | (mids == blk) | (mids == blk - 1)
        selc[qb, :, 0, :] = valid.astype(np.float32)
        selc[qb, :, 1, :] = np.where(valid, 1e4 * forced.astype(np.float32), -1e4)
    Ef = (np.arange(S)[None, :] // 64 == np.arange(64)[:, None]).astype(np.float32)
    Eb = np.zeros((64, NTILE, 128), np.float32)
    for c in range(NTILE):
        for sl in range(128):
            Eb[2 * c + sl // 64, c, sl] = 1.0
    s_ = np.arange(128)[:, None]; t_ = np.arange(128)[None, :]
    tri = np.zeros((128, 2, 128), np.float32)
    tri[:, 0, :] = np.where(s_ > t_, NEGB, 0.0)
    tri[:, 1, :] = np.where(s_ <= t_, NEGB, 0.0)
    cmp_idx = np.arange(255)[:, None] * 16 + np.arange(32)[None, :]
    slc_start = np.arange(64) * 64
    overlap = ((cmp_idx[:, :1] < slc_start[None, :] + 64) & (cmp_idx[:, -1:] >= slc_start[None, :])).astype(np.float32)
    rc = np.zeros((256, 65), np.float32)
    rc[:255, :64] = overlap
    rc[:255, 64] = 1.0
    rconst = np.ascontiguousarray(rc.reshape(2, 128, 65).transpose(1, 0, 2))
    inv = (1.0 / (np.float32(500000.0) ** (np.arange(0, 16, 2, dtype=np.float32) / np.float32(16)))).astype(np.float32)
    return {"cmask": cmask.astype(bf), "selc": selc, "Ef": Ef.astype(bf), "tri": tri.astype(bf), "rconst": rconst, "invf": bc128(inv)}


def prep_MC(layer, h, inputs):
    i = layer
    maps = []
    w = inputs["w_in"][i]
    cst = nsa_consts()
    for c in range(8):
        b, gi = c // 2, c % 2
        o = 1536
        cols = np.concatenate([o + np.arange(gi * 256, (gi + 1) * 256),
                               o + 512 + 2 * 128 + np.arange(gi * 64, (gi + 1) * 64),
                               o + 512 + 4 * 128 + np.arange(gi * 64, (gi + 1) * 64),
                               o + 512 + 0 * 128 + np.arange(gi * 64, (gi + 1) * 64),
                               o + 512 + 1 * 128 + np.arange(gi * 64, (gi + 1) * 64),
                               o + 512 + 3 * 128 + np.arange(gi * 64, (gi + 1) * 64),
                               o + 512 + 5 * 128 + np.arange(gi * 64, (gi + 1) * 64),
                               o + 512 + 6 * 128 + np.arange(gi * 12, (gi + 1) * 12)])
        m = {
            "h": np.ascontiguousarray(h[b]), "identf": np.eye(128, dtype=np.float32), "wc": np.ascontiguousarray(w[:, cols]),
            "g_mix": pc8(inputs["g_mix"][i]),
            "pos": np.ascontiguousarray(inputs["positions"][b].reshape(NTILE, 128).T.astype(np.int32)),
            "w1kc": inputs["nsa_kc_w1"][i], "w1vc": inputs["nsa_vc_w1"][i], "w2kc": inputs["nsa_kc_w2"][i], "w2vc": inputs["nsa_vc_w2"][i],
            "posT": np.ascontiguousarray(inputs["nsa_cmp_pos"][i].T),
        }
        m.update(cst)
        maps.append(m)
    return maps


PAIRS = [[0, 1], [2, 3], [4, 5], [6, 7]]


def build_fused():
    nc = bass.Bass("TRN2", target_bir_lowering=False)
    xfull = nc.dram_tensor("xfull", [S, D], F32, kind="ExternalInput").ap()
    xhalf = nc.dram_tensor("xhalf", [TF, D], F32, kind="ExternalInput").ap()
    h_out = nc.dram_tensor("h_out", [TF, D], F32, kind="ExternalOutput").ap()
    ysrc = nc.dram_tensor("ysrc", [S, 512], F32).ap()
    ydst = nc.dram_tensor("ydst", [2 * S, 512], F32).ap()
    hsrc = nc.dram_tensor("hsrc", [TF, D], F32).ap()
    hfull = nc.dram_tensor("hfull", [S, D], F32).ap()
    scr = nc.dram_tensor("scr", [20, S, 64], BF16).ap()
    with ExitStack() as st:
        P = Prog(nc, st)
        ps = [st.enter_context(nc.psum_tensor(f"ps{i}", [128, 512], F32)) for i in range(8)]
        psb = [p_[:].bitcast(BF16) for p_ in ps]
        for layer in range(2):
            moe = layer % 2 == 1
            final = layer == 1
            hin = xfull if layer == 0 else hfull
            hmap = None if layer == 0 else (lambda i: ((i * 128) % 2048) // 512 * 1024 + ((i * 128) // 2048) * 512 + (i * 128) % 512)
            P.barrier()
            phase_MB(nc, P, ps, psb, f"L{layer}B_", hin, ysrc, scr, hmap=hmap)
            P.barrier()
            phase_MC(nc, P, ps, psb, f"L{layer}C_", hin, ysrc, hmap=hmap)
            P.barrier()
            for q in range(4):
                P.collective(lambda e, q=q: e.collective_compute("AllGather", ALU.bypass, replica_groups=PAIRS, ins=[ysrc[q * 1024:(q + 1) * 1024, :]], outs=[ydst[q * 2048:(q + 1) * 2048, :]]),
                             reads=["ysrc"], writes=["ydst"])
            P.emit()
            P.barrier()
            phase_F(nc, P, ps, psb, f"L{layer}F_", 8 if moe else 1, moe, final, xhalf if layer == 0 else hsrc, ydst, h_out if final else hsrc, final)
            if not final:
                P.barrier()
                for q in range(4):
                    P.collective(lambda e, q=q: e.collective_compute("AllGather", ALU.bypass, replica_groups=PAIRS, ins=[hsrc[q * 512:(q + 1) * 512, :]], outs=[hfull[q * 1024:(q + 1) * 1024, :]]),
                                 reads=["f_out"], writes=["hfull"])
                P.emit()
    return nc


W_OUT_PERM = np.concatenate([np.arange(0, 128), np.arange(256, 384), np.arange(512, 768), np.arange(128, 256), np.arange(384, 512), np.arange(768, 1024)])


def kernel(**inputs):
    inputs = {k: np.asarray(v) for k, v in inputs.items()}
    x = np.ascontiguousarray(inputs["x"], dtype=np.float32)
    hd = np.zeros((4, 1, 1), np.float32)
    maps = [dict() for _ in range(8)]
    for layer in range(2):
        moe = layer % 2 == 1
        final = layer == 1
        for tag, prep in (("B", prep_MB), ("C", prep_MC)):
            pm = prep(layer, hd, inputs)
            for c in range(8):
                for k, v in pm[c].items():
                    if k != "h":
                        maps[c][f"L{layer}{tag}_{k}"] = v
        dummy = np.zeros((8 * TF, 1), np.float32)
        pf = prep_F_inputs(layer, dummy, dummy, inputs, moe, final)
        wperm = np.ascontiguousarray(inputs["w_out"][layer][W_OUT_PERM, :])
        for c in range(8):
            for k, v in pf[c].items():
                if k in ("h", "y"):
                    continue
                maps[c][f"L{layer}F_{k}"] = wperm if k == "w_out" else v
            sel = np.zeros((128, 2), np.float32)
            sel[:, c % 2] = 1.0
            maps[c][f"L{layer}F_selv"] = sel
    for c in range(8):
        b, gi = c // 2, c % 2
        maps[c]["xfull"] = x[b]
        maps[c]["xhalf"] = np.ascontiguousarray(x[b, gi * TF:(gi + 1) * TF])
    nc = build_fused()
    res = run_bass_kernel_spmd(nc, maps, core_ids=list(range(8))).results
    out = np.empty((4, S, D), np.float32)
    for c in range(8):
        b, gi = c // 2, c % 2
        out[b, gi * TF:(gi + 1) * TF] = res[c]["h_out"]
    return out
```

```python
import numpy as np
from contextlib import ExitStack
import concourse.bass as bass
import concourse.mybir as mybir
from concourse.bass_utils import run_bass_kernel_spmd

F32 = mybir.dt.float32
BF16 = mybir.dt.bfloat16
I32 = mybir.dt.int32
AF = mybir.ActivationFunctionType
ALU = mybir.AluOpType
AX = mybir.AxisListType

EPOCH = 20000
NDMA = 24


class Prog:
    ENGS = ("pe", "act", "dve", "pool", "sp")

    def __init__(self, nc, stack):
        self.nc = nc
        self.stack = stack
        self.ops = {e: [] for e in self.ENGS}
        self.count = {e: 0 for e in self.ENGS}
        self.sems = {}
        self.seen = {e: {} for e in self.ENGS}
        self.lastw = {}
        self.readers = {}
        self.ndma = 0
        self.dma_last = {}
        self.final_tokens = []
        self.barrier_toks = []

    def sem(self, key):
        if key not in self.sems:
            self.sems[key] = self.stack.enter_context(self.nc.semaphore("s_" + "_".join(map(str, key))))
        return self.sems[key]

    def barrier(self):
        toks = []
        for e in self.ENGS:
            n = self.count[e]
            if n > 0:
                ep, v = divmod(n - 1, EPOCH)
                toks.append((("c", e, ep), v + 1))
        for si, val in self.dma_last.items():
            toks.append((("d", si), val))
        toks.extend(getattr(self, "cc_toks", []))
        self.barrier_toks = toks

    def _deps(self, eng, reads, writes):
        toks = list(self.barrier_toks)
        for k in reads:
            if k in self.lastw:
                toks.append(self.lastw[k])
        for k in writes:
            if k in self.lastw:
                toks.append(self.lastw[k])
            toks.extend(self.readers.get(k, ()))
        need = {}
        for (sk, val) in toks:
            if self.seen[eng].get(sk, 0) >= val:
                continue
            if need.get(sk, 0) < val:
                need[sk] = val
        for sk, val in need.items():
            self.seen[eng][sk] = val
        return list(need.items())

    def _record(self, tok, reads, writes):
        for k in writes:
            self.lastw[k] = tok
            self.readers[k] = []
        for k in reads:
            if k in writes:
                continue
            self.readers.setdefault(k, []).append(tok)

    def op(self, eng, fn, reads=(), writes=(), same_ok=False):
        waits = self._deps(eng, reads, writes)
        if same_ok:
            waits = [(wk, v) for (wk, v) in waits if not (wk[0] == "c" and wk[1] == eng)]
        n = self.count[eng]
        ep, v = divmod(n, EPOCH)
        sk = ("c", eng, ep)
        self.sem(sk)
        tok = (sk, v + 1)
        self.count[eng] = n + 1
        self.ops[eng].append((fn, waits, sk, 1))
        self._record(tok, reads, writes)
        return tok

    def dma(self, eng, out, in_, reads=(), writes=(), **kw):
        i = self.ndma
        self.ndma += 1
        si = i % NDMA
        sk = ("d", si)
        self.sem(sk)
        prev = self.dma_last.get(si, 0)
        waits = self._deps(eng, reads, writes)
        if prev > 0 and self.seen[eng].get(sk, 0) < prev:
            waits.append((sk, prev))
            self.seen[eng][sk] = prev
        val = prev + 16
        self.dma_last[si] = val
        tok = (sk, val)

        def fn(e, out=out, in_=in_, kw=kw):
            return e.dma_start(out=out, in_=in_, **kw)
        self.ops[eng].append((fn, waits, sk, 16))
        self._record(tok, reads, writes)
        return tok

    def collective(self, fn, reads=(), writes=()):
        k = getattr(self, "ncc", 0)
        self.ncc = k + 1
        sk = ("cc", k)
        self.sem(sk)
        waits = self._deps("pool", reads, writes)
        self.ops["pool"].append((fn, waits, sk, None))
        tok = (sk, 1)
        self._record(tok, reads, writes)
        self.cc_toks = getattr(self, "cc_toks", []) + [tok]
        return tok

    def emit(self, last=False, final_waits_eng="sp"):
        nc = self.nc
        fin = []
        if last:
            for tok in self.final_tokens:
                fin.append(tok)
        with nc.Block() as block:
            def mk(engname):
                def body(e):
                    for (fn, waits, sk, inc) in self.ops[engname]:
                        for (wk, val) in waits:
                            e.wait_ge(self.sems[wk], val)
                        inst = fn(e)
                        if inc is None:
                            inst.then_inc(self.sems[sk])
                        else:
                            inst.then_inc(self.sems[sk], inc)
                    if engname == final_waits_eng:
                        for (wk, val) in fin:
                            e.wait_ge(self.sems[wk], val)
                return body
            block.tensor(mk("pe"))
            block.scalar(mk("act"))
            block.vector(mk("dve"))
            block.gpsimd(mk("pool"))
            block.sync(mk("sp"))
        self.ops = {e: [] for e in self.ENGS}


D = 1024
DFF = 2816
NFC = 22
TF = 2048
HALF = 1024
NT = 8


def phase_F(nc, P, ps, psb, pre, E, moe, final, h_in, ydst, out_ap, is_last):
    di = lambda n, s, dt=F32: nc.dram_tensor(pre + n, s, dt, kind="ExternalInput").ap()
    p_in = di("p", [TF, 256])
    w_out = di("w_out", [D, D])
    g_ffn = di("g_ffn", [128, 8])
    w1 = di("w1", [E, NFC, 128, 8, 128])
    w3 = di("w3", [E, NFC, 128, 8, 128])
    w2 = di("w2", [E, DFF, D])
    g_ple = di("g_ple", [128, 8])
    ple_gate = di("ple_gate", [D, D])
    ple_proj = di("ple_proj", [256, D])
    identf = di("identf", [128, 128])
    selv_in = di("selv", [128, 2])
    if moe:
        rw = di("rw", [128, 8, 8])
        rb = di("rb", [128, 8])
    if final:
        g_fin = di("g_fin", [128, D])

    with ExitStack() as st:
        sb = lambda name, shape, dt: st.enter_context(nc.sbuf_tensor(pre + "s_" + name, shape, dt))
        hacc = sb("hacc", [128, NT, D], F32)
        hnT = sb("hnT", [128, 8, HALF], BF16)
        actT = sb("actT", [128, NFC * HALF], BF16)
        w2b = sb("w2b", [128, NFC * D], BF16)
        stg = sb("stg", [128, 6, 8, 128], F32)
        w13b = sb("w13b", [128, 6, 8, 128], BF16)
        stg2 = sb("stg2", [128, 3, D], F32)
        idf = sb("idf", [128, 128], F32)
        idb = sb("idb", [128, 128], BF16)
        gf = sb("gf", [128, 8], F32)
        gp = sb("gp", [128, 8], F32)
        ss = sb("ss", [128, 4], F32)
        gates = sb("gates", [128, NT, 8], F32)
        sm = sb("sm", [128, 64], F32)
        silu = sb("silu", [128, 2, 512], BF16)
        if moe:
            rws = sb("rws", [128, 8, 8], F32)
            rbs = sb("rbs", [128, 8], F32)
        if final:
            gfin = sb("gfin", [128, D], F32)

        woutb = actT[:, 0:8 * D].rearrange("p (c n) -> p c n", c=8)
        pgb = actT[:, 8 * D:16 * D].rearrange("p (c n) -> p c n", c=8)
        ppb = actT[:, 16 * D:18 * D].rearrange("p (c n) -> p c n", c=2)
        o = 0
        def carve(nbytes_bf16, dt, pat=None, **kw):
            nonlocal o
            v = w2b[:, o:o + nbytes_bf16]
            o += nbytes_bf16
            if dt == F32:
                v = v.bitcast(F32)
            if pat:
                v = v.rearrange(pat, **kw)
            return v
        xs = carve(2 * D, F32)
        ys = carve(2 * D, F32)
        ys2 = carve(2 * D, F32)
        yb = carve(D, BF16)
        yT = carve(D, BF16, "p (c t) -> p c t", c=8)
        hnb = carve(D, BF16)
        hn32 = carve(2 * D, F32)
        hnT32 = carve(2 * D, F32, "p (c t) -> p c t", c=8)
        pst = carve(2 * 256, F32)
        pbf = carve(256, BF16)
        pT = carve(256, BF16, "p (c t) -> p c t", c=2)
        gsb = carve(2 * D, F32)
        junk = carve(2 * D, F32)
        osb = carve(2 * D, F32)

        P.dma("sp", idf[:], identf, writes=["idf"])
        P.dma("sp", gf[:], g_ffn, writes=["gf"])
        P.dma("sp", gp[:], g_ple, writes=["gp"])
        selv = sb("selv", [128, 2], F32)
        P.dma("sp", selv[:], selv_in, writes=["selv"])
        P.op("dve", lambda e: e.tensor_copy(out=idb[:], in_=idf[:]), reads=["idf"], writes=["idb"])
        if moe:
            P.dma("sp", rws[:], rw, writes=["rws"])
            P.dma("sp", rbs[:], rb, writes=["rbs"])
            P.op("dve", lambda e: e.tensor_tensor(out=rws[:], in0=rws[:], in1=gf[:].unsqueeze(2).to_broadcast([128, 8, 8]), op=ALU.mult),
                 reads=["rws", "gf"], writes=["rws"])
        if final:
            P.dma("sp", gfin[:], g_fin, writes=["gfin"])

        def load_w_bf16(dst, src, nchunks, gscale, tag):
            for c in range(nchunks):
                s = c % 2
                P.dma("sp", stg2[:, s, :], src[c * 128:(c + 1) * 128, :], writes=[("stg2", s)])
                if gscale is not None:
                    P.op("pool", lambda e, c=c, s=s: e.tensor_scalar(out=dst[:, c, :], in0=stg2[:, s, :], scalar1=gscale[:, c:c + 1], scalar2=None, op0=ALU.mult),
                         reads=[("stg2", s), "gp"], writes=[tag])
                else:
                    P.op("pool", lambda e, c=c, s=s: e.tensor_copy(out=dst[:, c, :], in_=stg2[:, s, :]), reads=[("stg2", s)], writes=[tag])

        def rms(src_ap, key, col):
            P.op("act", lambda e: e.activation(out=junk, in_=src_ap, func=AF.Square, accum_out=ss[:, col:col + 1]), reads=[key], writes=["junk", ("ss", col)])
            P.op("dve", lambda e: e.tensor_scalar(out=ss[:, col:col + 1], in0=ss[:, col:col + 1], scalar1=1.0 / D, scalar2=1e-6, op0=ALU.mult, op1=ALU.add),
                 reads=[("ss", col)], writes=[("ss", col)])
            P.op("act", lambda e: e.sqrt(out=ss[:, col:col + 1], in_=ss[:, col:col + 1]), reads=[("ss", col)], writes=[("ss", col)])
            P.op("dve", lambda e: e.reciprocal(out=ss[:, col:col + 1], in_=ss[:, col:col + 1]), reads=[("ss", col)], writes=[("ss", col)])

        def transposes(psbank, src_bf, n, ident, srckey, pskey):
            def f(e):
                for c in range(n):
                    i = e.transpose(out=psbank[:, c * 128:(c + 1) * 128], in_=src_bf[:, c * 128:(c + 1) * 128], identity=ident)
                return i
            P.op("pe", f, reads=[srckey, "idb", "idf"], writes=[pskey])

        for half in range(2):
            tb = half * HALF
            P.barrier()
            load_w_bf16(woutb, w_out, 8, None, "woutb")
            for i in range(NT):
                t0 = tb + i * 128
                P.dma("sp", xs, h_in[t0:t0 + 128, :], writes=["xs"])
                tl = i * 128 + half * HALF
                for k_, yk in ((0, ys), (1, ys2)):
                    for r_ in range(2):
                        tt = k_ * TF + tl
                        row = (tt // 1024) * 2048 + r_ * 1024 + (tt % 1024)
                        P.dma("sp", yk[:, r_ * 512:(r_ + 1) * 512], ydst[row:row + 128, :], writes=["ys" if k_ == 0 else "ys2"])
                P.op("dve", lambda e: e.tensor_scalar(out=ys, in0=ys, scalar1=selv[:, 0:1], scalar2=None, op0=ALU.mult), reads=["ys", "selv"], writes=["ys"])
                P.op("dve", lambda e: e.scalar_tensor_tensor(out=ys, in0=ys2, scalar=selv[:, 1:2], in1=ys, op0=ALU.mult, op1=ALU.add), reads=["ys", "ys2", "selv"], writes=["ys"])
                P.op("pool", lambda e: e.tensor_copy(out=yb, in_=ys), reads=["ys"], writes=["yb"])
                transposes(psb[0], yb, 8, idb[:], "yb", "ps0")
                P.op("act", lambda e: e.activation(out=yT.rearrange("p c t -> p (c t)"), in_=psb[0], func=AF.Copy), reads=["ps0"], writes=["yT"])
                for hf in range(2):
                    def mm(e, hf=hf):
                        for c in range(8):
                            ins = e.matmul(ps[1 + hf][:], lhsT=yT[:, c, :], rhs=woutb[:, c, hf * 512:(hf + 1) * 512], start=(c == 0), stop=(c == 7))
                        return ins
                    P.op("pe", mm, reads=["yT", "woutb"], writes=[f"ps{1 + hf}"])
                    P.op("dve", lambda e, hf=hf, i=i: e.tensor_tensor(out=hacc[:, i, hf * 512:(hf + 1) * 512], in0=ps[1 + hf][:], in1=xs[:, hf * 512:(hf + 1) * 512], op=ALU.add),
                         reads=[f"ps{1 + hf}", "xs"], writes=[("hacc", i)])
                rms(hacc[:, i, :], ("hacc", i), 0)
                P.op("dve", lambda e, i=i: e.tensor_scalar(out=hnb, in0=hacc[:, i, :], scalar1=ss[:, 0:1], scalar2=None, op0=ALU.mult),
                     reads=[("hacc", i), ("ss", 0)], writes=["hnb"])
                transposes(psb[3], hnb, 8, idb[:], "hnb", "ps3")
                P.op("act", lambda e, i=i: e.activation(out=hnT[:, :, i * 128:(i + 1) * 128], in_=psb[3].rearrange("p (c t) -> p c t", c=8), func=AF.Copy),
                     reads=["ps3"], writes=[("hnT", i)])
                if moe:
                    P.op("pool", lambda e, i=i: e.tensor_scalar(out=hn32, in0=hacc[:, i, :], scalar1=ss[:, 0:1], scalar2=None, op0=ALU.mult),
                         reads=[("hacc", i), ("ss", 0)], writes=["hn32"])
                    for q in range(2):
                        def trf(e, q=q):
                            for c in range(4):
                                cc = q * 4 + c
                                ins = e.transpose(out=ps[4 + q][:, c * 128:(c + 1) * 128], in_=hn32[:, cc * 128:(cc + 1) * 128], identity=idf[:])
                            return ins
                        P.op("pe", trf, reads=["hn32", "idf"], writes=[f"ps{4 + q}"])
                        P.op("act", lambda e, q=q: e.activation(out=hnT32[:, q * 4:(q + 1) * 4, :], in_=ps[4 + q][:].rearrange("p (c t) -> p c t", c=4), func=AF.Copy),
                             reads=[f"ps{4 + q}"], writes=[("hnT32", q)])
                    def mml(e):
                        for c in range(8):
                            ins = e.matmul(ps[6][:, 0:8], lhsT=hnT32[:, c, :], rhs=rws[:, c, :], start=(c == 0), stop=(c == 7))
                        return ins
                    P.op("pe", mml, reads=[("hnT32", 0), ("hnT32", 1), "rws"], writes=["ps6"])
                    lg = sm[:, 0:8]
                    mx = sm[:, 8:16]
                    dd = sm[:, 16:17]
                    p1 = sm[:, 17:18]
                    p2 = sm[:, 18:19]
                    t1 = sm[:, 24:32]
                    P.op("dve", lambda e: e.tensor_tensor(out=lg, in0=ps[6][:, 0:8], in1=rbs[:], op=ALU.add), reads=["ps6", "rbs"], writes=["lg"])
                    P.op("dve", lambda e: e.max(out=mx, in_=lg), reads=["lg"], writes=["mx"])
                    P.op("dve", lambda e: e.tensor_tensor(out=dd, in0=mx[:, 1:2], in1=mx[:, 0:1], op=ALU.subtract), reads=["mx"], writes=["dd"])
                    P.op("act", lambda e: e.activation(out=dd, in_=dd, func=AF.Exp), reads=["dd"], writes=["dd"])
                    P.op("dve", lambda e: e.tensor_scalar(out=p1, in0=dd, scalar1=1.0, scalar2=None, op0=ALU.add), reads=["dd"], writes=["p1"])
                    P.op("dve", lambda e: e.reciprocal(out=p1, in_=p1), reads=["p1"], writes=["p1"])
                    P.op("dve", lambda e: e.tensor_tensor(out=p2, in0=dd, in1=p1, op=ALU.mult), reads=["dd", "p1"], writes=["p2"])
                    P.op("dve", lambda e: e.tensor_scalar(out=t1, in0=lg, scalar1=mx[:, 0:1], scalar2=p1, op0=ALU.is_equal, op1=ALU.mult),
                         reads=["lg", "mx", "p1"], writes=["t1"])
                    P.op("dve", lambda e, i=i: e.tensor_scalar(out=gates[:, i, :], in0=lg, scalar1=mx[:, 1:2], scalar2=p2, op0=ALU.is_equal, op1=ALU.mult),
                         reads=["lg", "mx", "p2"], writes=[("gates", i)])
                    P.op("dve", lambda e, i=i: e.tensor_tensor(out=gates[:, i, :], in0=gates[:, i, :], in1=t1, op=ALU.add),
                         reads=[("gates", i), "t1"], writes=[("gates", i)])

            P.barrier()
            for ex in range(E):
                nslot = 0
                for fc in range(NFC):
                    s = fc % 3
                    P.dma("sp", stg[:, s, :, :], w1[ex, fc], writes=[("stg", s)])
                    P.dma("sp", stg[:, 3 + s, :, :], w3[ex, fc], writes=[("stg", 3 + s)])
                    gb = gf[:].unsqueeze(2).to_broadcast([128, 8, 128])
                    P.op("pool", lambda e, s=s: e.tensor_tensor(out=w13b[:, s, :, :], in0=stg[:, s, :, :], in1=gb, op=ALU.mult),
                         reads=[("stg", s), "gf"], writes=[("w13b", s)])
                    P.op("pool", lambda e, s=s: e.tensor_tensor(out=w13b[:, 3 + s, :, :], in0=stg[:, 3 + s, :, :], in1=gb, op=ALU.mult),
                         reads=[("stg", 3 + s), "gf"], writes=[("w13b", 3 + s)])
                    P.dma("sp", stg2[:, s, :], w2[ex, fc * 128:(fc + 1) * 128, :], writes=[("stg2", s)])
                    P.op("act", lambda e, s=s, fc=fc: e.activation(out=w2b[:, fc * D:(fc + 1) * D], in_=stg2[:, s, :], func=AF.Copy),
                         reads=[("stg2", s)], writes=[("w2b", fc)])
                    for g in range(2):
                        def mm13(e, g=g, s=s):
                            for wi in range(2):
                                for c in range(8):
                                    ins = e.matmul(ps[2 * g + wi][:], lhsT=w13b[:, 3 * wi + s, c, :], rhs=hnT[:, c, g * 512:(g + 1) * 512], start=(c == 0), stop=(c == 7))
                            return ins
                        P.op("pe", mm13, reads=[("w13b", s), ("w13b", 3 + s)] + [("hnT", i) for i in range(NT)], writes=[f"ps{2 * g}", f"ps{2 * g + 1}"])
                        P.op("act", lambda e, g=g: e.activation(out=silu[:, g, :], in_=ps[2 * g][:], func=AF.Silu), reads=[f"ps{2 * g}"], writes=[("silu", g)])
                        P.op("dve", lambda e, g=g, fc=fc: e.tensor_tensor(out=actT[:, fc * HALF + g * 512: fc * HALF + (g + 1) * 512], in0=silu[:, g, :], in1=ps[2 * g + 1][:], op=ALU.mult),
                             reads=[("silu", g), f"ps{2 * g + 1}"], writes=[("actT", fc)])
                for i in range(NT):
                    for hf in range(2):
                        b = 4 + (nslot % 4)
                        nslot += 1
                        def mm2(e, i=i, hf=hf, b=b):
                            for fc in range(NFC):
                                ins = e.matmul(ps[b][:], lhsT=actT[:, fc * HALF + i * 128: fc * HALF + (i + 1) * 128], rhs=w2b[:, fc * D + hf * 512: fc * D + (hf + 1) * 512],
                                               start=(fc == 0), stop=(fc == NFC - 1))
                            return ins
                        P.op("pe", mm2, reads=[("actT", fc) for fc in range(NFC)] + [("w2b", fc) for fc in range(NFC)], writes=[f"ps{b}"])
                        if moe:
                            P.op("dve", lambda e, i=i, hf=hf, b=b, ex=ex: e.scalar_tensor_tensor(out=hacc[:, i, hf * 512:(hf + 1) * 512], in0=ps[b][:], scalar=gates[:, i, ex:ex + 1],
                                                                                               in1=hacc[:, i, hf * 512:(hf + 1) * 512], op0=ALU.mult, op1=ALU.add),
                                 reads=[f"ps{b}", ("gates", i), ("hacc", i)], writes=[("hacc", i)])
                        else:
                            P.op("dve", lambda e, i=i, hf=hf, b=b: e.tensor_tensor(out=hacc[:, i, hf * 512:(hf + 1) * 512], in0=ps[b][:], in1=hacc[:, i, hf * 512:(hf + 1) * 512], op=ALU.add),
                                 reads=[f"ps{b}", ("hacc", i)], writes=[("hacc", i)])

            P.barrier()
            load_w_bf16(pgb, ple_gate, 8, gp, "pgb")
            load_w_bf16(ppb, ple_proj, 2, None, "ppb")
            for i in range(NT):
                t0 = tb + i * 128
                P.dma("sp", pst, p_in[t0:t0 + 128, :], writes=["pst"])
                P.op("pool", lambda e: e.tensor_copy(out=pbf, in_=pst), reads=["pst"], writes=["pbf"])
                transposes(psb[0], pbf, 2, idb[:], "pbf", "ps0")
                P.op("act", lambda e: e.activation(out=pT.rearrange("p c t -> p (c t)"), in_=psb[0][:, 0:256], func=AF.Copy), reads=["ps0"], writes=["pT"])
                rms(hacc[:, i, :], ("hacc", i), 1)
                P.op("dve", lambda e, i=i: e.tensor_scalar(out=hnb, in0=hacc[:, i, :], scalar1=ss[:, 1:2], scalar2=None, op0=ALU.mult),
                     reads=[("hacc", i), ("ss", 1)], writes=["hnb"])
                transposes(psb[3], hnb, 8, idb[:], "hnb", "ps3")
                P.op("act", lambda e: e.activation(out=yT.rearrange("p c t -> p (c t)"), in_=psb[3], func=AF.Copy), reads=["ps3"], writes=["yT"])
                for hf in range(2):
                    def mmg(e, hf=hf):
                        for c in range(8):
                            ins = e.matmul(ps[1 + hf][:], lhsT=yT[:, c, :], rhs=pgb[:, c, hf * 512:(hf + 1) * 512], start=(c == 0), stop=(c == 7))
                        return ins
                    P.op("pe", mmg, reads=["yT", "pgb"], writes=[f"ps{1 + hf}"])
                    P.op("act", lambda e, hf=hf: e.activation(out=gsb[:, hf * 512:(hf + 1) * 512], in_=ps[1 + hf][:], func=AF.Sigmoid), reads=[f"ps{1 + hf}"], writes=[("gsb", hf)])
                    def mmp(e, hf=hf):
                        for c in range(2):
                            ins = e.matmul(ps[4 + hf][:], lhsT=pT[:, c, :], rhs=ppb[:, c, hf * 512:(hf + 1) * 512], start=(c == 0), stop=(c == 1))
                        return ins
                    P.op("pe", mmp, reads=["pT", "ppb"], writes=[f"ps{4 + hf}"])
                    P.op("dve", lambda e, hf=hf: e.tensor_tensor(out=gsb[:, hf * 512:(hf + 1) * 512], in0=gsb[:, hf * 512:(hf + 1) * 512], in1=ps[4 + hf][:], op=ALU.mult),
                         reads=[("gsb", hf), f"ps{4 + hf}"], writes=[("gsb", hf)])
                P.op("dve", lambda e, i=i: e.tensor_tensor(out=osb, in0=gsb, in1=hacc[:, i, :], op=ALU.add), reads=[("gsb", 0), ("gsb", 1), ("hacc", i)], writes=["osb"])
                if final:
                    rms(osb, "osb", 2)
                    P.op("dve", lambda e: e.scalar_tensor_tensor(out=osb, in0=osb, scalar=ss[:, 2:3], in1=gfin[:], op0=ALU.mult, op1=ALU.mult),
                         reads=["osb", ("ss", 2), "gfin"], writes=["osb"])
                tk = P.dma("sp", out_ap[t0:t0 + 128, :], osb, reads=["osb"], writes=["f_out"])
                if is_last:
                    P.final_tokens.append(tk)
        P.emit(last=is_last)


def prep_F_inputs(layer, hs, ys, inputs, moe, final):
    i = layer
    j = i // 2
    def tochunks(w):
        E = w.shape[0]
        return np.ascontiguousarray(w.reshape(E, 8, 128, NFC, 128).transpose(0, 3, 2, 1, 4))
    if moe:
        w1, w3, w2 = inputs["moe_w1"][j], inputs["moe_w3"][j], inputs["moe_w2"][j]
    else:
        w1, w3, w2 = inputs["ffn_w1"][j][None], inputs["ffn_w3"][j][None], inputs["ffn_w2"][j][None]
    pc = lambda g: np.ascontiguousarray(g.reshape(8, 128).T)
    common = {
        "w_out": inputs["w_out"][i], "g_ffn": pc(inputs["g_ffn"][i]), "w1": tochunks(w1), "w3": tochunks(w3), "w2": np.ascontiguousarray(w2),
        "g_ple": pc(inputs["g_ple"][i]), "ple_gate": inputs["ple_gate_w"][i], "ple_proj": inputs["ple_proj_w"][i],
        "identf": np.eye(128, dtype=np.float32),
    }
    if moe:
        common["rw"] = np.ascontiguousarray(inputs["router_w"][j].reshape(8, 128, 8).transpose(1, 0, 2))
        common["rb"] = np.ascontiguousarray(np.broadcast_to(inputs["router_b"][j][None, :], (128, 8)))
    if final:
        common["g_fin"] = np.ascontiguousarray(np.broadcast_to(inputs["g_final"][None, :], (128, D)))
    pl = inputs["p"][i].reshape(-1, 256)
    maps = []
    for c in range(8):
        m = dict(common)
        m["h"] = np.ascontiguousarray(hs[c * TF:(c + 1) * TF])
        m["y"] = np.ascontiguousarray(ys[c * TF:(c + 1) * TF])
        m["p"] = np.ascontiguousarray(pl[c * TF:(c + 1) * TF])
        maps.append(m)
    return maps


S = 4096
NTILE = 32


class Proj:
    def __init__(self, nc, P, st, h_in, identf, ncol, w_dram, g_dram, shift_cols=None, mu_dram=None, pre="", hmap=None):
        self.nc, self.P = nc, P
        self.hmap = hmap if hmap is not None else (lambda i: i * 128)
        sb = lambda name, shape, dt: st.enter_context(nc.sbuf_tensor(pre + "s_" + name, shape, dt))
        self.h_in = h_in
        self.xs = sb("pj_xs", [128, 2, D], F32)
        self.junk = sb("pj_junk", [128, D], F32)
        self.ss = sb("pj_ss", [128, 2], F32)
        self.hnb = sb("pj_hnb", [128, D], BF16)
        self.hnT = sb("pj_hnT", [128, 3, 8, 129], BF16)
        self.idf = sb("pj_idf", [128, 128], F32)
        self.idb = sb("pj_idb", [128, 128], BF16)
        self.g = sb("pj_g", [128, 8], F32)
        self.wb = sb("pj_wb", [128, 8, ncol], BF16)
        self.ncol = ncol
        stg = sb("pj_stg", [128, 2, ncol], F32)
        P.dma("sp", self.idf[:], identf, writes=["idf"])
        P.dma("sp", self.g[:], g_dram, writes=["pj_g"])
        P.op("dve", lambda e: e.tensor_copy(out=self.idb[:], in_=self.idf[:]), reads=["idf"], writes=["idb"])
        P.op("pool", lambda e: e.memset(self.hnT[:], 0.0), writes=[("hnT", 0), ("hnT", 1), ("hnT", 2)])
        for c in range(8):
            s = c % 2
            P.dma("sp", stg[:, s, :], w_dram[c * 128:(c + 1) * 128, :], writes=[("pj_stg", s)])
            P.op("pool", lambda e, c=c, s=s: e.tensor_scalar(out=self.wb[:, c, :], in0=stg[:, s, :], scalar1=self.g[:, c:c + 1], scalar2=None, op0=ALU.mult),
                 reads=[("pj_stg", s), "pj_g"], writes=["pj_wb"])
        if shift_cols is not None:
            a, b = shift_cols
            n = b - a
            self.wprev = sb("pj_wprev", [128, 8, n], BF16)
            mu = sb("pj_mu", [128, n], F32)
            P.dma("sp", mu[:], mu_dram, writes=["pj_mu"])
            mub = mu[:].unsqueeze(1).to_broadcast([128, 8, n])
            P.op("dve", lambda e: e.tensor_tensor(out=self.wprev[:], in0=self.wb[:, :, a:b], in1=mub, op=ALU.mult), reads=["pj_wb", "pj_mu"], writes=["pj_wprev"])
            P.op("dve", lambda e: e.tensor_tensor(out=self.wb[:, :, a:b], in0=self.wb[:, :, a:b], in1=self.wprev[:], op=ALU.subtract), reads=["pj_wb", "pj_wprev"], writes=["pj_wb"])
        self.shift_cols = shift_cols

    def tile(self, i, psbank_bf, pskey):
        P = self.P
        s = i % 2
        xs = self.xs[:, s, :]
        r0 = self.hmap(i)
        P.dma("sp", xs, self.h_in[r0:r0 + 128, :], writes=[("pj_xs", s)])
        ssc = self.ss[:, s:s + 1]
        k = ("pj_ss", s)
        P.op("act", lambda e: e.activation(out=self.junk[:], in_=xs, func=AF.Square, accum_out=ssc), reads=[("pj_xs", s)], writes=["pj_junk", k])
        P.op("dve", lambda e: e.tensor_scalar(out=ssc, in0=ssc, scalar1=1.0 / D, scalar2=1e-6, op0=ALU.mult, op1=ALU.add), reads=[k], writes=[k])
        P.op("act", lambda e: e.sqrt(out=ssc, in_=ssc), reads=[k], writes=[k])
        P.op("dve", lambda e: e.reciprocal(out=ssc, in_=ssc), reads=[k], writes=[k])
        P.op("dve", lambda e: e.tensor_scalar(out=self.hnb[:], in0=xs, scalar1=ssc, scalar2=None, op0=ALU.mult), reads=[("pj_xs", s), k], writes=["pj_hnb"])

        def tr(e):
            for c in range(8):
                ins = e.transpose(out=psbank_bf[:, c * 128:(c + 1) * 128], in_=self.hnb[:, c * 128:(c + 1) * 128], identity=self.idb[:])
            return ins
        P.op("pe", tr, reads=["pj_hnb", "idb"], writes=[pskey])
        sh, sn = i % 3, (i + 1) % 3
        P.op("act", lambda e: e.activation(out=self.hnT[:, sh, :, 1:129], in_=psbank_bf.rearrange("p (c t) -> p c t", c=8), func=AF.Copy), reads=[pskey], writes=[("hnT", sh)])
        P.op("pool", lambda e: e.tensor_copy(out=self.hnT[:, sn, :, 0:1], in_=self.hnT[:, sh, :, 128:129]), reads=[("hnT", sh)], writes=[("hnT", sn)])

    def mm_tok(self, e, i, ps_ap, c0, c1, start=True, stop=True):
        s = i % 3
        sh = self.shift_cols is not None and c0 >= self.shift_cols[0] and c1 <= self.shift_cols[1]
        n = 16 if sh else 8
        k = 0
        for c in range(8):
            ins = e.matmul(ps_ap, lhsT=self.hnT[:, s, c, 1:129], rhs=self.wb[:, c, c0:c1], start=(start and k == 0), stop=(stop and k == n - 1))
            k += 1
        if sh:
            a = self.shift_cols[0]
            for c in range(8):
                ins = e.matmul(ps_ap, lhsT=self.hnT[:, s, c, 0:128], rhs=self.wprev[:, c, c0 - a:c1 - a], start=False, stop=(stop and k == n - 1))
                k += 1
        return ins

    def mm_feat(self, e, i, ps_ap, c0, c1):
        s = i % 3
        sh = self.shift_cols is not None and c0 >= self.shift_cols[0] and c1 <= self.shift_cols[1]
        n = 16 if sh else 8
        k = 0
        for c in range(8):
            ins = e.matmul(ps_ap, lhsT=self.wb[:, c, c0:c1], rhs=self.hnT[:, s, c, 1:129], start=(k == 0), stop=(k == n - 1))
            k += 1
        if sh:
            a = self.shift_cols[0]
            for c in range(8):
                ins = e.matmul(ps_ap, lhsT=self.wprev[:, c, c0 - a:c1 - a], rhs=self.hnT[:, s, c, 0:128], start=False, stop=(k == n - 1))
                k += 1
        return ins

    def keys(self, i):
        return [("hnT", i % 3), "pj_wb", "pj_wprev"]


def pc8(g):
    return np.ascontiguousarray(np.asarray(g).reshape(8, 128).T)


def bc128(v):
    v = np.asarray(v, dtype=np.float32).reshape(1, -1)
    return np.ascontiguousarray(np.broadcast_to(v, (128, v.shape[1])))


class MAops:
    def __init__(self, nc, P, st, pre, pj, c0, psA, keyA, psB, keyB, ysrc):
        self.P, self.pj, self.c0, self.psA, self.keyA, self.psB, self.keyB, self.ysrc = P, pj, c0, psA, keyA, psB, keyB, ysrc
        di = lambda n, s, dt=F32: nc.dram_tensor(pre + n, s, dt, kind="ExternalInput").ap()
        sb = lambda name, shape, dt: st.enter_context(nc.sbuf_tensor(pre + "s_" + name, shape, dt))
        lng = di("lng", [128, 128]); lnb = di("lnb", [128, 128]); ws = di("ws", [2, 128, 128]); tril = di("tril", [128, 128]); bs = di("bs", [128, 2])
        self.lngs = sb("lngs", [128, 128], F32); self.lnbs = sb("lnbs", [128, 128], F32)
        wss = sb("wss", [128, 2, 128], F32); trl = sb("trl", [128, 128], F32)
        self.wT = sb("wT", [128, 2, 128], BF16); self.bss = sb("bss", [128, 2], F32)
        self.uv = sb("uv", [128, 2, 256], F32); self.st1 = sb("st1", [128, 2, 8], F32)
        self.vc = sb("vc", [128, 2, 2, 64], F32); self.sq = sb("sq", [128, 2, 64], F32)
        self.vn = sb("vn", [128, 2, 2, 64], BF16); self.yo = sb("yo", [128, 2, 128], F32)
        P.dma("sp", self.lngs[:], lng, writes=["a_lngs"]); P.dma("sp", self.lnbs[:], lnb, writes=["a_lnbs"])
        P.dma("sp", trl[:], tril, writes=["a_trl"]); P.dma("sp", self.bss[:], bs, writes=["a_bss"])
        for hh in range(2):
            P.dma("sp", wss[:, hh, :], ws[hh], writes=[("a_wss", hh)])
            P.op("dve", lambda e, hh=hh: e.tensor_tensor(out=wss[:, hh, :], in0=wss[:, hh, :], in1=trl[:], op=ALU.mult), reads=[("a_wss", hh), "a_trl"], writes=[("a_wss", hh)])
            P.op("pe", lambda e, hh=hh: e.transpose(out=psB[:, hh * 128:(hh + 1) * 128], in_=wss[:, hh, :], identity=pj.idf[:]), reads=[("a_wss", hh), "idf"], writes=[keyB])
            P.op("act", lambda e, hh=hh: e.activation(out=self.wT[:, hh, :], in_=psB[:, hh * 128:(hh + 1) * 128], func=AF.Copy), reads=[keyB], writes=["a_wT"])

    def tile(self, i):
        P, pj, c0, psA, keyA, psB, keyB = self.P, self.pj, self.c0, self.psA, self.keyA, self.psB, self.keyB
        so = i % 2
        uv = self.uv[:, so, :]; st1 = self.st1[:, so, :]; vc = self.vc[:, so]; sq = self.sq; vn = self.vn[:, so]; yo = self.yo
        K = lambda n: ("a_" + n, so)
        P.op("pe", lambda e: pj.mm_tok(e, i, psA[:, 0:256], c0, c0 + 256), reads=pj.keys(i), writes=[keyA])
        P.op("act", lambda e: e.activation(out=uv, in_=psA[:, 0:256], func=AF.Gelu_apprx_tanh), reads=[keyA], writes=[K("uv")])
        v3 = uv[:, 128:256].rearrange("p (h d) -> p h d", h=2)
        g3 = lambda ap: ap.rearrange("p (h d) -> p h d", h=2)
        P.op("dve", lambda e: e.tensor_reduce(out=st1[:, 0:2], in_=v3, axis=AX.X, op=ALU.add), reads=[K("uv")], writes=[K("st_m")])
        P.op("dve", lambda e: e.tensor_scalar(out=st1[:, 0:2], in0=st1[:, 0:2], scalar1=1.0 / 64, scalar2=None, op0=ALU.mult), reads=[K("st_m")], writes=[K("st_m")])
        P.op("pool", lambda e: e.tensor_tensor(out=vc, in0=v3, in1=st1[:, 0:2].unsqueeze(2).to_broadcast([128, 2, 64]), op=ALU.subtract), reads=[K("uv"), K("st_m")], writes=[K("vc")])
        P.op("pool", lambda e: e.tensor_tensor(out=sq[:], in0=vc, in1=vc, op=ALU.mult), reads=[K("vc")], writes=["a_sq"])
        P.op("dve", lambda e: e.tensor_reduce(out=st1[:, 2:4], in_=sq[:], axis=AX.X, op=ALU.add), reads=["a_sq"], writes=[K("st_v")])
        P.op("dve", lambda e: e.tensor_scalar(out=st1[:, 2:4], in0=st1[:, 2:4], scalar1=1.0 / 64, scalar2=1e-5, op0=ALU.mult, op1=ALU.add), reads=[K("st_v")], writes=[K("st_v")])
        P.op("act", lambda e: e.sqrt(out=st1[:, 2:4], in_=st1[:, 2:4]), reads=[K("st_v")], writes=[K("st_v")])
        P.op("dve", lambda e: e.reciprocal(out=st1[:, 2:4], in_=st1[:, 2:4]), reads=[K("st_v")], writes=[K("st_v")])
        P.op("pool", lambda e: e.tensor_tensor(out=vc, in0=vc, in1=st1[:, 2:4].unsqueeze(2).to_broadcast([128, 2, 64]), op=ALU.mult), reads=[K("vc"), K("st_v")], writes=[K("vc")])
        P.op("pool", lambda e: e.tensor_tensor(out=vc, in0=vc, in1=g3(self.lngs[:]), op=ALU.mult), reads=[K("vc"), "a_lngs"], writes=[K("vc")])
        P.op("pool", lambda e: e.tensor_tensor(out=vn, in0=vc, in1=g3(self.lnbs[:]), op=ALU.add), reads=[K("vc"), "a_lnbs"], writes=[K("vn")])

        def mix(e):
            for hh in range(2):
                ins = e.matmul(psB[:, hh * 64:(hh + 1) * 64], lhsT=self.wT[:, hh, :], rhs=vn[:, hh, :], start=True, stop=True)
            return ins
        P.op("pe", mix, reads=["a_wT", K("vn")], writes=[keyB])
        for hh in range(2):
            P.op("dve", lambda e, hh=hh: e.scalar_tensor_tensor(out=yo[:, so, hh * 64:(hh + 1) * 64], in0=psB[:, hh * 64:(hh + 1) * 64], scalar=self.bss[:, hh:hh + 1],
                                                            in1=uv[:, hh * 64:(hh + 1) * 64], op0=ALU.add, op1=ALU.mult),
                 reads=[keyB, "a_bss", K("uv")], writes=[K("yo")])
        P.dma("sp", self.ysrc[i * 128:(i + 1) * 128, 0:128], yo[:, so, :], reads=[K("yo")], writes=["ysrc"])


def phase_MA(nc, P, ps, psb, pre, h_in, ysrc, hmap=None, is_last=False):
    di = lambda n, s, dt=F32: nc.dram_tensor(pre + n, s, dt, kind="ExternalInput").ap()
    identf = di("identf", [128, 128])
    wc = di("wc", [D, 256])
    g_mix = di("g_mix", [128, 8])
    lng = di("lng", [128, 128])
    lnb = di("lnb", [128, 128])
    ws = di("ws", [2, 128, 128])
    tril = di("tril", [128, 128])
    bs = di("bs", [128, 2])
    with ExitStack() as st:
        sb = lambda name, shape, dt: st.enter_context(nc.sbuf_tensor(pre + "s_" + name, shape, dt))
        pj = Proj(nc, P, st, h_in, identf, 256, wc, g_mix, pre=pre, hmap=hmap)
        lngs = sb("lngs", [128, 128], F32)
        lnbs = sb("lnbs", [128, 128], F32)
        wss = sb("wss", [128, 2, 128], F32)
        trl = sb("trl", [128, 128], F32)
        wT = sb("wT", [128, 2, 128], BF16)
        bss = sb("bss", [128, 2], F32)
        uv = sb("uv", [128, 256], F32)
        st1 = sb("st1", [128, 8], F32)
        vc = sb("vc", [128, 2, 64], F32)
        sq = sb("sq", [128, 2, 64], F32)
        vn = sb("vn", [128, 2, 64], BF16)
        yo = sb("yo", [128, 2, 128], F32)
        P.dma("sp", lngs[:], lng, writes=["lngs"])
        P.dma("sp", lnbs[:], lnb, writes=["lnbs"])
        P.dma("sp", trl[:], tril, writes=["trl"])
        P.dma("sp", bss[:], bs, writes=["bss"])
        for hh in range(2):
            P.dma("sp", wss[:, hh, :], ws[hh], writes=[("wss", hh)])
            P.op("dve", lambda e, hh=hh: e.tensor_tensor(out=wss[:, hh, :], in0=wss[:, hh, :], in1=trl[:], op=ALU.mult), reads=[("wss", hh), "trl"], writes=[("wss", hh)])
            P.op("pe", lambda e, hh=hh: e.transpose(out=ps[7][:, hh * 128:(hh + 1) * 128], in_=wss[:, hh, :], identity=pj.idf[:]), reads=[("wss", hh), "idf"], writes=["ps7"])
            P.op("act", lambda e, hh=hh: e.activation(out=wT[:, hh, :], in_=ps[7][:, hh * 128:(hh + 1) * 128], func=AF.Copy), reads=["ps7"], writes=["wT"])
        for i in range(NTILE):
            pj.tile(i, psb[0], "ps0")
            P.op("pe", lambda e, i=i: pj.mm_tok(e, i, ps[1][:, 0:256], 0, 256), reads=pj.keys(i), writes=["ps1"])
            P.op("act", lambda e: e.activation(out=uv[:], in_=ps[1][:, 0:256], func=AF.Gelu_apprx_tanh), reads=["ps1"], writes=["uv"])
            v3 = uv[:, 128:256].rearrange("p (h d) -> p h d", h=2)
            P.op("dve", lambda e: e.tensor_reduce(out=st1[:, 0:2], in_=v3, axis=AX.X, op=ALU.add), reads=["uv"], writes=["st_m"])
            P.op("dve", lambda e: e.tensor_scalar(out=st1[:, 0:2], in0=st1[:, 0:2], scalar1=1.0 / 64, scalar2=None, op0=ALU.mult), reads=["st_m"], writes=["st_m"])
            P.op("dve", lambda e: e.tensor_tensor(out=vc[:], in0=v3, in1=st1[:, 0:2].unsqueeze(2).to_broadcast([128, 2, 64]), op=ALU.subtract), reads=["uv", "st_m"], writes=["vc"])
            P.op("dve", lambda e: e.tensor_tensor(out=sq[:], in0=vc[:], in1=vc[:], op=ALU.mult), reads=["vc"], writes=["sq"])
            P.op("dve", lambda e: e.tensor_reduce(out=st1[:, 2:4], in_=sq[:], axis=AX.X, op=ALU.add), reads=["sq"], writes=["st_v"])
            P.op("dve", lambda e: e.tensor_scalar(out=st1[:, 2:4], in0=st1[:, 2:4], scalar1=1.0 / 64, scalar2=1e-5, op0=ALU.mult, op1=ALU.add), reads=["st_v"], writes=["st_v"])
            P.op("act", lambda e: e.sqrt(out=st1[:, 2:4], in_=st1[:, 2:4]), reads=["st_v"], writes=["st_v"])
            P.op("dve", lambda e: e.reciprocal(out=st1[:, 2:4], in_=st1[:, 2:4]), reads=["st_v"], writes=["st_v"])
            P.op("dve", lambda e: e.tensor_tensor(out=vc[:], in0=vc[:], in1=st1[:, 2:4].unsqueeze(2).to_broadcast([128, 2, 64]), op=ALU.mult), reads=["vc", "st_v"], writes=["vc"])
            P.op("dve", lambda e: e.tensor_tensor(out=vc[:], in0=vc[:], in1=lngs[:].rearrange("p (h d) -> p h d", h=2), op=ALU.mult), reads=["vc", "lngs"], writes=["vc"])
            P.op("dve", lambda e: e.tensor_tensor(out=vn[:], in0=vc[:], in1=lnbs[:].rearrange("p (h d) -> p h d", h=2), op=ALU.add), reads=["vc", "lnbs"], writes=["vn"])
            def mix(e):
                for hh in range(2):
                    ins = e.matmul(ps[2][:, hh * 64:(hh + 1) * 64], lhsT=wT[:, hh, :], rhs=vn[:, hh, :], start=True, stop=True)
                return ins
            P.op("pe", mix, reads=["wT", "vn"], writes=["ps2"])
            so = i % 2
            for hh in range(2):
                P.op("dve", lambda e, hh=hh, so=so: e.scalar_tensor_tensor(out=yo[:, so, hh * 64:(hh + 1) * 64], in0=ps[2][:, hh * 64:(hh + 1) * 64], scalar=bss[:, hh:hh + 1],
                                                                       in1=uv[:, hh * 64:(hh + 1) * 64], op0=ALU.add, op1=ALU.mult),
                     reads=["ps2", "bss", "uv"], writes=[("yo", so)])
            P.dma("sp", ysrc[i * 128:(i + 1) * 128, 0:128], yo[:, so, :], reads=[("yo", so)], writes=["ysrc"])
        P.emit(last=is_last)


def prep_MA(layer, h, inputs):
    i = layer
    maps = []
    for c in range(8):
        b, gi = c // 2, c % 2
        w = inputs["w_in"][i]
        wc = np.concatenate([w[:, gi * 128:(gi + 1) * 128], w[:, 256 + gi * 128:256 + (gi + 1) * 128]], axis=1)
        maps.append({
            "h": np.ascontiguousarray(h[b]), "identf": np.eye(128, dtype=np.float32), "wc": np.ascontiguousarray(wc),
            "g_mix": pc8(inputs["g_mix"][i]),
            "lng": bc128(inputs["gm_ln_g"][i][2 * gi:2 * gi + 2].reshape(-1)), "lnb": bc128(inputs["gm_ln_b"][i][2 * gi:2 * gi + 2].reshape(-1)),
            "ws": np.ascontiguousarray(inputs["gm_ws"][i][2 * gi:2 * gi + 2]), "tril": np.tril(np.ones((128, 128), np.float32)),
            "bs": np.ascontiguousarray(inputs["gm_bs"][i][2 * gi:2 * gi + 2].T),
        })
    return maps


def phase_MB(nc, P, ps, psb, pre, h_in, ysrc, scr, hmap=None, ntile=NTILE, upto=9, is_last=False):
    di = lambda n, s, dt=F32: nc.dram_tensor(pre + n, s, dt, kind="ExternalInput").ap()
    identf = di("identf", [128, 128])
    wc = di("wc", [D, 896])
    g_mix = di("g_mix", [128, 8])
    mu = di("mu", [128, 640])
    wa_up = di("wa_up", [128, 128])
    g_up = di("g_up", [128, 128])
    w0a0 = di("w0a0", [1, 256])
    cvec = di("cvec", [128, 5, 128])
    sel_in = di("sel", [20, 5, 128])
    mcum_in = di("mcum", [128, 128])
    NST = 32
    with ExitStack() as st:
        sb = lambda name, shape, dt: st.enter_context(nc.sbuf_tensor(pre + "s_" + name, shape, dt))
        pj = Proj(nc, P, st, h_in, identf, 896, wc, g_mix, shift_cols=(0, 640), mu_dram=mu, pre=pre, hmap=hmap)
        ma = MAops(nc, P, st, pre + "a_", pj, 640, ps[6], "ps6", ps[7], "ps7", ysrc)
        waf = sb("waf", [128, 128], F32); wab = sb("wab", [128, 128], BF16)
        guf = sb("guf", [128, 128], F32); gub = sb("gub", [128, 128], BF16)
        w0f = sb("w0f", [1, 256], F32); w0b = sb("w0b", [1, 256], BF16)
        ones = sb("ones", [1, 128], BF16)
        cv = sb("cv", [128, 5, 128], F32)
        self_ = sb("self", [20, 5, 128], F32)
        sel = sb("selt", [20, 5, 128], BF16)
        ldT = sb("ldT", [128, 128], BF16)
        gdT = sb("gdT", [128, 128], BF16)
        sg = sb("sg", [128, 128], F32)
        aa = sb("aa", [128, 128], F32)
        rkv = sb("rkv", [128, 384], F32)
        kk0 = sb("kk0", [128, 2, 64], F32)
        sq = sb("sq", [128, 2, 64], F32)
        st1 = sb("st1", [128, 8], F32)
        tmp = sb("tmp", [128, 128], F32)
        strm = sb("strm", [128, 2, 5, 128], F32)
        g_all = sb("g_all", [128, ntile, 128], F32)
        bon_all = sb("bon_all", [128, ntile, 128], F32)
        vT_all = sb("vT_all", [128, ntile * 128], F32)
        yT_all = sb("yT_all", [128, ntile * 128], F32)
        rows = sb("rows", [20, 2, NST * 64], BF16)
        shl = sb("shl", [128, 2, 2, 5, 128], BF16)
        sdf = sb("sdf", [128, 5, 128], F32)
        Sst = sb("Sst", [128, 64], F32)
        T1 = sb("T1", [128, 64], F32)
        junk = sb("junk", [128, 64], F32)
        sa = sb("sa", [128, 1], F32)
        fill = sb("fill", [128, 2], F32)
        junk2 = sb("junk2", [128, 64], F32)
        prev_step = None
        mcum = sb("mcum", [128, 128], F32)
        pinc = sb("pinc", [128, 128], F32); pinv = sb("pinv", [128, 128], F32); pexc = sb("pexc", [128, 128], F32); csx = sb("csx", [128, 128], F32)
        Sb2 = sb("Sb2", [128, 2, 64], F32); Ubuf = sb("Ubuf", [128, 64], F32)
        P.dma("sp", mcum[:], mcum_in, writes=["mcum"])
        T1p = sb("T1p", [128, 64], F32)
        wr_sb = sb("wr_sb", [128, 2, 512], F32)
        yo = sb("yo", [128, 2, 128], F32)
        yc = sb("yc", [128, 2, 64], F32)

        P.dma("sp", waf[:], wa_up, writes=["waf"]); P.op("dve", lambda e: e.tensor_copy(out=wab[:], in_=waf[:]), reads=["waf"], writes=["wab"])
        P.dma("sp", guf[:], g_up, writes=["guf"]); P.op("dve", lambda e: e.tensor_copy(out=gub[:], in_=guf[:]), reads=["guf"], writes=["gub"])
        P.dma("sp", w0f[:], w0a0, writes=["w0f"]); P.op("dve", lambda e: e.tensor_copy(out=w0b[:], in_=w0f[:]), reads=["w0f"], writes=["w0b"])
        P.op("dve", lambda e: e.memset(ones[:], 1.0), writes=["ones"])
        P.dma("sp", cv[:], cvec, writes=["cv"])
        P.dma("sp", self_[:], sel_in, writes=["self"])
        P.op("dve", lambda e: e.tensor_copy(out=sel[:], in_=self_[:]), reads=["self"], writes=["sel"])
        KK, KA, RK, GNG, GNB = range(5)
        h3 = lambda ap: ap.rearrange("p (h d) -> p h d", h=2)

        pj.tile(0, psb[0], "ps0")
        for i in range(ntile):
            so = i % 2
            if i + 1 < ntile:
                pj.tile(i + 1, psb[0], "ps0")
            P.op("pe", lambda e, i=i: pj.mm_tok(e, i, ps[1][:, 0:384], 0, 384), reads=pj.keys(i), writes=["ps1"])
            P.op("pe", lambda e, i=i: pj.mm_feat(e, i, ps[2][:, 0:128], 384, 512), reads=pj.keys(i), writes=["ps2"])
            P.op("pe", lambda e, i=i: pj.mm_feat(e, i, ps[3][:, 0:128], 512, 640), reads=pj.keys(i), writes=["ps3"])
            P.op("act", lambda e: e.activation(out=ldT[0:64, :], in_=ps[2][0:64, 0:128], func=AF.Tanh), reads=["ps2"], writes=["ldT0"])
            P.op("act", lambda e: e.activation(out=ldT[64:128, :], in_=ps[2][64:128, 0:128], func=AF.Copy), reads=["ps2"], writes=["ldT1"])
            P.op("act", lambda e: e.activation(out=gdT[:], in_=ps[3][:, 0:128], func=AF.Sigmoid), reads=["ps3"], writes=["gdT"])
            P.op("act", lambda e: e.activation(out=rkv[:], in_=ps[1][:, 0:384], func=AF.Copy), reads=["ps1"], writes=["rkv"])
            def ups(e):
                e.matmul(ps[4][:, 0:128], lhsT=ldT[0:64, :], rhs=wab[0:64, :], start=True, stop=False)
                e.matmul(ps[4][:, 0:128], lhsT=ones[:], rhs=w0b[:, 0:128], start=False, stop=True)
                e.matmul(ps[4][:, 128:256], lhsT=ldT[64:128, :], rhs=wab[64:128, :], start=True, stop=False)
                e.matmul(ps[4][:, 128:256], lhsT=ones[:], rhs=w0b[:, 128:256], start=False, stop=True)
                return e.matmul(ps[4][:, 256:384], lhsT=gdT[:], rhs=gub[:], start=True, stop=True)
            P.op("pe", ups, reads=["ldT0", "ldT1", "gdT", "wab", "gub", "w0b", "ones"], writes=["ps4"])
            P.op("act", lambda e: e.activation(out=sg[:], in_=ps[4][:, 0:128], func=AF.Sigmoid), reads=["ps4"], writes=["sg"])
            P.op("pe", lambda e: e.matmul(ps[6][:, 128:256], lhsT=mcum[:], rhs=sg[:], start=True, stop=True), reads=["mcum", "sg"], writes=["ps6"])
            P.op("act", lambda e, so=so: e.activation(out=strm[:, so, 0, :], in_=ps[6][:, 128:256], func=AF.Exp, scale=-0.6065306597126334), reads=["ps6"], writes=[("strm", so, 0)])
            P.op("act", lambda e: e.activation(out=pinv[:], in_=ps[6][:, 128:256], func=AF.Exp, scale=0.6065306597126334), reads=["ps6"], writes=["pinv"])
            P.op("act", lambda e: e.activation(out=csx[:], in_=ps[6][:, 128:256], func=AF.Copy), reads=["ps6"], writes=["csx"])
            P.op("pool", lambda e: e.tensor_tensor(out=csx[:], in0=csx[:], in1=sg[:], op=ALU.subtract), reads=["csx", "sg"], writes=["csx"])
            P.op("act", lambda e: e.activation(out=pexc[:], in_=csx[:], func=AF.Exp, scale=-0.6065306597126334), reads=["csx"], writes=["pexc"])
            P.op("act", lambda e: e.activation(out=aa[:], in_=ps[4][:, 128:256], func=AF.Sigmoid), reads=["ps4"], writes=["aa"])
            P.op("act", lambda e, i=i: e.activation(out=g_all[:, i, :], in_=ps[4][:, 256:384], func=AF.Copy), reads=["ps4"], writes=[("g_all", i)])
            r_ = rkv[:, 0:128]; k_ = rkv[:, 128:256]; v_ = rkv[:, 256:384]
            P.op("pe", lambda e: e.transpose(out=ps[5][:, 0:128], in_=v_, identity=pj.idf[:]), reads=["rkv", "idf"], writes=["ps5"])
            P.op("act", lambda e, i=i: e.activation(out=vT_all[:, i * 128:(i + 1) * 128], in_=ps[5][:, 0:128], func=AF.Copy), reads=["ps5"], writes=[("vT", i)])
            P.op("dve", lambda e: e.tensor_tensor(out=kk0[:], in0=h3(k_), in1=h3(cv[:, KK, :]), op=ALU.mult), reads=["rkv", "cv"], writes=["kk0"])
            P.op("dve", lambda e: e.tensor_tensor(out=sq[:], in0=kk0[:], in1=kk0[:], op=ALU.mult), reads=["kk0"], writes=["sq"])
            P.op("dve", lambda e: e.tensor_reduce(out=st1[:, 0:2], in_=sq[:], axis=AX.X, op=ALU.add), reads=["sq"], writes=["st_k"])
            P.op("dve", lambda e: e.tensor_scalar(out=st1[:, 0:2], in0=st1[:, 0:2], scalar1=1e-24, scalar2=None, op0=ALU.max), reads=["st_k"], writes=["st_k"])
            P.op("act", lambda e: e.sqrt(out=st1[:, 0:2], in_=st1[:, 0:2]), reads=["st_k"], writes=["st_k"])
            P.op("dve", lambda e: e.reciprocal(out=st1[:, 0:2], in_=st1[:, 0:2]), reads=["st_k"], writes=["st_k"])
            P.op("dve", lambda e, so=so: e.tensor_tensor(out=h3(strm[:, so, 1, :]), in0=kk0[:], in1=st1[:, 0:2].unsqueeze(2).to_broadcast([128, 2, 64]), op=ALU.mult),
                 reads=["kk0", "st_k"], writes=[("strm", so, 1)])
            P.op("dve", lambda e, so=so: e.scalar_tensor_tensor(out=strm[:, so, 2, :], in0=strm[:, so, 1, :], scalar=-1.0, in1=aa[:], op0=ALU.mult, op1=ALU.mult),
                 reads=[("strm", so, 1), "aa"], writes=[("strm", so, 2)])
            P.op("dve", lambda e: e.scalar_tensor_tensor(out=tmp[:], in0=aa[:], scalar=-1.0, in1=cv[:, KA, :], op0=ALU.add, op1=ALU.mult), reads=["aa", "cv"], writes=["tmp"])
            P.op("dve", lambda e, so=so: e.scalar_tensor_tensor(out=strm[:, so, 3, :], in0=tmp[:], scalar=1.0, in1=k_, op0=ALU.add, op1=ALU.mult),
                 reads=["tmp", "rkv"], writes=[("strm", so, 3)])
            P.op("pool", lambda e, so=so: e.tensor_tensor(out=strm[:, so, 4, :], in0=r_, in1=strm[:, so, 0, :], op=ALU.mult), reads=["rkv", ("strm", so, 0)], writes=[("strm", so, 4)])
            P.op("dve", lambda e, so=so: e.tensor_tensor(out=tmp[:], in0=r_, in1=strm[:, so, 3, :], op=ALU.mult), reads=["rkv", ("strm", so, 3), "tmp"], writes=["tmp"])
            P.op("dve", lambda e: e.tensor_tensor(out=tmp[:], in0=tmp[:], in1=cv[:, RK, :], op=ALU.mult), reads=["tmp", "cv"], writes=["tmp"])
            P.op("dve", lambda e: e.tensor_reduce(out=st1[:, 2:4], in_=h3(tmp[:]), axis=AX.X, op=ALU.add), reads=["tmp"], writes=["st_b"])
            P.op("dve", lambda e, i=i: e.tensor_tensor(out=h3(bon_all[:, i, :]), in0=h3(v_), in1=st1[:, 2:4].unsqueeze(2).to_broadcast([128, 2, 64]), op=ALU.mult),
                 reads=["rkv", "st_b"], writes=[("bon", i)])
            P.op("pool", lambda e, so=so: e.tensor_tensor(out=strm[:, so, 1, :], in0=strm[:, so, 1, :], in1=pexc[:], op=ALU.mult), reads=[("strm", so, 1), ("strm", so, 2), "pexc"], writes=[("strm", so, 1)])
            P.op("pool", lambda e, so=so: e.tensor_tensor(out=strm[:, so, 2, :], in0=strm[:, so, 2, :], in1=pinv[:], op=ALU.mult), reads=[("strm", so, 2), "pinv"], writes=[("strm", so, 2)])
            P.op("pool", lambda e, so=so: e.tensor_tensor(out=strm[:, so, 3, :], in0=strm[:, so, 3, :], in1=pinv[:], op=ALU.mult), reads=[("strm", so, 3), "pinv", "tmp"], writes=[("strm", so, 3)])
            ma.tile(i)
            skeys = [("strm", so, s_) for s_ in range(5)]
            P.op("pool", lambda e, so=so: e.tensor_copy(out=shl[:, so, 0], in_=strm[:, so]), reads=skeys, writes=[("shl", so, 0)])
            P.op("pool", lambda e, so=so: e.tensor_tensor(out=sdf[:], in0=strm[:, so], in1=shl[:, so, 0], op=ALU.subtract), reads=skeys + [("shl", so, 0)], writes=["sdf"])
            P.op("pool", lambda e, so=so: e.tensor_copy(out=shl[:, so, 1], in_=sdf[:]), reads=["sdf"], writes=[("shl", so, 1)])
            for hl in range(2):
                for s_ in range(5):
                    r0 = hl * 10 + 2 * s_
                    P.dma("sp" if hl == 0 else "pool", scr[r0:r0 + 2, i * 128:(i + 1) * 128, :].rearrange("h t k -> t h k"), h3(shl[:, so, hl, s_, :]),
                          reads=[("shl", so, hl)], writes=[("scr", i)])

        P.barrier()
        P.op("dve", lambda e: e.memset(Sb2[:], 0.0), writes=[("S", 0), ("S", 1)])
        nsteps = ntile * 128
        SW, SKK, SNB, SK, SR = range(5)
        SLOT = {0: 0, 4: 1, 1: 2, 2: 3, 3: 4}
        def bcap(par, s_, j):
            f = SLOT[s_] * 256
            return ps[par * 3 + f // 512][:, (f % 512) + j * 64:(f % 512) + (j + 1) * 64]
        def bcblk(par, s_):
            f = SLOT[s_] * 256
            return ps[par * 3 + f // 512][:, (f % 512):(f % 512) + 256]
        if upto < 3:
            P.op('dve', lambda e: e.memset(yT_all[:], 0.0), writes=[('yT', i) for i in range(ntile)])
        for ch in range(nsteps // NST if upto >= 2 else 0):
            slot = ch % 2
            P.dma("sp", rows[:, slot, :].rearrange("p (t k) -> p t k", k=64), scr[:, ch * NST:(ch + 1) * NST, :],
                  reads=[("scr", (ch * NST) // 128)], writes=[("rows", slot)])
            for gg in range(NST // 4):
                g = ch * (NST // 4) + gg
                par = g % 2
                def bc(e, par=par, slot=slot, gg=gg):
                    for s_ in range(5):
                        ins = e.matmul(bcblk(par, s_), lhsT=sel[:, s_, :], rhs=rows[:, slot, gg * 256:(gg + 1) * 256], start=True, stop=True)
                    return ins
                P.op("pe", bc, reads=[("rows", slot), "sel"], writes=[("bc", par)])
                if upto >= 3:
                    pass
                for j in range(4 if upto >= 3 else 0):
                    t = g * 4 + j
                    ti = t // 128
                    cb = ch % 2
                    Sx = Sb2[:, cb, :]
                    kS = ("S", cb)
                    last_in_chunk = (t % NST == NST - 1)

                    def yop(pt, ppar, pj_, pcb, same=True):
                        P.op("dve", lambda e, ppar=ppar, pj_=pj_, pt=pt, pcb=pcb: e.scalar_tensor_tensor(out=junk2[:], in0=Sb2[:, pcb, :], scalar=1.0, in1=bcap(ppar, SR, pj_), op0=ALU.mult, op1=ALU.mult,
                                                                                                   accum_out=yT_all[:, pt:pt + 1]),
                             reads=[("S", pcb), ("bc", ppar)], writes=["junk2", ("yT", pt // 128)], same_ok=same)
                    P.op("dve", lambda e, par=par, j=j, Sx=Sx: e.scalar_tensor_tensor(out=junk[:], in0=Sx, scalar=1.0, in1=bcap(par, SKK, j), op0=ALU.mult, op1=ALU.mult, accum_out=sa[:]),
                         reads=[kS, ("bc", par)], writes=["junk", "sa"], same_ok=(t > 0))
                    P.op("dve", lambda e, par=par, j=j, t=t, Sx=Sx: e.scalar_tensor_tensor(out=Ubuf[:], in0=bcap(par, SK, j), scalar=vT_all[:, t:t + 1], in1=Sx, op0=ALU.mult, op1=ALU.add),
                         reads=[kS, ("bc", par), ("vT", ti)], writes=["Ubuf"], same_ok=(t > 0))
                    if prev_step is not None:
                        yop(*prev_step)
                    P.op("dve", lambda e, par=par, j=j, Sx=Sx: e.scalar_tensor_tensor(out=Sx, in0=bcap(par, SNB, j), scalar=sa[:], in1=Ubuf[:], op0=ALU.mult, op1=ALU.add),
                         reads=["sa", "Ubuf", ("bc", par)], writes=[kS], same_ok=True)
                    prev_step = (t, par, j, cb)
                    if last_in_chunk:
                        yop(*prev_step)
                        prev_step = None
                        P.op("dve", lambda e, par=par, j=j, cb=cb: e.tensor_tensor(out=Sb2[:, 1 - cb, :], in0=Sb2[:, cb, :], in1=bcap(par, SW, j), op=ALU.mult),
                             reads=[("S", cb), ("bc", par)], writes=[("S", 1 - cb)], same_ok=True)
                        P.op("dve", lambda e: e.memset(fill[:, 0:1], 0.0), writes=["fill"], same_ok=True)

        P.barrier()
        for i in range(ntile):
            so = i % 2
            P.op("pe", lambda e, i=i: e.transpose(out=ps[5][:, 0:128], in_=yT_all[:, i * 128:(i + 1) * 128], identity=pj.idf[:]), reads=[("yT", i), "idf"], writes=["ps5"])
            y3 = ps[5][:, 0:128].rearrange("p (h d) -> p h d", h=2)
            P.op("dve", lambda e: e.tensor_reduce(out=st1[:, 0:2], in_=y3, axis=AX.X, op=ALU.add), reads=["ps5"], writes=["st_k"])
            P.op("dve", lambda e: e.tensor_scalar(out=st1[:, 0:2], in0=st1[:, 0:2], scalar1=1.0 / 64, scalar2=None, op0=ALU.mult), reads=["st_k"], writes=["st_k"])
            P.op("dve", lambda e: e.tensor_tensor(out=yc[:], in0=y3, in1=st1[:, 0:2].unsqueeze(2).to_broadcast([128, 2, 64]), op=ALU.subtract), reads=["ps5", "st_k"], writes=["yc"])
            P.op("dve", lambda e: e.tensor_tensor(out=sq[:], in0=yc[:], in1=yc[:], op=ALU.mult), reads=["yc"], writes=["sq"])
            P.op("dve", lambda e: e.tensor_reduce(out=st1[:, 2:4], in_=sq[:], axis=AX.X, op=ALU.add), reads=["sq"], writes=["st_b"])
            P.op("dve", lambda e: e.tensor_scalar(out=st1[:, 2:4], in0=st1[:, 2:4], scalar1=1.0 / 64, scalar2=64e-5, op0=ALU.mult, op1=ALU.add), reads=["st_b"], writes=["st_b"])
            P.op("act", lambda e: e.sqrt(out=st1[:, 2:4], in_=st1[:, 2:4]), reads=["st_b"], writes=["st_b"])
            P.op("dve", lambda e: e.reciprocal(out=st1[:, 2:4], in_=st1[:, 2:4]), reads=["st_b"], writes=["st_b"])
            P.op("dve", lambda e: e.tensor_tensor(out=yc[:], in0=yc[:], in1=st1[:, 2:4].unsqueeze(2).to_broadcast([128, 2, 64]), op=ALU.mult), reads=["yc", "st_b"], writes=["yc"])
            ycf = yc[:].rearrange("p h d -> p (h d)")
            P.op("dve", lambda e: e.tensor_tensor(out=ycf, in0=ycf, in1=cv[:, GNG, :], op=ALU.mult), reads=["yc", "cv"], writes=["yc"])
            P.op("dve", lambda e: e.tensor_tensor(out=ycf, in0=ycf, in1=cv[:, GNB, :], op=ALU.add), reads=["yc", "cv"], writes=["yc"])
            P.op("dve", lambda e, i=i: e.tensor_tensor(out=ycf, in0=ycf, in1=bon_all[:, i, :], op=ALU.add), reads=["yc", ("bon", i)], writes=["yc"])
            P.op("dve", lambda e, i=i, so=so: e.tensor_tensor(out=yo[:, so, :], in0=ycf, in1=g_all[:, i, :], op=ALU.mult), reads=["yc", ("g_all", i)], writes=[("yo", so)])
            P.dma("sp", ysrc[i * 128:(i + 1) * 128, 128:256], yo[:, so, :], reads=[("yo", so)], writes=["ysrc"])
        P.emit(last=is_last)


def prep_MB(layer, h, inputs):
    i = layer
    maps = []
    w = inputs["w_in"][i]
    sel = np.zeros((20, 5, 128), np.float32)
    for hl in range(2):
        for s_ in range(5):
            for hh in range(2):
                sel[hl * 10 + s_ * 2 + hh, s_, hh * 64:(hh + 1) * 64] = 1.0
    for c in range(8):
        b, gi = c // 2, c % 2
        hc = slice(gi * 128, (gi + 1) * 128)
        cols = np.concatenate([512 + np.arange(gi * 128, (gi + 1) * 128), 768 + np.arange(gi * 128, (gi + 1) * 128), 1024 + np.arange(gi * 128, (gi + 1) * 128),
                               np.arange(1280, 1536)])
        maps.append({
            "h": np.ascontiguousarray(h[b]), "identf": np.eye(128, dtype=np.float32),
            "wc": np.ascontiguousarray(np.concatenate([w[:, cols], w[:, gi * 128:(gi + 1) * 128], w[:, 256 + gi * 128:256 + (gi + 1) * 128]], axis=1)),
            "a_lng": bc128(inputs["gm_ln_g"][i][2 * gi:2 * gi + 2].reshape(-1)), "a_lnb": bc128(inputs["gm_ln_b"][i][2 * gi:2 * gi + 2].reshape(-1)),
            "a_ws": np.ascontiguousarray(inputs["gm_ws"][i][2 * gi:2 * gi + 2]), "a_tril": np.tril(np.ones((128, 128), np.float32)),
            "a_bs": np.ascontiguousarray(inputs["gm_bs"][i][2 * gi:2 * gi + 2].T),
            "g_mix": pc8(inputs["g_mix"][i]), "mu": bc128(inputs["rw_mu"][i][cols - 512]),
            "wa_up": np.ascontiguousarray(np.concatenate([inputs["rw_w_up"][i][:, hc], inputs["rw_a_up"][i][:, hc]], axis=0)),
            "g_up": np.ascontiguousarray(inputs["rw_g_up"][i][:, hc]),
            "w0a0": np.concatenate([inputs["rw_w0"][i][hc], inputs["rw_a0"][i][hc]])[None, :].astype(np.float32),
            "cvec": np.ascontiguousarray(np.stack([bc128(inputs["rw_k_k"][i][hc]), bc128(inputs["rw_k_a"][i][hc]), bc128(inputs["rw_r_k"][i].reshape(-1)[hc]),
                                                   bc128(inputs["rw_gn_g"][i][hc]), bc128(inputs["rw_gn_b"][i][hc])], axis=1)),
            "sel": sel,
            "mcum": ((np.arange(128)[:, None] // 32 == np.arange(128)[None, :] // 32) & (np.arange(128)[:, None] <= np.arange(128)[None, :])).astype(np.float32),
        })
    return maps


NEGB = -30000.0
MC_BR = 'csw'


def phase_MC(nc, P, ps, psb, pre, h_in, ysrc, hmap=None, ntile=NTILE, upto=9, is_last=False):
    import ml_dtypes
    di = lambda n, s, dt=F32: nc.dram_tensor(pre + n, s, dt, kind="ExternalInput").ap()
    identf = di("identf", [128, 128])
    wc = di("wc", [D, 652])
    g_mix = di("g_mix", [128, 8])
    pos_in = di("pos", [128, NTILE], I32)
    invf = di("invf", [128, 8])
    w1kc = di("w1kc", [2048, 128]); w1vc = di("w1vc", [2048, 128])
    w2kc = di("w2kc", [128, 64]); w2vc = di("w2vc", [128, 64])
    posT = di("posT", [64, 32])
    rconst = di("rconst", [128, 2, 65])
    cmask = di("cmask", [NTILE, 128, 2, 128], BF16)
    selc = di("selc", [NTILE, 128, 2, 64])
    Ef_in = di("Ef", [64, S], BF16)
    tri_in = di("tri", [128, 2, 128], BF16)
    with ExitStack() as st:
        sb = lambda name, shape, dt: st.enter_context(nc.sbuf_tensor(pre + "s_" + name, shape, dt))
        pj = Proj(nc, P, st, h_in, identf, 652, wc, g_mix, pre=pre, hmap=hmap)
        qrT = sb("qrT", [64, 4, S], BF16)
        qwT = sb("qwT", [64, 4, S], BF16)
        kT4 = sb("kT4", [64, 4, S], BF16)
        vaug = sb("vaug", [128, 2, NTILE, 65], BF16)
        ksE = sb("ksE", [128, S], BF16)
        qs = sb("qs", [128, 2, 4, 128], BF16)
        tri = sb("tris", [128, 2, 128], BF16)
        w1s = sb("w1s", [64, 32, 128], F32)
        w1b = sb("w1b", [64, 2, 32, 128], BF16)
        w2f = sb("w2f", [128, 2, 64], F32); w2b = sb("w2b", [128, 2, 64], BF16)
        posf = sb("posf", [64, 32], F32); posb = sb("posb", [64, 32], BF16)
        rcf = sb("rcf", [128, 2, 65], F32)
        R_ = sb("R_", [128, 2, 129], BF16)
        posi = sb("posi", [128, NTILE], I32); posfl = sb("posfl", [128, NTILE], F32)
        inv = sb("inv", [128, 8], F32)
        ang = sb("ang", [128, NTILE, 8], F32)
        cs = sb("cs", [128, NTILE, 8], F32); sn = sb("sn", [128, NTILE, 8], F32)
        gsig = sb("gsig", [128, NTILE, 12], F32)
        xq = sb("xq", [128, 512], F32)
        xqb = sb("xqb", [128, 512], BF16)
        qkr = sb("qkr", [128, 6, 64], BF16)
        ra = sb("ra", [128, 6, 8], F32); rb_ = sb("rb_", [128, 6, 8], F32)
        bias_sb = sb("bias_sb", [128, 2], F32)
        gelT = sb("gelT", [128, 2, 256], BF16)
        kcmpT = sb("kcmpT", [64, 256], BF16)
        cm = sb("cm", [128, 2, 2, 128], BF16)
        scs = sb("scs", [128, 2, 2, 64], F32)
        eT = sb("eT", [128, 2, 4, 128], BF16)
        imp = sb("imp", [128, 64], F32)
        imp2 = sb("imp2", [128, 64], F32)
        rep = sb("rep", [128, 64], F32)
        mx = sb("mx", [128, 8], F32)
        selbb = sb("selbb", [128, 128], BF16)
        selbT = sb("selbT", [64, 128], BF16)
        sm = sb("sm", [128, 16], F32)
        oacc = sb("oacc", [128, 2, 4, 64], F32)

        ld = lambda dst, src, key: P.dma("sp", dst, src, writes=[key])
        ld(ksE[64:128, :], Ef_in, "ksE_E"); ld(tri[:], tri_in, "tri")
        P.op("pool", lambda e: e.memset(selbb[:], 0.0), writes=["selbb"])
        ld(posi[:], pos_in, "posi"); ld(inv[:], invf, "inv")
        ld(rcf[:], rconst, "rcf"); ld(posf[:], posT, "posf")
        ld(w2f[:, 0, :], w2kc, "w2f0"); ld(w2f[:, 1, :], w2vc, "w2f1")
        P.op("dve", lambda e: e.tensor_copy(out=w2b[:], in_=w2f[:]), reads=["w2f0", "w2f1"], writes=["w2b"])
        P.op("dve", lambda e: e.tensor_copy(out=posb[:], in_=posf[:]), reads=["posf"], writes=["posb"])
        P.op("dve", lambda e: e.memset(R_[:], 0.0), writes=["R"])
        P.op("dve", lambda e: e.tensor_copy(out=R_[:, :, 0:65], in_=rcf[:]), reads=["rcf", "R"], writes=["R"])
        P.op("pool", lambda e: e.memset(vaug[:], 1.0), writes=["vaug_init"])
        P.op("pool", lambda e: e.memset(gelT[:], 0.0), writes=["gelT0", "gelT1"])
        for w in range(2):
            P.dma("sp", w1s[:], (w1kc if w == 0 else w1vc).rearrange("(l d) h -> d l h", d=64), writes=["w1s"])
            P.op("pool", lambda e, w=w: e.tensor_copy(out=w1b[:, w, :, :], in_=w1s[:]), reads=["w1s"], writes=[("w1b", w)])
        P.op("dve", lambda e: e.tensor_copy(out=posfl[:], in_=posi[:]), reads=["posi"], writes=["posfl"])
        P.op("dve", lambda e: e.tensor_tensor(out=ang[:], in0=posfl[:].unsqueeze(2).to_broadcast([128, NTILE, 8]), in1=inv[:].unsqueeze(1).to_broadcast([128, NTILE, 8]), op=ALU.mult),
             reads=["posfl", "inv"], writes=["ang"])
        PI = float(np.pi)
        angi = sb("angi", [128, NTILE, 8], I32)
        angf = sb("angf", [128, NTILE, 8], F32)
        for (dst, off, key) in ((sn, 0.5, "sn"), (cs, 0.75, "cs")):
            P.op("dve", lambda e, dst=dst, off=off: e.tensor_scalar(out=dst[:], in0=ang[:], scalar1=1.0 / (2 * PI), scalar2=off, op0=ALU.mult, op1=ALU.add), reads=["ang"], writes=[key])
            P.op("dve", lambda e, dst=dst: e.tensor_copy(out=angi[:], in_=dst[:]), reads=[key], writes=["angi"])
            P.op("dve", lambda e: e.tensor_copy(out=angf[:], in_=angi[:]), reads=["angi"], writes=["angf"])
            P.op("dve", lambda e, dst=dst: e.tensor_tensor(out=dst[:], in0=dst[:], in1=angf[:], op=ALU.subtract), reads=[key, "angf"], writes=[key])
            P.op("dve", lambda e, dst=dst: e.tensor_scalar(out=angf[:], in0=dst[:], scalar1=0.0, scalar2=None, op0=ALU.is_lt), reads=[key], writes=["angf"])
            P.op("dve", lambda e, dst=dst: e.tensor_tensor(out=dst[:], in0=dst[:], in1=angf[:], op=ALU.add), reads=[key, "angf"], writes=[key])
            P.op("dve", lambda e, dst=dst: e.tensor_scalar(out=dst[:], in0=dst[:], scalar1=2 * PI, scalar2=-PI, op0=ALU.mult, op1=ALU.add), reads=[key], writes=[key])
            P.op("dve", lambda e, dst=dst: e.tensor_scalar(out=dst[:], in0=dst[:], scalar1=PI, scalar2=-PI, op0=ALU.min, op1=ALU.max), reads=[key], writes=[key])
            P.op("act", lambda e, dst=dst: e.activation(out=dst[:], in_=dst[:], func=AF.Sin), reads=[key], writes=[key])

        pj.tile(0, psb[0], "ps0")
        for i in range(ntile):
            if i + 1 < ntile:
                pj.tile(i + 1, psb[0], "ps0")
            P.op("pe", lambda e, i=i: pj.mm_tok(e, i, ps[1][:], 0, 512), reads=pj.keys(i), writes=["ps1"])
            P.op("pe", lambda e, i=i: pj.mm_tok(e, i, ps[2][:, 0:140], 512, 652), reads=pj.keys(i), writes=["ps2"])
            P.op("act", lambda e: e.activation(out=xq[:], in_=ps[1][:], func=AF.Copy), reads=["ps1"], writes=["xq"])
            P.op("act", lambda e, i=i: e.activation(out=vaug[:, :, i, 0:64], in_=ps[2][:, 0:128].rearrange("p (a d) -> p a d", a=2), func=AF.Copy), reads=["ps2", "vaug_init"], writes=[("vaug", i)])
            P.op("act", lambda e, i=i: e.activation(out=gsig[:, i, :], in_=ps[2][:, 128:140], func=AF.Sigmoid), reads=["ps2"], writes=[("gsig", i)])
            P.op("pool", lambda e: e.tensor_copy(out=xqb[:], in_=xq[:]), reads=["xq"], writes=["xqb"])
            X = xq[:, 0:384].rearrange("p (h d) -> p h d", h=6)
            cb = cs[:, i, :].unsqueeze(1).to_broadcast([128, 6, 8])
            sbb = sn[:, i, :].unsqueeze(1).to_broadcast([128, 6, 8])
            P.op("pool", lambda e, X=X: e.tensor_copy(out=qkr[:, :, 16:64], in_=X[:, :, 16:64]), reads=["xq"], writes=["qkr_c"])
            P.op("dve", lambda e, X=X, cb=cb: e.tensor_tensor(out=ra[:], in0=X[:, :, 0:8], in1=cb, op=ALU.mult), reads=["xq", "cs"], writes=["ra"])
            P.op("dve", lambda e, X=X, sbb=sbb: e.tensor_tensor(out=rb_[:], in0=X[:, :, 8:16], in1=sbb, op=ALU.mult), reads=["xq", "sn"], writes=["rb"])
            P.op("dve", lambda e: e.tensor_tensor(out=qkr[:, :, 0:8], in0=ra[:], in1=rb_[:], op=ALU.subtract), reads=["ra", "rb"], writes=["qkr_a"])
            P.op("dve", lambda e, X=X, sbb=sbb: e.tensor_tensor(out=ra[:], in0=X[:, :, 0:8], in1=sbb, op=ALU.mult), reads=["xq", "sn", "ra"], writes=["ra"])
            P.op("dve", lambda e, X=X, cb=cb: e.tensor_tensor(out=rb_[:], in0=X[:, :, 8:16], in1=cb, op=ALU.mult), reads=["xq", "cs", "rb"], writes=["rb"])
            P.op("dve", lambda e: e.tensor_tensor(out=qkr[:, :, 8:16], in0=ra[:], in1=rb_[:], op=ALU.add), reads=["ra", "rb"], writes=["qkr_b"])
            def trs(e):
                for j in range(4):
                    e.transpose(out=psb[3][0:64, j * 128:(j + 1) * 128], in_=qkr[:, j, :], identity=pj.idb[:])
                for j in range(4):
                    e.transpose(out=psb[5][0:64, j * 128:(j + 1) * 128], in_=xqb[:, j * 64:(j + 1) * 64], identity=pj.idb[:])
                e.transpose(out=psb[4][0:64, 0:128], in_=qkr[:, 4, :], identity=pj.idb[:])
                e.transpose(out=psb[4][0:64, 128:256], in_=qkr[:, 5, :], identity=pj.idb[:])
                e.transpose(out=psb[4][0:64, 256:384], in_=xqb[:, 384:448], identity=pj.idb[:])
                return e.transpose(out=psb[4][0:64, 384:512], in_=xqb[:, 448:512], identity=pj.idb[:])
            P.op("pe", trs, reads=["qkr_a", "qkr_b", "qkr_c", "xqb", "idb"], writes=["ps3", "ps4", "ps5"])
            tsl = slice(i * 128, (i + 1) * 128)
            P.op("act", lambda e, tsl=tsl: e.activation(out=qrT[:, :, tsl], in_=psb[3][0:64, 0:512].rearrange("p (j t) -> p j t", j=4), func=AF.Copy), reads=["ps3"], writes=[("qrT", i)])
            P.op("dve", lambda e, tsl=tsl: e.tensor_copy(out=qwT[:, :, tsl], in_=psb[5][0:64, 0:512].rearrange("p (j t) -> p j t", j=4)), reads=["ps5"], writes=[("qwT", i)])
            P.op("act", lambda e, tsl=tsl: e.activation(out=kT4[:, :, tsl], in_=psb[4][0:64, 0:512].rearrange("p (j t) -> p j t", j=4), func=AF.Copy), reads=["ps4"], writes=[("kT4", i)])
            P.op("act", lambda e, tsl=tsl: e.activation(out=ksE[0:64, tsl], in_=psb[4][0:64, 0:128], func=AF.Copy), reads=["ps4"], writes=[("ksE", i)])

        P.barrier()
        ncmp = (ntile * 128 - 32) // 16 + 1
        for w in range(2 if upto >= 2 else 0):
            src = 2 + w
            def hid(e, w=w, src=src):
                for l in range(32):
                    ins = e.matmul(ps[0][:, 0:ncmp], lhsT=w1b[:, w, l, :], rhs=kT4[:, src, l:l + 16 * (ncmp - 1) + 1:16], start=(l == 0), stop=(l == 31))
                return ins
            P.op("pe", hid, reads=[("w1b", w)], writes=["ps0"])
            def pbias(e, w=w):
                for l in range(32):
                    ins = e.matmul(ps[1][:, 0:1], lhsT=w1b[:, w, l, :], rhs=posb[:, l:l + 1], start=(l == 0), stop=(l == 31))
                return ins
            P.op("pe", pbias, reads=[("w1b", w), "posb"], writes=["ps1"])
            P.op("dve", lambda e, w=w: e.tensor_copy(out=bias_sb[:, w:w + 1], in_=ps[1][:, 0:1]), reads=["ps1"], writes=[("bias", w)])
            P.op("act", lambda e, w=w: e.activation(out=gelT[:, w, 0:ncmp], in_=ps[0][:, 0:ncmp], func=AF.Gelu_apprx_tanh, bias=bias_sb[:, w:w + 1]), reads=["ps0", ("bias", w), f"gelT{w}"], writes=[f"gelT{w}"])
        if upto >= 2:
            P.op("pe", lambda e: e.matmul(ps[2][0:64, 0:256], lhsT=w2b[:, 0, :], rhs=gelT[:, 0, :], start=True, stop=True), reads=["w2b", "gelT0"], writes=["ps2"])
            P.op("act", lambda e: e.activation(out=kcmpT[:], in_=ps[2][0:64, 0:256], func=AF.Copy), reads=["ps2"], writes=["kcmpT"])
        for cn in range(2 if upto >= 2 else 0):
            P.op("pe", lambda e, cn=cn: e.matmul(ps[3][:, cn * 64:(cn + 1) * 64], lhsT=gelT[:, 1, cn * 128:(cn + 1) * 128], rhs=w2b[:, 1, :], start=True, stop=True), reads=["w2b", "gelT1"], writes=["ps3"])
            P.op("dve", lambda e, cn=cn: e.tensor_copy(out=R_[:, cn, 65:129], in_=ps[3][:, cn * 64:(cn + 1) * 64]), reads=["ps3", "R"], writes=["R"])

        P.barrier()
        nsc = 0
        if upto < 3:
            P.op('dve', lambda e: e.memset(oacc[:], 0.0), writes=[('oacc', 0), ('oacc', 1)])
            P.dma('sp', ysrc[0:128, 256:512], oacc[:, 0].rearrange('p j d -> p (j d)'), reads=[('oacc', 0)], writes=['ysrc'])
        for qb in range(ntile if upto >= 3 else 0):
            tsl = slice(qb * 128, (qb + 1) * 128)
            so = qb % 2
            P.dma("sp", cm[:, so], cmask[qb], writes=[("cm", so)])
            P.dma("sp", scs[:, so], selc[qb], writes=[("scs", so)])
            ncn = 2 if qb >= 16 else 1
            for cn in range(ncn):
                sbk = nsc % 2; nsc += 1
                def sc(e, cn=cn, sbk=sbk, tsl=tsl, so=so):
                    e.matmul(ps[sbk][:], lhsT=kcmpT[:, cn * 128:(cn + 1) * 128], rhs=qwT[:, :, tsl], start=True, stop=False)
                    return e.matmul(ps[sbk][:], lhsT=pj.idb[:], rhs=cm[:, so, cn, :].unsqueeze(1).to_broadcast([128, 4, 128]), start=False, stop=True)
                P.op("pe", sc, reads=["kcmpT", ("qwT", qb), ("cm", so), "idb"], writes=[f"ps{sbk}"])
                P.op("act", lambda e, sbk=sbk: e.activation(out=eT[:, sbk].rearrange("p j t -> p (j t)"), in_=ps[sbk][:], func=AF.Exp, scale=0.125), reads=[f"ps{sbk}"], writes=[("eT", sbk)])
                def pv(e, cn=cn, sbk=sbk, ncn=ncn):
                    for j in range(4):
                        ins = e.matmul(ps[2 + j // 2][:, (j % 2) * 129:(j % 2) * 129 + 129], lhsT=eT[:, sbk, j, :], rhs=R_[:, cn, :], start=(cn == 0 and j % 2 == 0), stop=(cn == ncn - 1), skip_group_check=True)
                    return ins
                P.op("pe", pv, reads=[("eT", sbk), "R"], writes=["ps2", "ps3"])
            P.op("dve", lambda e: e.memset(imp[:], 0.0), writes=["imp"])
            for j in range(4):
                pso = ps[2 + j // 2][:, (j % 2) * 129:(j % 2) * 129 + 129]
                ri = sm[:, j:j + 1]; rg = sm[:, 4 + j:5 + j]
                P.op("dve", lambda e, pso=pso, ri=ri: e.tensor_scalar(out=ri, in0=pso[:, 64:65], scalar1=1e-30, scalar2=None, op0=ALU.add), reads=["ps2", "ps3"], writes=[("ri", j)])
                P.op("dve", lambda e, ri=ri: e.reciprocal(out=ri, in_=ri), reads=[("ri", j)], writes=[("ri", j)])
                P.op("dve", lambda e, pso=pso, ri=ri: e.scalar_tensor_tensor(out=imp[:], in0=pso[:, 0:64], scalar=ri, in1=imp[:], op0=ALU.mult, op1=ALU.add), reads=["ps2", "ps3", ("ri", j), "imp"], writes=["imp"])
                P.op("dve", lambda e, ri=ri, rg=rg, j=j, qb=qb: e.tensor_tensor(out=rg, in0=ri, in1=gsig[:, qb, 3 * j:3 * j + 1], op=ALU.mult), reads=[("ri", j), ("gsig", qb)], writes=[("rg", j)])
                P.op("dve", lambda e, pso=pso, rg=rg, j=j, so=so: e.tensor_scalar(out=oacc[:, so, j, :], in0=pso[:, 65:129], scalar1=rg, scalar2=None, op0=ALU.mult), reads=["ps2", "ps3", ("rg", j)], writes=[("oacc", so)])
            if upto < 4:
                P.dma('sp', ysrc[tsl, 256:512], oacc[:, so].rearrange('p j d -> p (j d)'), reads=[('oacc', so)], writes=['ysrc'])
                continue
            P.op("dve", lambda e, so=so: e.tensor_tensor(out=imp2[:], in0=imp[:], in1=scs[:, so, 0, :], op=ALU.mult), reads=["imp", ("scs", so)], writes=["imp2"])
            P.op("dve", lambda e, so=so: e.tensor_tensor(out=imp2[:], in0=imp2[:], in1=scs[:, so, 1, :], op=ALU.add), reads=["imp2", ("scs", so)], writes=["imp2"])
            P.op("dve", lambda e: e.max(out=mx[:], in_=imp2[:]), reads=["imp2"], writes=["mx"])
            P.op("dve", lambda e: e.match_replace(out=rep[:], in_to_replace=mx[:], in_values=imp2[:], imm_value=-1e30), reads=["imp2", "mx"], writes=["rep"])
            P.op("dve", lambda e: e.max(out=mx[:], in_=rep[:]), reads=["rep"], writes=["mx"])
            P.op("dve", lambda e: e.tensor_scalar(out=mx[:, 7:8], in0=mx[:, 7:8], scalar1=-5000.0, scalar2=None, op0=ALU.max), reads=["mx"], writes=["mx"])
            P.op("dve", lambda e: e.tensor_scalar(out=rep[:], in0=imp2[:], scalar1=mx[:, 7:8], scalar2=None, op0=ALU.is_ge), reads=["imp2", "mx", "rep"], writes=["rep"])
            P.op("dve", lambda e: e.tensor_scalar(out=selbb[:, 64:128], in0=rep[:], scalar1=-NEGB, scalar2=NEGB, op0=ALU.mult, op1=ALU.add), reads=["rep", "selbb"], writes=["selbb"])
            P.op("pe", lambda e: e.transpose(out=psb[6][:, 0:128], in_=selbb[:], identity=pj.idb[:]), reads=["selbb", "idb"], writes=["ps6"])
            P.op("act", lambda e, so=so: e.activation(out=qs[64:128, so], in_=psb[6][64:128, 0:128].unsqueeze(1).to_broadcast([64, 4, 128]), func=AF.Copy), reads=["ps6"], writes=[("qs_b", so)])
            P.op("pool", lambda e, so=so, tsl=tsl: e.tensor_copy(out=qs[0:64, so], in_=qrT[:, :, tsl]), reads=[("qrT", qb)], writes=[("qs_a", so)])
            jobs = [("s", c) for c in range(qb + 1)] + [("w", c) for c in range(max(0, qb - 4), qb + 1)]
            pend = None
            first = {"s": True, "w": True}
            last_c = {"s": qb, "w": qb}
            for job in jobs + [(None, None)]:
                kind, c = job
                cur = None
                if kind is not None:
                    sbk = nsc % 2; nsc += 1
                    ksrc = 0 if kind == "s" else 1
                    def sc2(e, kind=kind, c=c, sbk=sbk, ksrc=ksrc, tsl=tsl, qb=qb):
                        extra = []
                        if c == qb:
                            extra.append((pj.idb[:], tri[:, 0, :].unsqueeze(1).to_broadcast([128, 4, 128])))
                        if kind == "w" and c == qb - 4:
                            extra.append((pj.idb[:], tri[:, 1, :].unsqueeze(1).to_broadcast([128, 4, 128])))
                        if kind == "s":
                            ins = e.matmul(ps[sbk][:], lhsT=ksE[:, c * 128:(c + 1) * 128], rhs=qs[:, qb % 2], start=True, stop=(len(extra) == 0))
                        else:
                            ins = e.matmul(ps[sbk][:], lhsT=kT4[:, ksrc, c * 128:(c + 1) * 128], rhs=qrT[:, :, tsl], start=True, stop=(len(extra) == 0))
                        for n_, (l_, r_) in enumerate(extra):
                            ins = e.matmul(ps[sbk][:], lhsT=l_, rhs=r_, start=False, stop=(n_ == len(extra) - 1))
                        return ins
                    P.op("pe", sc2, reads=[("kT4", c), ("ksE", c), "ksE_E", ("qrT", qb), ("qs_a", qb % 2), ("qs_b", qb % 2), "tri", "idb"], writes=[f"ps{sbk}"])
                    P.op("act", lambda e, sbk=sbk: e.activation(out=eT[:, sbk].rearrange("p j t -> p (j t)"), in_=ps[sbk][:], func=AF.Exp, scale=0.125), reads=[f"ps{sbk}"], writes=[("eT", sbk)])
                    cur = (kind, c, sbk)
                if pend is not None:
                    pk, pc, pb = pend
                    bank = 4 if pk == "s" else 5
                    vi = 0 if pk == "s" else 1
                    c0 = 0 if pk == "s" else max(0, qb - 4)
                    def pv2(e, pk=pk, pc=pc, pb=pb, bank=bank, vi=vi, c0=c0, qb=qb):
                        for j in range(4):
                            ins = e.matmul(ps[bank][:, j * 65:(j + 1) * 65], lhsT=eT[:, pb, j, :], rhs=vaug[:, vi, pc, :], start=(pc == c0 and j == 0), stop=(pc == qb), skip_group_check=True)
                        return ins
                    P.op("pe", pv2, reads=[("eT", pb), ("vaug", pc)], writes=[f"ps{bank}"])
                pend = cur
            for j in range(4):
                for (bank, gcol, tag) in ((4, 1, "s"), (5, 2, "w")):
                    if tag not in MC_BR:
                        continue
                    pso = ps[bank][:, j * 65:(j + 1) * 65]
                    ri = sm[:, 8:9]
                    P.op("dve", lambda e, pso=pso, ri=ri: e.reciprocal(out=ri, in_=pso[:, 64:65]), reads=[f"ps{bank}"], writes=["ri2"])
                    P.op("dve", lambda e, ri=ri, j=j, gcol=gcol, qb=qb: e.tensor_tensor(out=ri, in0=ri, in1=gsig[:, qb, 3 * j + gcol:3 * j + gcol + 1], op=ALU.mult), reads=["ri2", ("gsig", qb)], writes=["ri2"])
                    P.op("dve", lambda e, pso=pso, ri=ri, j=j, so=so: e.scalar_tensor_tensor(out=oacc[:, so, j, :], in0=pso[:, 0:64], scalar=ri, in1=oacc[:, so, j, :], op0=ALU.mult, op1=ALU.add),
                         reads=[f"ps{bank}", "ri2", ("oacc", so)], writes=[("oacc", so)])
            P.dma("sp", ysrc[tsl, 256:512], oacc[:, so].rearrange("p j d -> p (j d)"), reads=[("oacc", so)], writes=["ysrc"])
        P.emit(last=is_last)


def nsa_consts():
    import ml_dtypes
    bf = ml_dtypes.bfloat16
    n = np.arange(256)[:, None]
    cmask = np.zeros((NTILE, 128, 2, 128), np.float32)
    for qb in range(NTILE):
        t = qb * 128 + np.arange(128)[None, :]
        ok = (16 * n + 31 <= t) & (n < 255)
        m = np.where(ok, 0.0, NEGB).astype(np.float32)
        cmask[qb] = m.reshape(2, 128, 128).transpose(1, 0, 2)
    selc = np.zeros((NTILE, 128, 2, 64), np.float32)
    mids = np.arange(64)[None, :]
    for qb in range(NTILE):
        t = qb * 128 + np.arange(128)[:, None]
        blk = t // 64
        valid = mids <= blk
        forced = (mids == 0) | (mids == blk) | (mids == blk - 1)
        selc[qb, :, 0, :] = valid.astype(np.float32)
        selc[qb, :, 1, :] = np.where(valid, 1e4 * forced.astype(np.float32), -1e4)
    Ef = (np.arange(S)[None, :] // 64 == np.arange(64)[:, None]).astype(np.float32)
    Eb = np.zeros((64, NTILE, 128), np.float32)
    for c in range(NTILE):
        for sl in range(128):
            Eb[2 * c + sl // 64, c, sl] = 1.0
    s_ = np.arange(128)[:, None]; t_ = np.arange(128)[None, :]
    tri = np.zeros((128, 2, 128), np.float32)
    tri[:, 0, :] = np.where(s_ > t_, NEGB, 0.0)
    tri[:, 1, :] = np.where(s_ <= t_, NEGB, 0.0)
    cmp_idx = np.arange(255)[:, None] * 16 + np.arange(32)[None, :]
    slc_start = np.arange(64) * 64
    overlap = ((cmp_idx[:, :1] < slc_start[None, :] + 64) & (cmp_idx[:, -1:] >= slc_start[None, :])).astype(np.float32)
    rc = np.zeros((256, 65), np.float32)
    rc[:255, :64] = overlap
    rc[:255, 64] = 1.0
    rconst = np.ascontiguousarray(rc.reshape(2, 128, 65).transpose(1, 0, 2))
    inv = (1.0 / (np.float32(500000.0) ** (np.arange(0, 16, 2, dtype=np.float32) / np.float32(16)))).astype(np.float32)
    return {"cmask": cmask.astype(bf), "selc": selc, "Ef": Ef.astype(bf), "tri": tri.astype(bf), "rconst": rconst, "invf": bc128(inv)}


def prep_MC(layer, h, inputs):
    i = layer
    maps = []
    w = inputs["w_in"][i]
    cst = nsa_consts()
    for c in range(8):
        b, gi = c // 2, c % 2
        o = 1536
        cols = np.concatenate([o + np.arange(gi * 256, (gi + 1) * 256),
                               o + 512 + 2 * 128 + np.arange(gi * 64, (gi + 1) * 64),
                               o + 512 + 4 * 128 + np.arange(gi * 64, (gi + 1) * 64),
                               o + 512 + 0 * 128 + np.arange(gi * 64, (gi + 1) * 64),
                               o + 512 + 1 * 128 + np.arange(gi * 64, (gi + 1) * 64),
                               o + 512 + 3 * 128 + np.arange(gi * 64, (gi + 1) * 64),
                               o + 512 + 5 * 128 + np.arange(gi * 64, (gi + 1) * 64),
                               o + 512 + 6 * 128 + np.arange(gi * 12, (gi + 1) * 12)])
        m = {
            "h": np.ascontiguousarray(h[b]), "identf": np.eye(128, dtype=np.float32), "wc": np.ascontiguousarray(w[:, cols]),
            "g_mix": pc8(inputs["g_mix"][i]),
            "pos": np.ascontiguousarray(inputs["positions"][b].reshape(NTILE, 128).T.astype(np.int32)),
            "w1kc": inputs["nsa_kc_w1"][i], "w1vc": inputs["nsa_vc_w1"][i], "w2kc": inputs["nsa_kc_w2"][i], "w2vc": inputs["nsa_vc_w2"][i],
            "posT": np.ascontiguousarray(inputs["nsa_cmp_pos"][i].T),
        }
        m.update(cst)
        maps.append(m)
    return maps


PAIRS = [[0, 1], [2, 3], [4, 5], [6, 7]]


def build_fused():
    nc = bass.Bass("TRN2", target_bir_lowering=False)
    xfull = nc.dram_tensor("xfull", [S, D], F32, kind="ExternalInput").ap()
    xhalf = nc.dram_tensor("xhalf", [TF, D], F32, kind="ExternalInput").ap()
    h_out = nc.dram_tensor("h_out", [TF, D], F32, kind="ExternalOutput").ap()
    ysrc = nc.dram_tensor("ysrc", [S, 512], F32).ap()
    ydst = nc.dram_tensor("ydst", [2 * S, 512], F32).ap()
    hsrc = nc.dram_tensor("hsrc", [TF, D], F32).ap()
    hfull = nc.dram_tensor("hfull", [S, D], F32).ap()
    scr = nc.dram_tensor("scr", [20, S, 64], BF16).ap()
    with ExitStack() as st:
        P = Prog(nc, st)
        ps = [st.enter_context(nc.psum_tensor(f"ps{i}", [128, 512], F32)) for i in range(8)]
        psb = [p_[:].bitcast(BF16) for p_ in ps]
        for layer in range(2):
            moe = layer % 2 == 1
            final = layer == 1
            hin = xfull if layer == 0 else hfull
            hmap = None if layer == 0 else (lambda i: ((i * 128) % 2048) // 512 * 1024 + ((i * 128) // 2048) * 512 + (i * 128) % 512)
            P.barrier()
            phase_MB(nc, P, ps, psb, f"L{layer}B_", hin, ysrc, scr, hmap=hmap)
            P.barrier()
            phase_MC(nc, P, ps, psb, f"L{layer}C_", hin, ysrc, hmap=hmap)
            P.barrier()
            for q in range(4):
                P.collective(lambda e, q=q: e.collective_compute("AllGather", ALU.bypass, replica_groups=PAIRS, ins=[ysrc[q * 1024:(q + 1) * 1024, :]], outs=[ydst[q * 2048:(q + 1) * 2048, :]]),
                             reads=["ysrc"], writes=["ydst"])
            P.emit()
            P.barrier()
            phase_F(nc, P, ps, psb, f"L{layer}F_", 8 if moe else 1, moe, final, xhalf if layer == 0 else hsrc, ydst, h_out if final else hsrc, final)
            if not final:
                P.barrier()
                for q in range(4):
                    P.collective(lambda e, q=q: e.collective_compute("AllGather", ALU.bypass, replica_groups=PAIRS, ins=[hsrc[q * 512:(q + 1) * 512, :]], outs=[hfull[q * 1024:(q + 1) * 1024, :]]),
                                 reads=["f_out"], writes=["hfull"])
                P.emit()
    return nc


W_OUT_PERM = np.concatenate([np.arange(0, 128), np.arange(256, 384), np.arange(512, 768), np.arange(128, 256), np.arange(384, 512), np.arange(768, 1024)])


def kernel(**inputs):
    inputs = {k: np.asarray(v) for k, v in inputs.items()}
    x = np.ascontiguousarray(inputs["x"], dtype=np.float32)
    hd = np.zeros((4, 1, 1), np.float32)
    maps = [dict() for _ in range(8)]
    for layer in range(2):
        moe = layer % 2 == 1
        final = layer == 1
        for tag, prep in (("B", prep_MB), ("C", prep_MC)):
            pm = prep(layer, hd, inputs)
            for c in range(8):
                for k, v in pm[c].items():
                    if k != "h":
                        maps[c][f"L{layer}{tag}_{k}"] = v
        dummy = np.zeros((8 * TF, 1), np.float32)
        pf = prep_F_inputs(layer, dummy, dummy, inputs, moe, final)
        wperm = np.ascontiguousarray(inputs["w_out"][layer][W_OUT_PERM, :])
        for c in range(8):
            for k, v in pf[c].items():
                if k in ("h", "y"):
                    continue
                maps[c][f"L{layer}F_{k}"] = wperm if k == "w_out" else v
            sel = np.zeros((128, 2), np.float32)
            sel[:, c % 2] = 1.0
            maps[c][f"L{layer}F_selv"] = sel
    for c in range(8):
        b, gi = c // 2, c % 2
        maps[c]["xfull"] = x[b]
        maps[c]["xhalf"] = np.ascontiguousarray(x[b, gi * TF:(gi + 1) * TF])
    nc = build_fused()
    res = run_bass_kernel_spmd(nc, maps, core_ids=list(range(8))).results
    out = np.empty((4, S, D), np.float32)
    for c in range(8):
        b, gi = c // 2, c % 2
        out[b, gi * TF:(gi + 1) * TF] = res[c]["h_out"]
    return out
```

```python
import numpy as np
from contextlib import ExitStack
import concourse.bass as bass
import concourse.mybir as mybir
from concourse.bass_utils import run_bass_kernel_spmd

F32 = mybir.dt.float32
BF16 = mybir.dt.bfloat16
I32 = mybir.dt.int32
AF = mybir.ActivationFunctionType
ALU = mybir.AluOpType
AX = mybir.AxisListType

EPOCH = 20000
NDMA = 24


class Prog:
    ENGS = ("pe", "act", "dve", "pool", "sp")

    def __init__(self, nc, stack):
        self.nc = nc
        self.stack = stack
        self.ops = {e: [] for e in self.ENGS}
        self.count = {e: 0 for e in self.ENGS}
        self.sems = {}
        self.seen = {e: {} for e in self.ENGS}
        self.lastw = {}
        self.readers = {}
        self.ndma = 0
        self.dma_last = {}
        self.final_tokens = []
        self.barrier_toks = []

    def sem(self, key):
        if key not in self.sems:
            self.sems[key] = self.stack.enter_context(self.nc.semaphore("s_" + "_".join(map(str, key))))
        return self.sems[key]

    def barrier(self):
        toks = []
        for e in self.ENGS:
            n = self.count[e]
            if n > 0:
                ep, v = divmod(n - 1, EPOCH)
                toks.append((("c", e, ep), v + 1))
        for si, val in self.dma_last.items():
            toks.append((("d", si), val))
        toks.extend(getattr(self, "cc_toks", []))
        self.barrier_toks = toks

    def _deps(self, eng, reads, writes):
        toks = list(self.barrier_toks)
        for k in reads:
            if k in self.lastw:
                toks.append(self.lastw[k])
        for k in writes:
            if k in self.lastw:
                toks.append(self.lastw[k])
            toks.extend(self.readers.get(k, ()))
        need = {}
        for (sk, val) in toks:
            if self.seen[eng].get(sk, 0) >= val:
                continue
            if need.get(sk, 0) < val:
                need[sk] = val
        for sk, val in need.items():
            self.seen[eng][sk] = val
        return list(need.items())

    def _record(self, tok, reads, writes):
        for k in writes:
            self.lastw[k] = tok
            self.readers[k] = []
        for k in reads:
            if k in writes:
                continue
            self.readers.setdefault(k, []).append(tok)

    def op(self, eng, fn, reads=(), writes=(), same_ok=False):
        waits = self._deps(eng, reads, writes)
        if same_ok:
            waits = [(wk, v) for (wk, v) in waits if not (wk[0] == "c" and wk[1] == eng)]
        n = self.count[eng]
        ep, v = divmod(n, EPOCH)
        sk = ("c", eng, ep)
        self.sem(sk)
        tok = (sk, v + 1)
        self.count[eng] = n + 1
        self.ops[eng].append((fn, waits, sk, 1))
        self._record(tok, reads, writes)
        return tok

    def dma(self, eng, out, in_, reads=(), writes=(), **kw):
        i = self.ndma
        self.ndma += 1
        si = i % NDMA
        sk = ("d", si)
        self.sem(sk)
        prev = self.dma_last.get(si, 0)
        waits = self._deps(eng, reads, writes)
        if prev > 0 and self.seen[eng].get(sk, 0) < prev:
            waits.append((sk, prev))
            self.seen[eng][sk] = prev
        val = prev + 16
        self.dma_last[si] = val
        tok = (sk, val)

        def fn(e, out=out, in_=in_, kw=kw):
            return e.dma_start(out=out, in_=in_, **kw)
        self.ops[eng].append((fn, waits, sk, 16))
        self._record(tok, reads, writes)
        return tok

    def collective(self, fn, reads=(), writes=()):
        k = getattr(self, "ncc", 0)
        self.ncc = k + 1
        sk = ("cc", k)
        self.sem(sk)
        waits = self._deps("pool", reads, writes)
        self.ops["pool"].append((fn, waits, sk, None))
        tok = (sk, 1)
        self._record(tok, reads, writes)
        self.cc_toks = getattr(self, "cc_toks", []) + [tok]
        return tok

    def emit(self, last=False, final_waits_eng="sp"):
        nc = self.nc
        fin = []
        if last:
            for tok in self.final_tokens:
                fin.append(tok)
        with nc.Block() as block:
            def mk(engname):
                def body(e):
                    for (fn, waits, sk, inc) in self.ops[engname]:
                        for (wk, val) in waits:
                            e.wait_ge(self.sems[wk], val)
                        inst = fn(e)
                        if inc is None:
                            inst.then_inc(self.sems[sk])
                        else:
                            inst.then_inc(self.sems[sk], inc)
                    if engname == final_waits_eng:
                        for (wk, val) in fin:
                            e.wait_ge(self.sems[wk], val)
                return body
            block.tensor(mk("pe"))
            block.scalar(mk("act"))
            block.vector(mk("dve"))
            block.gpsimd(mk("pool"))
            block.sync(mk("sp"))
        self.ops = {e: [] for e in self.ENGS}


D = 1024
DFF = 2816
NFC = 22
TF = 2048
HALF = 1024
NT = 8


def phase_F(nc, P, ps, psb, pre, E, moe, final, h_in, ydst, out_ap, is_last):
    di = lambda n, s, dt=F32: nc.dram_tensor(pre + n, s, dt, kind="ExternalInput").ap()
    p_in = di("p", [TF, 256])
    w_out = di("w_out", [D, D])
    g_ffn = di("g_ffn", [128, 8])
    w1 = di("w1", [E, NFC, 128, 8, 128])
    w3 = di("w3", [E, NFC, 128, 8, 128])
    w2 = di("w2", [E, DFF, D])
    g_ple = di("g_ple", [128, 8])
    ple_gate = di("ple_gate", [D, D])
    ple_proj = di("ple_proj", [256, D])
    identf = di("identf", [128, 128])
    selv_in = di("selv", [128, 2])
    if moe:
        rw = di("rw", [128, 8, 8])
        rb = di("rb", [128, 8])
    if final:
        g_fin = di("g_fin", [128, D])

    with ExitStack() as st:
        sb = lambda name, shape, dt: st.enter_context(nc.sbuf_tensor(pre + "s_" + name, shape, dt))
        hacc = sb("hacc", [128, NT, D], F32)
        hnT = sb("hnT", [128, 8, HALF], BF16)
        actT = sb("actT", [128, NFC * HALF], BF16)
        w2b = sb("w2b", [128, NFC * D], BF16)
        stg = sb("stg", [128, 4, 8, 128], F32)
        w13b = sb("w13b", [128, 4, 8, 128], BF16)
        stg2 = sb("stg2", [128, 2, D], F32)
        idf = sb("idf", [128, 128], F32)
        idb = sb("idb", [128, 128], BF16)
        gf = sb("gf", [128, 8], F32)
        gp = sb("gp", [128, 8], F32)
        ss = sb("ss", [128, 4], F32)
        gates = sb("gates", [128, NT, 8], F32)
        sm = sb("sm", [128, 64], F32)
        silu = sb("silu", [128, 2, 512], BF16)
        if moe:
            rws = sb("rws", [128, 8, 8], F32)
            rbs = sb("rbs", [128, 8], F32)
        if final:
            gfin = sb("gfin", [128, D], F32)

        wsm = sb("wsm", [128, 16 * D], BF16)
        woutb = wsm[:, 0:8 * D].rearrange("p (c n) -> p c n", c=8)
        pgb = wsm[:, 8 * D:16 * D].rearrange("p (c n) -> p c n", c=8)
        ppb = actT[:, 0:2 * D].rearrange("p (c n) -> p c n", c=2)
        o = 0
        def carve(nbytes_bf16, dt, pat=None, **kw):
            nonlocal o
            v = w2b[:, o:o + nbytes_bf16]
            o += nbytes_bf16
            if dt == F32:
                v = v.bitcast(F32)
            if pat:
                v = v.rearrange(pat, **kw)
            return v
        xs = carve(2 * D, F32)
        ys = carve(2 * D, F32)
        ys2 = carve(2 * D, F32)
        yb = carve(D, BF16)
        yT = carve(D, BF16, "p (c t) -> p c t", c=8)
        hnb = carve(D, BF16)
        hn32 = carve(2 * D, F32)
        hnT32 = carve(2 * D, F32, "p (c t) -> p c t", c=8)
        pst = carve(2 * 256, F32)
        pbf = carve(256, BF16)
        pT = carve(256, BF16, "p (c t) -> p c t", c=2)
        gsb = carve(2 * D, F32)
        junk = carve(2 * D, F32)
        osb = carve(2 * D, F32)

        P.dma("sp", idf[:], identf, writes=["idf"])
        P.dma("sp", gf[:], g_ffn, writes=["gf"])
        P.dma("sp", gp[:], g_ple, writes=["gp"])
        selv = sb("selv", [128, 2], F32)
        P.dma("sp", selv[:], selv_in, writes=["selv"])
        P.op("dve", lambda e: e.tensor_copy(out=idb[:], in_=idf[:]), reads=["idf"], writes=["idb"])
        if moe:
            P.dma("sp", rws[:], rw, writes=["rws"])
            P.dma("sp", rbs[:], rb, writes=["rbs"])
            P.op("dve", lambda e: e.tensor_tensor(out=rws[:], in0=rws[:], in1=gf[:].unsqueeze(2).to_broadcast([128, 8, 8]), op=ALU.mult),
                 reads=["rws", "gf"], writes=["rws"])
        if final:
            P.dma("sp", gfin[:], g_fin, writes=["gfin"])

        def load_w_bf16(dst, src, nchunks, gscale, tag):
            for c in range(nchunks):
                s = c % 2
                P.dma("sp", stg2[:, s, :], src[c * 128:(c + 1) * 128, :], writes=[("stg2", s)])
                if gscale is not None:
                    P.op("pool", lambda e, c=c, s=s: e.tensor_scalar(out=dst[:, c, :], in0=stg2[:, s, :], scalar1=gscale[:, c:c + 1], scalar2=None, op0=ALU.mult),
                         reads=[("stg2", s), "gp"], writes=[tag])
                else:
                    P.op("pool", lambda e, c=c, s=s: e.tensor_copy(out=dst[:, c, :], in_=stg2[:, s, :]), reads=[("stg2", s)], writes=[tag])

        def rms(src_ap, key, col):
            P.op("act", lambda e: e.activation(out=junk, in_=src_ap, func=AF.Square, accum_out=ss[:, col:col + 1]), reads=[key], writes=["junk", ("ss", col)])
            P.op("dve", lambda e: e.tensor_scalar(out=ss[:, col:col + 1], in0=ss[:, col:col + 1], scalar1=1.0 / D, scalar2=1e-6, op0=ALU.mult, op1=ALU.add),
                 reads=[("ss", col)], writes=[("ss", col)])
            P.op("act", lambda e: e.sqrt(out=ss[:, col:col + 1], in_=ss[:, col:col + 1]), reads=[("ss", col)], writes=[("ss", col)])
            P.op("dve", lambda e: e.reciprocal(out=ss[:, col:col + 1], in_=ss[:, col:col + 1]), reads=[("ss", col)], writes=[("ss", col)])

        def transposes(psbank, src_bf, n, ident, srckey, pskey):
            def f(e):
                for c in range(n):
                    i = e.transpose(out=psbank[:, c * 128:(c + 1) * 128], in_=src_bf[:, c * 128:(c + 1) * 128], identity=ident)
                return i
            P.op("pe", f, reads=[srckey, "idb", "idf"], writes=[pskey])

        for half in range(2):
            tb = half * HALF
            P.barrier()
            if half == 0:
                load_w_bf16(woutb, w_out, 8, None, "woutb")
                load_w_bf16(pgb, ple_gate, 8, gp, "pgb")
            for i in range(NT):
                t0 = tb + i * 128
                P.dma("sp", xs, h_in[t0:t0 + 128, :], writes=["xs"])
                tl = i * 128 + half * HALF
                for k_, yk in ((0, ys), (1, ys2)):
                    for r_ in range(2):
                        tt = k_ * TF + tl
                        row = (tt // 1024) * 2048 + r_ * 1024 + (tt % 1024)
                        P.dma("sp", yk[:, r_ * 512:(r_ + 1) * 512], ydst[row:row + 128, :], writes=["ys" if k_ == 0 else "ys2"])
                P.op("dve", lambda e: e.tensor_scalar(out=ys, in0=ys, scalar1=selv[:, 0:1], scalar2=None, op0=ALU.mult), reads=["ys", "selv"], writes=["ys"])
                P.op("dve", lambda e: e.scalar_tensor_tensor(out=ys, in0=ys2, scalar=selv[:, 1:2], in1=ys, op0=ALU.mult, op1=ALU.add), reads=["ys", "ys2", "selv"], writes=["ys"])
                P.op("pool", lambda e: e.tensor_copy(out=yb, in_=ys), reads=["ys"], writes=["yb"])
                transposes(psb[0], yb, 8, idb[:], "yb", "ps0")
                P.op("act", lambda e: e.activation(out=yT.rearrange("p c t -> p (c t)"), in_=psb[0], func=AF.Copy), reads=["ps0"], writes=["yT"])
                for hf in range(2):
                    def mm(e, hf=hf):
                        for c in range(8):
                            ins = e.matmul(ps[1 + hf][:], lhsT=yT[:, c, :], rhs=woutb[:, c, hf * 512:(hf + 1) * 512], start=(c == 0), stop=(c == 7))
                        return ins
                    P.op("pe", mm, reads=["yT", "woutb"], writes=[f"ps{1 + hf}"])
                    P.op("dve", lambda e, hf=hf, i=i: e.tensor_tensor(out=hacc[:, i, hf * 512:(hf + 1) * 512], in0=ps[1 + hf][:], in1=xs[:, hf * 512:(hf + 1) * 512], op=ALU.add),
                         reads=[f"ps{1 + hf}", "xs"], writes=[("hacc", i)])
                rms(hacc[:, i, :], ("hacc", i), 0)
                P.op("dve", lambda e, i=i: e.tensor_scalar(out=hnb, in0=hacc[:, i, :], scalar1=ss[:, 0:1], scalar2=None, op0=ALU.mult),
                     reads=[("hacc", i), ("ss", 0)], writes=["hnb"])
                transposes(psb[3], hnb, 8, idb[:], "hnb", "ps3")
                P.op("act", lambda e, i=i: e.activation(out=hnT[:, :, i * 128:(i + 1) * 128], in_=psb[3].rearrange("p (c t) -> p c t", c=8), func=AF.Copy),
                     reads=["ps3"], writes=[("hnT", i)])
                if moe:
                    P.op("pool", lambda e, i=i: e.tensor_scalar(out=hn32, in0=hacc[:, i, :], scalar1=ss[:, 0:1], scalar2=None, op0=ALU.mult),
                         reads=[("hacc", i), ("ss", 0)], writes=["hn32"])
                    for q in range(2):
                        def trf(e, q=q):
                            for c in range(4):
                                cc = q * 4 + c
                                ins = e.transpose(out=ps[4 + q][:, c * 128:(c + 1) * 128], in_=hn32[:, cc * 128:(cc + 1) * 128], identity=idf[:])
                            return ins
                        P.op("pe", trf, reads=["hn32", "idf"], writes=[f"ps{4 + q}"])
                        P.op("act", lambda e, q=q: e.activation(out=hnT32[:, q * 4:(q + 1) * 4, :], in_=ps[4 + q][:].rearrange("p (c t) -> p c t", c=4), func=AF.Copy),
                             reads=[f"ps{4 + q}"], writes=[("hnT32", q)])
                    def mml(e):
                        for c in range(8):
                            ins = e.matmul(ps[6][:, 0:8], lhsT=hnT32[:, c, :], rhs=rws[:, c, :], start=(c == 0), stop=(c == 7))
                        return ins
                    P.op("pe", mml, reads=[("hnT32", 0), ("hnT32", 1), "rws"], writes=["ps6"])
                    lg = sm[:, 0:8]
                    mx = sm[:, 8:16]
                    dd = sm[:, 16:17]
                    p1 = sm[:, 17:18]
                    p2 = sm[:, 18:19]
                    t1 = sm[:, 24:32]
                    P.op("dve", lambda e: e.tensor_tensor(out=lg, in0=ps[6][:, 0:8], in1=rbs[:], op=ALU.add), reads=["ps6", "rbs"], writes=["lg"])
                    P.op("dve", lambda e: e.max(out=mx, in_=lg), reads=["lg"], writes=["mx"])
                    P.op("dve", lambda e: e.tensor_tensor(out=dd, in0=mx[:, 1:2], in1=mx[:, 0:1], op=ALU.subtract), reads=["mx"], writes=["dd"])
                    P.op("act", lambda e: e.activation(out=dd, in_=dd, func=AF.Exp), reads=["dd"], writes=["dd"])
                    P.op("dve", lambda e: e.tensor_scalar(out=p1, in0=dd, scalar1=1.0, scalar2=None, op0=ALU.add), reads=["dd"], writes=["p1"])
                    P.op("dve", lambda e: e.reciprocal(out=p1, in_=p1), reads=["p1"], writes=["p1"])
                    P.op("dve", lambda e: e.tensor_tensor(out=p2, in0=dd, in1=p1, op=ALU.mult), reads=["dd", "p1"], writes=["p2"])
                    P.op("dve", lambda e: e.tensor_scalar(out=t1, in0=lg, scalar1=mx[:, 0:1], scalar2=p1, op0=ALU.is_equal, op1=ALU.mult),
                         reads=["lg", "mx", "p1"], writes=["t1"])
                    P.op("dve", lambda e, i=i: e.tensor_scalar(out=gates[:, i, :], in0=lg, scalar1=mx[:, 1:2], scalar2=p2, op0=ALU.is_equal, op1=ALU.mult),
                         reads=["lg", "mx", "p2"], writes=[("gates", i)])
                    P.op("dve", lambda e, i=i: e.tensor_tensor(out=gates[:, i, :], in0=gates[:, i, :], in1=t1, op=ALU.add),
                         reads=[("gates", i), "t1"], writes=[("gates", i)])

            P.barrier()
            for ex in range(E):
                nslot = 0
                for fc in range(NFC):
                    s = fc % 2
                    P.dma("sp", stg[:, s, :, :], w1[ex, fc], writes=[("stg", s)])
                    P.dma("sp", stg[:, 2 + s, :, :], w3[ex, fc], writes=[("stg", 2 + s)])
                    gb = gf[:].unsqueeze(2).to_broadcast([128, 8, 128])
                    P.op("pool", lambda e, s=s: e.tensor_tensor(out=w13b[:, s, :, :], in0=stg[:, s, :, :], in1=gb, op=ALU.mult),
                         reads=[("stg", s), "gf"], writes=[("w13b", s)])
                    P.op("pool", lambda e, s=s: e.tensor_tensor(out=w13b[:, 2 + s, :, :], in0=stg[:, 2 + s, :, :], in1=gb, op=ALU.mult),
                         reads=[("stg", 2 + s), "gf"], writes=[("w13b", 2 + s)])
                    P.dma("sp", stg2[:, s, :], w2[ex, fc * 128:(fc + 1) * 128, :], writes=[("stg2", s)])
                    P.op("act", lambda e, s=s, fc=fc: e.activation(out=w2b[:, fc * D:(fc + 1) * D], in_=stg2[:, s, :], func=AF.Copy),
                         reads=[("stg2", s)], writes=[("w2b", fc)])
                    for g in range(2):
                        def mm13(e, g=g, s=s):
                            for wi in range(2):
                                for c in range(8):
                                    ins = e.matmul(ps[2 * g + wi][:], lhsT=w13b[:, 2 * wi + s, c, :], rhs=hnT[:, c, g * 512:(g + 1) * 512], start=(c == 0), stop=(c == 7))
                            return ins
                        P.op("pe", mm13, reads=[("w13b", s), ("w13b", 2 + s)] + [("hnT", i) for i in range(NT)], writes=[f"ps{2 * g}", f"ps{2 * g + 1}"])
                        P.op("act", lambda e, g=g: e.activation(out=silu[:, g, :], in_=ps[2 * g][:], func=AF.Silu), reads=[f"ps{2 * g}"], writes=[("silu", g)])
                        P.op("dve", lambda e, g=g, fc=fc: e.tensor_tensor(out=actT[:, fc * HALF + g * 512: fc * HALF + (g + 1) * 512], in0=silu[:, g, :], in1=ps[2 * g + 1][:], op=ALU.mult),
                             reads=[("silu", g), f"ps{2 * g + 1}"], writes=[("actT", fc)])
                for i in range(NT):
                    for hf in range(2):
                        b = 4 + (nslot % 4)
                        nslot += 1
                        def mm2(e, i=i, hf=hf, b=b):
                            for fc in range(NFC):
                                ins = e.matmul(ps[b][:], lhsT=actT[:, fc * HALF + i * 128: fc * HALF + (i + 1) * 128], rhs=w2b[:, fc * D + hf * 512: fc * D + (hf + 1) * 512],
                                               start=(fc == 0), stop=(fc == NFC - 1))
                            return ins
                        P.op("pe", mm2, reads=[("actT", fc) for fc in range(NFC)] + [("w2b", fc) for fc in range(NFC)], writes=[f"ps{b}"])
                        if moe:
                            P.op("dve", lambda e, i=i, hf=hf, b=b, ex=ex: e.scalar_tensor_tensor(out=hacc[:, i, hf * 512:(hf + 1) * 512], in0=ps[b][:], scalar=gates[:, i, ex:ex + 1],
                                                                                               in1=hacc[:, i, hf * 512:(hf + 1) * 512], op0=ALU.mult, op1=ALU.add),
                                 reads=[f"ps{b}", ("gates", i), ("hacc", i)], writes=[("hacc", i)])
                        else:
                            P.op("dve", lambda e, i=i, hf=hf, b=b: e.tensor_tensor(out=hacc[:, i, hf * 512:(hf + 1) * 512], in0=ps[b][:], in1=hacc[:, i, hf * 512:(hf + 1) * 512], op=ALU.add),
                                 reads=[f"ps{b}", ("hacc", i)], writes=[("hacc", i)])

            P.barrier()
            load_w_bf16(ppb, ple_proj, 2, None, "ppb")
            for i in range(NT):
                t0 = tb + i * 128
                P.dma("sp", pst, p_in[t0:t0 + 128, :], writes=["pst"])
                P.op("pool", lambda e: e.tensor_copy(out=pbf, in_=pst), reads=["pst"], writes=["pbf"])
                transposes(psb[0], pbf, 2, idb[:], "pbf", "ps0")
                P.op("act", lambda e: e.activation(out=pT.rearrange("p c t -> p (c t)"), in_=psb[0][:, 0:256], func=AF.Copy), reads=["ps0"], writes=["pT"])
                rms(hacc[:, i, :], ("hacc", i), 1)
                P.op("dve", lambda e, i=i: e.tensor_scalar(out=hnb, in0=hacc[:, i, :], scalar1=ss[:, 1:2], scalar2=None, op0=ALU.mult),
                     reads=[("hacc", i), ("ss", 1)], writes=["hnb"])
                transposes(psb[3], hnb, 8, idb[:], "hnb", "ps3")
                P.op("act", lambda e: e.activation(out=yT.rearrange("p c t -> p (c t)"), in_=psb[3], func=AF.Copy), reads=["ps3"], writes=["yT"])
                for hf in range(2):
                    def mmg(e, hf=hf):
                        for c in range(8):
                            ins = e.matmul(ps[1 + hf][:], lhsT=yT[:, c, :], rhs=pgb[:, c, hf * 512:(hf + 1) * 512], start=(c == 0), stop=(c == 7))
                        return ins
                    P.op("pe", mmg, reads=["yT", "pgb"], writes=[f"ps{1 + hf}"])
                    P.op("act", lambda e, hf=hf: e.activation(out=gsb[:, hf * 512:(hf + 1) * 512], in_=ps[1 + hf][:], func=AF.Sigmoid), reads=[f"ps{1 + hf}"], writes=[("gsb", hf)])
                    def mmp(e, hf=hf):
                        for c in range(2):
                            ins = e.matmul(ps[4 + hf][:], lhsT=pT[:, c, :], rhs=ppb[:, c, hf * 512:(hf + 1) * 512], start=(c == 0), stop=(c == 1))
                        return ins
                    P.op("pe", mmp, reads=["pT", "ppb"], writes=[f"ps{4 + hf}"])
                    P.op("dve", lambda e, hf=hf: e.tensor_tensor(out=gsb[:, hf * 512:(hf + 1) * 512], in0=gsb[:, hf * 512:(hf + 1) * 512], in1=ps[4 + hf][:], op=ALU.mult),
                         reads=[("gsb", hf), f"ps{4 + hf}"], writes=[("gsb", hf)])
                P.op("dve", lambda e, i=i: e.tensor_tensor(out=osb, in0=gsb, in1=hacc[:, i, :], op=ALU.add), reads=[("gsb", 0), ("gsb", 1), ("hacc", i)], writes=["osb"])
                if final:
                    rms(osb, "osb", 2)
                    P.op("dve", lambda e: e.scalar_tensor_tensor(out=osb, in0=osb, scalar=ss[:, 2:3], in1=gfin[:], op0=ALU.mult, op1=ALU.mult),
                         reads=["osb", ("ss", 2), "gfin"], writes=["osb"])
                tk = P.dma("sp", out_ap[t0:t0 + 128, :], osb, reads=["osb"], writes=["f_out"])
                if is_last:
                    P.final_tokens.append(tk)
        P.emit(last=is_last)


def prep_F_inputs(layer, hs, ys, inputs, moe, final):
    i = layer
    j = i // 2
    def tochunks(w):
        E = w.shape[0]
        return np.ascontiguousarray(w.reshape(E, 8, 128, NFC, 128).transpose(0, 3, 2, 1, 4))
    if moe:
        w1, w3, w2 = inputs["moe_w1"][j], inputs["moe_w3"][j], inputs["moe_w2"][j]
    else:
        w1, w3, w2 = inputs["ffn_w1"][j][None], inputs["ffn_w3"][j][None], inputs["ffn_w2"][j][None]
    pc = lambda g: np.ascontiguousarray(g.reshape(8, 128).T)
    common = {
        "w_out": inputs["w_out"][i], "g_ffn": pc(inputs["g_ffn"][i]), "w1": tochunks(w1), "w3": tochunks(w3), "w2": np.ascontiguousarray(w2),
        "g_ple": pc(inputs["g_ple"][i]), "ple_gate": inputs["ple_gate_w"][i], "ple_proj": inputs["ple_proj_w"][i],
        "identf": np.eye(128, dtype=np.float32),
    }
    if moe:
        common["rw"] = np.ascontiguousarray(inputs["router_w"][j].reshape(8, 128, 8).transpose(1, 0, 2))
        common["rb"] = np.ascontiguousarray(np.broadcast_to(inputs["router_b"][j][None, :], (128, 8)))
    if final:
        common["g_fin"] = np.ascontiguousarray(np.broadcast_to(inputs["g_final"][None, :], (128, D)))
    pl = inputs["p"][i].reshape(-1, 256)
    maps = []
    for c in range(8):
        m = dict(common)
        m["h"] = np.ascontiguousarray(hs[c * TF:(c + 1) * TF])
        m["y"] = np.ascontiguousarray(ys[c * TF:(c + 1) * TF])
        m["p"] = np.ascontiguousarray(pl[c * TF:(c + 1) * TF])
        maps.append(m)
    return maps


S = 4096
NTILE = 32


class Proj:
    def __init__(self, nc, P, st, h_in, identf, ncol, w_dram, g_dram, shift_cols=None, mu_dram=None, pre="", hmap=None):
        self.nc, self.P = nc, P
        self.hmap = hmap if hmap is not None else (lambda i: i * 128)
        sb = lambda name, shape, dt: st.enter_context(nc.sbuf_tensor(pre + "s_" + name, shape, dt))
        self.h_in = h_in
        self.xs = sb("pj_xs", [128, 2, D], F32)
        self.junk = sb("pj_junk", [128, D], F32)
        self.ss = sb("pj_ss", [128, 2], F32)
        self.hnb = sb("pj_hnb", [128, D], BF16)
        self.hnT = sb("pj_hnT", [128, 3, 8, 129], BF16)
        self.idf = sb("pj_idf", [128, 128], F32)
        self.idb = sb("pj_idb", [128, 128], BF16)
        self.g = sb("pj_g", [128, 8], F32)
        self.wb = sb("pj_wb", [128, 8, ncol], BF16)
        self.ncol = ncol
        stg = sb("pj_stg", [128, 2, ncol], F32)
        P.dma("sp", self.idf[:], identf, writes=["idf"])
        P.dma("sp", self.g[:], g_dram, writes=["pj_g"])
        P.op("dve", lambda e: e.tensor_copy(out=self.idb[:], in_=self.idf[:]), reads=["idf"], writes=["idb"])
        P.op("pool", lambda e: e.memset(self.hnT[:], 0.0), writes=[("hnT", 0), ("hnT", 1), ("hnT", 2)])
        for c in range(8):
            s = c % 2
            P.dma("sp", stg[:, s, :], w_dram[c * 128:(c + 1) * 128, :], writes=[("pj_stg", s)])
            P.op("pool", lambda e, c=c, s=s: e.tensor_scalar(out=self.wb[:, c, :], in0=stg[:, s, :], scalar1=self.g[:, c:c + 1], scalar2=None, op0=ALU.mult),
                 reads=[("pj_stg", s), "pj_g"], writes=["pj_wb"])
        if shift_cols is not None:
            a, b = shift_cols
            n = b - a
            self.wprev = sb("pj_wprev", [128, 8, n], BF16)
            mu = sb("pj_mu", [128, n], F32)
            P.dma("sp", mu[:], mu_dram, writes=["pj_mu"])
            mub = mu[:].unsqueeze(1).to_broadcast([128, 8, n])
            P.op("dve", lambda e: e.tensor_tensor(out=self.wprev[:], in0=self.wb[:, :, a:b], in1=mub, op=ALU.mult), reads=["pj_wb", "pj_mu"], writes=["pj_wprev"])
            P.op("dve", lambda e: e.tensor_tensor(out=self.wb[:, :, a:b], in0=self.wb[:, :, a:b], in1=self.wprev[:], op=ALU.subtract), reads=["pj_wb", "pj_wprev"], writes=["pj_wb"])
        self.shift_cols = shift_cols

    def tile(self, i, psbank_bf, pskey):
        P = self.P
        s = i % 2
        xs = self.xs[:, s, :]
        r0 = self.hmap(i)
        P.dma("sp", xs, self.h_in[r0:r0 + 128, :], writes=[("pj_xs", s)])
        ssc = self.ss[:, s:s + 1]
        k = ("pj_ss", s)
        P.op("act", lambda e: e.activation(out=self.junk[:], in_=xs, func=AF.Square, accum_out=ssc), reads=[("pj_xs", s)], writes=["pj_junk", k])
        P.op("dve", lambda e: e.tensor_scalar(out=ssc, in0=ssc, scalar1=1.0 / D, scalar2=1e-6, op0=ALU.mult, op1=ALU.add), reads=[k], writes=[k])
        P.op("act", lambda e: e.sqrt(out=ssc, in_=ssc), reads=[k], writes=[k])
        P.op("dve", lambda e: e.reciprocal(out=ssc, in_=ssc), reads=[k], writes=[k])
        P.op("dve", lambda e: e.tensor_scalar(out=self.hnb[:], in0=xs, scalar1=ssc, scalar2=None, op0=ALU.mult), reads=[("pj_xs", s), k], writes=["pj_hnb"])

        def tr(e):
            for c in range(8):
                ins = e.transpose(out=psbank_bf[:, c * 128:(c + 1) * 128], in_=self.hnb[:, c * 128:(c + 1) * 128], identity=self.idb[:])
            return ins
        P.op("pe", tr, reads=["pj_hnb", "idb"], writes=[pskey])
        sh, sn = i % 3, (i + 1) % 3
        P.op("act", lambda e: e.activation(out=self.hnT[:, sh, :, 1:129], in_=psbank_bf.rearrange("p (c t) -> p c t", c=8), func=AF.Copy), reads=[pskey], writes=[("hnT", sh)])
        P.op("pool", lambda e: e.tensor_copy(out=self.hnT[:, sn, :, 0:1], in_=self.hnT[:, sh, :, 128:129]), reads=[("hnT", sh)], writes=[("hnT", sn)])

    def mm_tok(self, e, i, ps_ap, c0, c1, start=True, stop=True):
        s = i % 3
        sh = self.shift_cols is not None and c0 >= self.shift_cols[0] and c1 <= self.shift_cols[1]
        n = 16 if sh else 8
        k = 0
        for c in range(8):
            ins = e.matmul(ps_ap, lhsT=self.hnT[:, s, c, 1:129], rhs=self.wb[:, c, c0:c1], start=(start and k == 0), stop=(stop and k == n - 1))
            k += 1
        if sh:
            a = self.shift_cols[0]
            for c in range(8):
                ins = e.matmul(ps_ap, lhsT=self.hnT[:, s, c, 0:128], rhs=self.wprev[:, c, c0 - a:c1 - a], start=False, stop=(stop and k == n - 1))
                k += 1
        return ins

    def mm_feat(self, e, i, ps_ap, c0, c1):
        s = i % 3
        sh = self.shift_cols is not None and c0 >= self.shift_cols[0] and c1 <= self.shift_cols[1]
        n = 16 if sh else 8
        k = 0
        for c in range(8):
            ins = e.matmul(ps_ap, lhsT=self.wb[:, c, c0:c1], rhs=self.hnT[:, s, c, 1:129], start=(k == 0), stop=(k == n - 1))
            k += 1
        if sh:
            a = self.shift_cols[0]
            for c in range(8):
                ins = e.matmul(ps_ap, lhsT=self.wprev[:, c, c0 - a:c1 - a], rhs=self.hnT[:, s, c, 0:128], start=False, stop=(k == n - 1))
                k += 1
        return ins

    def keys(self, i):
        return [("hnT", i % 3), "pj_wb", "pj_wprev"]


def pc8(g):
    return np.ascontiguousarray(np.asarray(g).reshape(8, 128).T)


def bc128(v):
    v = np.asarray(v, dtype=np.float32).reshape(1, -1)
    return np.ascontiguousarray(np.broadcast_to(v, (128, v.shape[1])))


class MAops:
    def __init__(self, nc, P, st, pre, pj, c0, psA, keyA, psB, keyB, ysrc):
        self.P, self.pj, self.c0, self.psA, self.keyA, self.psB, self.keyB, self.ysrc = P, pj, c0, psA, keyA, psB, keyB, ysrc
        di = lambda n, s, dt=F32: nc.dram_tensor(pre + n, s, dt, kind="ExternalInput").ap()
        sb = lambda name, shape, dt: st.enter_context(nc.sbuf_tensor(pre + "s_" + name, shape, dt))
        lng = di("lng", [128, 128]); lnb = di("lnb", [128, 128]); ws = di("ws", [2, 128, 128]); tril = di("tril", [128, 128]); bs = di("bs", [128, 2])
        self.lngs = sb("lngs", [128, 128], F32); self.lnbs = sb("lnbs", [128, 128], F32)
        wss = sb("wss", [128, 2, 128], F32); trl = sb("trl", [128, 128], F32)
        self.wT = sb("wT", [128, 2, 128], BF16); self.bss = sb("bss", [128, 2], F32)
        self.uv = sb("uv", [128, 2, 256], F32); self.st1 = sb("st1", [128, 2, 8], F32)
        self.vc = sb("vc", [128, 2, 2, 64], F32); self.sq = sb("sq", [128, 2, 64], F32)
        self.vn = sb("vn", [128, 2, 2, 64], BF16); self.yo = sb("yo", [128, 2, 128], F32)
        P.dma("sp", self.lngs[:], lng, writes=["a_lngs"]); P.dma("sp", self.lnbs[:], lnb, writes=["a_lnbs"])
        P.dma("sp", trl[:], tril, writes=["a_trl"]); P.dma("sp", self.bss[:], bs, writes=["a_bss"])
        for hh in range(2):
            P.dma("sp", wss[:, hh, :], ws[hh], writes=[("a_wss", hh)])
            P.op("dve", lambda e, hh=hh: e.tensor_tensor(out=wss[:, hh, :], in0=wss[:, hh, :], in1=trl[:], op=ALU.mult), reads=[("a_wss", hh), "a_trl"], writes=[("a_wss", hh)])
            P.op("pe", lambda e, hh=hh: e.transpose(out=psB[:, hh * 128:(hh + 1) * 128], in_=wss[:, hh, :], identity=pj.idf[:]), reads=[("a_wss", hh), "idf"], writes=[keyB])
            P.op("act", lambda e, hh=hh: e.activation(out=self.wT[:, hh, :], in_=psB[:, hh * 128:(hh + 1) * 128], func=AF.Copy), reads=[keyB], writes=["a_wT"])

    def tile(self, i):
        P, pj, c0, psA, keyA, psB, keyB = self.P, self.pj, self.c0, self.psA, self.keyA, self.psB, self.keyB
        so = i % 2
        uv = self.uv[:, so, :]; st1 = self.st1[:, so, :]; vc = self.vc[:, so]; sq = self.sq; vn = self.vn[:, so]; yo = self.yo
        K = lambda n: ("a_" + n, so)
        P.op("pe", lambda e: pj.mm_tok(e, i, psA[:, 0:256], c0, c0 + 256), reads=pj.keys(i), writes=[keyA])
        P.op("act", lambda e: e.activation(out=uv, in_=psA[:, 0:256], func=AF.Gelu_apprx_tanh), reads=[keyA], writes=[K("uv")])
        v3 = uv[:, 128:256].rearrange("p (h d) -> p h d", h=2)
        g3 = lambda ap: ap.rearrange("p (h d) -> p h d", h=2)
        P.op("dve", lambda e: e.tensor_reduce(out=st1[:, 0:2], in_=v3, axis=AX.X, op=ALU.add), reads=[K("uv")], writes=[K("st_m")])
        P.op("dve", lambda e: e.tensor_scalar(out=st1[:, 0:2], in0=st1[:, 0:2], scalar1=1.0 / 64, scalar2=None, op0=ALU.mult), reads=[K("st_m")], writes=[K("st_m")])
        P.op("pool", lambda e: e.tensor_tensor(out=vc, in0=v3, in1=st1[:, 0:2].unsqueeze(2).to_broadcast([128, 2, 64]), op=ALU.subtract), reads=[K("uv"), K("st_m")], writes=[K("vc")])
        P.op("pool", lambda e: e.tensor_tensor(out=sq[:], in0=vc, in1=vc, op=ALU.mult), reads=[K("vc")], writes=["a_sq"])
        P.op("dve", lambda e: e.tensor_reduce(out=st1[:, 2:4], in_=sq[:], axis=AX.X, op=ALU.add), reads=["a_sq"], writes=[K("st_v")])
        P.op("dve", lambda e: e.tensor_scalar(out=st1[:, 2:4], in0=st1[:, 2:4], scalar1=1.0 / 64, scalar2=1e-5, op0=ALU.mult, op1=ALU.add), reads=[K("st_v")], writes=[K("st_v")])
        P.op("act", lambda e: e.sqrt(out=st1[:, 2:4], in_=st1[:, 2:4]), reads=[K("st_v")], writes=[K("st_v")])
        P.op("dve", lambda e: e.reciprocal(out=st1[:, 2:4], in_=st1[:, 2:4]), reads=[K("st_v")], writes=[K("st_v")])
        P.op("pool", lambda e: e.tensor_tensor(out=vc, in0=vc, in1=st1[:, 2:4].unsqueeze(2).to_broadcast([128, 2, 64]), op=ALU.mult), reads=[K("vc"), K("st_v")], writes=[K("vc")])
        P.op("pool", lambda e: e.tensor_tensor(out=vc, in0=vc, in1=g3(self.lngs[:]), op=ALU.mult), reads=[K("vc"), "a_lngs"], writes=[K("vc")])
        P.op("pool", lambda e: e.tensor_tensor(out=vn, in0=vc, in1=g3(self.lnbs[:]), op=ALU.add), reads=[K("vc"), "a_lnbs"], writes=[K("vn")])

        def mix(e):
            for hh in range(2):
                ins = e.matmul(psB[:, hh * 64:(hh + 1) * 64], lhsT=self.wT[:, hh, :], rhs=vn[:, hh, :], start=True, stop=True)
            return ins
        P.op("pe", mix, reads=["a_wT", K("vn")], writes=[keyB])
        for hh in range(2):
            P.op("dve", lambda e, hh=hh: e.scalar_tensor_tensor(out=yo[:, so, hh * 64:(hh + 1) * 64], in0=psB[:, hh * 64:(hh + 1) * 64], scalar=self.bss[:, hh:hh + 1],
                                                            in1=uv[:, hh * 64:(hh + 1) * 64], op0=ALU.add, op1=ALU.mult),
                 reads=[keyB, "a_bss", K("uv")], writes=[K("yo")])
        P.dma("sp", self.ysrc[i * 128:(i + 1) * 128, 0:128], yo[:, so, :], reads=[K("yo")], writes=["ysrc"])


def phase_MA(nc, P, ps, psb, pre, h_in, ysrc, hmap=None, is_last=False):
    di = lambda n, s, dt=F32: nc.dram_tensor(pre + n, s, dt, kind="ExternalInput").ap()
    identf = di("identf", [128, 128])
    wc = di("wc", [D, 256])
    g_mix = di("g_mix", [128, 8])
    lng = di("lng", [128, 128])
    lnb = di("lnb", [128, 128])
    ws = di("ws", [2, 128, 128])
    tril = di("tril", [128, 128])
    bs = di("bs", [128, 2])
    with ExitStack() as st:
        sb = lambda name, shape, dt: st.enter_context(nc.sbuf_tensor(pre + "s_" + name, shape, dt))
        pj = Proj(nc, P, st, h_in, identf, 256, wc, g_mix, pre=pre, hmap=hmap)
        lngs = sb("lngs", [128, 128], F32)
        lnbs = sb("lnbs", [128, 128], F32)
        wss = sb("wss", [128, 2, 128], F32)
        trl = sb("trl", [128, 128], F32)
        wT = sb("wT", [128, 2, 128], BF16)
        bss = sb("bss", [128, 2], F32)
        uv = sb("uv", [128, 256], F32)
        st1 = sb("st1", [128, 8], F32)
        vc = sb("vc", [128, 2, 64], F32)
        sq = sb("sq", [128, 2, 64], F32)
        vn = sb("vn", [128, 2, 64], BF16)
        yo = sb("yo", [128, 2, 128], F32)
        P.dma("sp", lngs[:], lng, writes=["lngs"])
        P.dma("sp", lnbs[:], lnb, writes=["lnbs"])
        P.dma("sp", trl[:], tril, writes=["trl"])
        P.dma("sp", bss[:], bs, writes=["bss"])
        for hh in range(2):
            P.dma("sp", wss[:, hh, :], ws[hh], writes=[("wss", hh)])
            P.op("dve", lambda e, hh=hh: e.tensor_tensor(out=wss[:, hh, :], in0=wss[:, hh, :], in1=trl[:], op=ALU.mult), reads=[("wss", hh), "trl"], writes=[("wss", hh)])
            P.op("pe", lambda e, hh=hh: e.transpose(out=ps[7][:, hh * 128:(hh + 1) * 128], in_=wss[:, hh, :], identity=pj.idf[:]), reads=[("wss", hh), "idf"], writes=["ps7"])
            P.op("act", lambda e, hh=hh: e.activation(out=wT[:, hh, :], in_=ps[7][:, hh * 128:(hh + 1) * 128], func=AF.Copy), reads=["ps7"], writes=["wT"])
        for i in range(NTILE):
            pj.tile(i, psb[0], "ps0")
            P.op("pe", lambda e, i=i: pj.mm_tok(e, i, ps[1][:, 0:256], 0, 256), reads=pj.keys(i), writes=["ps1"])
            P.op("act", lambda e: e.activation(out=uv[:], in_=ps[1][:, 0:256], func=AF.Gelu_apprx_tanh), reads=["ps1"], writes=["uv"])
            v3 = uv[:, 128:256].rearrange("p (h d) -> p h d", h=2)
            P.op("dve", lambda e: e.tensor_reduce(out=st1[:, 0:2], in_=v3, axis=AX.X, op=ALU.add), reads=["uv"], writes=["st_m"])
            P.op("dve", lambda e: e.tensor_scalar(out=st1[:, 0:2], in0=st1[:, 0:2], scalar1=1.0 / 64, scalar2=None, op0=ALU.mult), reads=["st_m"], writes=["st_m"])
            P.op("dve", lambda e: e.tensor_tensor(out=vc[:], in0=v3, in1=st1[:, 0:2].unsqueeze(2).to_broadcast([128, 2, 64]), op=ALU.subtract), reads=["uv", "st_m"], writes=["vc"])
            P.op("dve", lambda e: e.tensor_tensor(out=sq[:], in0=vc[:], in1=vc[:], op=ALU.mult), reads=["vc"], writes=["sq"])
            P.op("dve", lambda e: e.tensor_reduce(out=st1[:, 2:4], in_=sq[:], axis=AX.X, op=ALU.add), reads=["sq"], writes=["st_v"])
            P.op("dve", lambda e: e.tensor_scalar(out=st1[:, 2:4], in0=st1[:, 2:4], scalar1=1.0 / 64, scalar2=1e-5, op0=ALU.mult, op1=ALU.add), reads=["st_v"], writes=["st_v"])
            P.op("act", lambda e: e.sqrt(out=st1[:, 2:4], in_=st1[:, 2:4]), reads=["st_v"], writes=["st_v"])
            P.op("dve", lambda e: e.reciprocal(out=st1[:, 2:4], in_=st1[:, 2:4]), reads=["st_v"], writes=["st_v"])
            P.op("dve", lambda e: e.tensor_tensor(out=vc[:], in0=vc[:], in1=st1[:, 2:4].unsqueeze(2).to_broadcast([128, 2, 64]), op=ALU.mult), reads=["vc", "st_v"], writes=["vc"])
            P.op("dve", lambda e: e.tensor_tensor(out=vc[:], in0=vc[:], in1=lngs[:].rearrange("p (h d) -> p h d", h=2), op=ALU.mult), reads=["vc", "lngs"], writes=["vc"])
            P.op("dve", lambda e: e.tensor_tensor(out=vn[:], in0=vc[:], in1=lnbs[:].rearrange("p (h d) -> p h d", h=2), op=ALU.add), reads=["vc", "lnbs"], writes=["vn"])
            def mix(e):
                for hh in range(2):
                    ins = e.matmul(ps[2][:, hh * 64:(hh + 1) * 64], lhsT=wT[:, hh, :], rhs=vn[:, hh, :], start=True, stop=True)
                return ins
            P.op("pe", mix, reads=["wT", "vn"], writes=["ps2"])
            so = i % 2
            for hh in range(2):
                P.op("dve", lambda e, hh=hh, so=so: e.scalar_tensor_tensor(out=yo[:, so, hh * 64:(hh + 1) * 64], in0=ps[2][:, hh * 64:(hh + 1) * 64], scalar=bss[:, hh:hh + 1],
                                                                       in1=uv[:, hh * 64:(hh + 1) * 64], op0=ALU.add, op1=ALU.mult),
                     reads=["ps2", "bss", "uv"], writes=[("yo", so)])
            P.dma("sp", ysrc[i * 128:(i + 1) * 128, 0:128], yo[:, so, :], reads=[("yo", so)], writes=["ysrc"])
        P.emit(last=is_last)


def prep_MA(layer, h, inputs):
    i = layer
    maps = []
    for c in range(8):
        b, gi = c // 2, c % 2
        w = inputs["w_in"][i]
        wc = np.concatenate([w[:, gi * 128:(gi + 1) * 128], w[:, 256 + gi * 128:256 + (gi + 1) * 128]], axis=1)
        maps.append({
            "h": np.ascontiguousarray(h[b]), "identf": np.eye(128, dtype=np.float32), "wc": np.ascontiguousarray(wc),
            "g_mix": pc8(inputs["g_mix"][i]),
            "lng": bc128(inputs["gm_ln_g"][i][2 * gi:2 * gi + 2].reshape(-1)), "lnb": bc128(inputs["gm_ln_b"][i][2 * gi:2 * gi + 2].reshape(-1)),
            "ws": np.ascontiguousarray(inputs["gm_ws"][i][2 * gi:2 * gi + 2]), "tril": np.tril(np.ones((128, 128), np.float32)),
            "bs": np.ascontiguousarray(inputs["gm_bs"][i][2 * gi:2 * gi + 2].T),
        })
    return maps


def phase_MB(nc, P, ps, psb, pre, h_in, ysrc, scr, hmap=None, ntile=NTILE, upto=9, is_last=False):
    di = lambda n, s, dt=F32: nc.dram_tensor(pre + n, s, dt, kind="ExternalInput").ap()
    identf = di("identf", [128, 128])
    wc = di("wc", [D, 896])
    g_mix = di("g_mix", [128, 8])
    mu = di("mu", [128, 640])
    wa_up = di("wa_up", [128, 128])
    g_up = di("g_up", [128, 128])
    w0a0 = di("w0a0", [1, 256])
    cvec = di("cvec", [128, 5, 128])
    sel_in = di("sel", [20, 5, 128])
    mcum_in = di("mcum", [128, 128])
    NST = 32
    with ExitStack() as st:
        sb = lambda name, shape, dt: st.enter_context(nc.sbuf_tensor(pre + "s_" + name, shape, dt))
        pj = Proj(nc, P, st, h_in, identf, 896, wc, g_mix, shift_cols=(0, 640), mu_dram=mu, pre=pre, hmap=hmap)
        ma = MAops(nc, P, st, pre + "a_", pj, 640, ps[6], "ps6", ps[7], "ps7", ysrc)
        waf = sb("waf", [128, 128], F32); wab = sb("wab", [128, 128], BF16)
        guf = sb("guf", [128, 128], F32); gub = sb("gub", [128, 128], BF16)
        w0f = sb("w0f", [1, 256], F32); w0b = sb("w0b", [1, 256], BF16)
        ones = sb("ones", [1, 128], BF16)
        cv = sb("cv", [128, 5, 128], F32)
        self_ = sb("self", [20, 5, 128], F32)
        sel = sb("selt", [20, 5, 128], BF16)
        ldT = sb("ldT", [128, 128], BF16)
        gdT = sb("gdT", [128, 128], BF16)
        sg = sb("sg", [128, 128], F32)
        aa = sb("aa", [128, 128], F32)
        rkv = sb("rkv", [128, 384], F32)
        kk0 = sb("kk0", [128, 2, 64], F32)
        sq = sb("sq", [128, 2, 64], F32)
        st1 = sb("st1", [128, 8], F32)
        tmp = sb("tmp", [128, 128], F32)
        strm = sb("strm", [128, 2, 5, 128], F32)
        g_all = sb("g_all", [128, ntile, 128], F32)
        bon_all = sb("bon_all", [128, ntile, 128], F32)
        vT_all = sb("vT_all", [128, ntile * 128], F32)
        yT_all = sb("yT_all", [128, ntile * 128], F32)
        rows = sb("rows", [20, 2, NST * 64], BF16)
        shl = sb("shl", [128, 2, 2, 5, 128], BF16)
        sdf = sb("sdf", [128, 5, 128], F32)
        Sst = sb("Sst", [128, 64], F32)
        T1 = sb("T1", [128, 64], F32)
        junk = sb("junk", [128, 64], F32)
        sa = sb("sa", [128, 1], F32)
        fill = sb("fill", [128, 2], F32)
        junk2 = sb("junk2", [128, 64], F32)
        prev_step = None
        mcum = sb("mcum", [128, 128], F32)
        pinc = sb("pinc", [128, 128], F32); pinv = sb("pinv", [128, 128], F32); pexc = sb("pexc", [128, 128], F32); csx = sb("csx", [128, 128], F32)
        Sb2 = sb("Sb2", [128, 2, 64], F32); Ubuf = sb("Ubuf", [128, 64], F32)
        P.dma("sp", mcum[:], mcum_in, writes=["mcum"])
        T1p = sb("T1p", [128, 64], F32)
        wr_sb = sb("wr_sb", [128, 2, 512], F32)
        yo = sb("yo", [128, 2, 128], F32)
        yc = sb("yc", [128, 2, 64], F32)

        P.dma("sp", waf[:], wa_up, writes=["waf"]); P.op("dve", lambda e: e.tensor_copy(out=wab[:], in_=waf[:]), reads=["waf"], writes=["wab"])
        P.dma("sp", guf[:], g_up, writes=["guf"]); P.op("dve", lambda e: e.tensor_copy(out=gub[:], in_=guf[:]), reads=["guf"], writes=["gub"])
        P.dma("sp", w0f[:], w0a0, writes=["w0f"]); P.op("dve", lambda e: e.tensor_copy(out=w0b[:], in_=w0f[:]), reads=["w0f"], writes=["w0b"])
        P.op("dve", lambda e: e.memset(ones[:], 1.0), writes=["ones"])
        P.dma("sp", cv[:], cvec, writes=["cv"])
        P.dma("sp", self_[:], sel_in, writes=["self"])
        P.op("dve", lambda e: e.tensor_copy(out=sel[:], in_=self_[:]), reads=["self"], writes=["sel"])
        KK, KA, RK, GNG, GNB = range(5)
        h3 = lambda ap: ap.rearrange("p (h d) -> p h d", h=2)

        pj.tile(0, psb[0], "ps0")
        for i in range(ntile):
            so = i % 2
            if i + 1 < ntile:
                pj.tile(i + 1, psb[0], "ps0")
            P.op("pe", lambda e, i=i: pj.mm_tok(e, i, ps[1][:, 0:384], 0, 384), reads=pj.keys(i), writes=["ps1"])
            P.op("pe", lambda e, i=i: pj.mm_feat(e, i, ps[2][:, 0:128], 384, 512), reads=pj.keys(i), writes=["ps2"])
            P.op("pe", lambda e, i=i: pj.mm_feat(e, i, ps[3][:, 0:128], 512, 640), reads=pj.keys(i), writes=["ps3"])
            P.op("act", lambda e: e.activation(out=ldT[0:64, :], in_=ps[2][0:64, 0:128], func=AF.Tanh), reads=["ps2"], writes=["ldT0"])
            P.op("act", lambda e: e.activation(out=ldT[64:128, :], in_=ps[2][64:128, 0:128], func=AF.Copy), reads=["ps2"], writes=["ldT1"])
            P.op("act", lambda e: e.activation(out=gdT[:], in_=ps[3][:, 0:128], func=AF.Sigmoid), reads=["ps3"], writes=["gdT"])
            P.op("act", lambda e: e.activation(out=rkv[:], in_=ps[1][:, 0:384], func=AF.Copy), reads=["ps1"], writes=["rkv"])
            def ups(e):
                e.matmul(ps[4][:, 0:128], lhsT=ldT[0:64, :], rhs=wab[0:64, :], start=True, stop=False)
                e.matmul(ps[4][:, 0:128], lhsT=ones[:], rhs=w0b[:, 0:128], start=False, stop=True)
                e.matmul(ps[4][:, 128:256], lhsT=ldT[64:128, :], rhs=wab[64:128, :], start=True, stop=False)
                e.matmul(ps[4][:, 128:256], lhsT=ones[:], rhs=w0b[:, 128:256], start=False, stop=True)
                return e.matmul(ps[4][:, 256:384], lhsT=gdT[:], rhs=gub[:], start=True, stop=True)
            P.op("pe", ups, reads=["ldT0", "ldT1", "gdT", "wab", "gub", "w0b", "ones"], writes=["ps4"])
            P.op("act", lambda e: e.activation(out=sg[:], in_=ps[4][:, 0:128], func=AF.Sigmoid), reads=["ps4"], writes=["sg"])
            P.op("pe", lambda e: e.matmul(ps[6][:, 128:256], lhsT=mcum[:], rhs=sg[:], start=True, stop=True), reads=["mcum", "sg"], writes=["ps6"])
            P.op("act", lambda e, so=so: e.activation(out=strm[:, so, 0, :], in_=ps[6][:, 128:256], func=AF.Exp, scale=-0.6065306597126334), reads=["ps6"], writes=[("strm", so, 0)])
            P.op("act", lambda e: e.activation(out=pinv[:], in_=ps[6][:, 128:256], func=AF.Exp, scale=0.6065306597126334), reads=["ps6"], writes=["pinv"])
            P.op("act", lambda e: e.activation(out=csx[:], in_=ps[6][:, 128:256], func=AF.Copy), reads=["ps6"], writes=["csx"])
            P.op("pool", lambda e: e.tensor_tensor(out=csx[:], in0=csx[:], in1=sg[:], op=ALU.subtract), reads=["csx", "sg"], writes=["csx"])
            P.op("act", lambda e: e.activation(out=pexc[:], in_=csx[:], func=AF.Exp, scale=-0.6065306597126334), reads=["csx"], writes=["pexc"])
            P.op("act", lambda e: e.activation(out=aa[:], in_=ps[4][:, 128:256], func=AF.Sigmoid), reads=["ps4"], writes=["aa"])
            P.op("act", lambda e, i=i: e.activation(out=g_all[:, i, :], in_=ps[4][:, 256:384], func=AF.Copy), reads=["ps4"], writes=[("g_all", i)])
            r_ = rkv[:, 0:128]; k_ = rkv[:, 128:256]; v_ = rkv[:, 256:384]
            P.op("pe", lambda e: e.transpose(out=ps[5][:, 0:128], in_=v_, identity=pj.idf[:]), reads=["rkv", "idf"], writes=["ps5"])
            P.op("act", lambda e, i=i: e.activation(out=vT_all[:, i * 128:(i + 1) * 128], in_=ps[5][:, 0:128], func=AF.Copy), reads=["ps5"], writes=[("vT", i)])
            P.op("dve", lambda e: e.tensor_tensor(out=kk0[:], in0=h3(k_), in1=h3(cv[:, KK, :]), op=ALU.mult), reads=["rkv", "cv"], writes=["kk0"])
            P.op("dve", lambda e: e.tensor_tensor(out=sq[:], in0=kk0[:], in1=kk0[:], op=ALU.mult), reads=["kk0"], writes=["sq"])
            P.op("dve", lambda e: e.tensor_reduce(out=st1[:, 0:2], in_=sq[:], axis=AX.X, op=ALU.add), reads=["sq"], writes=["st_k"])
            P.op("dve", lambda e: e.tensor_scalar(out=st1[:, 0:2], in0=st1[:, 0:2], scalar1=1e-24, scalar2=None, op0=ALU.max), reads=["st_k"], writes=["st_k"])
            P.op("act", lambda e: e.sqrt(out=st1[:, 0:2], in_=st1[:, 0:2]), reads=["st_k"], writes=["st_k"])
            P.op("dve", lambda e: e.reciprocal(out=st1[:, 0:2], in_=st1[:, 0:2]), reads=["st_k"], writes=["st_k"])
            P.op("dve", lambda e, so=so: e.tensor_tensor(out=h3(strm[:, so, 1, :]), in0=kk0[:], in1=st1[:, 0:2].unsqueeze(2).to_broadcast([128, 2, 64]), op=ALU.mult),
                 reads=["kk0", "st_k"], writes=[("strm", so, 1)])
            P.op("dve", lambda e, so=so: e.scalar_tensor_tensor(out=strm[:, so, 2, :], in0=strm[:, so, 1, :], scalar=-1.0, in1=aa[:], op0=ALU.mult, op1=ALU.mult),
                 reads=[("strm", so, 1), "aa"], writes=[("strm", so, 2)])
            P.op("dve", lambda e: e.scalar_tensor_tensor(out=tmp[:], in0=aa[:], scalar=-1.0, in1=cv[:, KA, :], op0=ALU.add, op1=ALU.mult), reads=["aa", "cv"], writes=["tmp"])
            P.op("dve", lambda e, so=so: e.scalar_tensor_tensor(out=strm[:, so, 3, :], in0=tmp[:], scalar=1.0, in1=k_, op0=ALU.add, op1=ALU.mult),
                 reads=["tmp", "rkv"], writes=[("strm", so, 3)])
            P.op("pool", lambda e, so=so: e.tensor_tensor(out=strm[:, so, 4, :], in0=r_, in1=strm[:, so, 0, :], op=ALU.mult), reads=["rkv", ("strm", so, 0)], writes=[("strm", so, 4)])
            P.op("dve", lambda e, so=so: e.tensor_tensor(out=tmp[:], in0=r_, in1=strm[:, so, 3, :], op=ALU.mult), reads=["rkv", ("strm", so, 3), "tmp"], writes=["tmp"])
            P.op("dve", lambda e: e.tensor_tensor(out=tmp[:], in0=tmp[:], in1=cv[:, RK, :], op=ALU.mult), reads=["tmp", "cv"], writes=["tmp"])
            P.op("dve", lambda e: e.tensor_reduce(out=st1[:, 2:4], in_=h3(tmp[:]), axis=AX.X, op=ALU.add), reads=["tmp"], writes=["st_b"])
            P.op("dve", lambda e, i=i: e.tensor_tensor(out=h3(bon_all[:, i, :]), in0=h3(v_), in1=st1[:, 2:4].unsqueeze(2).to_broadcast([128, 2, 64]), op=ALU.mult),
                 reads=["rkv", "st_b"], writes=[("bon", i)])
            P.op("pool", lambda e, so=so: e.tensor_tensor(out=strm[:, so, 1, :], in0=strm[:, so, 1, :], in1=pexc[:], op=ALU.mult), reads=[("strm", so, 1), ("strm", so, 2), "pexc"], writes=[("strm", so, 1)])
            P.op("pool", lambda e, so=so: e.tensor_tensor(out=strm[:, so, 2, :], in0=strm[:, so, 2, :], in1=pinv[:], op=ALU.mult), reads=[("strm", so, 2), "pinv"], writes=[("strm", so, 2)])
            P.op("pool", lambda e, so=so: e.tensor_tensor(out=strm[:, so, 3, :], in0=strm[:, so, 3, :], in1=pinv[:], op=ALU.mult), reads=[("strm", so, 3), "pinv", "tmp"], writes=[("strm", so, 3)])
            ma.tile(i)
            skeys = [("strm", so, s_) for s_ in range(5)]
            P.op("pool", lambda e, so=so: e.tensor_copy(out=shl[:, so, 0], in_=strm[:, so]), reads=skeys, writes=[("shl", so, 0)])
            P.op("pool", lambda e, so=so: e.tensor_tensor(out=sdf[:], in0=strm[:, so], in1=shl[:, so, 0], op=ALU.subtract), reads=skeys + [("shl", so, 0)], writes=["sdf"])
            P.op("pool", lambda e, so=so: e.tensor_copy(out=shl[:, so, 1], in_=sdf[:]), reads=["sdf"], writes=[("shl", so, 1)])
            for hl in range(2):
                for s_ in range(5):
                    r0 = hl * 10 + 2 * s_
                    P.dma("sp" if hl == 0 else "pool", scr[r0:r0 + 2, i * 128:(i + 1) * 128, :].rearrange("h t k -> t h k"), h3(shl[:, so, hl, s_, :]),
                          reads=[("shl", so, hl)], writes=[("scr", i)])

        P.barrier()
        P.op("dve", lambda e: e.memset(Sb2[:], 0.0), writes=[("S", 0), ("S", 1)])
        nsteps = ntile * 128
        SW, SKK, SNB, SK, SR = range(5)
        SLOT = {0: 0, 4: 1, 1: 2, 2: 3, 3: 4}
        def bcap(par, s_, j):
            f = SLOT[s_] * 256
            return ps[par * 3 + f // 512][:, (f % 512) + j * 64:(f % 512) + (j + 1) * 64]
        def bcblk(par, s_):
            f = SLOT[s_] * 256
            return ps[par * 3 + f // 512][:, (f % 512):(f % 512) + 256]
        if upto < 3:
            P.op('dve', lambda e: e.memset(yT_all[:], 0.0), writes=[('yT', i) for i in range(ntile)])
        for ch in range(nsteps // NST if upto >= 2 else 0):
            slot = ch % 2
            P.dma("sp", rows[:, slot, :].rearrange("p (t k) -> p t k", k=64), scr[:, ch * NST:(ch + 1) * NST, :],
                  reads=[("scr", (ch * NST) // 128)], writes=[("rows", slot)])
            for gg in range(NST // 4):
                g = ch * (NST // 4) + gg
                par = g % 2
                def bc(e, par=par, slot=slot, gg=gg):
                    for s_ in range(5):
                        ins = e.matmul(bcblk(par, s_), lhsT=sel[:, s_, :], rhs=rows[:, slot, gg * 256:(gg + 1) * 256], start=True, stop=True)
                    return ins
                P.op("pe", bc, reads=[("rows", slot), "sel"], writes=[("bc", par)])
                if upto >= 3:
                    pass
                for j in range(4 if upto >= 3 else 0):
                    t = g * 4 + j
                    ti = t // 128
                    cb = ch % 2
                    Sx = Sb2[:, cb, :]
                    kS = ("S", cb)
                    last_in_chunk = (t % NST == NST - 1)

                    def yop(pt, ppar, pj_, pcb, same=True):
                        P.op("dve", lambda e, ppar=ppar, pj_=pj_, pt=pt, pcb=pcb: e.scalar_tensor_tensor(out=junk2[:], in0=Sb2[:, pcb, :], scalar=1.0, in1=bcap(ppar, SR, pj_), op0=ALU.mult, op1=ALU.mult,
                                                                                                   accum_out=yT_all[:, pt:pt + 1]),
                             reads=[("S", pcb), ("bc", ppar)], writes=["junk2", ("yT", pt // 128)], same_ok=same)
                    P.op("dve", lambda e, par=par, j=j, Sx=Sx: e.scalar_tensor_tensor(out=junk[:], in0=Sx, scalar=1.0, in1=bcap(par, SKK, j), op0=ALU.mult, op1=ALU.mult, accum_out=sa[:]),
                         reads=[kS, ("bc", par)], writes=["junk", "sa"], same_ok=(t > 0))
                    P.op("dve", lambda e, par=par, j=j, t=t, Sx=Sx: e.scalar_tensor_tensor(out=Ubuf[:], in0=bcap(par, SK, j), scalar=vT_all[:, t:t + 1], in1=Sx, op0=ALU.mult, op1=ALU.add),
                         reads=[kS, ("bc", par), ("vT", ti)], writes=["Ubuf"], same_ok=(t > 0))
                    if prev_step is not None:
                        yop(*prev_step)
                    P.op("dve", lambda e, par=par, j=j, Sx=Sx: e.scalar_tensor_tensor(out=Sx, in0=bcap(par, SNB, j), scalar=sa[:], in1=Ubuf[:], op0=ALU.mult, op1=ALU.add),
                         reads=["sa", "Ubuf", ("bc", par)], writes=[kS], same_ok=True)
                    prev_step = (t, par, j, cb)
                    if last_in_chunk:
                        yop(*prev_step)
                        prev_step = None
                        P.op("dve", lambda e, par=par, j=j, cb=cb: e.tensor_tensor(out=Sb2[:, 1 - cb, :], in0=Sb2[:, cb, :], in1=bcap(par, SW, j), op=ALU.mult),
                             reads=[("S", cb), ("bc", par)], writes=[("S", 1 - cb)], same_ok=True)
                        P.op("dve", lambda e: e.memset(fill[:, 0:1], 0.0), writes=["fill"], same_ok=True)

        P.barrier()
        for i in range(ntile):
            so = i % 2
            P.op("pe", lambda e, i=i: e.transpose(out=ps[5][:, 0:128], in_=yT_all[:, i * 128:(i + 1) * 128], identity=pj.idf[:]), reads=[("yT", i), "idf"], writes=["ps5"])
            y3 = ps[5][:, 0:128].rearrange("p (h d) -> p h d", h=2)
            P.op("dve", lambda e: e.tensor_reduce(out=st1[:, 0:2], in_=y3, axis=AX.X, op=ALU.add), reads=["ps5"], writes=["st_k"])
            P.op("dve", lambda e: e.tensor_scalar(out=st1[:, 0:2], in0=st1[:, 0:2], scalar1=1.0 / 64, scalar2=None, op0=ALU.mult), reads=["st_k"], writes=["st_k"])
            P.op("dve", lambda e: e.tensor_tensor(out=yc[:], in0=y3, in1=st1[:, 0:2].unsqueeze(2).to_broadcast([128, 2, 64]), op=ALU.subtract), reads=["ps5", "st_k"], writes=["yc"])
            P.op("dve", lambda e: e.tensor_tensor(out=sq[:], in0=yc[:], in1=yc[:], op=ALU.mult), reads=["yc"], writes=["sq"])
            P.op("dve", lambda e: e.tensor_reduce(out=st1[:, 2:4], in_=sq[:], axis=AX.X, op=ALU.add), reads=["sq"], writes=["st_b"])
            P.op("dve", lambda e: e.tensor_scalar(out=st1[:, 2:4], in0=st1[:, 2:4], scalar1=1.0 / 64, scalar2=64e-5, op0=ALU.mult, op1=ALU.add), reads=["st_b"], writes=["st_b"])
            P.op("act", lambda e: e.sqrt(out=st1[:, 2:4], in_=st1[:, 2:4]), reads=["st_b"], writes=["st_b"])
            P.op("dve", lambda e: e.reciprocal(out=st1[:, 2:4], in_=st1[:, 2:4]), reads=["st_b"], writes=["st_b"])
            P.op("dve", lambda e: e.tensor_tensor(out=yc[:], in0=yc[:], in1=st1[:, 2:4].unsqueeze(2).to_broadcast([128, 2, 64]), op=ALU.mult), reads=["yc", "st_b"], writes=["yc"])
            ycf = yc[:].rearrange("p h d -> p (h d)")
            P.op("dve", lambda e: e.tensor_tensor(out=ycf, in0=ycf, in1=cv[:, GNG, :], op=ALU.mult), reads=["yc", "cv"], writes=["yc"])
            P.op("dve", lambda e: e.tensor_tensor(out=ycf, in0=ycf, in1=cv[:, GNB, :], op=ALU.add), reads=["yc", "cv"], writes=["yc"])
            P.op("dve", lambda e, i=i: e.tensor_tensor(out=ycf, in0=ycf, in1=bon_all[:, i, :], op=ALU.add), reads=["yc", ("bon", i)], writes=["yc"])
            P.op("dve", lambda e, i=i, so=so: e.tensor_tensor(out=yo[:, so, :], in0=ycf, in1=g_all[:, i, :], op=ALU.mult), reads=["yc", ("g_all", i)], writes=[("yo", so)])
            P.dma("sp", ysrc[i * 128:(i + 1) * 128, 128:256], yo[:, so, :], reads=[("yo", so)], writes=["ysrc"])
        P.emit(last=is_last)


def prep_MB(layer, h, inputs):
    i = layer
    maps = []
    w = inputs["w_in"][i]
    sel = np.zeros((20, 5, 128), np.float32)
    for hl in range(2):
        for s_ in range(5):
            for hh in range(2):
                sel[hl * 10 + s_ * 2 + hh, s_, hh * 64:(hh + 1) * 64] = 1.0
    for c in range(8):
        b, gi = c // 2, c % 2
        hc = slice(gi * 128, (gi + 1) * 128)
        cols = np.concatenate([512 + np.arange(gi * 128, (gi + 1) * 128), 768 + np.arange(gi * 128, (gi + 1) * 128), 1024 + np.arange(gi * 128, (gi + 1) * 128),
                               np.arange(1280, 1536)])
        maps.append({
            "h": np.ascontiguousarray(h[b]), "identf": np.eye(128, dtype=np.float32),
            "wc": np.ascontiguousarray(np.concatenate([w[:, cols], w[:, gi * 128:(gi + 1) * 128], w[:, 256 + gi * 128:256 + (gi + 1) * 128]], axis=1)),
            "a_lng": bc128(inputs["gm_ln_g"][i][2 * gi:2 * gi + 2].reshape(-1)), "a_lnb": bc128(inputs["gm_ln_b"][i][2 * gi:2 * gi + 2].reshape(-1)),
            "a_ws": np.ascontiguousarray(inputs["gm_ws"][i][2 * gi:2 * gi + 2]), "a_tril": np.tril(np.ones((128, 128), np.float32)),
            "a_bs": np.ascontiguousarray(inputs["gm_bs"][i][2 * gi:2 * gi + 2].T),
            "g_mix": pc8(inputs["g_mix"][i]), "mu": bc128(inputs["rw_mu"][i][cols - 512]),
            "wa_up": np.ascontiguousarray(np.concatenate([inputs["rw_w_up"][i][:, hc], inputs["rw_a_up"][i][:, hc]], axis=0)),
            "g_up": np.ascontiguousarray(inputs["rw_g_up"][i][:, hc]),
            "w0a0": np.concatenate([inputs["rw_w0"][i][hc], inputs["rw_a0"][i][hc]])[None, :].astype(np.float32),
            "cvec": np.ascontiguousarray(np.stack([bc128(inputs["rw_k_k"][i][hc]), bc128(inputs["rw_k_a"][i][hc]), bc128(inputs["rw_r_k"][i].reshape(-1)[hc]),
                                                   bc128(inputs["rw_gn_g"][i][hc]), bc128(inputs["rw_gn_b"][i][hc])], axis=1)),
            "sel": sel,
            "mcum": ((np.arange(128)[:, None] // 32 == np.arange(128)[None, :] // 32) & (np.arange(128)[:, None] <= np.arange(128)[None, :])).astype(np.float32),
        })
    return maps


NEGB = -30000.0
MC_BR = 'csw'


def phase_MC(nc, P, ps, psb, pre, h_in, ysrc, hmap=None, ntile=NTILE, upto=9, is_last=False):
    import ml_dtypes
    di = lambda n, s, dt=F32: nc.dram_tensor(pre + n, s, dt, kind="ExternalInput").ap()
    identf = di("identf", [128, 128])
    wc = di("wc", [D, 652])
    g_mix = di("g_mix", [128, 8])
    pos_in = di("pos", [128, NTILE], I32)
    invf = di("invf", [128, 8])
    w1kc = di("w1kc", [2048, 128]); w1vc = di("w1vc", [2048, 128])
    w2kc = di("w2kc", [128, 64]); w2vc = di("w2vc", [128, 64])
    posT = di("posT", [64, 32])
    rconst = di("rconst", [128, 2, 65])
    cmask = di("cmask", [NTILE, 128, 2, 128], BF16)
    selc = di("selc", [NTILE, 128, 2, 64])
    Ef_in = di("Ef", [64, S], BF16)
    tri_in = di("tri", [128, 2, 128], BF16)
    with ExitStack() as st:
        sb = lambda name, shape, dt: st.enter_context(nc.sbuf_tensor(pre + "s_" + name, shape, dt))
        pj = Proj(nc, P, st, h_in, identf, 652, wc, g_mix, pre=pre, hmap=hmap)
        qrT = sb("qrT", [64, 4, S], BF16)
        qwT = sb("qwT", [64, 4, S], BF16)
        kT4 = sb("kT4", [64, 4, S], BF16)
        vaug = sb("vaug", [128, 2, NTILE, 65], BF16)
        ksE = sb("ksE", [128, S], BF16)
        qs = sb("qs", [128, 2, 4, 128], BF16)
        tri = sb("tris", [128, 2, 128], BF16)
        w1s = sb("w1s", [64, 32, 128], F32)
        w1b = sb("w1b", [64, 2, 32, 128], BF16)
        w2f = sb("w2f", [128, 2, 64], F32); w2b = sb("w2b", [128, 2, 64], BF16)
        posf = sb("posf", [64, 32], F32); posb = sb("posb", [64, 32], BF16)
        rcf = sb("rcf", [128, 2, 65], F32)
        R_ = sb("R_", [128, 2, 129], BF16)
        posi = sb("posi", [128, NTILE], I32); posfl = sb("posfl", [128, NTILE], F32)
        inv = sb("inv", [128, 8], F32)
        ang = sb("ang", [128, NTILE, 8], F32)
        cs = sb("cs", [128, NTILE, 8], F32); sn = sb("sn", [128, NTILE, 8], F32)
        gsig = sb("gsig", [128, NTILE, 12], F32)
        xq = sb("xq", [128, 512], F32)
        xqb = sb("xqb", [128, 512], BF16)
        qkr = sb("qkr", [128, 6, 64], BF16)
        ra = sb("ra", [128, 6, 8], F32); rb_ = sb("rb_", [128, 6, 8], F32)
        bias_sb = sb("bias_sb", [128, 2], F32)
        gelT = sb("gelT", [128, 2, 256], BF16)
        kcmpT = sb("kcmpT", [64, 256], BF16)
        cm = sb("cm", [128, 2, 2, 128], BF16)
        scs = sb("scs", [128, 2, 2, 64], F32)
        eT = sb("eT", [128, 2, 4, 128], BF16)
        imp = sb("imp", [128, 64], F32)
        imp2 = sb("imp2", [128, 64], F32)
        rep = sb("rep", [128, 64], F32)
        mx = sb("mx", [128, 8], F32)
        selbb = sb("selbb", [128, 128], BF16)
        selbT = sb("selbT", [64, 128], BF16)
        sm = sb("sm", [128, 16], F32)
        oacc = sb("oacc", [128, 2, 4, 64], F32)

        ld = lambda dst, src, key: P.dma("sp", dst, src, writes=[key])
        ld(ksE[64:128, :], Ef_in, "ksE_E"); ld(tri[:], tri_in, "tri")
        P.op("pool", lambda e: e.memset(selbb[:], 0.0), writes=["selbb"])
        ld(posi[:], pos_in, "posi"); ld(inv[:], invf, "inv")
        ld(rcf[:], rconst, "rcf"); ld(posf[:], posT, "posf")
        ld(w2f[:, 0, :], w2kc, "w2f0"); ld(w2f[:, 1, :], w2vc, "w2f1")
        P.op("dve", lambda e: e.tensor_copy(out=w2b[:], in_=w2f[:]), reads=["w2f0", "w2f1"], writes=["w2b"])
        P.op("dve", lambda e: e.tensor_copy(out=posb[:], in_=posf[:]), reads=["posf"], writes=["posb"])
        P.op("dve", lambda e: e.memset(R_[:], 0.0), writes=["R"])
        P.op("dve", lambda e: e.tensor_copy(out=R_[:, :, 0:65], in_=rcf[:]), reads=["rcf", "R"], writes=["R"])
        P.op("pool", lambda e: e.memset(vaug[:], 1.0), writes=["vaug_init"])
        P.op("pool", lambda e: e.memset(gelT[:], 0.0), writes=["gelT0", "gelT1"])
        for w in range(2):
            P.dma("sp", w1s[:], (w1kc if w == 0 else w1vc).rearrange("(l d) h -> d l h", d=64), writes=["w1s"])
            P.op("pool", lambda e, w=w: e.tensor_copy(out=w1b[:, w, :, :], in_=w1s[:]), reads=["w1s"], writes=[("w1b", w)])
        P.op("dve", lambda e: e.tensor_copy(out=posfl[:], in_=posi[:]), reads=["posi"], writes=["posfl"])
        P.op("dve", lambda e: e.tensor_tensor(out=ang[:], in0=posfl[:].unsqueeze(2).to_broadcast([128, NTILE, 8]), in1=inv[:].unsqueeze(1).to_broadcast([128, NTILE, 8]), op=ALU.mult),
             reads=["posfl", "inv"], writes=["ang"])
        PI = float(np.pi)
        angi = sb("angi", [128, NTILE, 8], I32)
        angf = sb("angf", [128, NTILE, 8], F32)
        for (dst, off, key) in ((sn, 0.5, "sn"), (cs, 0.75, "cs")):
            P.op("dve", lambda e, dst=dst, off=off: e.tensor_scalar(out=dst[:], in0=ang[:], scalar1=1.0 / (2 * PI), scalar2=off, op0=ALU.mult, op1=ALU.add), reads=["ang"], writes=[key])
            P.op("dve", lambda e, dst=dst: e.tensor_copy(out=angi[:], in_=dst[:]), reads=[key], writes=["angi"])
            P.op("dve", lambda e: e.tensor_copy(out=angf[:], in_=angi[:]), reads=["angi"], writes=["angf"])
            P.op("dve", lambda e, dst=dst: e.tensor_tensor(out=dst[:], in0=dst[:], in1=angf[:], op=ALU.subtract), reads=[key, "angf"], writes=[key])
            P.op("dve", lambda e, dst=dst: e.tensor_scalar(out=angf[:], in0=dst[:], scalar1=0.0, scalar2=None, op0=ALU.is_lt), reads=[key], writes=["angf"])
            P.op("dve", lambda e, dst=dst: e.tensor_tensor(out=dst[:], in0=dst[:], in1=angf[:], op=ALU.add), reads=[key, "angf"], writes=[key])
            P.op("dve", lambda e, dst=dst: e.tensor_scalar(out=dst[:], in0=dst[:], scalar1=2 * PI, scalar2=-PI, op0=ALU.mult, op1=ALU.add), reads=[key], writes=[key])
            P.op("dve", lambda e, dst=dst: e.tensor_scalar(out=dst[:], in0=dst[:], scalar1=PI, scalar2=-PI, op0=ALU.min, op1=ALU.max), reads=[key], writes=[key])
            P.op("act", lambda e, dst=dst: e.activation(out=dst[:], in_=dst[:], func=AF.Sin), reads=[key], writes=[key])

        pj.tile(0, psb[0], "ps0")
        for i in range(ntile):
            if i + 1 < ntile:
                pj.tile(i + 1, psb[0], "ps0")
            P.op("pe", lambda e, i=i: pj.mm_tok(e, i, ps[1][:], 0, 512), reads=pj.keys(i), writes=["ps1"])
            P.op("pe", lambda e, i=i: pj.mm_tok(e, i, ps[2][:, 0:140], 512, 652), reads=pj.keys(i), writes=["ps2"])
            P.op("act", lambda e: e.activation(out=xq[:], in_=ps[1][:], func=AF.Copy), reads=["ps1"], writes=["xq"])
            P.op("act", lambda e, i=i: e.activation(out=vaug[:, :, i, 0:64], in_=ps[2][:, 0:128].rearrange("p (a d) -> p a d", a=2), func=AF.Copy), reads=["ps2", "vaug_init"], writes=[("vaug", i)])
            P.op("act", lambda e, i=i: e.activation(out=gsig[:, i, :], in_=ps[2][:, 128:140], func=AF.Sigmoid), reads=["ps2"], writes=[("gsig", i)])
            P.op("pool", lambda e: e.tensor_copy(out=xqb[:], in_=xq[:]), reads=["xq"], writes=["xqb"])
            X = xq[:, 0:384].rearrange("p (h d) -> p h d", h=6)
            cb = cs[:, i, :].unsqueeze(1).to_broadcast([128, 6, 8])
            sbb = sn[:, i, :].unsqueeze(1).to_broadcast([128, 6, 8])
            P.op("pool", lambda e, X=X: e.tensor_copy(out=qkr[:, :, 16:64], in_=X[:, :, 16:64]), reads=["xq"], writes=["qkr_c"])
            P.op("dve", lambda e, X=X, cb=cb: e.tensor_tensor(out=ra[:], in0=X[:, :, 0:8], in1=cb, op=ALU.mult), reads=["xq", "cs"], writes=["ra"])
            P.op("dve", lambda e, X=X, sbb=sbb: e.tensor_tensor(out=rb_[:], in0=X[:, :, 8:16], in1=sbb, op=ALU.mult), reads=["xq", "sn"], writes=["rb"])
            P.op("dve", lambda e: e.tensor_tensor(out=qkr[:, :, 0:8], in0=ra[:], in1=rb_[:], op=ALU.subtract), reads=["ra", "rb"], writes=["qkr_a"])
            P.op("dve", lambda e, X=X, sbb=sbb: e.tensor_tensor(out=ra[:], in0=X[:, :, 0:8], in1=sbb, op=ALU.mult), reads=["xq", "sn", "ra"], writes=["ra"])
            P.op("dve", lambda e, X=X, cb=cb: e.tensor_tensor(out=rb_[:], in0=X[:, :, 8:16], in1=cb, op=ALU.mult), reads=["xq", "cs", "rb"], writes=["rb"])
            P.op("dve", lambda e: e.tensor_tensor(out=qkr[:, :, 8:16], in0=ra[:], in1=rb_[:], op=ALU.add), reads=["ra", "rb"], writes=["qkr_b"])
            def trs(e):
                for j in range(4):
                    e.transpose(out=psb[3][0:64, j * 128:(j + 1) * 128], in_=qkr[:, j, :], identity=pj.idb[:])
                for j in range(4):
                    e.transpose(out=psb[5][0:64, j * 128:(j + 1) * 128], in_=xqb[:, j * 64:(j + 1) * 64], identity=pj.idb[:])
                e.transpose(out=psb[4][0:64, 0:128], in_=qkr[:, 4, :], identity=pj.idb[:])
                e.transpose(out=psb[4][0:64, 128:256], in_=qkr[:, 5, :], identity=pj.idb[:])
                e.transpose(out=psb[4][0:64, 256:384], in_=xqb[:, 384:448], identity=pj.idb[:])
                return e.transpose(out=psb[4][0:64, 384:512], in_=xqb[:, 448:512], identity=pj.idb[:])
            P.op("pe", trs, reads=["qkr_a", "qkr_b", "qkr_c", "xqb", "idb"], writes=["ps3", "ps4", "ps5"])
            tsl = slice(i * 128, (i + 1) * 128)
            P.op("act", lambda e, tsl=tsl: e.activation(out=qrT[:, :, tsl], in_=psb[3][0:64, 0:512].rearrange("p (j t) -> p j t", j=4), func=AF.Copy), reads=["ps3"], writes=[("qrT", i)])
            P.op("dve", lambda e, tsl=tsl: e.tensor_copy(out=qwT[:, :, tsl], in_=psb[5][0:64, 0:512].rearrange("p (j t) -> p j t", j=4)), reads=["ps5"], writes=[("qwT", i)])
            P.op("act", lambda e, tsl=tsl: e.activation(out=kT4[:, :, tsl], in_=psb[4][0:64, 0:512].rearrange("p (j t) -> p j t", j=4), func=AF.Copy), reads=["ps4"], writes=[("kT4", i)])
            P.op("act", lambda e, tsl=tsl: e.activation(out=ksE[0:64, tsl], in_=psb[4][0:64, 0:128], func=AF.Copy), reads=["ps4"], writes=[("ksE", i)])

        P.barrier()
        ncmp = (ntile * 128 - 32) // 16 + 1
        for w in range(2 if upto >= 2 else 0):
            src = 2 + w
            def hid(e, w=w, src=src):
                for l in range(32):
                    ins = e.matmul(ps[0][:, 0:ncmp], lhsT=w1b[:, w, l, :], rhs=kT4[:, src, l:l + 16 * (ncmp - 1) + 1:16], start=(l == 0), stop=(l == 31))
                return ins
            P.op("pe", hid, reads=[("w1b", w)], writes=["ps0"])
            def pbias(e, w=w):
                for l in range(32):
                    ins = e.matmul(ps[1][:, 0:1], lhsT=w1b[:, w, l, :], rhs=posb[:, l:l + 1], start=(l == 0), stop=(l == 31))
                return ins
            P.op("pe", pbias, reads=[("w1b", w), "posb"], writes=["ps1"])
            P.op("dve", lambda e, w=w: e.tensor_copy(out=bias_sb[:, w:w + 1], in_=ps[1][:, 0:1]), reads=["ps1"], writes=[("bias", w)])
            P.op("act", lambda e, w=w: e.activation(out=gelT[:, w, 0:ncmp], in_=ps[0][:, 0:ncmp], func=AF.Gelu_apprx_tanh, bias=bias_sb[:, w:w + 1]), reads=["ps0", ("bias", w), f"gelT{w}"], writes=[f"gelT{w}"])
        if upto >= 2:
            P.op("pe", lambda e: e.matmul(ps[2][0:64, 0:256], lhsT=w2b[:, 0, :], rhs=gelT[:, 0, :], start=True, stop=True), reads=["w2b", "gelT0"], writes=["ps2"])
            P.op("act", lambda e: e.activation(out=kcmpT[:], in_=ps[2][0:64, 0:256], func=AF.Copy), reads=["ps2"], writes=["kcmpT"])
        for cn in range(2 if upto >= 2 else 0):
            P.op("pe", lambda e, cn=cn: e.matmul(ps[3][:, cn * 64:(cn + 1) * 64], lhsT=gelT[:, 1, cn * 128:(cn + 1) * 128], rhs=w2b[:, 1, :], start=True, stop=True), reads=["w2b", "gelT1"], writes=["ps3"])
            P.op("dve", lambda e, cn=cn: e.tensor_copy(out=R_[:, cn, 65:129], in_=ps[3][:, cn * 64:(cn + 1) * 64]), reads=["ps3", "R"], writes=["R"])

        P.barrier()
        nsc = 0
        if upto < 3:
            P.op('dve', lambda e: e.memset(oacc[:], 0.0), writes=[('oacc', 0), ('oacc', 1)])
            P.dma('sp', ysrc[0:128, 256:512], oacc[:, 0].rearrange('p j d -> p (j d)'), reads=[('oacc', 0)], writes=['ysrc'])
        for qb in range(ntile if upto >= 3 else 0):
            tsl = slice(qb * 128, (qb + 1) * 128)
            so = qb % 2
            P.dma("sp", cm[:, so], cmask[qb], writes=[("cm", so)])
            P.dma("sp", scs[:, so], selc[qb], writes=[("scs", so)])
            ncn = 2 if qb >= 16 else 1
            for cn in range(ncn):
                sbk = nsc % 2; nsc += 1
                def sc(e, cn=cn, sbk=sbk, tsl=tsl, so=so):
                    e.matmul(ps[sbk][:], lhsT=kcmpT[:, cn * 128:(cn + 1) * 128], rhs=qwT[:, :, tsl], start=True, stop=False)
                    return e.matmul(ps[sbk][:], lhsT=pj.idb[:], rhs=cm[:, so, cn, :].unsqueeze(1).to_broadcast([128, 4, 128]), start=False, stop=True)
                P.op("pe", sc, reads=["kcmpT", ("qwT", qb), ("cm", so), "idb"], writes=[f"ps{sbk}"])
                P.op("act", lambda e, sbk=sbk: e.activation(out=eT[:, sbk].rearrange("p j t -> p (j t)"), in_=ps[sbk][:], func=AF.Exp, scale=0.125), reads=[f"ps{sbk}"], writes=[("eT", sbk)])
                def pv(e, cn=cn, sbk=sbk, ncn=ncn):
                    for j in range(4):
                        ins = e.matmul(ps[2 + j // 2][:, (j % 2) * 129:(j % 2) * 129 + 129], lhsT=eT[:, sbk, j, :], rhs=R_[:, cn, :], start=(cn == 0 and j % 2 == 0), stop=(cn == ncn - 1), skip_group_check=True)
                    return ins
                P.op("pe", pv, reads=[("eT", sbk), "R"], writes=["ps2", "ps3"])
            P.op("dve", lambda e: e.memset(imp[:], 0.0), writes=["imp"])
            for j in range(4):
                pso = ps[2 + j // 2][:, (j % 2) * 129:(j % 2) * 129 + 129]
                ri = sm[:, j:j + 1]; rg = sm[:, 4 + j:5 + j]
                P.op("dve", lambda e, pso=pso, ri=ri: e.tensor_scalar(out=ri, in0=pso[:, 64:65], scalar1=1e-30, scalar2=None, op0=ALU.add), reads=["ps2", "ps3"], writes=[("ri", j)])
                P.op("dve", lambda e, ri=ri: e.reciprocal(out=ri, in_=ri), reads=[("ri", j)], writes=[("ri", j)])
                P.op("dve", lambda e, pso=pso, ri=ri: e.scalar_tensor_tensor(out=imp[:], in0=pso[:, 0:64], scalar=ri, in1=imp[:], op0=ALU.mult, op1=ALU.add), reads=["ps2", "ps3", ("ri", j), "imp"], writes=["imp"])
                P.op("dve", lambda e, ri=ri, rg=rg, j=j, qb=qb: e.tensor_tensor(out=rg, in0=ri, in1=gsig[:, qb, 3 * j:3 * j + 1], op=ALU.mult), reads=[("ri", j), ("gsig", qb)], writes=[("rg", j)])
                P.op("dve", lambda e, pso=pso, rg=rg, j=j, so=so: e.tensor_scalar(out=oacc[:, so, j, :], in0=pso[:, 65:129], scalar1=rg, scalar2=None, op0=ALU.mult), reads=["ps2", "ps3", ("rg", j)], writes=[("oacc", so)])
            if upto < 4:
                P.dma('sp', ysrc[tsl, 256:512], oacc[:, so].rearrange('p j d -> p (j d)'), reads=[('oacc', so)], writes=['ysrc'])
                continue
            P.op("dve", lambda e, so=so: e.tensor_tensor(out=imp2[:], in0=imp[:], in1=scs[:, so, 0, :], op=ALU.mult), reads=["imp", ("scs", so)], writes=["imp2"])
            P.op("dve", lambda e, so=so: e.tensor_tensor(out=imp2[:], in0=imp2[:], in1=scs[:, so, 1, :], op=ALU.add), reads=["imp2", ("scs", so)], writes=["imp2"])
            P.op("dve", lambda e: e.max(out=mx[:], in_=imp2[:]), reads=["imp2"], writes=["mx"])
            P.op("dve", lambda e: e.match_replace(out=rep[:], in_to_replace=mx[:], in_values=imp2[:], imm_value=-1e30), reads=["imp2", "mx"], writes=["rep"])
            P.op("dve", lambda e: e.max(out=mx[:], in_=rep[:]), reads=["rep"], writes=["mx"])
            P.op("dve", lambda e: e.tensor_scalar(out=mx[:, 7:8], in0=mx[:, 7:8], scalar1=-5000.0, scalar2=None, op0=ALU.max), reads=["mx"], writes=["mx"])
            P.op("dve", lambda e: e.tensor_scalar(out=rep[:], in0=imp2[:], scalar1=mx[:, 7:8], scalar2=None, op0=ALU.is_ge), reads=["imp2", "mx", "rep"], writes=["rep"])
            P.op("dve", lambda e: e.tensor_scalar(out=selbb[:, 64:128], in0=rep[:], scalar1=-NEGB, scalar2=NEGB, op0=ALU.mult, op1=ALU.add), reads=["rep", "selbb"], writes=["selbb"])
            P.op("pe", lambda e: e.transpose(out=psb[6][:, 0:128], in_=selbb[:], identity=pj.idb[:]), reads=["selbb", "idb"], writes=["ps6"])
            P.op("act", lambda e, so=so: e.activation(out=qs[64:128, so], in_=psb[6][64:128, 0:128].unsqueeze(1).to_broadcast([64, 4, 128]), func=AF.Copy), reads=["ps6"], writes=[("qs_b", so)])
            P.op("pool", lambda e, so=so, tsl=tsl: e.tensor_copy(out=qs[0:64, so], in_=qrT[:, :, tsl]), reads=[("qrT", qb)], writes=[("qs_a", so)])
            jobs = [("s", c) for c in range(qb + 1)] + [("w", c) for c in range(max(0, qb - 4), qb + 1)]
            pend = None
            first = {"s": True, "w": True}
            last_c = {"s": qb, "w": qb}
            for job in jobs + [(None, None)]:
                kind, c = job
                cur = None
                if kind is not None:
                    sbk = nsc % 2; nsc += 1
                    ksrc = 0 if kind == "s" else 1
                    def sc2(e, kind=kind, c=c, sbk=sbk, ksrc=ksrc, tsl=tsl, qb=qb):
                        extra = []
                        if c == qb:
                            extra.append((pj.idb[:], tri[:, 0, :].unsqueeze(1).to_broadcast([128, 4, 128])))
                        if kind == "w" and c == qb - 4:
                            extra.append((pj.idb[:], tri[:, 1, :].unsqueeze(1).to_broadcast([128, 4, 128])))
                        if kind == "s":
                            ins = e.matmul(ps[sbk][:], lhsT=ksE[:, c * 128:(c + 1) * 128], rhs=qs[:, qb % 2], start=True, stop=(len(extra) == 0))
                        else:
                            ins = e.matmul(ps[sbk][:], lhsT=kT4[:, ksrc, c * 128:(c + 1) * 128], rhs=qrT[:, :, tsl], start=True, stop=(len(extra) == 0))
                        for n_, (l_, r_) in enumerate(extra):
                            ins = e.matmul(ps[sbk][:], lhsT=l_, rhs=r_, start=False, stop=(n_ == len(extra) - 1))
                        return ins
                    P.op("pe", sc2, reads=[("kT4", c), ("ksE", c), "ksE_E", ("qrT", qb), ("qs_a", qb % 2), ("qs_b", qb % 2), "tri", "idb"], writes=[f"ps{sbk}"])
                    P.op("act", lambda e, sbk=sbk: e.activation(out=eT[:, sbk].rearrange("p j t -> p (j t)"), in_=ps[sbk][:], func=AF.Exp, scale=0.125), reads=[f"ps{sbk}"], writes=[("eT", sbk)])
                    cur = (kind, c, sbk)
                if pend is not None:
                    pk, pc, pb = pend
                    bank = 4 if pk == "s" else 5
                    vi = 0 if pk == "s" else 1
                    c0 = 0 if pk == "s" else max(0, qb - 4)
                    def pv2(e, pk=pk, pc=pc, pb=pb, bank=bank, vi=vi, c0=c0, qb=qb):
                        for j in range(4):
                            ins = e.matmul(ps[bank][:, j * 65:(j + 1) * 65], lhsT=eT[:, pb, j, :], rhs=vaug[:, vi, pc, :], start=(pc == c0 and j == 0), stop=(pc == qb), skip_group_check=True)
                        return ins
                    P.op("pe", pv2, reads=[("eT", pb), ("vaug", pc)], writes=[f"ps{bank}"])
                pend = cur
            for j in range(4):
                for (bank, gcol, tag) in ((4, 1, "s"), (5, 2, "w")):
                    if tag not in MC_BR:
                        continue
                    pso = ps[bank][:, j * 65:(j + 1) * 65]
                    ri = sm[:, 8:9]
                    P.op("dve", lambda e, pso=pso, ri=ri: e.reciprocal(out=ri, in_=pso[:, 64:65]), reads=[f"ps{bank}"], writes=["ri2"])
                    P.op("dve", lambda e, ri=ri, j=j, gcol=gcol, qb=qb: e.tensor_tensor(out=ri, in0=ri, in1=gsig[:, qb, 3 * j + gcol:3 * j + gcol + 1], op=ALU.mult), reads=["ri2", ("gsig", qb)], writes=["ri2"])
                    P.op("dve", lambda e, pso=pso, ri=ri, j=j, so=so: e.scalar_tensor_tensor(out=oacc[:, so, j, :], in0=pso[:, 0:64], scalar=ri, in1=oacc[:, so, j, :], op0=ALU.mult, op1=ALU.add),
                         reads=[f"ps{bank}", "ri2", ("oacc", so)], writes=[("oacc", so)])
            P.dma("sp", ysrc[tsl, 256:512], oacc[:, so].rearrange("p j d -> p (j d)"), reads=[("oacc", so)], writes=["ysrc"])
        P.emit(last=is_last)


def nsa_consts():
    import ml_dtypes
    bf = ml_dtypes.bfloat16
    n = np.arange(256)[:, None]
    cmask = np.zeros((NTILE, 128, 2, 128), np.float32)
    for qb in range(NTILE):
        t = qb * 128 + np.arange(128)[None, :]
        ok = (16 * n + 31 <= t) & (n < 255)
        m = np.where(ok, 0.0, NEGB).astype(np.float32)
        cmask[qb] = m.reshape(2, 128, 128).transpose(1, 0, 2)
    selc = np.zeros((NTILE, 128, 2, 64), np.float32)
    mids = np.arange(64)[None, :]
    for qb in range(NTILE):
        t = qb * 128 + np.arange(128)[:, None]
        blk = t // 64
        valid = mids <= blk
        forced = (mids == 0) | (mids == blk) | (mids == blk - 1)
        selc[qb, :, 0, :] = valid.astype(np.float32)
        selc[qb, :, 1, :] = np.where(valid, 1e4 * forced.astype(np.float32), -1e4)
    Ef = (np.arange(S)[None, :] // 64 == np.arange(64)[:, None]).astype(np.float32)
    Eb = np.zeros((64, NTILE, 128), np.float32)
    for c in range(NTILE):
        for sl in range(128):
            Eb[2 * c + sl // 64, c, sl] = 1.0
    s_ = np.arange(128)[:, None]; t_ = np.arange(128)[None, :]
    tri = np.zeros((128, 2, 128), np.float32)
    tri[:, 0, :] = np.where(s_ > t_, NEGB, 0.0)
    tri[:, 1, :] = np.where(s_ <= t_, NEGB, 0.0)
    cmp_idx = np.arange(255)[:, None] * 16 + np.arange(32)[None, :]
    slc_start = np.arange(64) * 64
    overlap = ((cmp_idx[:, :1] < slc_start[None, :] + 64) & (cmp_idx[:, -1:] >= slc_start[None, :])).astype(np.float32)
    rc = np.zeros((256, 65), np.float32)
    rc[:255, :64] = overlap
    rc[:255, 64] = 1.0
    rconst = np.ascontiguousarray(rc.reshape(2, 128, 65).transpose(1, 0, 2))
    inv = (1.0 / (np.float32(500000.0) ** (np.arange(0, 16, 2, dtype=np.float32) / np.float32(16)))).astype(np.float32)
    return {"cmask": cmask.astype(bf), "selc": selc, "Ef": Ef.astype(bf), "tri": tri.astype(bf), "rconst": rconst, "invf": bc128(inv)}


def prep_MC(layer, h, inputs):
    i = layer
    maps = []
    w = inputs["w_in"][i]
    cst = nsa_consts()
    for c in range(8):
        b, gi = c // 2, c % 2
        o = 1536
        cols = np.concatenate([o + np.arange(gi * 256, (gi + 1) * 256),
                               o + 512 + 2 * 128 + np.arange(gi * 64, (gi + 1) * 64),
                               o + 512 + 4 * 128 + np.arange(gi * 64, (gi + 1) * 64),
                               o + 512 + 0 * 128 + np.arange(gi * 64, (gi + 1) * 64),
                               o + 512 + 1 * 128 + np.arange(gi * 64, (gi + 1) * 64),
                               o + 512 + 3 * 128 + np.arange(gi * 64, (gi + 1) * 64),
                               o + 512 + 5 * 128 + np.arange(gi * 64, (gi + 1) * 64),
                               o + 512 + 6 * 128 + np.arange(gi * 12, (gi + 1) * 12)])
        m = {
            "h": np.ascontiguousarray(h[b]), "identf": np.eye(128, dtype=np.float32), "wc": np.ascontiguousarray(w[:, cols]),
            "g_mix": pc8(inputs["g_mix"][i]),
            "pos": np.ascontiguousarray(inputs["positions"][b].reshape(NTILE, 128).T.astype(np.int32)),
            "w1kc": inputs["nsa_kc_w1"][i], "w1vc": inputs["nsa_vc_w1"][i], "w2kc": inputs["nsa_kc_w2"][i], "w2vc": inputs["nsa_vc_w2"][i],
            "posT": np.ascontiguousarray(inputs["nsa_cmp_pos"][i].T),
        }
        m.update(cst)
        maps.append(m)
    return maps


PAIRS = [[0, 1], [2, 3], [4, 5], [6, 7]]


def build_fused():
    nc = bass.Bass("TRN2", target_bir_lowering=False)
    xfull = nc.dram_tensor("xfull", [S, D], F32, kind="ExternalInput").ap()
    xhalf = nc.dram_tensor("xhalf", [TF, D], F32, kind="ExternalInput").ap()
    h_out = nc.dram_tensor("h_out", [TF, D], F32, kind="ExternalOutput").ap()
    ysrc = nc.dram_tensor("ysrc", [S, 512], F32).ap()
    ydst = nc.dram_tensor("ydst", [2 * S, 512], F32).ap()
    hsrc = nc.dram_tensor("hsrc", [TF, D], F32).ap()
    hfull = nc.dram_tensor("hfull", [S, D], F32).ap()
    scr = nc.dram_tensor("scr", [20, S, 64], BF16).ap()
    with ExitStack() as st:
        P = Prog(nc, st)
        ps = [st.enter_context(nc.psum_tensor(f"ps{i}", [128, 512], F32)) for i in range(8)]
        psb = [p_[:].bitcast(BF16) for p_ in ps]
        for layer in range(2):
            moe = layer % 2 == 1
            final = layer == 1
            hin = xfull if layer == 0 else hfull
            hmap = None if layer == 0 else (lambda i: ((i * 128) % 2048) // 512 * 1024 + ((i * 128) // 2048) * 512 + (i * 128) % 512)
            P.barrier()
            phase_MB(nc, P, ps, psb, f"L{layer}B_", hin, ysrc, scr, hmap=hmap)
            P.barrier()
            phase_MC(nc, P, ps, psb, f"L{layer}C_", hin, ysrc, hmap=hmap)
            P.barrier()
            for q in range(4):
                P.collective(lambda e, q=q: e.collective_compute("AllGather", ALU.bypass, replica_groups=PAIRS, ins=[ysrc[q * 1024:(q + 1) * 1024, :]], outs=[ydst[q * 2048:(q + 1) * 2048, :]]),
                             reads=["ysrc"], writes=["ydst"])
            P.emit()
            P.barrier()
            phase_F(nc, P, ps, psb, f"L{layer}F_", 8 if moe else 1, moe, final, xhalf if layer == 0 else hsrc, ydst, h_out if final else hsrc, final)
            if not final:
                P.barrier()
                for q in range(4):
                    P.collective(lambda e, q=q: e.collective_compute("AllGather", ALU.bypass, replica_groups=PAIRS, ins=[hsrc[q * 512:(q + 1) * 512, :]], outs=[hfull[q * 1024:(q + 1) * 1024, :]]),
                                 reads=["f_out"], writes=["hfull"])
                P.emit()
    return nc


W_OUT_PERM = np.concatenate([np.arange(0, 128), np.arange(256, 384), np.arange(512, 768), np.arange(128, 256), np.arange(384, 512), np.arange(768, 1024)])


def kernel(**inputs):
    inputs = {k: np.asarray(v) for k, v in inputs.items()}
    x = np.ascontiguousarray(inputs["x"], dtype=np.float32)
    hd = np.zeros((4, 1, 1), np.float32)
    maps = [dict() for _ in range(8)]
    for layer in range(2):
        moe = layer % 2 == 1
        final = layer == 1
        for tag, prep in (("B", prep_MB), ("C", prep_MC)):
            pm = prep(layer, hd, inputs)
            for c in range(8):
                for k, v in pm[c].items():
                    if k != "h":
                        maps[c][f"L{layer}{tag}_{k}"] = v
        dummy = np.zeros((8 * TF, 1), np.float32)
        pf = prep_F_inputs(layer, dummy, dummy, inputs, moe, final)
        wperm = np.ascontiguousarray(inputs["w_out"][layer][W_OUT_PERM, :])
        for c in range(8):
            for k, v in pf[c].items():
                if k in ("h", "y"):
                    continue
                maps[c][f"L{layer}F_{k}"] = wperm if k == "w_out" else v
            sel = np.zeros((128, 2), np.float32)
            sel[:, c % 2] = 1.0
            maps[c][f"L{layer}F_selv"] = sel
    for c in range(8):
        b, gi = c // 2, c % 2
        maps[c]["xfull"] = x[b]
        maps[c]["xhalf"] = np.ascontiguousarray(x[b, gi * TF:(gi + 1) * TF])
    nc = build_fused()
    res = run_bass_kernel_spmd(nc, maps, core_ids=list(range(8))).results
    out = np.empty((4, S, D), np.float32)
    for c in range(8):
        b, gi = c // 2, c % 2
        out[b, gi * TF:(gi + 1) * TF] = res[c]["h_out"]
    return out
```

```python
import numpy as np
from contextlib import ExitStack
import concourse.bass as bass
import concourse.mybir as mybir
from concourse.bass_utils import run_bass_kernel_spmd

F32 = mybir.dt.float32
BF16 = mybir.dt.bfloat16
I32 = mybir.dt.int32
AF = mybir.ActivationFunctionType
ALU = mybir.AluOpType
AX = mybir.AxisListType

EPOCH = 20000
NDMA = 24


class Prog:
    ENGS = ("pe", "act", "dve", "pool", "sp")

    def __init__(self, nc, stack):
        self.nc = nc
        self.stack = stack
        self.ops = {e: [] for e in self.ENGS}
        self.count = {e: 0 for e in self.ENGS}
        self.sems = {}
        self.seen = {e: {} for e in self.ENGS}
        self.lastw = {}
        self.readers = {}
        self.ndma = 0
        self.dma_last = {}
        self.final_tokens = []
        self.barrier_toks = []

    def sem(self, key):
        if key not in self.sems:
            self.sems[key] = self.stack.enter_context(self.nc.semaphore("s_" + "_".join(map(str, key))))
        return self.sems[key]

    def barrier(self):
        toks = []
        for e in self.ENGS:
            n = self.count[e]
            if n > 0:
                ep, v = divmod(n - 1, EPOCH)
                toks.append((("c", e, ep), v + 1))
        for si, val in self.dma_last.items():
            toks.append((("d", si), val))
        toks.extend(getattr(self, "cc_toks", []))
        self.barrier_toks = toks

    def _deps(self, eng, reads, writes):
        toks = list(self.barrier_toks)
        for k in reads:
            if k in self.lastw:
                toks.append(self.lastw[k])
        for k in writes:
            if k in self.lastw:
                toks.append(self.lastw[k])
            toks.extend(self.readers.get(k, ()))
        need = {}
        for (sk, val) in toks:
            if self.seen[eng].get(sk, 0) >= val:
                continue
            if need.get(sk, 0) < val:
                need[sk] = val
        for sk, val in need.items():
            self.seen[eng][sk] = val
        return list(need.items())

    def _record(self, tok, reads, writes):
        for k in writes:
            self.lastw[k] = tok
            self.readers[k] = []
        for k in reads:
            if k in writes:
                continue
            self.readers.setdefault(k, []).append(tok)

    def op(self, eng, fn, reads=(), writes=(), same_ok=False):
        waits = self._deps(eng, reads, writes)
        if same_ok:
            waits = [(wk, v) for (wk, v) in waits if not (wk[0] == "c" and wk[1] == eng)]
        n = self.count[eng]
        ep, v = divmod(n, EPOCH)
        sk = ("c", eng, ep)
        self.sem(sk)
        tok = (sk, v + 1)
        self.count[eng] = n + 1
        self.ops[eng].append((fn, waits, sk, 1))
        self._record(tok, reads, writes)
        return tok

    def dma(self, eng, out, in_, reads=(), writes=(), **kw):
        i = self.ndma
        self.ndma += 1
        si = i % NDMA
        sk = ("d", si)
        self.sem(sk)
        prev = self.dma_last.get(si, 0)
        waits = self._deps(eng, reads, writes)
        if prev > 0 and self.seen[eng].get(sk, 0) < prev:
            waits.append((sk, prev))
            self.seen[eng][sk] = prev
        val = prev + 16
        self.dma_last[si] = val
        tok = (sk, val)

        def fn(e, out=out, in_=in_, kw=kw):
            return e.dma_start(out=out, in_=in_, **kw)
        self.ops[eng].append((fn, waits, sk, 16))
        self._record(tok, reads, writes)
        return tok

    def collective(self, fn, reads=(), writes=()):
        k = getattr(self, "ncc", 0)
        self.ncc = k + 1
        sk = ("cc", k)
        self.sem(sk)
        waits = self._deps("pool", reads, writes)
        self.ops["pool"].append((fn, waits, sk, None))
        tok = (sk, 1)
        self._record(tok, reads, writes)
        self.cc_toks = getattr(self, "cc_toks", []) + [tok]
        return tok

    def emit(self, last=False, final_waits_eng="sp"):
        nc = self.nc
        fin = []
        if last:
            for tok in self.final_tokens:
                fin.append(tok)
        with nc.Block() as block:
            def mk(engname):
                def body(e):
                    for (fn, waits, sk, inc) in self.ops[engname]:
                        for (wk, val) in waits:
                            e.wait_ge(self.sems[wk], val)
                        inst = fn(e)
                        if inc is None:
                            inst.then_inc(self.sems[sk])
                        else:
                            inst.then_inc(self.sems[sk], inc)
                    if engname == final_waits_eng:
                        for (wk, val) in fin:
                            e.wait_ge(self.sems[wk], val)
                return body
            block.tensor(mk("pe"))
            block.scalar(mk("act"))
            block.vector(mk("dve"))
            block.gpsimd(mk("pool"))
            block.sync(mk("sp"))
        self.ops = {e: [] for e in self.ENGS}


D = 1024
DFF = 2816
NFC = 22
TF = 2048
HALF = 1024
NT = 8


def phase_F(nc, P, ps, psb, pre, E, moe, final, h_in, ydst, out_ap, is_last):
    di = lambda n, s, dt=F32: nc.dram_tensor(pre + n, s, dt, kind="ExternalInput").ap()
    p_in = di("p", [TF, 256])
    w_out = di("w_out", [D, D])
    g_ffn = di("g_ffn", [128, 8])
    w1 = di("w1", [E, NFC, 128, 8, 128])
    w3 = di("w3", [E, NFC, 128, 8, 128])
    w2 = di("w2", [E, DFF, D])
    g_ple = di("g_ple", [128, 8])
    ple_gate = di("ple_gate", [D, D])
    ple_proj = di("ple_proj", [256, D])
    identf = di("identf", [128, 128])
    selv_in = di("selv", [128, 2])
    if moe:
        rw = di("rw", [128, 8, 8])
        rb = di("rb", [128, 8])
    if final:
        g_fin = di("g_fin", [128, D])

    with ExitStack() as st:
        sb = lambda name, shape, dt: st.enter_context(nc.sbuf_tensor(pre + "s_" + name, shape, dt))
        hacc = sb("hacc", [128, NT, D], F32)
        hnT = sb("hnT", [128, 8, HALF], BF16)
        actT = sb("actT", [128, NFC * HALF], BF16)
        w2b = sb("w2b", [128, NFC * D], BF16)
        stg = sb("stg", [128, 4, 8, 128], F32)
        w13b = sb("w13b", [128, 4, 8, 128], BF16)
        stg2 = sb("stg2", [128, 2, D], F32)
        idf = sb("idf", [128, 128], F32)
        idb = sb("idb", [128, 128], BF16)
        gf = sb("gf", [128, 8], F32)
        gp = sb("gp", [128, 8], F32)
        ss = sb("ss", [128, 4], F32)
        epsf = sb("epsf", [128, 1], F32)
        P.op("pool", lambda e: e.memset(epsf[:], 1e-6), writes=["epsf"])
        gates = sb("gates", [128, NT, 8], F32)
        sm = sb("sm", [128, 64], F32)
        silu = sb("silu", [128, 2, 512], BF16)
        if moe:
            rws = sb("rws", [128, 8, 8], F32)
            rbs = sb("rbs", [128, 8], F32)
        if final:
            gfin = sb("gfin", [128, D], F32)

        wsm = sb("wsm", [128, 16 * D], BF16)
        woutb = wsm[:, 0:8 * D].rearrange("p (c n) -> p c n", c=8)
        pgb = wsm[:, 8 * D:16 * D].rearrange("p (c n) -> p c n", c=8)
        ppb = actT[:, 0:2 * D].rearrange("p (c n) -> p c n", c=2)
        o = 0
        def carve(nbytes_bf16, dt, pat=None, **kw):
            nonlocal o
            v = w2b[:, o:o + nbytes_bf16]
            o += nbytes_bf16
            if dt == F32:
                v = v.bitcast(F32)
            if pat:
                v = v.rearrange(pat, **kw)
            return v
        xs = carve(2 * D, F32)
        ys = carve(2 * D, F32)
        ys2 = carve(2 * D, F32)
        yb = carve(D, BF16)
        yT = carve(D, BF16, "p (c t) -> p c t", c=8)
        hnb = carve(D, BF16)
        hn32 = carve(2 * D, F32)
        hnT32 = carve(2 * D, F32, "p (c t) -> p c t", c=8)
        pst = carve(2 * 256, F32)
        pbf = carve(256, BF16)
        pT = carve(256, BF16, "p (c t) -> p c t", c=2)
        gsb = carve(2 * D, F32)
        junk = carve(2 * D, F32)
        osb = carve(2 * D, F32)

        P.dma("sp", idf[:], identf, writes=["idf"])
        P.dma("sp", gf[:], g_ffn, writes=["gf"])
        P.dma("sp", gp[:], g_ple, writes=["gp"])
        selv = sb("selv", [128, 2], F32)
        P.dma("sp", selv[:], selv_in, writes=["selv"])
        P.op("dve", lambda e: e.tensor_copy(out=idb[:], in_=idf[:]), reads=["idf"], writes=["idb"])
        if moe:
            P.dma("sp", rws[:], rw, writes=["rws"])
            P.dma("sp", rbs[:], rb, writes=["rbs"])
            P.op("dve", lambda e: e.tensor_tensor(out=rws[:], in0=rws[:], in1=gf[:].unsqueeze(2).to_broadcast([128, 8, 8]), op=ALU.mult),
                 reads=["rws", "gf"], writes=["rws"])
        if final:
            P.dma("sp", gfin[:], g_fin, writes=["gfin"])

        def load_w_bf16(dst, src, nchunks, gscale, tag):
            for c in range(nchunks):
                s = c % 2
                P.dma("sp", stg2[:, s, :], src[c * 128:(c + 1) * 128, :], writes=[("stg2", s)])
                if gscale is not None:
                    P.op("pool", lambda e, c=c, s=s: e.tensor_scalar(out=dst[:, c, :], in0=stg2[:, s, :], scalar1=gscale[:, c:c + 1], scalar2=None, op0=ALU.mult),
                         reads=[("stg2", s), "gp"], writes=[tag])
                else:
                    P.op("pool", lambda e, c=c, s=s: e.tensor_copy(out=dst[:, c, :], in_=stg2[:, s, :]), reads=[("stg2", s)], writes=[tag])

        def rms(src_ap, key, col):
            P.op("act", lambda e: e.activation(out=junk, in_=src_ap, func=AF.Square, accum_out=ss[:, col:col + 1]), reads=[key], writes=["junk", ("ss", col)])
            P.op("act", lambda e: e.activation(out=ss[:, col:col + 1], in_=ss[:, col:col + 1], func=AF.Sqrt, scale=1.0 / D, bias=epsf[:]), reads=[("ss", col), "epsf"], writes=[("ss", col)])
            P.op("dve", lambda e: e.reciprocal(out=ss[:, col:col + 1], in_=ss[:, col:col + 1]), reads=[("ss", col)], writes=[("ss", col)])

        def transposes(psbank, src_bf, n, ident, srckey, pskey):
            def f(e):
                for c in range(n):
                    i = e.transpose(out=psbank[:, c * 128:(c + 1) * 128], in_=src_bf[:, c * 128:(c + 1) * 128], identity=ident)
                return i
            P.op("pe", f, reads=[srckey, "idb", "idf"], writes=[pskey])

        for half in range(2):
            tb = half * HALF
            P.barrier()
            if half == 0:
                load_w_bf16(woutb, w_out, 8, None, "woutb")
                load_w_bf16(pgb, ple_gate, 8, gp, "pgb")
            for i in range(NT):
                t0 = tb + i * 128
                P.dma("sp", xs, h_in[t0:t0 + 128, :], writes=["xs"])
                tl = i * 128 + half * HALF
                for k_, yk in ((0, ys), (1, ys2)):
                    for r_ in range(2):
                        tt = k_ * TF + tl
                        row = (tt // 1024) * 2048 + r_ * 1024 + (tt % 1024)
                        P.dma("sp", yk[:, r_ * 512:(r_ + 1) * 512], ydst[row:row + 128, :], writes=["ys" if k_ == 0 else "ys2"])
                P.op("dve", lambda e: e.tensor_scalar(out=ys, in0=ys, scalar1=selv[:, 0:1], scalar2=None, op0=ALU.mult), reads=["ys", "selv"], writes=["ys"])
                P.op("dve", lambda e: e.scalar_tensor_tensor(out=ys, in0=ys2, scalar=selv[:, 1:2], in1=ys, op0=ALU.mult, op1=ALU.add), reads=["ys", "ys2", "selv"], writes=["ys"])
                P.op("pool", lambda e: e.tensor_copy(out=yb, in_=ys), reads=["ys"], writes=["yb"])
                transposes(psb[0], yb, 8, idb[:], "yb", "ps0")
                P.op("act", lambda e: e.activation(out=yT.rearrange("p c t -> p (c t)"), in_=psb[0], func=AF.Copy), reads=["ps0"], writes=["yT"])
                for hf in range(2):
                    def mm(e, hf=hf):
                        for c in range(8):
                            ins = e.matmul(ps[1 + hf][:], lhsT=yT[:, c, :], rhs=woutb[:, c, hf * 512:(hf + 1) * 512], start=(c == 0), stop=(c == 7))
                        return ins
                    P.op("pe", mm, reads=["yT", "woutb"], writes=[f"ps{1 + hf}"])
                    P.op("dve", lambda e, hf=hf, i=i: e.tensor_tensor(out=hacc[:, i, hf * 512:(hf + 1) * 512], in0=ps[1 + hf][:], in1=xs[:, hf * 512:(hf + 1) * 512], op=ALU.add),
                         reads=[f"ps{1 + hf}", "xs"], writes=[("hacc", i)])
                rms(hacc[:, i, :], ("hacc", i), 0)
                P.op("dve", lambda e, i=i: e.tensor_scalar(out=hnb, in0=hacc[:, i, :], scalar1=ss[:, 0:1], scalar2=None, op0=ALU.mult),
                     reads=[("hacc", i), ("ss", 0)], writes=["hnb"])
                transposes(psb[3], hnb, 8, idb[:], "hnb", "ps3")
                P.op("act", lambda e, i=i: e.activation(out=hnT[:, :, i * 128:(i + 1) * 128], in_=psb[3].rearrange("p (c t) -> p c t", c=8), func=AF.Copy),
                     reads=["ps3"], writes=[("hnT", i)])
                if moe:
                    P.op("pool", lambda e, i=i: e.tensor_scalar(out=hn32, in0=hacc[:, i, :], scalar1=ss[:, 0:1], scalar2=None, op0=ALU.mult),
                         reads=[("hacc", i), ("ss", 0)], writes=["hn32"])
                    for q in range(2):
                        def trf(e, q=q):
                            for c in range(4):
                                cc = q * 4 + c
                                ins = e.transpose(out=ps[4 + q][:, c * 128:(c + 1) * 128], in_=hn32[:, cc * 128:(cc + 1) * 128], identity=idf[:])
                            return ins
                        P.op("pe", trf, reads=["hn32", "idf"], writes=[f"ps{4 + q}"])
                        P.op("act", lambda e, q=q: e.activation(out=hnT32[:, q * 4:(q + 1) * 4, :], in_=ps[4 + q][:].rearrange("p (c t) -> p c t", c=4), func=AF.Copy),
                             reads=[f"ps{4 + q}"], writes=[("hnT32", q)])
                    def mml(e):
                        for c in range(8):
                            ins = e.matmul(ps[6][:, 0:8], lhsT=hnT32[:, c, :], rhs=rws[:, c, :], start=(c == 0), stop=(c == 7))
                        return ins
                    P.op("pe", mml, reads=[("hnT32", 0), ("hnT32", 1), "rws"], writes=["ps6"])
                    lg = sm[:, 0:8]
                    mx = sm[:, 8:16]
                    dd = sm[:, 16:17]
                    p1 = sm[:, 17:18]
                    p2 = sm[:, 18:19]
                    t1 = sm[:, 24:32]
                    P.op("dve", lambda e: e.tensor_tensor(out=lg, in0=ps[6][:, 0:8], in1=rbs[:], op=ALU.add), reads=["ps6", "rbs"], writes=["lg"])
                    P.op("dve", lambda e: e.max(out=mx, in_=lg), reads=["lg"], writes=["mx"])
                    P.op("dve", lambda e: e.tensor_tensor(out=dd, in0=mx[:, 1:2], in1=mx[:, 0:1], op=ALU.subtract), reads=["mx"], writes=["dd"])
                    P.op("act", lambda e: e.activation(out=dd, in_=dd, func=AF.Exp), reads=["dd"], writes=["dd"])
                    P.op("dve", lambda e: e.tensor_scalar(out=p1, in0=dd, scalar1=1.0, scalar2=None, op0=ALU.add), reads=["dd"], writes=["p1"])
                    P.op("dve", lambda e: e.reciprocal(out=p1, in_=p1), reads=["p1"], writes=["p1"])
                    P.op("dve", lambda e: e.tensor_tensor(out=p2, in0=dd, in1=p1, op=ALU.mult), reads=["dd", "p1"], writes=["p2"])
                    P.op("dve", lambda e: e.tensor_scalar(out=t1, in0=lg, scalar1=mx[:, 0:1], scalar2=p1, op0=ALU.is_equal, op1=ALU.mult),
                         reads=["lg", "mx", "p1"], writes=["t1"])
                    P.op("dve", lambda e, i=i: e.tensor_scalar(out=gates[:, i, :], in0=lg, scalar1=mx[:, 1:2], scalar2=p2, op0=ALU.is_equal, op1=ALU.mult),
                         reads=["lg", "mx", "p2"], writes=[("gates", i)])
                    P.op("dve", lambda e, i=i: e.tensor_tensor(out=gates[:, i, :], in0=gates[:, i, :], in1=t1, op=ALU.add),
                         reads=[("gates", i), "t1"], writes=[("gates", i)])

            P.barrier()
            for ex in range(E):
                nslot = 0
                for fc in range(NFC):
                    s = fc % 2
                    P.dma("sp", stg[:, s, :, :], w1[ex, fc], writes=[("stg", s)])
                    P.dma("sp", stg[:, 2 + s, :, :], w3[ex, fc], writes=[("stg", 2 + s)])
                    gb = gf[:].unsqueeze(2).to_broadcast([128, 8, 128])
                    P.op("pool", lambda e, s=s: e.tensor_tensor(out=w13b[:, s, :, :], in0=stg[:, s, :, :], in1=gb, op=ALU.mult),
                         reads=[("stg", s), "gf"], writes=[("w13b", s)])
                    P.op("pool", lambda e, s=s: e.tensor_tensor(out=w13b[:, 2 + s, :, :], in0=stg[:, 2 + s, :, :], in1=gb, op=ALU.mult),
                         reads=[("stg", 2 + s), "gf"], writes=[("w13b", 2 + s)])
                    P.dma("sp", stg2[:, s, :], w2[ex, fc * 128:(fc + 1) * 128, :], writes=[("stg2", s)])
                    P.op("act", lambda e, s=s, fc=fc: e.activation(out=w2b[:, fc * D:(fc + 1) * D], in_=stg2[:, s, :], func=AF.Copy),
                         reads=[("stg2", s)], writes=[("w2b", fc)])
                    for g in range(2):
                        def mm13(e, g=g, s=s):
                            for wi in range(2):
                                for c in range(8):
                                    ins = e.matmul(ps[2 * g + wi][:], lhsT=w13b[:, 2 * wi + s, c, :], rhs=hnT[:, c, g * 512:(g + 1) * 512], start=(c == 0), stop=(c == 7))
                            return ins
                        P.op("pe", mm13, reads=[("w13b", s), ("w13b", 2 + s)] + [("hnT", i) for i in range(NT)], writes=[f"ps{2 * g}", f"ps{2 * g + 1}"])
                        P.op("act", lambda e, g=g: e.activation(out=silu[:, g, :], in_=ps[2 * g][:], func=AF.Silu), reads=[f"ps{2 * g}"], writes=[("silu", g)])
                        P.op("dve", lambda e, g=g, fc=fc: e.tensor_tensor(out=actT[:, fc * HALF + g * 512: fc * HALF + (g + 1) * 512], in0=silu[:, g, :], in1=ps[2 * g + 1][:], op=ALU.mult),
                             reads=[("silu", g), f"ps{2 * g + 1}"], writes=[("actT", fc)])
                for i in range(NT):
                    for hf in range(2):
                        b = 4 + (nslot % 4)
                        nslot += 1
                        def mm2(e, i=i, hf=hf, b=b):
                            for fc in range(NFC):
                                ins = e.matmul(ps[b][:], lhsT=actT[:, fc * HALF + i * 128: fc * HALF + (i + 1) * 128], rhs=w2b[:, fc * D + hf * 512: fc * D + (hf + 1) * 512],
                                               start=(fc == 0), stop=(fc == NFC - 1))
                            return ins
                        P.op("pe", mm2, reads=[("actT", fc) for fc in range(NFC)] + [("w2b", fc) for fc in range(NFC)], writes=[f"ps{b}"])
                        if moe:
                            P.op("dve", lambda e, i=i, hf=hf, b=b, ex=ex: e.scalar_tensor_tensor(out=hacc[:, i, hf * 512:(hf + 1) * 512], in0=ps[b][:], scalar=gates[:, i, ex:ex + 1],
                                                                                               in1=hacc[:, i, hf * 512:(hf + 1) * 512], op0=ALU.mult, op1=ALU.add),
                                 reads=[f"ps{b}", ("gates", i), ("hacc", i)], writes=[("hacc", i)])
                        else:
                            P.op("dve", lambda e, i=i, hf=hf, b=b: e.tensor_tensor(out=hacc[:, i, hf * 512:(hf + 1) * 512], in0=ps[b][:], in1=hacc[:, i, hf * 512:(hf + 1) * 512], op=ALU.add),
                                 reads=[f"ps{b}", ("hacc", i)], writes=[("hacc", i)])

            P.barrier()
            load_w_bf16(ppb, ple_proj, 2, None, "ppb")
            for i in range(NT):
                t0 = tb + i * 128
                P.dma("sp", pst, p_in[t0:t0 + 128, :], writes=["pst"])
                P.op("pool", lambda e: e.tensor_copy(out=pbf, in_=pst), reads=["pst"], writes=["pbf"])
                transposes(psb[0], pbf, 2, idb[:], "pbf", "ps0")
                P.op("act", lambda e: e.activation(out=pT.rearrange("p c t -> p (c t)"), in_=psb[0][:, 0:256], func=AF.Copy), reads=["ps0"], writes=["pT"])
                rms(hacc[:, i, :], ("hacc", i), 1)
                P.op("dve", lambda e, i=i: e.tensor_scalar(out=hnb, in0=hacc[:, i, :], scalar1=ss[:, 1:2], scalar2=None, op0=ALU.mult),
                     reads=[("hacc", i), ("ss", 1)], writes=["hnb"])
                transposes(psb[3], hnb, 8, idb[:], "hnb", "ps3")
                P.op("act", lambda e: e.activation(out=yT.rearrange("p c t -> p (c t)"), in_=psb[3], func=AF.Copy), reads=["ps3"], writes=["yT"])
                for hf in range(2):
                    def mmg(e, hf=hf):
                        for c in range(8):
                            ins = e.matmul(ps[1 + hf][:], lhsT=yT[:, c, :], rhs=pgb[:, c, hf * 512:(hf + 1) * 512], start=(c == 0), stop=(c == 7))
                        return ins
                    P.op("pe", mmg, reads=["yT", "pgb"], writes=[f"ps{1 + hf}"])
                    P.op("act", lambda e, hf=hf: e.activation(out=gsb[:, hf * 512:(hf + 1) * 512], in_=ps[1 + hf][:], func=AF.Sigmoid), reads=[f"ps{1 + hf}"], writes=[("gsb", hf)])
                    def mmp(e, hf=hf):
                        for c in range(2):
                            ins = e.matmul(ps[4 + hf][:], lhsT=pT[:, c, :], rhs=ppb[:, c, hf * 512:(hf + 1) * 512], start=(c == 0), stop=(c == 1))
                        return ins
                    P.op("pe", mmp, reads=["pT", "ppb"], writes=[f"ps{4 + hf}"])
                    P.op("dve", lambda e, hf=hf: e.tensor_tensor(out=gsb[:, hf * 512:(hf + 1) * 512], in0=gsb[:, hf * 512:(hf + 1) * 512], in1=ps[4 + hf][:], op=ALU.mult),
                         reads=[("gsb", hf), f"ps{4 + hf}"], writes=[("gsb", hf)])
                P.op("dve", lambda e, i=i: e.tensor_tensor(out=osb, in0=gsb, in1=hacc[:, i, :], op=ALU.add), reads=[("gsb", 0), ("gsb", 1), ("hacc", i)], writes=["osb"])
                if final:
                    rms(osb, "osb", 2)
                    P.op("dve", lambda e: e.scalar_tensor_tensor(out=osb, in0=osb, scalar=ss[:, 2:3], in1=gfin[:], op0=ALU.mult, op1=ALU.mult),
                         reads=["osb", ("ss", 2), "gfin"], writes=["osb"])
                tk = P.dma("sp", out_ap[t0:t0 + 128, :], osb, reads=["osb"], writes=["f_out"])
                if is_last:
                    P.final_tokens.append(tk)
        P.emit(last=is_last)


def prep_F_inputs(layer, hs, ys, inputs, moe, final):
    i = layer
    j = i // 2
    def tochunks(w):
        E = w.shape[0]
        return np.ascontiguousarray(w.reshape(E, 8, 128, NFC, 128).transpose(0, 3, 2, 1, 4))
    if moe:
        w1, w3, w2 = inputs["moe_w1"][j], inputs["moe_w3"][j], inputs["moe_w2"][j]
    else:
        w1, w3, w2 = inputs["ffn_w1"][j][None], inputs["ffn_w3"][j][None], inputs["ffn_w2"][j][None]
    pc = lambda g: np.ascontiguousarray(g.reshape(8, 128).T)
    common = {
        "w_out": inputs["w_out"][i], "g_ffn": pc(inputs["g_ffn"][i]), "w1": tochunks(w1), "w3": tochunks(w3), "w2": np.ascontiguousarray(w2),
        "g_ple": pc(inputs["g_ple"][i]), "ple_gate": inputs["ple_gate_w"][i], "ple_proj": inputs["ple_proj_w"][i],
        "identf": np.eye(128, dtype=np.float32),
    }
    if moe:
        common["rw"] = np.ascontiguousarray(inputs["router_w"][j].reshape(8, 128, 8).transpose(1, 0, 2))
        common["rb"] = np.ascontiguousarray(np.broadcast_to(inputs["router_b"][j][None, :], (128, 8)))
    if final:
        common["g_fin"] = np.ascontiguousarray(np.broadcast_to(inputs["g_final"][None, :], (128, D)))
    pl = inputs["p"][i].reshape(-1, 256)
    maps = []
    for c in range(8):
        m = dict(common)
        m["h"] = np.ascontiguousarray(hs[c * TF:(c + 1) * TF])
        m["y"] = np.ascontiguousarray(ys[c * TF:(c + 1) * TF])
        m["p"] = np.ascontiguousarray(pl[c * TF:(c + 1) * TF])
        maps.append(m)
    return maps


S = 4096
NTILE = 32


class Proj:
    def __init__(self, nc, P, st, h_in, identf, ncol, w_dram, g_dram, shift_cols=None, mu_dram=None, pre="", hmap=None):
        self.nc, self.P = nc, P
        self.hmap = hmap if hmap is not None else (lambda i: i * 128)
        sb = lambda name, shape, dt: st.enter_context(nc.sbuf_tensor(pre + "s_" + name, shape, dt))
        self.h_in = h_in
        self.xs = sb("pj_xs", [128, 2, D], F32)
        self.junk = sb("pj_junk", [128, D], F32)
        self.ss = sb("pj_ss", [128, 2], F32)
        self.hnb = sb("pj_hnb", [128, D], BF16)
        self.hnT = sb("pj_hnT", [128, 3, 8, 129], BF16)
        self.idf = sb("pj_idf", [128, 128], F32)
        self.idb = sb("pj_idb", [128, 128], BF16)
        self.g = sb("pj_g", [128, 8], F32)
        self.eps = sb("pj_eps", [128, 1], F32)
        P.op("pool", lambda e: e.memset(self.eps[:], 1e-6), writes=["pj_eps"])
        self.wb = sb("pj_wb", [128, 8, ncol], BF16)
        self.ncol = ncol
        stg = sb("pj_stg", [128, 2, ncol], F32)
        P.dma("sp", self.idf[:], identf, writes=["idf"])
        P.dma("sp", self.g[:], g_dram, writes=["pj_g"])
        P.op("dve", lambda e: e.tensor_copy(out=self.idb[:], in_=self.idf[:]), reads=["idf"], writes=["idb"])
        P.op("pool", lambda e: e.memset(self.hnT[:], 0.0), writes=[("hnT", 0), ("hnT", 1), ("hnT", 2)])
        for c in range(8):
            s = c % 2
            P.dma("sp", stg[:, s, :], w_dram[c * 128:(c + 1) * 128, :], writes=[("pj_stg", s)])
            P.op("pool", lambda e, c=c, s=s: e.tensor_scalar(out=self.wb[:, c, :], in0=stg[:, s, :], scalar1=self.g[:, c:c + 1], scalar2=None, op0=ALU.mult),
                 reads=[("pj_stg", s), "pj_g"], writes=["pj_wb"])
        if shift_cols is not None:
            a, b = shift_cols
            n = b - a
            self.wprev = sb("pj_wprev", [128, 8, n], BF16)
            mu = sb("pj_mu", [128, n], F32)
            P.dma("sp", mu[:], mu_dram, writes=["pj_mu"])
            mub = mu[:].unsqueeze(1).to_broadcast([128, 8, n])
            P.op("dve", lambda e: e.tensor_tensor(out=self.wprev[:], in0=self.wb[:, :, a:b], in1=mub, op=ALU.mult), reads=["pj_wb", "pj_mu"], writes=["pj_wprev"])
            P.op("dve", lambda e: e.tensor_tensor(out=self.wb[:, :, a:b], in0=self.wb[:, :, a:b], in1=self.wprev[:], op=ALU.subtract), reads=["pj_wb", "pj_wprev"], writes=["pj_wb"])
        self.shift_cols = shift_cols

    def tile(self, i, psbank_bf, pskey):
        P = self.P
        s = i % 2
        xs = self.xs[:, s, :]
        r0 = self.hmap(i)
        P.dma("sp", xs, self.h_in[r0:r0 + 128, :], writes=[("pj_xs", s)])
        ssc = self.ss[:, s:s + 1]
        k = ("pj_ss", s)
        P.op("act", lambda e: e.activation(out=self.junk[:], in_=xs, func=AF.Square, accum_out=ssc), reads=[("pj_xs", s)], writes=["pj_junk", k])
        P.op("act", lambda e: e.activation(out=ssc, in_=ssc, func=AF.Sqrt, scale=1.0 / D, bias=self.eps[:]), reads=[k, "pj_eps"], writes=[k])
        P.op("dve", lambda e: e.reciprocal(out=ssc, in_=ssc), reads=[k], writes=[k])
        P.op("dve", lambda e: e.tensor_scalar(out=self.hnb[:], in0=xs, scalar1=ssc, scalar2=None, op0=ALU.mult), reads=[("pj_xs", s), k], writes=["pj_hnb"])

        def tr(e):
            for c in range(8):
                ins = e.transpose(out=psbank_bf[:, c * 128:(c + 1) * 128], in_=self.hnb[:, c * 128:(c + 1) * 128], identity=self.idb[:])
            return ins
        P.op("pe", tr, reads=["pj_hnb", "idb"], writes=[pskey])
        sh, sn = i % 3, (i + 1) % 3
        P.op("act", lambda e: e.activation(out=self.hnT[:, sh, :, 1:129], in_=psbank_bf.rearrange("p (c t) -> p c t", c=8), func=AF.Copy), reads=[pskey], writes=[("hnT", sh)])
        P.op("pool", lambda e: e.tensor_copy(out=self.hnT[:, sn, :, 0:1], in_=self.hnT[:, sh, :, 128:129]), reads=[("hnT", sh)], writes=[("hnT", sn)])

    def mm_tok(self, e, i, ps_ap, c0, c1, start=True, stop=True):
        s = i % 3
        sh = self.shift_cols is not None and c0 >= self.shift_cols[0] and c1 <= self.shift_cols[1]
        n = 16 if sh else 8
        k = 0
        for c in range(8):
            ins = e.matmul(ps_ap, lhsT=self.hnT[:, s, c, 1:129], rhs=self.wb[:, c, c0:c1], start=(start and k == 0), stop=(stop and k == n - 1))
            k += 1
        if sh:
            a = self.shift_cols[0]
            for c in range(8):
                ins = e.matmul(ps_ap, lhsT=self.hnT[:, s, c, 0:128], rhs=self.wprev[:, c, c0 - a:c1 - a], start=False, stop=(stop and k == n - 1))
                k += 1
        return ins

    def mm_feat(self, e, i, ps_ap, c0, c1):
        s = i % 3
        sh = self.shift_cols is not None and c0 >= self.shift_cols[0] and c1 <= self.shift_cols[1]
        n = 16 if sh else 8
        k = 0
        for c in range(8):
            ins = e.matmul(ps_ap, lhsT=self.wb[:, c, c0:c1], rhs=self.hnT[:, s, c, 1:129], start=(k == 0), stop=(k == n - 1))
            k += 1
        if sh:
            a = self.shift_cols[0]
            for c in range(8):
                ins = e.matmul(ps_ap, lhsT=self.wprev[:, c, c0 - a:c1 - a], rhs=self.hnT[:, s, c, 0:128], start=False, stop=(k == n - 1))
                k += 1
        return ins

    def keys(self, i):
        return [("hnT", i % 3), "pj_wb", "pj_wprev"]


def pc8(g):
    return np.ascontiguousarray(np.asarray(g).reshape(8, 128).T)


def bc128(v):
    v = np.asarray(v, dtype=np.float32).reshape(1, -1)
    return np.ascontiguousarray(np.broadcast_to(v, (128, v.shape[1])))


class MAops:
    def __init__(self, nc, P, st, pre, pj, c0, psA, keyA, psB, keyB, ysrc):
        self.P, self.pj, self.c0, self.psA, self.keyA, self.psB, self.keyB, self.ysrc = P, pj, c0, psA, keyA, psB, keyB, ysrc
        di = lambda n, s, dt=F32: nc.dram_tensor(pre + n, s, dt, kind="ExternalInput").ap()
        sb = lambda name, shape, dt: st.enter_context(nc.sbuf_tensor(pre + "s_" + name, shape, dt))
        lng = di("lng", [128, 128]); lnb = di("lnb", [128, 128]); ws = di("ws", [2, 128, 128]); tril = di("tril", [128, 128]); bs = di("bs", [128, 2])
        self.lngs = sb("lngs", [128, 128], F32); self.lnbs = sb("lnbs", [128, 128], F32)
        wss = sb("wss", [128, 2, 128], F32); trl = sb("trl", [128, 128], F32)
        self.wT = sb("wT", [128, 2, 128], BF16); self.bss = sb("bss", [128, 2], F32)
        self.uv = sb("uv", [128, 2, 256], F32); self.st1 = sb("st1", [128, 2, 8], F32)
        self.vc = sb("vc", [128, 2, 2, 64], F32); self.sq = sb("sq", [128, 2, 64], F32)
        self.vn = sb("vn", [128, 2, 2, 64], BF16); self.yo = sb("yo", [128, 2, 128], F32)
        P.dma("sp", self.lngs[:], lng, writes=["a_lngs"]); P.dma("sp", self.lnbs[:], lnb, writes=["a_lnbs"])
        P.dma("sp", trl[:], tril, writes=["a_trl"]); P.dma("sp", self.bss[:], bs, writes=["a_bss"])
        for hh in range(2):
            P.dma("sp", wss[:, hh, :], ws[hh], writes=[("a_wss", hh)])
            P.op("dve", lambda e, hh=hh: e.tensor_tensor(out=wss[:, hh, :], in0=wss[:, hh, :], in1=trl[:], op=ALU.mult), reads=[("a_wss", hh), "a_trl"], writes=[("a_wss", hh)])
            P.op("pe", lambda e, hh=hh: e.transpose(out=psB[:, hh * 128:(hh + 1) * 128], in_=wss[:, hh, :], identity=pj.idf[:]), reads=[("a_wss", hh), "idf"], writes=[keyB])
            P.op("act", lambda e, hh=hh: e.activation(out=self.wT[:, hh, :], in_=psB[:, hh * 128:(hh + 1) * 128], func=AF.Copy), reads=[keyB], writes=["a_wT"])

    def tile(self, i):
        P, pj, c0, psA, keyA, psB, keyB = self.P, self.pj, self.c0, self.psA, self.keyA, self.psB, self.keyB
        so = i % 2
        uv = self.uv[:, so, :]; st1 = self.st1[:, so, :]; vc = self.vc[:, so]; sq = self.sq; vn = self.vn[:, so]; yo = self.yo
        K = lambda n: ("a_" + n, so)
        P.op("pe", lambda e: pj.mm_tok(e, i, psA[:, 0:256], c0, c0 + 256), reads=pj.keys(i), writes=[keyA])
        P.op("act", lambda e: e.activation(out=uv, in_=psA[:, 0:256], func=AF.Gelu_apprx_tanh), reads=[keyA], writes=[K("uv")])
        v3 = uv[:, 128:256].rearrange("p (h d) -> p h d", h=2)
        g3 = lambda ap: ap.rearrange("p (h d) -> p h d", h=2)
        P.op("dve", lambda e: e.tensor_reduce(out=st1[:, 0:2], in_=v3, axis=AX.X, op=ALU.add), reads=[K("uv")], writes=[K("st_m")])
        P.op("dve", lambda e: e.tensor_scalar(out=st1[:, 0:2], in0=st1[:, 0:2], scalar1=1.0 / 64, scalar2=None, op0=ALU.mult), reads=[K("st_m")], writes=[K("st_m")])
        P.op("pool", lambda e: e.tensor_tensor(out=vc, in0=v3, in1=st1[:, 0:2].unsqueeze(2).to_broadcast([128, 2, 64]), op=ALU.subtract), reads=[K("uv"), K("st_m")], writes=[K("vc")])
        P.op("pool", lambda e: e.tensor_tensor(out=sq[:], in0=vc, in1=vc, op=ALU.mult), reads=[K("vc")], writes=["a_sq"])
        P.op("dve", lambda e: e.tensor_reduce(out=st1[:, 2:4], in_=sq[:], axis=AX.X, op=ALU.add), reads=["a_sq"], writes=[K("st_v")])
        P.op("dve", lambda e: e.tensor_scalar(out=st1[:, 2:4], in0=st1[:, 2:4], scalar1=1.0 / 64, scalar2=1e-5, op0=ALU.mult, op1=ALU.add), reads=[K("st_v")], writes=[K("st_v")])
        P.op("act", lambda e: e.sqrt(out=st1[:, 2:4], in_=st1[:, 2:4]), reads=[K("st_v")], writes=[K("st_v")])
        P.op("dve", lambda e: e.reciprocal(out=st1[:, 2:4], in_=st1[:, 2:4]), reads=[K("st_v")], writes=[K("st_v")])
        P.op("pool", lambda e: e.tensor_tensor(out=vc, in0=vc, in1=st1[:, 2:4].unsqueeze(2).to_broadcast([128, 2, 64]), op=ALU.mult), reads=[K("vc"), K("st_v")], writes=[K("vc")])
        P.op("pool", lambda e: e.tensor_tensor(out=vc, in0=vc, in1=g3(self.lngs[:]), op=ALU.mult), reads=[K("vc"), "a_lngs"], writes=[K("vc")])
        P.op("pool", lambda e: e.tensor_tensor(out=vn, in0=vc, in1=g3(self.lnbs[:]), op=ALU.add), reads=[K("vc"), "a_lnbs"], writes=[K("vn")])

        def mix(e):
            for hh in range(2):
                ins = e.matmul(psB[:, hh * 64:(hh + 1) * 64], lhsT=self.wT[:, hh, :], rhs=vn[:, hh, :], start=True, stop=True)
            return ins
        P.op("pe", mix, reads=["a_wT", K("vn")], writes=[keyB])
        for hh in range(2):
            P.op("dve", lambda e, hh=hh: e.scalar_tensor_tensor(out=yo[:, so, hh * 64:(hh + 1) * 64], in0=psB[:, hh * 64:(hh + 1) * 64], scalar=self.bss[:, hh:hh + 1],
                                                            in1=uv[:, hh * 64:(hh + 1) * 64], op0=ALU.add, op1=ALU.mult),
                 reads=[keyB, "a_bss", K("uv")], writes=[K("yo")])
        P.dma("sp", self.ysrc[i * 128:(i + 1) * 128, 0:128], yo[:, so, :], reads=[K("yo")], writes=["ysrc"])


def phase_MA(nc, P, ps, psb, pre, h_in, ysrc, hmap=None, is_last=False):
    di = lambda n, s, dt=F32: nc.dram_tensor(pre + n, s, dt, kind="ExternalInput").ap()
    identf = di("identf", [128, 128])
    wc = di("wc", [D, 256])
    g_mix = di("g_mix", [128, 8])
    lng = di("lng", [128, 128])
    lnb = di("lnb", [128, 128])
    ws = di("ws", [2, 128, 128])
    tril = di("tril", [128, 128])
    bs = di("bs", [128, 2])
    with ExitStack() as st:
        sb = lambda name, shape, dt: st.enter_context(nc.sbuf_tensor(pre + "s_" + name, shape, dt))
        pj = Proj(nc, P, st, h_in, identf, 256, wc, g_mix, pre=pre, hmap=hmap)
        lngs = sb("lngs", [128, 128], F32)
        lnbs = sb("lnbs", [128, 128], F32)
        wss = sb("wss", [128, 2, 128], F32)
        trl = sb("trl", [128, 128], F32)
        wT = sb("wT", [128, 2, 128], BF16)
        bss = sb("bss", [128, 2], F32)
        uv = sb("uv", [128, 256], F32)
        st1 = sb("st1", [128, 8], F32)
        vc = sb("vc", [128, 2, 64], F32)
        sq = sb("sq", [128, 2, 64], F32)
        vn = sb("vn", [128, 2, 64], BF16)
        yo = sb("yo", [128, 2, 128], F32)
        P.dma("sp", lngs[:], lng, writes=["lngs"])
        P.dma("sp", lnbs[:], lnb, writes=["lnbs"])
        P.dma("sp", trl[:], tril, writes=["trl"])
        P.dma("sp", bss[:], bs, writes=["bss"])
        for hh in range(2):
            P.dma("sp", wss[:, hh, :], ws[hh], writes=[("wss", hh)])
            P.op("dve", lambda e, hh=hh: e.tensor_tensor(out=wss[:, hh, :], in0=wss[:, hh, :], in1=trl[:], op=ALU.mult), reads=[("wss", hh), "trl"], writes=[("wss", hh)])
            P.op("pe", lambda e, hh=hh: e.transpose(out=ps[7][:, hh * 128:(hh + 1) * 128], in_=wss[:, hh, :], identity=pj.idf[:]), reads=[("wss", hh), "idf"], writes=["ps7"])
            P.op("act", lambda e, hh=hh: e.activation(out=wT[:, hh, :], in_=ps[7][:, hh * 128:(hh + 1) * 128], func=AF.Copy), reads=["ps7"], writes=["wT"])
        for i in range(NTILE):
            pj.tile(i, psb[0], "ps0")
            P.op("pe", lambda e, i=i: pj.mm_tok(e, i, ps[1][:, 0:256], 0, 256), reads=pj.keys(i), writes=["ps1"])
            P.op("act", lambda e: e.activation(out=uv[:], in_=ps[1][:, 0:256], func=AF.Gelu_apprx_tanh), reads=["ps1"], writes=["uv"])
            v3 = uv[:, 128:256].rearrange("p (h d) -> p h d", h=2)
            P.op("dve", lambda e: e.tensor_reduce(out=st1[:, 0:2], in_=v3, axis=AX.X, op=ALU.add), reads=["uv"], writes=["st_m"])
            P.op("dve", lambda e: e.tensor_scalar(out=st1[:, 0:2], in0=st1[:, 0:2], scalar1=1.0 / 64, scalar2=None, op0=ALU.mult), reads=["st_m"], writes=["st_m"])
            P.op("dve", lambda e: e.tensor_tensor(out=vc[:], in0=v3, in1=st1[:, 0:2].unsqueeze(2).to_broadcast([128, 2, 64]), op=ALU.subtract), reads=["uv", "st_m"], writes=["vc"])
            P.op("dve", lambda e: e.tensor_tensor(out=sq[:], in0=vc[:], in1=vc[:], op=ALU.mult), reads=["vc"], writes=["sq"])
            P.op("dve", lambda e: e.tensor_reduce(out=st1[:, 2:4], in_=sq[:], axis=AX.X, op=ALU.add), reads=["sq"], writes=["st_v"])
            P.op("dve", lambda e: e.tensor_scalar(out=st1[:, 2:4], in0=st1[:, 2:4], scalar1=1.0 / 64, scalar2=1e-5, op0=ALU.mult, op1=ALU.add), reads=["st_v"], writes=["st_v"])
            P.op("act", lambda e: e.sqrt(out=st1[:, 2:4], in_=st1[:, 2:4]), reads=["st_v"], writes=["st_v"])
            P.op("dve", lambda e: e.reciprocal(out=st1[:, 2:4], in_=st1[:, 2:4]), reads=["st_v"], writes=["st_v"])
            P.op("dve", lambda e: e.tensor_tensor(out=vc[:], in0=vc[:], in1=st1[:, 2:4].unsqueeze(2).to_broadcast([128, 2, 64]), op=ALU.mult), reads=["vc", "st_v"], writes=["vc"])
            P.op("dve", lambda e: e.tensor_tensor(out=vc[:], in0=vc[:], in1=lngs[:].rearrange("p (h d) -> p h d", h=2), op=ALU.mult), reads=["vc", "lngs"], writes=["vc"])
            P.op("dve", lambda e: e.tensor_tensor(out=vn[:], in0=vc[:], in1=lnbs[:].rearrange("p (h d) -> p h d", h=2), op=ALU.add), reads=["vc", "lnbs"], writes=["vn"])
            def mix(e):
                for hh in range(2):
                    ins = e.matmul(ps[2][:, hh * 64:(hh + 1) * 64], lhsT=wT[:, hh, :], rhs=vn[:, hh, :], start=True, stop=True)
                return ins
            P.op("pe", mix, reads=["wT", "vn"], writes=["ps2"])
            so = i % 2
            for hh in range(2):
                P.op("dve", lambda e, hh=hh, so=so: e.scalar_tensor_tensor(out=yo[:, so, hh * 64:(hh + 1) * 64], in0=ps[2][:, hh * 64:(hh + 1) * 64], scalar=bss[:, hh:hh + 1],
                                                                       in1=uv[:, hh * 64:(hh + 1) * 64], op0=ALU.add, op1=ALU.mult),
                     reads=["ps2", "bss", "uv"], writes=[("yo", so)])
            P.dma("sp", ysrc[i * 128:(i + 1) * 128, 0:128], yo[:, so, :], reads=[("yo", so)], writes=["ysrc"])
        P.emit(last=is_last)


def prep_MA(layer, h, inputs):
    i = layer
    maps = []
    for c in range(8):
        b, gi = c // 2, c % 2
        w = inputs["w_in"][i]
        wc = np.concatenate([w[:, gi * 128:(gi + 1) * 128], w[:, 256 + gi * 128:256 + (gi + 1) * 128]], axis=1)
        maps.append({
            "h": np.ascontiguousarray(h[b]), "identf": np.eye(128, dtype=np.float32), "wc": np.ascontiguousarray(wc),
            "g_mix": pc8(inputs["g_mix"][i]),
            "lng": bc128(inputs["gm_ln_g"][i][2 * gi:2 * gi + 2].reshape(-1)), "lnb": bc128(inputs["gm_ln_b"][i][2 * gi:2 * gi + 2].reshape(-1)),
            "ws": np.ascontiguousarray(inputs["gm_ws"][i][2 * gi:2 * gi + 2]), "tril": np.tril(np.ones((128, 128), np.float32)),
            "bs": np.ascontiguousarray(inputs["gm_bs"][i][2 * gi:2 * gi + 2].T),
        })
    return maps


def phase_MB(nc, P, ps, psb, pre, h_in, ysrc, scr, hmap=None, ntile=NTILE, upto=9, is_last=False):
    di = lambda n, s, dt=F32: nc.dram_tensor(pre + n, s, dt, kind="ExternalInput").ap()
    identf = di("identf", [128, 128])
    wc = di("wc", [D, 896])
    g_mix = di("g_mix", [128, 8])
    mu = di("mu", [128, 640])
    wa_up = di("wa_up", [128, 128])
    g_up = di("g_up", [128, 128])
    w0a0 = di("w0a0", [1, 256])
    cvec = di("cvec", [128, 5, 128])
    sel_in = di("sel", [20, 5, 128])
    mcum_in = di("mcum", [128, 128])
    NST = 32
    with ExitStack() as st:
        sb = lambda name, shape, dt: st.enter_context(nc.sbuf_tensor(pre + "s_" + name, shape, dt))
        pj = Proj(nc, P, st, h_in, identf, 896, wc, g_mix, shift_cols=(0, 640), mu_dram=mu, pre=pre, hmap=hmap)
        ma = MAops(nc, P, st, pre + "a_", pj, 640, ps[6], "ps6", ps[7], "ps7", ysrc)
        waf = sb("waf", [128, 128], F32); wab = sb("wab", [128, 128], BF16)
        guf = sb("guf", [128, 128], F32); gub = sb("gub", [128, 128], BF16)
        w0f = sb("w0f", [1, 256], F32); w0b = sb("w0b", [1, 256], BF16)
        ones = sb("ones", [1, 128], BF16)
        cv = sb("cv", [128, 5, 128], F32)
        self_ = sb("self", [20, 5, 128], F32)
        sel = sb("selt", [20, 5, 128], BF16)
        ldT = sb("ldT", [128, 128], BF16)
        gdT = sb("gdT", [128, 128], BF16)
        sg = sb("sg", [128, 128], F32)
        aa = sb("aa", [128, 128], F32)
        rkv = sb("rkv", [128, 384], F32)
        kk0 = sb("kk0", [128, 2, 64], F32)
        sq = sb("sq", [128, 2, 64], F32)
        st1 = sb("st1", [128, 8], F32)
        tmp = sb("tmp", [128, 128], F32)
        strm = sb("strm", [128, 2, 5, 128], F32)
        g_all = sb("g_all", [128, ntile, 128], F32)
        bon_all = sb("bon_all", [128, ntile, 128], F32)
        vT_all = sb("vT_all", [128, ntile * 128], F32)
        yT_all = sb("yT_all", [128, ntile * 128], F32)
        rows = sb("rows", [20, 2, NST * 64], BF16)
        shl = sb("shl", [128, 2, 2, 5, 128], BF16)
        sdf = sb("sdf", [128, 5, 128], F32)
        Sst = sb("Sst", [128, 64], F32)
        T1 = sb("T1", [128, 64], F32)
        junk = sb("junk", [128, 64], F32)
        sa = sb("sa", [128, 1], F32)
        fill = sb("fill", [128, 2], F32)
        junk2 = sb("junk2", [128, 64], F32)
        prev_step = None
        mcum = sb("mcum", [128, 128], F32)
        pinc = sb("pinc", [128, 128], F32); pinv = sb("pinv", [128, 128], F32); pexc = sb("pexc", [128, 128], F32); csx = sb("csx", [128, 128], F32)
        Sb2 = sb("Sb2", [128, 2, 64], F32); Ubuf = sb("Ubuf", [128, 64], F32)
        P.dma("sp", mcum[:], mcum_in, writes=["mcum"])
        T1p = sb("T1p", [128, 64], F32)
        wr_sb = sb("wr_sb", [128, 2, 512], F32)
        yo = sb("yo", [128, 2, 128], F32)
        yc = sb("yc", [128, 2, 64], F32)

        P.dma("sp", waf[:], wa_up, writes=["waf"]); P.op("dve", lambda e: e.tensor_copy(out=wab[:], in_=waf[:]), reads=["waf"], writes=["wab"])
        P.dma("sp", guf[:], g_up, writes=["guf"]); P.op("dve", lambda e: e.tensor_copy(out=gub[:], in_=guf[:]), reads=["guf"], writes=["gub"])
        P.dma("sp", w0f[:], w0a0, writes=["w0f"]); P.op("dve", lambda e: e.tensor_copy(out=w0b[:], in_=w0f[:]), reads=["w0f"], writes=["w0b"])
        P.op("dve", lambda e: e.memset(ones[:], 1.0), writes=["ones"])
        P.dma("sp", cv[:], cvec, writes=["cv"])
        P.dma("sp", self_[:], sel_in, writes=["self"])
        P.op("dve", lambda e: e.tensor_copy(out=sel[:], in_=self_[:]), reads=["self"], writes=["sel"])
        KK, KA, RK, GNG, GNB = range(5)
        h3 = lambda ap: ap.rearrange("p (h d) -> p h d", h=2)

        pj.tile(0, psb[0], "ps0")
        for i in range(ntile):
            so = i % 2
            if i + 1 < ntile:
                pj.tile(i + 1, psb[0], "ps0")
            P.op("pe", lambda e, i=i: pj.mm_tok(e, i, ps[1][:, 0:384], 0, 384), reads=pj.keys(i), writes=["ps1"])
            P.op("pe", lambda e, i=i: pj.mm_feat(e, i, ps[2][:, 0:128], 384, 512), reads=pj.keys(i), writes=["ps2"])
            P.op("pe", lambda e, i=i: pj.mm_feat(e, i, ps[3][:, 0:128], 512, 640), reads=pj.keys(i), writes=["ps3"])
            P.op("act", lambda e: e.activation(out=ldT[0:64, :], in_=ps[2][0:64, 0:128], func=AF.Tanh), reads=["ps2"], writes=["ldT0"])
            P.op("act", lambda e: e.activation(out=ldT[64:128, :], in_=ps[2][64:128, 0:128], func=AF.Copy), reads=["ps2"], writes=["ldT1"])
            P.op("act", lambda e: e.activation(out=gdT[:], in_=ps[3][:, 0:128], func=AF.Sigmoid), reads=["ps3"], writes=["gdT"])
            P.op("act", lambda e: e.activation(out=rkv[:], in_=ps[1][:, 0:384], func=AF.Copy), reads=["ps1"], writes=["rkv"])
            def ups(e):
                e.matmul(ps[4][:, 0:128], lhsT=ldT[0:64, :], rhs=wab[0:64, :], start=True, stop=False)
                e.matmul(ps[4][:, 0:128], lhsT=ones[:], rhs=w0b[:, 0:128], start=False, stop=True)
                e.matmul(ps[4][:, 128:256], lhsT=ldT[64:128, :], rhs=wab[64:128, :], start=True, stop=False)
                e.matmul(ps[4][:, 128:256], lhsT=ones[:], rhs=w0b[:, 128:256], start=False, stop=True)
                return e.matmul(ps[4][:, 256:384], lhsT=gdT[:], rhs=gub[:], start=True, stop=True)
            P.op("pe", ups, reads=["ldT0", "ldT1", "gdT", "wab", "gub", "w0b", "ones"], writes=["ps4"])
            P.op("act", lambda e: e.activation(out=sg[:], in_=ps[4][:, 0:128], func=AF.Sigmoid), reads=["ps4"], writes=["sg"])
            P.op("pe", lambda e: e.matmul(ps[6][:, 128:256], lhsT=mcum[:], rhs=sg[:], start=True, stop=True), reads=["mcum", "sg"], writes=["ps6"])
            P.op("act", lambda e, so=so: e.activation(out=strm[:, so, 0, :], in_=ps[6][:, 128:256], func=AF.Exp, scale=-0.6065306597126334), reads=["ps6"], writes=[("strm", so, 0)])
            P.op("act", lambda e: e.activation(out=pinv[:], in_=ps[6][:, 128:256], func=AF.Exp, scale=0.6065306597126334), reads=["ps6"], writes=["pinv"])
            P.op("act", lambda e: e.activation(out=csx[:], in_=ps[6][:, 128:256], func=AF.Copy), reads=["ps6"], writes=["csx"])
            P.op("pool", lambda e: e.tensor_tensor(out=csx[:], in0=csx[:], in1=sg[:], op=ALU.subtract), reads=["csx", "sg"], writes=["csx"])
            P.op("act", lambda e: e.activation(out=pexc[:], in_=csx[:], func=AF.Exp, scale=-0.6065306597126334), reads=["csx"], writes=["pexc"])
            P.op("act", lambda e: e.activation(out=aa[:], in_=ps[4][:, 128:256], func=AF.Sigmoid), reads=["ps4"], writes=["aa"])
            P.op("act", lambda e, i=i: e.activation(out=g_all[:, i, :], in_=ps[4][:, 256:384], func=AF.Copy), reads=["ps4"], writes=[("g_all", i)])
            r_ = rkv[:, 0:128]; k_ = rkv[:, 128:256]; v_ = rkv[:, 256:384]
            P.op("pe", lambda e: e.transpose(out=ps[5][:, 0:128], in_=v_, identity=pj.idf[:]), reads=["rkv", "idf"], writes=["ps5"])
            P.op("act", lambda e, i=i: e.activation(out=vT_all[:, i * 128:(i + 1) * 128], in_=ps[5][:, 0:128], func=AF.Copy), reads=["ps5"], writes=[("vT", i)])
            P.op("dve", lambda e: e.tensor_tensor(out=kk0[:], in0=h3(k_), in1=h3(cv[:, KK, :]), op=ALU.mult), reads=["rkv", "cv"], writes=["kk0"])
            P.op("dve", lambda e: e.tensor_tensor(out=sq[:], in0=kk0[:], in1=kk0[:], op=ALU.mult), reads=["kk0"], writes=["sq"])
            P.op("dve", lambda e: e.tensor_reduce(out=st1[:, 0:2], in_=sq[:], axis=AX.X, op=ALU.add), reads=["sq"], writes=["st_k"])
            P.op("dve", lambda e: e.tensor_scalar(out=st1[:, 0:2], in0=st1[:, 0:2], scalar1=1e-24, scalar2=None, op0=ALU.max), reads=["st_k"], writes=["st_k"])
            P.op("act", lambda e: e.sqrt(out=st1[:, 0:2], in_=st1[:, 0:2]), reads=["st_k"], writes=["st_k"])
            P.op("dve", lambda e: e.reciprocal(out=st1[:, 0:2], in_=st1[:, 0:2]), reads=["st_k"], writes=["st_k"])
            P.op("dve", lambda e, so=so: e.tensor_tensor(out=h3(strm[:, so, 1, :]), in0=kk0[:], in1=st1[:, 0:2].unsqueeze(2).to_broadcast([128, 2, 64]), op=ALU.mult),
                 reads=["kk0", "st_k"], writes=[("strm", so, 1)])
            P.op("dve", lambda e, so=so: e.scalar_tensor_tensor(out=strm[:, so, 2, :], in0=strm[:, so, 1, :], scalar=-1.0, in1=aa[:], op0=ALU.mult, op1=ALU.mult),
                 reads=[("strm", so, 1), "aa"], writes=[("strm", so, 2)])
            P.op("dve", lambda e: e.scalar_tensor_tensor(out=tmp[:], in0=aa[:], scalar=-1.0, in1=cv[:, KA, :], op0=ALU.add, op1=ALU.mult), reads=["aa", "cv"], writes=["tmp"])
            P.op("dve", lambda e, so=so: e.scalar_tensor_tensor(out=strm[:, so, 3, :], in0=tmp[:], scalar=1.0, in1=k_, op0=ALU.add, op1=ALU.mult),
                 reads=["tmp", "rkv"], writes=[("strm", so, 3)])
            P.op("pool", lambda e, so=so: e.tensor_tensor(out=strm[:, so, 4, :], in0=r_, in1=strm[:, so, 0, :], op=ALU.mult), reads=["rkv", ("strm", so, 0)], writes=[("strm", so, 4)])
            P.op("dve", lambda e, so=so: e.tensor_tensor(out=tmp[:], in0=r_, in1=strm[:, so, 3, :], op=ALU.mult), reads=["rkv", ("strm", so, 3), "tmp"], writes=["tmp"])
            P.op("dve", lambda e: e.tensor_tensor(out=tmp[:], in0=tmp[:], in1=cv[:, RK, :], op=ALU.mult), reads=["tmp", "cv"], writes=["tmp"])
            P.op("dve", lambda e: e.tensor_reduce(out=st1[:, 2:4], in_=h3(tmp[:]), axis=AX.X, op=ALU.add), reads=["tmp"], writes=["st_b"])
            P.op("dve", lambda e, i=i: e.tensor_tensor(out=h3(bon_all[:, i, :]), in0=h3(v_), in1=st1[:, 2:4].unsqueeze(2).to_broadcast([128, 2, 64]), op=ALU.mult),
                 reads=["rkv", "st_b"], writes=[("bon", i)])
            P.op("pool", lambda e, so=so: e.tensor_tensor(out=strm[:, so, 1, :], in0=strm[:, so, 1, :], in1=pexc[:], op=ALU.mult), reads=[("strm", so, 1), ("strm", so, 2), "pexc"], writes=[("strm", so, 1)])
            P.op("pool", lambda e, so=so: e.tensor_tensor(out=strm[:, so, 2, :], in0=strm[:, so, 2, :], in1=pinv[:], op=ALU.mult), reads=[("strm", so, 2), "pinv"], writes=[("strm", so, 2)])
            P.op("pool", lambda e, so=so: e.tensor_tensor(out=strm[:, so, 3, :], in0=strm[:, so, 3, :], in1=pinv[:], op=ALU.mult), reads=[("strm", so, 3), "pinv", "tmp"], writes=[("strm", so, 3)])
            ma.tile(i)
            skeys = [("strm", so, s_) for s_ in range(5)]
            P.op("pool", lambda e, so=so: e.tensor_copy(out=shl[:, so, 0], in_=strm[:, so]), reads=skeys, writes=[("shl", so, 0)])
            P.op("pool", lambda e, so=so: e.tensor_tensor(out=sdf[:], in0=strm[:, so], in1=shl[:, so, 0], op=ALU.subtract), reads=skeys + [("shl", so, 0)], writes=["sdf"])
            P.op("pool", lambda e, so=so: e.tensor_copy(out=shl[:, so, 1], in_=sdf[:]), reads=["sdf"], writes=[("shl", so, 1)])
            for hl in range(2):
                for s_ in range(5):
                    r0 = hl * 10 + 2 * s_
                    P.dma("sp" if hl == 0 else "pool", scr[r0:r0 + 2, i * 128:(i + 1) * 128, :].rearrange("h t k -> t h k"), h3(shl[:, so, hl, s_, :]),
                          reads=[("shl", so, hl)], writes=[("scr", i)])

        P.barrier()
        P.op("dve", lambda e: e.memset(Sb2[:], 0.0), writes=[("S", 0), ("S", 1)])
        nsteps = ntile * 128
        SW, SKK, SNB, SK, SR = range(5)
        SLOT = {0: 0, 4: 1, 1: 2, 2: 3, 3: 4}
        def bcap(par, s_, j):
            f = SLOT[s_] * 256
            return ps[par * 3 + f // 512][:, (f % 512) + j * 64:(f % 512) + (j + 1) * 64]
        def bcblk(par, s_):
            f = SLOT[s_] * 256
            return ps[par * 3 + f // 512][:, (f % 512):(f % 512) + 256]
        if upto < 3:
            P.op('dve', lambda e: e.memset(yT_all[:], 0.0), writes=[('yT', i) for i in range(ntile)])
        for ch in range(nsteps // NST if upto >= 2 else 0):
            slot = ch % 2
            P.dma("sp", rows[:, slot, :].rearrange("p (t k) -> p t k", k=64), scr[:, ch * NST:(ch + 1) * NST, :],
                  reads=[("scr", (ch * NST) // 128)], writes=[("rows", slot)])
            for gg in range(NST // 4):
                g = ch * (NST // 4) + gg
                par = g % 2
                def bc(e, par=par, slot=slot, gg=gg):
                    for s_ in range(5):
                        ins = e.matmul(bcblk(par, s_), lhsT=sel[:, s_, :], rhs=rows[:, slot, gg * 256:(gg + 1) * 256], start=True, stop=True)
                    return ins
                P.op("pe", bc, reads=[("rows", slot), "sel"], writes=[("bc", par)])
                if upto >= 3:
                    pass
                for j in range(4 if upto >= 3 else 0):
                    t = g * 4 + j
                    ti = t // 128
                    cb = ch % 2
                    Sx = Sb2[:, cb, :]
                    kS = ("S", cb)
                    last_in_chunk = (t % NST == NST - 1)

                    def yop(pt, ppar, pj_, pcb, same=True):
                        P.op("dve", lambda e, ppar=ppar, pj_=pj_, pt=pt, pcb=pcb: e.scalar_tensor_tensor(out=junk2[:], in0=Sb2[:, pcb, :], scalar=1.0, in1=bcap(ppar, SR, pj_), op0=ALU.mult, op1=ALU.mult,
                                                                                                   accum_out=yT_all[:, pt:pt + 1]),
                             reads=[("S", pcb), ("bc", ppar)], writes=["junk2", ("yT", pt // 128)], same_ok=same)
                    P.op("dve", lambda e, par=par, j=j, Sx=Sx: e.scalar_tensor_tensor(out=junk[:], in0=Sx, scalar=1.0, in1=bcap(par, SKK, j), op0=ALU.mult, op1=ALU.mult, accum_out=sa[:]),
                         reads=[kS, ("bc", par)], writes=["junk", "sa"], same_ok=(t > 0))
                    P.op("dve", lambda e, par=par, j=j, t=t, Sx=Sx: e.scalar_tensor_tensor(out=Ubuf[:], in0=bcap(par, SK, j), scalar=vT_all[:, t:t + 1], in1=Sx, op0=ALU.mult, op1=ALU.add),
                         reads=[kS, ("bc", par), ("vT", ti)], writes=["Ubuf"], same_ok=(t > 0))
                    if prev_step is not None:
                        yop(*prev_step)
                    P.op("dve", lambda e, par=par, j=j, Sx=Sx: e.scalar_tensor_tensor(out=Sx, in0=bcap(par, SNB, j), scalar=sa[:], in1=Ubuf[:], op0=ALU.mult, op1=ALU.add),
                         reads=["sa", "Ubuf", ("bc", par)], writes=[kS], same_ok=True)
                    prev_step = (t, par, j, cb)
                    if last_in_chunk:
                        yop(*prev_step)
                        prev_step = None
                        P.op("dve", lambda e, par=par, j=j, cb=cb: e.tensor_tensor(out=Sb2[:, 1 - cb, :], in0=Sb2[:, cb, :], in1=bcap(par, SW, j), op=ALU.mult),
                             reads=[("S", cb), ("bc", par)], writes=[("S", 1 - cb)], same_ok=True)
                        P.op("dve", lambda e: e.memset(fill[:, 0:1], 0.0), writes=["fill"], same_ok=True)

        P.barrier()
        for i in range(ntile):
            so = i % 2
            P.op("pe", lambda e, i=i: e.transpose(out=ps[5][:, 0:128], in_=yT_all[:, i * 128:(i + 1) * 128], identity=pj.idf[:]), reads=[("yT", i), "idf"], writes=["ps5"])
            y3 = ps[5][:, 0:128].rearrange("p (h d) -> p h d", h=2)
            P.op("dve", lambda e: e.tensor_reduce(out=st1[:, 0:2], in_=y3, axis=AX.X, op=ALU.add), reads=["ps5"], writes=["st_k"])
            P.op("dve", lambda e: e.tensor_scalar(out=st1[:, 0:2], in0=st1[:, 0:2], scalar1=1.0 / 64, scalar2=None, op0=ALU.mult), reads=["st_k"], writes=["st_k"])
            P.op("dve", lambda e: e.tensor_tensor(out=yc[:], in0=y3, in1=st1[:, 0:2].unsqueeze(2).to_broadcast([128, 2, 64]), op=ALU.subtract), reads=["ps5", "st_k"], writes=["yc"])
            P.op("dve", lambda e: e.tensor_tensor(out=sq[:], in0=yc[:], in1=yc[:], op=ALU.mult), reads=["yc"], writes=["sq"])
            P.op("dve", lambda e: e.tensor_reduce(out=st1[:, 2:4], in_=sq[:], axis=AX.X, op=ALU.add), reads=["sq"], writes=["st_b"])
            P.op("dve", lambda e: e.tensor_scalar(out=st1[:, 2:4], in0=st1[:, 2:4], scalar1=1.0 / 64, scalar2=64e-5, op0=ALU.mult, op1=ALU.add), reads=["st_b"], writes=["st_b"])
            P.op("act", lambda e: e.sqrt(out=st1[:, 2:4], in_=st1[:, 2:4]), reads=["st_b"], writes=["st_b"])
            P.op("dve", lambda e: e.reciprocal(out=st1[:, 2:4], in_=st1[:, 2:4]), reads=["st_b"], writes=["st_b"])
            P.op("dve", lambda e: e.tensor_tensor(out=yc[:], in0=yc[:], in1=st1[:, 2:4].unsqueeze(2).to_broadcast([128, 2, 64]), op=ALU.mult), reads=["yc", "st_b"], writes=["yc"])
            ycf = yc[:].rearrange("p h d -> p (h d)")
            P.op("dve", lambda e: e.tensor_tensor(out=ycf, in0=ycf, in1=cv[:, GNG, :], op=ALU.mult), reads=["yc", "cv"], writes=["yc"])
            P.op("dve", lambda e: e.tensor_tensor(out=ycf, in0=ycf, in1=cv[:, GNB, :], op=ALU.add), reads=["yc", "cv"], writes=["yc"])
            P.op("dve", lambda e, i=i: e.tensor_tensor(out=ycf, in0=ycf, in1=bon_all[:, i, :], op=ALU.add), reads=["yc", ("bon", i)], writes=["yc"])
            P.op("dve", lambda e, i=i, so=so: e.tensor_tensor(out=yo[:, so, :], in0=ycf, in1=g_all[:, i, :], op=ALU.mult), reads=["yc", ("g_all", i)], writes=[("yo", so)])
            P.dma("sp", ysrc[i * 128:(i + 1) * 128, 128:256], yo[:, so, :], reads=[("yo", so)], writes=["ysrc"])
        P.emit(last=is_last)


def prep_MB(layer, h, inputs):
    i = layer
    maps = []
    w = inputs["w_in"][i]
    sel = np.zeros((20, 5, 128), np.float32)
    for hl in range(2):
        for s_ in range(5):
            for hh in range(2):
                sel[hl * 10 + s_ * 2 + hh, s_, hh * 64:(hh + 1) * 64] = 1.0
    for c in range(8):
        b, gi = c // 2, c % 2
        hc = slice(gi * 128, (gi + 1) * 128)
        cols = np.concatenate([512 + np.arange(gi * 128, (gi + 1) * 128), 768 + np.arange(gi * 128, (gi + 1) * 128), 1024 + np.arange(gi * 128, (gi + 1) * 128),
                               np.arange(1280, 1536)])
        maps.append({
            "h": np.ascontiguousarray(h[b]), "identf": np.eye(128, dtype=np.float32),
            "wc": np.ascontiguousarray(np.concatenate([w[:, cols], w[:, gi * 128:(gi + 1) * 128], w[:, 256 + gi * 128:256 + (gi + 1) * 128]], axis=1)),
            "a_lng": bc128(inputs["gm_ln_g"][i][2 * gi:2 * gi + 2].reshape(-1)), "a_lnb": bc128(inputs["gm_ln_b"][i][2 * gi:2 * gi + 2].reshape(-1)),
            "a_ws": np.ascontiguousarray(inputs["gm_ws"][i][2 * gi:2 * gi + 2]), "a_tril": np.tril(np.ones((128, 128), np.float32)),
            "a_bs": np.ascontiguousarray(inputs["gm_bs"][i][2 * gi:2 * gi + 2].T),
            "g_mix": pc8(inputs["g_mix"][i]), "mu": bc128(inputs["rw_mu"][i][cols - 512]),
            "wa_up": np.ascontiguousarray(np.concatenate([inputs["rw_w_up"][i][:, hc], inputs["rw_a_up"][i][:, hc]], axis=0)),
            "g_up": np.ascontiguousarray(inputs["rw_g_up"][i][:, hc]),
            "w0a0": np.concatenate([inputs["rw_w0"][i][hc], inputs["rw_a0"][i][hc]])[None, :].astype(np.float32),
            "cvec": np.ascontiguousarray(np.stack([bc128(inputs["rw_k_k"][i][hc]), bc128(inputs["rw_k_a"][i][hc]), bc128(inputs["rw_r_k"][i].reshape(-1)[hc]),
                                                   bc128(inputs["rw_gn_g"][i][hc]), bc128(inputs["rw_gn_b"][i][hc])], axis=1)),
            "sel": sel,
            "mcum": ((np.arange(128)[:, None] // 32 == np.arange(128)[None, :] // 32) & (np.arange(128)[:, None] <= np.arange(128)[None, :])).astype(np.float32),
        })
    return maps


NEGB = -30000.0
MC_BR = 'csw'


def phase_MC(nc, P, ps, psb, pre, h_in, ysrc, hmap=None, ntile=NTILE, upto=9, is_last=False):
    import ml_dtypes
    di = lambda n, s, dt=F32: nc.dram_tensor(pre + n, s, dt, kind="ExternalInput").ap()
    identf = di("identf", [128, 128])
    wc = di("wc", [D, 652])
    g_mix = di("g_mix", [128, 8])
    pos_in = di("pos", [128, NTILE], I32)
    invf = di("invf", [128, 8])
    w1kc = di("w1kc", [2048, 128]); w1vc = di("w1vc", [2048, 128])
    w2kc = di("w2kc", [128, 64]); w2vc = di("w2vc", [128, 64])
    posT = di("posT", [64, 32])
    rconst = di("rconst", [128, 2, 65])
    cmask = di("cmask", [NTILE, 128, 2, 128], BF16)
    selc = di("selc", [NTILE, 128, 2, 64])
    Ef_in = di("Ef", [64, S], BF16)
    tri_in = di("tri", [128, 2, 128], BF16)
    with ExitStack() as st:
        sb = lambda name, shape, dt: st.enter_context(nc.sbuf_tensor(pre + "s_" + name, shape, dt))
        pj = Proj(nc, P, st, h_in, identf, 652, wc, g_mix, pre=pre, hmap=hmap)
        qrT = sb("qrT", [64, 4, S], BF16)
        qwT = sb("qwT", [64, 4, S], BF16)
        kT4 = sb("kT4", [64, 4, S], BF16)
        vaug = sb("vaug", [128, 2, NTILE, 65], BF16)
        ksE = sb("ksE", [128, S], BF16)
        qs = sb("qs", [128, 2, 4, 128], BF16)
        tri = sb("tris", [128, 2, 128], BF16)
        w1s = sb("w1s", [64, 32, 128], F32)
        w1b = sb("w1b", [64, 2, 32, 128], BF16)
        w2f = sb("w2f", [128, 2, 64], F32); w2b = sb("w2b", [128, 2, 64], BF16)
        posf = sb("posf", [64, 32], F32); posb = sb("posb", [64, 32], BF16)
        rcf = sb("rcf", [128, 2, 65], F32)
        R_ = sb("R_", [128, 2, 129], BF16)
        posi = sb("posi", [128, NTILE], I32); posfl = sb("posfl", [128, NTILE], F32)
        inv = sb("inv", [128, 8], F32)
        ang = sb("ang", [128, NTILE, 8], F32)
        cs = sb("cs", [128, NTILE, 8], F32); sn = sb("sn", [128, NTILE, 8], F32)
        gsig = sb("gsig", [128, NTILE, 12], F32)
        xq = sb("xq", [128, 512], F32)
        xqb = sb("xqb", [128, 512], BF16)
        qkr = sb("qkr", [128, 6, 64], BF16)
        ra = sb("ra", [128, 6, 8], F32); rb_ = sb("rb_", [128, 6, 8], F32)
        bias_sb = sb("bias_sb", [128, 2], F32)
        gelT = sb("gelT", [128, 2, 256], BF16)
        kcmpT = sb("kcmpT", [64, 256], BF16)
        cm = sb("cm", [128, 2, 2, 128], BF16)
        scs = sb("scs", [128, 2, 2, 64], F32)
        eT = sb("eT", [128, 2, 4, 128], BF16)
        imp = sb("imp", [128, 64], F32)
        imp2 = sb("imp2", [128, 64], F32)
        rep = sb("rep", [128, 64], F32)
        mx = sb("mx", [128, 8], F32)
        selbb = sb("selbb", [128, 128], BF16)
        selbT = sb("selbT", [64, 128], BF16)
        sm = sb("sm", [128, 16], F32)
        oacc = sb("oacc", [128, 2, 4, 64], F32)

        ld = lambda dst, src, key: P.dma("sp", dst, src, writes=[key])
        ld(ksE[64:128, :], Ef_in, "ksE_E"); ld(tri[:], tri_in, "tri")
        P.op("pool", lambda e: e.memset(selbb[:], 0.0), writes=["selbb"])
        ld(posi[:], pos_in, "posi"); ld(inv[:], invf, "inv")
        ld(rcf[:], rconst, "rcf"); ld(posf[:], posT, "posf")
        ld(w2f[:, 0, :], w2kc, "w2f0"); ld(w2f[:, 1, :], w2vc, "w2f1")
        P.op("dve", lambda e: e.tensor_copy(out=w2b[:], in_=w2f[:]), reads=["w2f0", "w2f1"], writes=["w2b"])
        P.op("dve", lambda e: e.tensor_copy(out=posb[:], in_=posf[:]), reads=["posf"], writes=["posb"])
        P.op("dve", lambda e: e.memset(R_[:], 0.0), writes=["R"])
        P.op("dve", lambda e: e.tensor_copy(out=R_[:, :, 0:65], in_=rcf[:]), reads=["rcf", "R"], writes=["R"])
        P.op("pool", lambda e: e.memset(vaug[:], 1.0), writes=["vaug_init"])
        P.op("pool", lambda e: e.memset(gelT[:], 0.0), writes=["gelT0", "gelT1"])
        for w in range(2):
            P.dma("sp", w1s[:], (w1kc if w == 0 else w1vc).rearrange("(l d) h -> d l h", d=64), writes=["w1s"])
            P.op("pool", lambda e, w=w: e.tensor_copy(out=w1b[:, w, :, :], in_=w1s[:]), reads=["w1s"], writes=[("w1b", w)])
        P.op("dve", lambda e: e.tensor_copy(out=posfl[:], in_=posi[:]), reads=["posi"], writes=["posfl"])
        P.op("dve", lambda e: e.tensor_tensor(out=ang[:], in0=posfl[:].unsqueeze(2).to_broadcast([128, NTILE, 8]), in1=inv[:].unsqueeze(1).to_broadcast([128, NTILE, 8]), op=ALU.mult),
             reads=["posfl", "inv"], writes=["ang"])
        PI = float(np.pi)
        angi = sb("angi", [128, NTILE, 8], I32)
        angf = sb("angf", [128, NTILE, 8], F32)
        for (dst, off, key) in ((sn, 0.5, "sn"), (cs, 0.75, "cs")):
            P.op("dve", lambda e, dst=dst, off=off: e.tensor_scalar(out=dst[:], in0=ang[:], scalar1=1.0 / (2 * PI), scalar2=off, op0=ALU.mult, op1=ALU.add), reads=["ang"], writes=[key])
            P.op("dve", lambda e, dst=dst: e.tensor_copy(out=angi[:], in_=dst[:]), reads=[key], writes=["angi"])
            P.op("dve", lambda e: e.tensor_copy(out=angf[:], in_=angi[:]), reads=["angi"], writes=["angf"])
            P.op("dve", lambda e, dst=dst: e.tensor_tensor(out=dst[:], in0=dst[:], in1=angf[:], op=ALU.subtract), reads=[key, "angf"], writes=[key])
            P.op("dve", lambda e, dst=dst: e.tensor_scalar(out=angf[:], in0=dst[:], scalar1=0.0, scalar2=None, op0=ALU.is_lt), reads=[key], writes=["angf"])
            P.op("dve", lambda e, dst=dst: e.tensor_tensor(out=dst[:], in0=dst[:], in1=angf[:], op=ALU.add), reads=[key, "angf"], writes=[key])
            P.op("dve", lambda e, dst=dst: e.tensor_scalar(out=dst[:], in0=dst[:], scalar1=2 * PI, scalar2=-PI, op0=ALU.mult, op1=ALU.add), reads=[key], writes=[key])
            P.op("dve", lambda e, dst=dst: e.tensor_scalar(out=dst[:], in0=dst[:], scalar1=PI, scalar2=-PI, op0=ALU.min, op1=ALU.max), reads=[key], writes=[key])
            P.op("act", lambda e, dst=dst: e.activation(out=dst[:], in_=dst[:], func=AF.Sin), reads=[key], writes=[key])

        pj.tile(0, psb[0], "ps0")
        for i in range(ntile):
            if i + 1 < ntile:
                pj.tile(i + 1, psb[0], "ps0")
            P.op("pe", lambda e, i=i: pj.mm_tok(e, i, ps[1][:], 0, 512), reads=pj.keys(i), writes=["ps1"])
            P.op("pe", lambda e, i=i: pj.mm_tok(e, i, ps[2][:, 0:140], 512, 652), reads=pj.keys(i), writes=["ps2"])
            P.op("act", lambda e: e.activation(out=xq[:], in_=ps[1][:], func=AF.Copy), reads=["ps1"], writes=["xq"])
            P.op("act", lambda e, i=i: e.activation(out=vaug[:, :, i, 0:64], in_=ps[2][:, 0:128].rearrange("p (a d) -> p a d", a=2), func=AF.Copy), reads=["ps2", "vaug_init"], writes=[("vaug", i)])
            P.op("act", lambda e, i=i: e.activation(out=gsig[:, i, :], in_=ps[2][:, 128:140], func=AF.Sigmoid), reads=["ps2"], writes=[("gsig", i)])
            P.op("pool", lambda e: e.tensor_copy(out=xqb[:], in_=xq[:]), reads=["xq"], writes=["xqb"])
            X = xq[:, 0:384].rearrange("p (h d) -> p h d", h=6)
            cb = cs[:, i, :].unsqueeze(1).to_broadcast([128, 6, 8])
            sbb = sn[:, i, :].unsqueeze(1).to_broadcast([128, 6, 8])
            P.op("pool", lambda e, X=X: e.tensor_copy(out=qkr[:, :, 16:64], in_=X[:, :, 16:64]), reads=["xq"], writes=["qkr_c"])
            P.op("dve", lambda e, X=X, cb=cb: e.tensor_tensor(out=ra[:], in0=X[:, :, 0:8], in1=cb, op=ALU.mult), reads=["xq", "cs"], writes=["ra"])
            P.op("dve", lambda e, X=X, sbb=sbb: e.tensor_tensor(out=rb_[:], in0=X[:, :, 8:16], in1=sbb, op=ALU.mult), reads=["xq", "sn"], writes=["rb"])
            P.op("dve", lambda e: e.tensor_tensor(out=qkr[:, :, 0:8], in0=ra[:], in1=rb_[:], op=ALU.subtract), reads=["ra", "rb"], writes=["qkr_a"])
            P.op("dve", lambda e, X=X, sbb=sbb: e.tensor_tensor(out=ra[:], in0=X[:, :, 0:8], in1=sbb, op=ALU.mult), reads=["xq", "sn", "ra"], writes=["ra"])
            P.op("dve", lambda e, X=X, cb=cb: e.tensor_tensor(out=rb_[:], in0=X[:, :, 8:16], in1=cb, op=ALU.mult), reads=["xq", "cs", "rb"], writes=["rb"])
            P.op("dve", lambda e: e.tensor_tensor(out=qkr[:, :, 8:16], in0=ra[:], in1=rb_[:], op=ALU.add), reads=["ra", "rb"], writes=["qkr_b"])
            def trs(e):
                for j in range(4):
                    e.transpose(out=psb[3][0:64, j * 128:(j + 1) * 128], in_=qkr[:, j, :], identity=pj.idb[:])
                for j in range(4):
                    e.transpose(out=psb[5][0:64, j * 128:(j + 1) * 128], in_=xqb[:, j * 64:(j + 1) * 64], identity=pj.idb[:])
                e.transpose(out=psb[4][0:64, 0:128], in_=qkr[:, 4, :], identity=pj.idb[:])
                e.transpose(out=psb[4][0:64, 128:256], in_=qkr[:, 5, :], identity=pj.idb[:])
                e.transpose(out=psb[4][0:64, 256:384], in_=xqb[:, 384:448], identity=pj.idb[:])
                return e.transpose(out=psb[4][0:64, 384:512], in_=xqb[:, 448:512], identity=pj.idb[:])
            P.op("pe", trs, reads=["qkr_a", "qkr_b", "qkr_c", "xqb", "idb"], writes=["ps3", "ps4", "ps5"])
            tsl = slice(i * 128, (i + 1) * 128)
            P.op("act", lambda e, tsl=tsl: e.activation(out=qrT[:, :, tsl], in_=psb[3][0:64, 0:512].rearrange("p (j t) -> p j t", j=4), func=AF.Copy), reads=["ps3"], writes=[("qrT", i)])
            P.op("dve", lambda e, tsl=tsl: e.tensor_copy(out=qwT[:, :, tsl], in_=psb[5][0:64, 0:512].rearrange("p (j t) -> p j t", j=4)), reads=["ps5"], writes=[("qwT", i)])
            P.op("act", lambda e, tsl=tsl: e.activation(out=kT4[:, :, tsl], in_=psb[4][0:64, 0:512].rearrange("p (j t) -> p j t", j=4), func=AF.Copy), reads=["ps4"], writes=[("kT4", i)])
            P.op("act", lambda e, tsl=tsl: e.activation(out=ksE[0:64, tsl], in_=psb[4][0:64, 0:128], func=AF.Copy), reads=["ps4"], writes=[("ksE", i)])

        P.barrier()
        ncmp = (ntile * 128 - 32) // 16 + 1
        for w in range(2 if upto >= 2 else 0):
            src = 2 + w
            def hid(e, w=w, src=src):
                for l in range(32):
                    ins = e.matmul(ps[0][:, 0:ncmp], lhsT=w1b[:, w, l, :], rhs=kT4[:, src, l:l + 16 * (ncmp - 1) + 1:16], start=(l == 0), stop=(l == 31))
                return ins
            P.op("pe", hid, reads=[("w1b", w)], writes=["ps0"])
            def pbias(e, w=w):
                for l in range(32):
                    ins = e.matmul(ps[1][:, 0:1], lhsT=w1b[:, w, l, :], rhs=posb[:, l:l + 1], start=(l == 0), stop=(l == 31))
                return ins
            P.op("pe", pbias, reads=[("w1b", w), "posb"], writes=["ps1"])
            P.op("dve", lambda e, w=w: e.tensor_copy(out=bias_sb[:, w:w + 1], in_=ps[1][:, 0:1]), reads=["ps1"], writes=[("bias", w)])
            P.op("act", lambda e, w=w: e.activation(out=gelT[:, w, 0:ncmp], in_=ps[0][:, 0:ncmp], func=AF.Gelu_apprx_tanh, bias=bias_sb[:, w:w + 1]), reads=["ps0", ("bias", w), f"gelT{w}"], writes=[f"gelT{w}"])
        if upto >= 2:
            P.op("pe", lambda e: e.matmul(ps[2][0:64, 0:256], lhsT=w2b[:, 0, :], rhs=gelT[:, 0, :], start=True, stop=True), reads=["w2b", "gelT0"], writes=["ps2"])
            P.op("act", lambda e: e.activation(out=kcmpT[:], in_=ps[2][0:64, 0:256], func=AF.Copy), reads=["ps2"], writes=["kcmpT"])
        for cn in range(2 if upto >= 2 else 0):
            P.op("pe", lambda e, cn=cn: e.matmul(ps[3][:, cn * 64:(cn + 1) * 64], lhsT=gelT[:, 1, cn * 128:(cn + 1) * 128], rhs=w2b[:, 1, :], start=True, stop=True), reads=["w2b", "gelT1"], writes=["ps3"])
            P.op("dve", lambda e, cn=cn: e.tensor_copy(out=R_[:, cn, 65:129], in_=ps[3][:, cn * 64:(cn + 1) * 64]), reads=["ps3", "R"], writes=["R"])

        P.barrier()
        nsc = 0
        if upto < 3:
            P.op('dve', lambda e: e.memset(oacc[:], 0.0), writes=[('oacc', 0), ('oacc', 1)])
            P.dma('sp', ysrc[0:128, 256:512], oacc[:, 0].rearrange('p j d -> p (j d)'), reads=[('oacc', 0)], writes=['ysrc'])
        for qb in range(ntile if upto >= 3 else 0):
            tsl = slice(qb * 128, (qb + 1) * 128)
            so = qb % 2
            P.dma("sp", cm[:, so], cmask[qb], writes=[("cm", so)])
            P.dma("sp", scs[:, so], selc[qb], writes=[("scs", so)])
            ncn = 2 if qb >= 16 else 1
            for cn in range(ncn):
                sbk = nsc % 2; nsc += 1
                def sc(e, cn=cn, sbk=sbk, tsl=tsl, so=so):
                    e.matmul(ps[sbk][:], lhsT=kcmpT[:, cn * 128:(cn + 1) * 128], rhs=qwT[:, :, tsl], start=True, stop=False)
                    return e.matmul(ps[sbk][:], lhsT=pj.idb[:], rhs=cm[:, so, cn, :].unsqueeze(1).to_broadcast([128, 4, 128]), start=False, stop=True)
                P.op("pe", sc, reads=["kcmpT", ("qwT", qb), ("cm", so), "idb"], writes=[f"ps{sbk}"])
                P.op("act", lambda e, sbk=sbk: e.activation(out=eT[:, sbk].rearrange("p j t -> p (j t)"), in_=ps[sbk][:], func=AF.Exp, scale=0.125), reads=[f"ps{sbk}"], writes=[("eT", sbk)])
                def pv(e, cn=cn, sbk=sbk, ncn=ncn):
                    for j in range(4):
                        ins = e.matmul(ps[2 + j // 2][:, (j % 2) * 129:(j % 2) * 129 + 129], lhsT=eT[:, sbk, j, :], rhs=R_[:, cn, :], start=(cn == 0 and j % 2 == 0), stop=(cn == ncn - 1), skip_group_check=True)
                    return ins
                P.op("pe", pv, reads=[("eT", sbk), "R"], writes=["ps2", "ps3"])
            P.op("dve", lambda e: e.memset(imp[:], 0.0), writes=["imp"])
            for j in range(4):
                pso = ps[2 + j // 2][:, (j % 2) * 129:(j % 2) * 129 + 129]
                ri = sm[:, j:j + 1]; rg = sm[:, 4 + j:5 + j]
                P.op("dve", lambda e, pso=pso, ri=ri: e.tensor_scalar(out=ri, in0=pso[:, 64:65], scalar1=1e-30, scalar2=None, op0=ALU.add), reads=["ps2", "ps3"], writes=[("ri", j)])
                P.op("dve", lambda e, ri=ri: e.reciprocal(out=ri, in_=ri), reads=[("ri", j)], writes=[("ri", j)])
                P.op("dve", lambda e, pso=pso, ri=ri: e.scalar_tensor_tensor(out=imp[:], in0=pso[:, 0:64], scalar=ri, in1=imp[:], op0=ALU.mult, op1=ALU.add), reads=["ps2", "ps3", ("ri", j), "imp"], writes=["imp"])
                P.op("dve", lambda e, ri=ri, rg=rg, j=j, qb=qb: e.tensor_tensor(out=rg, in0=ri, in1=gsig[:, qb, 3 * j:3 * j + 1], op=ALU.mult), reads=[("ri", j), ("gsig", qb)], writes=[("rg", j)])
                P.op("dve", lambda e, pso=pso, rg=rg, j=j, so=so: e.tensor_scalar(out=oacc[:, so, j, :], in0=pso[:, 65:129], scalar1=rg, scalar2=None, op0=ALU.mult), reads=["ps2", "ps3", ("rg", j)], writes=[("oacc", so)])
            if upto < 4:
                P.dma('sp', ysrc[tsl, 256:512], oacc[:, so].rearrange('p j d -> p (j d)'), reads=[('oacc', so)], writes=['ysrc'])
                continue
            P.op("dve", lambda e, so=so: e.tensor_tensor(out=imp2[:], in0=imp[:], in1=scs[:, so, 0, :], op=ALU.mult), reads=["imp", ("scs", so)], writes=["imp2"])
            P.op("dve", lambda e, so=so: e.tensor_tensor(out=imp2[:], in0=imp2[:], in1=scs[:, so, 1, :], op=ALU.add), reads=["imp2", ("scs", so)], writes=["imp2"])
            P.op("dve", lambda e: e.max(out=mx[:], in_=imp2[:]), reads=["imp2"], writes=["mx"])
            P.op("dve", lambda e: e.match_replace(out=rep[:], in_to_replace=mx[:], in_values=imp2[:], imm_value=-1e30), reads=["imp2", "mx"], writes=["rep"])
            P.op("dve", lambda e: e.max(out=mx[:], in_=rep[:]), reads=["rep"], writes=["mx"])
            P.op("dve", lambda e: e.tensor_scalar(out=mx[:, 7:8], in0=mx[:, 7:8], scalar1=-5000.0, scalar2=None, op0=ALU.max), reads=["mx"], writes=["mx"])
            P.op("dve", lambda e: e.tensor_scalar(out=rep[:], in0=imp2[:], scalar1=mx[:, 7:8], scalar2=None, op0=ALU.is_ge), reads=["imp2", "mx", "rep"], writes=["rep"])
            P.op("dve", lambda e: e.tensor_scalar(out=selbb[:, 64:128], in0=rep[:], scalar1=-NEGB, scalar2=NEGB, op0=ALU.mult, op1=ALU.add), reads=["rep", "selbb"], writes=["selbb"])
            P.op("pe", lambda e: e.transpose(out=psb[6][:, 0:128], in_=selbb[:], identity=pj.idb[:]), reads=["selbb", "idb"], writes=["ps6"])
            P.op("act", lambda e, so=so: e.activation(out=qs[64:128, so], in_=psb[6][64:128, 0:128].unsqueeze(1).to_broadcast([64, 4, 128]), func=AF.Copy), reads=["ps6"], writes=[("qs_b", so)])
            P.op("pool", lambda e, so=so, tsl=tsl: e.tensor_copy(out=qs[0:64, so], in_=qrT[:, :, tsl]), reads=[("qrT", qb)], writes=[("qs_a", so)])
            jobs = [("s", c) for c in range(qb + 1)] + [("w", c) for c in range(max(0, qb - 4), qb + 1)]
            pend = None
            first = {"s": True, "w": True}
            last_c = {"s": qb, "w": qb}
            for job in jobs + [(None, None)]:
                kind, c = job
                cur = None
                if kind is not None:
                    sbk = nsc % 2; nsc += 1
                    ksrc = 0 if kind == "s" else 1
                    def sc2(e, kind=kind, c=c, sbk=sbk, ksrc=ksrc, tsl=tsl, qb=qb):
                        extra = []
                        if c == qb:
                            extra.append((pj.idb[:], tri[:, 0, :].unsqueeze(1).to_broadcast([128, 4, 128])))
                        if kind == "w" and c == qb - 4:
                            extra.append((pj.idb[:], tri[:, 1, :].unsqueeze(1).to_broadcast([128, 4, 128])))
                        if kind == "s":
                            ins = e.matmul(ps[sbk][:], lhsT=ksE[:, c * 128:(c + 1) * 128], rhs=qs[:, qb % 2], start=True, stop=(len(extra) == 0))
                        else:
                            ins = e.matmul(ps[sbk][:], lhsT=kT4[:, ksrc, c * 128:(c + 1) * 128], rhs=qrT[:, :, tsl], start=True, stop=(len(extra) == 0))
                        for n_, (l_, r_) in enumerate(extra):
                            ins = e.matmul(ps[sbk][:], lhsT=l_, rhs=r_, start=False, stop=(n_ == len(extra) - 1))
                        return ins
                    P.op("pe", sc2, reads=[("kT4", c), ("ksE", c), "ksE_E", ("qrT", qb), ("qs_a", qb % 2), ("qs_b", qb % 2), "tri", "idb"], writes=[f"ps{sbk}"])
                    P.op("act", lambda e, sbk=sbk: e.activation(out=eT[:, sbk].rearrange("p j t -> p (j t)"), in_=ps[sbk][:], func=AF.Exp, scale=0.125), reads=[f"ps{sbk}"], writes=[("eT", sbk)])
                    cur = (kind, c, sbk)
                if pend is not None:
                    pk, pc, pb = pend
                    bank = 4 if pk == "s" else 5
                    vi = 0 if pk == "s" else 1
                    c0 = 0 if pk == "s" else max(0, qb - 4)
                    def pv2(e, pk=pk, pc=pc, pb=pb, bank=bank, vi=vi, c0=c0, qb=qb):
                        for j in range(4):
                            ins = e.matmul(ps[bank][:, j * 65:(j + 1) * 65], lhsT=eT[:, pb, j, :], rhs=vaug[:, vi, pc, :], start=(pc == c0 and j == 0), stop=(pc == qb), skip_group_check=True)
                        return ins
                    P.op("pe", pv2, reads=[("eT", pb), ("vaug", pc)], writes=[f"ps{bank}"])
                pend = cur
            for j in range(4):
                for (bank, gcol, tag) in ((4, 1, "s"), (5, 2, "w")):
                    if tag not in MC_BR:
                        continue
                    pso = ps[bank][:, j * 65:(j + 1) * 65]
                    ri = sm[:, 8:9]
                    P.op("dve", lambda e, pso=pso, ri=ri: e.reciprocal(out=ri, in_=pso[:, 64:65]), reads=[f"ps{bank}"], writes=["ri2"])
                    P.op("dve", lambda e, ri=ri, j=j, gcol=gcol, qb=qb: e.tensor_tensor(out=ri, in0=ri, in1=gsig[:, qb, 3 * j + gcol:3 * j + gcol + 1], op=ALU.mult), reads=["ri2", ("gsig", qb)], writes=["ri2"])
                    P.op("dve", lambda e, pso=pso, ri=ri, j=j, so=so: e.scalar_tensor_tensor(out=oacc[:, so, j, :], in0=pso[:, 0:64], scalar=ri, in1=oacc[:, so, j, :], op0=ALU.mult, op1=ALU.add),
                         reads=[f"ps{bank}", "ri2", ("oacc", so)], writes=[("oacc", so)])
            P.dma("sp", ysrc[tsl, 256:512], oacc[:, so].rearrange("p j d -> p (j d)"), reads=[("oacc", so)], writes=["ysrc"])
        P.emit(last=is_last)


def nsa_consts():
    import ml_dtypes
    bf = ml_dtypes.bfloat16
    n = np.arange(256)[:, None]
    cmask = np.zeros((NTILE, 128, 2, 128), np.float32)
    for qb in range(NTILE):
        t = qb * 128 + np.arange(128)[None, :]
        ok = (16 * n + 31 <= t) & (n < 255)
        m = np.where(ok, 0.0, NEGB).astype(np.float32)
        cmask[qb] = m.reshape(2, 128, 128).transpose(1, 0, 2)
    selc = np.zeros((NTILE, 128, 2, 64), np.float32)
    mids = np.arange(64)[None, :]
    for qb in range(NTILE):
        t = qb * 128 + np.arange(128)[:, None]
        blk = t // 64
        valid = mids <= blk
        forced = (mids == 0) | (mids == blk) | (mids == blk - 1)
        selc[qb, :, 0, :] = valid.astype(np.float32)
        selc[qb, :, 1, :] = np.where(valid, 1e4 * forced.astype(np.float32), -1e4)
    Ef = (np.arange(S)[None, :] // 64 == np.arange(64)[:, None]).astype(np.float32)
    Eb = np.zeros((64, NTILE, 128), np.float32)
    for c in range(NTILE):
        for sl in range(128):
            Eb[2 * c + sl // 64, c, sl] = 1.0
    s_ = np.arange(128)[:, None]; t_ = np.arange(128)[None, :]
    tri = np.zeros((128, 2, 128), np.float32)
    tri[:, 0, :] = np.where(s_ > t_, NEGB, 0.0)
    tri[:, 1, :] = np.where(s_ <= t_, NEGB, 0.0)
    cmp_idx = np.arange(255)[:, None] * 16 + np.arange(32)[None, :]
    slc_start = np.arange(64) * 64
    overlap = ((cmp_idx[:, :1] < slc_start[None, :] + 64) & (cmp_idx[:, -1:] >= slc_start[None, :])).astype(np.float32)
    rc = np.zeros((256, 65), np.float32)
    rc[:255, :64] = overlap
    rc[:255, 64] = 1.0
    rconst = np.ascontiguousarray(rc.reshape(2, 128, 65).transpose(1, 0, 2))
    inv = (1.0 / (np.float32(500000.0) ** (np.arange(0, 16, 2, dtype=np.float32) / np.float32(16)))).astype(np.float32)
    return {"cmask": cmask.astype(bf), "selc": selc, "Ef": Ef.astype(bf), "tri": tri.astype(bf), "rconst": rconst, "invf": bc128(inv)}


def prep_MC(layer, h, inputs):
    i = layer
    maps = []
    w = inputs["w_in"][i]
    cst = nsa_consts()
    for c in range(8):
        b, gi = c // 2, c % 2
        o = 1536
        cols = np.concatenate([o + np.arange(gi * 256, (gi + 1) * 256),
                               o + 512 + 2 * 128 + np.arange(gi * 64, (gi + 1) * 64),
                               o + 512 + 4 * 128 + np.arange(gi * 64, (gi + 1) * 64),
                               o + 512 + 0 * 128 + np.arange(gi * 64, (gi + 1) * 64),
                               o + 512 + 1 * 128 + np.arange(gi * 64, (gi + 1) * 64),
                               o + 512 + 3 * 128 + np.arange(gi * 64, (gi + 1) * 64),
                               o + 512 + 5 * 128 + np.arange(gi * 64, (gi + 1) * 64),
                               o + 512 + 6 * 128 + np.arange(gi * 12, (gi + 1) * 12)])
        m = {
            "h": np.ascontiguousarray(h[b]), "identf": np.eye(128, dtype=np.float32), "wc": np.ascontiguousarray(w[:, cols]),
            "g_mix": pc8(inputs["g_mix"][i]),
            "pos": np.ascontiguousarray(inputs["positions"][b].reshape(NTILE, 128).T.astype(np.int32)),
            "w1kc": inputs["nsa_kc_w1"][i], "w1vc": inputs["nsa_vc_w1"][i], "w2kc": inputs["nsa_kc_w2"][i], "w2vc": inputs["nsa_vc_w2"][i],
            "posT": np.ascontiguousarray(inputs["nsa_cmp_pos"][i].T),
        }
        m.update(cst)
        maps.append(m)
    return maps


PAIRS = [[0, 1], [2, 3], [4, 5], [6, 7]]


def build_fused():
    nc = bass.Bass("TRN2", target_bir_lowering=False)
    xfull = nc.dram_tensor("xfull", [S, D], F32, kind="ExternalInput").ap()
    xhalf = nc.dram_tensor("xhalf", [TF, D], F32, kind="ExternalInput").ap()
    h_out = nc.dram_tensor("h_out", [TF, D], F32, kind="ExternalOutput").ap()
    ysrc = nc.dram_tensor("ysrc", [S, 512], F32).ap()
    ydst = nc.dram_tensor("ydst", [2 * S, 512], F32).ap()
    hsrc = nc.dram_tensor("hsrc", [TF, D], F32).ap()
    hfull = nc.dram_tensor("hfull", [S, D], F32).ap()
    scr = nc.dram_tensor("scr", [20, S, 64], BF16).ap()
    with ExitStack() as st:
        P = Prog(nc, st)
        ps = [st.enter_context(nc.psum_tensor(f"ps{i}", [128, 512], F32)) for i in range(8)]
        psb = [p_[:].bitcast(BF16) for p_ in ps]
        for layer in range(2):
            moe = layer % 2 == 1
            final = layer == 1
            hin = xfull if layer == 0 else hfull
            hmap = None if layer == 0 else (lambda i: ((i * 128) % 2048) // 512 * 1024 + ((i * 128) // 2048) * 512 + (i * 128) % 512)
            P.barrier()
            phase_MB(nc, P, ps, psb, f"L{layer}B_", hin, ysrc, scr, hmap=hmap)
            P.barrier()
            phase_MC(nc, P, ps, psb, f"L{layer}C_", hin, ysrc, hmap=hmap)
            P.barrier()
            for q in range(4):
                P.collective(lambda e, q=q: e.collective_compute("AllGather", ALU.bypass, replica_groups=PAIRS, ins=[ysrc[q * 1024:(q + 1) * 1024, :]], outs=[ydst[q * 2048:(q + 1) * 2048, :]]),
                             reads=["ysrc"], writes=["ydst"])
            P.emit()
            P.barrier()
            phase_F(nc, P, ps, psb, f"L{layer}F_", 8 if moe else 1, moe, final, xhalf if layer == 0 else hsrc, ydst, h_out if final else hsrc, final)
            if not final:
                P.barrier()
                for q in range(4):
                    P.collective(lambda e, q=q: e.collective_compute("AllGather", ALU.bypass, replica_groups=PAIRS, ins=[hsrc[q * 512:(q + 1) * 512, :]], outs=[hfull[q * 1024:(q + 1) * 1024, :]]),
                                 reads=["f_out"], writes=["hfull"])
                P.emit()
    return nc


W_OUT_PERM = np.concatenate([np.arange(0, 128), np.arange(256, 384), np.arange(512, 768), np.arange(128, 256), np.arange(384, 512), np.arange(768, 1024)])


def kernel(**inputs):
    inputs = {k: np.asarray(v) for k, v in inputs.items()}
    x = np.ascontiguousarray(inputs["x"], dtype=np.float32)
    hd = np.zeros((4, 1, 1), np.float32)
    maps = [dict() for _ in range(8)]
    for layer in range(2):
        moe = layer % 2 == 1
        final = layer == 1
        for tag, prep in (("B", prep_MB), ("C", prep_MC)):
            pm = prep(layer, hd, inputs)
            for c in range(8):
                for k, v in pm[c].items():
                    if k != "h":
                        maps[c][f"L{layer}{tag}_{k}"] = v
        dummy = np.zeros((8 * TF, 1), np.float32)
        pf = prep_F_inputs(layer, dummy, dummy, inputs, moe, final)
        wperm = np.ascontiguousarray(inputs["w_out"][layer][W_OUT_PERM, :])
        for c in range(8):
            for k, v in pf[c].items():
                if k in ("h", "y"):
                    continue
                maps[c][f"L{layer}F_{k}"] = wperm if k == "w_out" else v
            sel = np.zeros((128, 2), np.float32)
            sel[:, c % 2] = 1.0
            maps[c][f"L{layer}F_selv"] = sel
    for c in range(8):
        b, gi = c // 2, c % 2
        maps[c]["xfull"] = x[b]
        maps[c]["xhalf"] = np.ascontiguousarray(x[b, gi * TF:(gi + 1) * TF])
    nc = build_fused()
    res = run_bass_kernel_spmd(nc, maps, core_ids=list(range(8))).results
    out = np.empty((4, S, D), np.float32)
    for c in range(8):
        b, gi = c // 2, c % 2
        out[b, gi * TF:(gi + 1) * TF] = res[c]["h_out"]
    return out
```
